# Optimizing a Trainium2 kernel written in Bass

```python
import jax, jax.numpy as jnp
from jax import lax
import numpy as np

D_MODEL = 2048
BATCH = 8
SEQ = 2048
DEPTH = 1

MIX_WIDTH = D_MODEL
HGRN_WIDTH = MIX_WIDTH // 2
HGRN_DK = 128
HGRN_HEADS = HGRN_WIDTH // HGRN_DK
HGRN_DV = HGRN_WIDTH // HGRN_HEADS
HGRN_CHUNK = 64
MLA_WIDTH = MIX_WIDTH - HGRN_WIDTH
MLA_V_DIM = 128
MLA_HEADS = MLA_WIDTH // MLA_V_DIM
MLA_NOPE = 128
MLA_ROPE = 64
MLA_QK = MLA_NOPE + MLA_ROPE
Q_LORA = D_MODEL // 4
KV_LORA = D_MODEL // 8
ATTN_BLOCK = 128
ROPE_BASE = 10000.0
IN_COLS = 4 * HGRN_WIDTH + Q_LORA + KV_LORA + MLA_ROPE
N_GROUPS = 4
EXPERTS_PER_GROUP = 8
N_EXPERTS = N_GROUPS * EXPERTS_PER_GROUP
TOP_K_IN_GROUP = 2
D_EXPERT = D_MODEL // 4
MOE_BLOCK = 256
EPS = 1e-6

kernel_name = "hymba_hgrn2_mla_hmoe_adaln"


def _rmsnorm(x, g):
    xf = x.astype(jnp.float32)
    y = xf * lax.rsqrt(jnp.mean(xf * xf, axis=-1, keepdims=True) + EPS)
    return (y * g.astype(jnp.float32)).astype(x.dtype)


def _modulate(h, shift, scale):
    return h * (1 + scale[:, None, :]) + shift[:, None, :]


def _rope(x, positions):
    inv_freq = ROPE_BASE ** (-jnp.arange(0, MLA_ROPE, 2, dtype=jnp.float32) / MLA_ROPE)
    ang = positions.astype(jnp.float32)[..., None] * inv_freq
    cos = jnp.cos(ang)[:, :, None, :]
    sin = jnp.sin(ang)[:, :, None, :]
    xf = x.astype(jnp.float32)
    x1, x2 = xf[..., : MLA_ROPE // 2], xf[..., MLA_ROPE // 2:]
    out = jnp.concatenate([x1 * cos - x2 * sin, x2 * cos + x1 * sin], axis=-1)
    return out.astype(x.dtype)


def _hgrn2_chunked(q, f_logit, v, lb):
    B, S = q.shape[0], q.shape[1]
    C = HGRN_CHUNK
    N = S // C
    f = lb + (1.0 - lb) * jax.nn.sigmoid(f_logit.astype(jnp.float32))
    log_f = jnp.log(f)
    k = 1.0 - f
    qf = q.astype(jnp.float32) * HGRN_DK ** -0.5
    vf = v.astype(jnp.float32)

    def chunks(t):
        return t.reshape(B, N, C, t.shape[2], t.shape[3]).transpose(0, 3, 1, 2, 4)

    qc, kc, vc, lfc = chunks(qf), chunks(k), chunks(vf), chunks(log_f)
    b = jnp.cumsum(lfc, axis=3)
    b_last = b[..., C - 1:C, :]
    b_mid = b[..., C // 2 - 1:C // 2, :]
    q_in = qc * jnp.exp(b - b_mid)
    k_in = kc * jnp.exp(b_mid - b)
    A = jnp.einsum('bhnqd,bhnkd->bhnqk', q_in, k_in)
    causal = jnp.tril(jnp.ones((C, C), dtype=bool))
    A = jnp.where(causal, A, 0.0)
    o_intra = jnp.einsum('bhnqk,bhnkv->bhnqv', A, vc)
    U = jnp.einsum('bhnkd,bhnkv->bhndv', kc * jnp.exp(b_last - b), vc)
    decay = jnp.exp(b_last[..., 0, :])

    def step(state, xs):
        d, u = xs
        return d[..., None] * state + u, state

    s0 = jnp.zeros((B, q.shape[2], HGRN_DK, v.shape[3]), jnp.float32)
    _, s_prev = lax.scan(step, s0, (jnp.moveaxis(decay, 2, 0), jnp.moveaxis(U, 2, 0)))
    s_prev = jnp.moveaxis(s_prev, 0, 2)
    o_inter = jnp.einsum('bhnqd,bhndv->bhnqv', qc * jnp.exp(b), s_prev)
    o = o_intra + o_inter
    return o.transpose(0, 2, 3, 1, 4).reshape(B, S, q.shape[2], v.shape[3])


def _causal_block_attention(q, k, v):
    B, H, S, dqk = q.shape
    dv = v.shape[-1]
    nb = S // ATTN_BLOCK
    scale = dqk ** -0.5
    qb = q.reshape(B, H, nb, ATTN_BLOCK, dqk).transpose(2, 0, 1, 3, 4)
    k_pos = jnp.arange(S)
    neg = jnp.finfo(jnp.float32).min

    def one_block(args):
        qblk, blk = args
        s = jnp.einsum('bhqd,bhkd->bhqk', qblk, k).astype(jnp.float32) * scale
        q_pos = blk * ATTN_BLOCK + jnp.arange(ATTN_BLOCK)
        s = jnp.where(k_pos[None, :] <= q_pos[:, None], s, neg)
        p = jax.nn.softmax(s, axis=-1)
        return jnp.einsum('bhqk,bhkv->bhqv', p.astype(v.dtype), v)

    out = lax.map(one_block, (qb, jnp.arange(nb)))
    return out.transpose(1, 0, 3, 2, 4).reshape(B, S, H, dv)


def _hier_moe(h, w_group, b_group, w_router, b_router, w_gate, w_up, w_down):
    B, S, D = h.shape
    T = B * S
    xt = h.reshape(T, D)
    g_logits = (xt @ w_group).astype(jnp.float32) + b_group.astype(jnp.float32)
    g_prob = jax.nn.softmax(g_logits, axis=-1)
    g_sel = jnp.argmax(g_logits, axis=-1)
    p_group = jnp.take_along_axis(g_prob, g_sel[:, None], axis=-1)[:, 0]
    e_logits = ((xt @ w_router).astype(jnp.float32) + b_router.astype(jnp.float32))
    e_logits = e_logits.reshape(T, N_GROUPS, EXPERTS_PER_GROUP)
    e_in = jnp.take_along_axis(e_logits, g_sel[:, None, None], axis=1)[:, 0]
    top_val, top_idx = lax.top_k(e_in, TOP_K_IN_GROUP)
    weights = p_group[:, None] * jax.nn.softmax(top_val, axis=-1)
    expert_ids = g_sel[:, None] * EXPERTS_PER_GROUP + top_idx

    A = T * TOP_K_IN_GROUP
    e_flat = expert_ids.reshape(A).astype(jnp.int32)
    w_flat = weights.reshape(A)
    tok = jnp.arange(A, dtype=jnp.int32) // TOP_K_IN_GROUP
    counts = jax.ops.segment_sum(jnp.ones((A,), jnp.int32), e_flat, num_segments=N_EXPERTS)
    padded = ((counts + MOE_BLOCK - 1) // MOE_BLOCK) * MOE_BLOCK
    pad_end = jnp.cumsum(padded)
    pad_start = pad_end - padded
    start = jnp.cumsum(counts) - counts
    order = jnp.argsort(e_flat)
    e_sorted = e_flat[order]
    dest_sorted = pad_start[e_sorted] + (jnp.arange(A, dtype=jnp.int32) - start[e_sorted])
    dest = jnp.zeros((A,), jnp.int32).at[order].set(dest_sorted.astype(jnp.int32))
    n_blocks = -(-A // MOE_BLOCK) + N_EXPERTS
    R = n_blocks * MOE_BLOCK
    x_buf = jnp.zeros((R, D), xt.dtype).at[dest].set(xt[tok])
    blk_start = jnp.arange(n_blocks, dtype=jnp.int32) * MOE_BLOCK
    blk_e = jnp.minimum(jnp.searchsorted(pad_end, blk_start, side='right'), N_EXPERTS - 1)

    def expert_block(args):
        xb, e = args
        hid = jax.nn.silu(xb @ w_gate[e]) * (xb @ w_up[e])
        return hid @ w_down[e]

    y_buf = lax.map(expert_block, (x_buf.reshape(n_blocks, MOE_BLOCK, D), blk_e)).reshape(R, D)
    contrib = y_buf[dest] * w_flat[:, None].astype(y_buf.dtype)
    y = jax.ops.segment_sum(contrib, tok, num_segments=T)
    return y.reshape(B, S, D)


def setup_inputs(seed: int = 0) -> dict:
    key = jax.random.key(seed)
    ks = jax.random.split(key, 24)
    L = DEPTH

    def w(k, shape, fan_in, mult=1.0):
        return jax.random.normal(k, shape, jnp.float32) * (mult * fan_in ** -0.5)

    def gain(k, shape):
        return 1.0 + 0.02 * jax.random.normal(k, shape, jnp.float32)

    x = jax.random.normal(ks[0], (BATCH, SEQ, D_MODEL), jnp.float32)
    c = jax.random.normal(ks[1], (BATCH, D_MODEL), jnp.float32)
    offsets = jax.random.randint(ks[2], (BATCH, 1), 0, 512, dtype=jnp.int32)
    positions = offsets + jnp.arange(SEQ, dtype=jnp.int32)[None, :]
    return {
        "x": x,
        "c": c,
        "positions": positions,
        "w_ada": w(ks[3], (L, D_MODEL, 6 * D_MODEL), D_MODEL, 0.5),
        "b_ada": 0.01 * jax.random.normal(ks[4], (L, 6 * D_MODEL), jnp.float32),
        "norm1_g": gain(ks[5], (L, D_MODEL)),
        "w_in": w(ks[6], (L, D_MODEL, IN_COLS), D_MODEL),
        "hgrn_lb_logits": 1.0 + 0.1 * jax.random.normal(ks[7], (L + 1, HGRN_WIDTH), jnp.float32),
        "hgrn_onorm_g": gain(ks[8], (L, HGRN_DV)),
        "q_a_norm_g": gain(ks[9], (L, Q_LORA)),
        "w_q_up": w(ks[10], (L, Q_LORA, MLA_HEADS * MLA_QK), Q_LORA),
        "kv_a_norm_g": gain(ks[11], (L, KV_LORA)),
        "w_kv_up": w(ks[12], (L, KV_LORA, MLA_HEADS * (MLA_NOPE + MLA_V_DIM)), KV_LORA),
        "q_norm_g": gain(ks[13], (L, MLA_QK)),
        "k_norm_g": gain(ks[14], (L, MLA_QK)),
        "attn_onorm_g": gain(ks[15], (L, MLA_V_DIM)),
        "w_out": w(ks[16], (L, MIX_WIDTH, D_MODEL), MIX_WIDTH),
        "norm2_g": gain(ks[17], (L, D_MODEL)),
        "w_group": w(ks[18], (L, D_MODEL, N_GROUPS), D_MODEL),
        "b_group": 0.01 * jax.random.normal(ks[19], (L, N_GROUPS), jnp.float32),
        "w_router": w(ks[20], (L, D_MODEL, N_EXPERTS), D_MODEL),
        "b_router": 0.01 * jax.random.normal(ks[21], (L, N_EXPERTS), jnp.float32),
        "w_gate": w(ks[22], (L, N_EXPERTS, D_MODEL, D_EXPERT), D_MODEL),
        "w_up": w(jax.random.fold_in(ks[22], 1), (L, N_EXPERTS, D_MODEL, D_EXPERT), D_MODEL),
        "w_down": w(ks[23], (L, N_EXPERTS, D_EXPERT, D_MODEL), D_EXPERT),
    }


def reference(x, c, positions, w_ada, b_ada, norm1_g, w_in, hgrn_lb_logits, hgrn_onorm_g,
              q_a_norm_g, w_q_up, kv_a_norm_g, w_kv_up, q_norm_g, k_norm_g, attn_onorm_g,
              w_out, norm2_g, w_group, b_group, w_router, b_router, w_gate, w_up, w_down):
    B, S, D = x.shape
    lb_all = jnp.cumsum(jax.nn.softmax(hgrn_lb_logits.astype(jnp.float32), axis=0), axis=0)
    c_act = jax.nn.silu(c)
    split_idx = [HGRN_WIDTH, 2 * HGRN_WIDTH, 3 * HGRN_WIDTH, 4 * HGRN_WIDTH,
                 4 * HGRN_WIDTH + Q_LORA, 4 * HGRN_WIDTH + Q_LORA + KV_LORA]
    for l in range(DEPTH):
        mod = c_act @ w_ada[l] + b_ada[l]
        sh1, sc1, g1, sh2, sc2, g2 = jnp.split(mod, 6, axis=-1)

        h = _modulate(_rmsnorm(x, norm1_g[l]), sh1, sc1)
        proj = h @ w_in[l]
        hq, hf, hi, hg, q_a, kv_a, k_pe = jnp.split(proj, split_idx, axis=-1)

        def heads_a(t):
            return t.reshape(B, S, HGRN_HEADS, HGRN_DK)
        lb = lb_all[l].reshape(HGRN_HEADS, HGRN_DK)
        o_a = _hgrn2_chunked(heads_a(hq), heads_a(hf), heads_a(hi), lb).astype(x.dtype)
        o_a = _rmsnorm(o_a, hgrn_onorm_g[l]) * jax.nn.silu(heads_a(hg))
        o_a = o_a.reshape(B, S, HGRN_WIDTH)

        qh = (_rmsnorm(q_a, q_a_norm_g[l]) @ w_q_up[l]).reshape(B, S, MLA_HEADS, MLA_QK)
        kv = (_rmsnorm(kv_a, kv_a_norm_g[l]) @ w_kv_up[l]).reshape(B, S, MLA_HEADS, MLA_NOPE + MLA_V_DIM)
        k_nope, v = kv[..., :MLA_NOPE], kv[..., MLA_NOPE:]
        kh = jnp.concatenate(
            [k_nope, jnp.broadcast_to(k_pe[:, :, None, :], (B, S, MLA_HEADS, MLA_ROPE))], axis=-1)
        qh = _rmsnorm(qh, q_norm_g[l])
        kh = _rmsnorm(kh, k_norm_g[l])
        qh = jnp.concatenate([qh[..., :MLA_NOPE], _rope(qh[..., MLA_NOPE:], positions)], axis=-1)
        kh = jnp.concatenate([kh[..., :MLA_NOPE], _rope(kh[..., MLA_NOPE:], positions)], axis=-1)
        o_b = _causal_block_attention(qh.transpose(0, 2, 1, 3), kh.transpose(0, 2, 1, 3),
                                      v.transpose(0, 2, 1, 3))
        o_b = _rmsnorm(o_b, attn_onorm_g[l]).reshape(B, S, MLA_WIDTH)

        mix = jnp.concatenate([o_a, o_b], axis=-1) @ w_out[l]
        x = x + g1[:, None, :] * mix

        h2 = _modulate(_rmsnorm(x, norm2_g[l]), sh2, sc2)
        y = _hier_moe(h2, w_group[l], b_group[l], w_router[l], b_router[l],
                      w_gate[l], w_up[l], w_down[l])
        x = x + g2[:, None, :] * y
    return x
```

```python
import numpy as np
import ml_dtypes
import concourse.bass as bass
import concourse.mybir as mybir
from concourse.bass_utils import run_bass_kernel_spmd

F32 = mybir.dt.float32
BF16 = mybir.dt.bfloat16
I32 = mybir.dt.int32
AF = mybir.ActivationFunctionType
ALU = mybir.AluOpType
AX = mybir.AxisListType

D = 2048
S_LEN = 2048
NT = 16
EPS = 1e-6
IN_COLS = 4928
BLK = 128
NBLK = 64
NSLOT = NBLK * BLK
DEBUG = None


class Sync:
    def __init__(self, nc, n_dma_sems=32):
        self.nc = nc
        self.eng = {"pe": nc.tensor, "dve": nc.vector, "act": nc.scalar,
                    "pool": nc.gpsimd, "sp": nc.sync}
        self.sem = {}
        self.cnt = {}
        self._ctx = []
        for e in self.eng:
            cm = nc.semaphore("s_" + e)
            self.sem[e] = cm.__enter__()
            self._ctx.append(cm)
            self.cnt[e] = 0
        self.dma_sems = []
        self.dma_pool = {"sp": [], "pool": [], "act": []}
        self.dma_rr = {"sp": 0, "pool": 0, "act": 0}
        for q, n in (("sp", n_dma_sems // 2), ("pool", n_dma_sems // 2), ("act", 2)):
            for i in range(n):
                cm = nc.semaphore("d%s%d" % (q, i))
                slot = [cm.__enter__(), 0, None]
                self.dma_sems.append(slot)
                self.dma_pool[q].append(slot)
                self._ctx.append(cm)
        self.waited = {}
        self.last_w = {}
        self.readers = {}
        self.prog = {e: [] for e in self.eng}

    def close(self):
        for cm in reversed(self._ctx):
            cm.__exit__(None, None, None)

    def _wait(self, e, tok):
        if tok is None:
            return
        sem, sid, val, src = tok
        if src == e and e == "pe":
            return
        k = (e, sid)
        if self.waited.get(k, 0) >= val:
            return
        self.waited[k] = val
        self.prog[e].append(("w", sem, val))

    def _deps(self, e, reads, writes, skip_same_war=True):
        for r in reads:
            self._wait(e, self.last_w.get(r))
        for w in writes:
            self._wait(e, self.last_w.get(w))
            for tok in self.readers.get(w, ()):
                if skip_same_war and tok[3] == e and e != "pool":
                    continue
                self._wait(e, tok)

    def _commit(self, tok, reads, writes):
        for w in writes:
            self.last_w[w] = tok
            self.readers[w] = []
        for r in reads:
            self.readers.setdefault(r, []).append(tok)

    def op(self, e, fn, reads=(), writes=()):
        self._deps(e, reads, writes)
        self.cnt[e] += 1
        self.prog[e].append(("i", fn, self.sem[e], 1))
        tok = (self.sem[e], e, self.cnt[e], e)
        self._commit(tok, reads, writes)
        return tok

    def dma(self, e, fn, reads=(), writes=()):
        pool = self.dma_pool[e]
        slot = pool[self.dma_rr[e]]
        self.dma_rr[e] = (self.dma_rr[e] + 1) % len(pool)
        self._wait(e, slot[2])
        self._deps(e, reads, writes, skip_same_war=False)
        slot[1] += 16
        self.prog[e].append(("i", fn, slot[0], 16))
        tok = (slot[0], id(slot), slot[1], None)
        slot[2] = tok
        self._commit(tok, reads, writes)
        return tok

    def barrier(self):
        toks = [(self.sem[e], e, self.cnt[e], e) for e in self.eng if self.cnt[e] > 0]
        toks += [s[2] for s in self.dma_sems if s[2] is not None]
        for e in self.eng:
            for t in toks:
                if t[3] == e:
                    continue
                self._wait(e, t)

    def emit(self):
        nc = self.nc
        self.barrier()
        prog = self.prog

        def run(engine, lst):
            for it in lst:
                if it[0] == "w":
                    engine.wait_ge(it[1], it[2])
                else:
                    it[1](engine).then_inc(it[2], it[3])

        with nc.Block() as block:
            @block.sync
            def _(eng):
                run(eng, prog["sp"])

            @block.scalar
            def _(eng):
                run(eng, prog["act"])

            @block.vector
            def _(eng):
                run(eng, prog["dve"])

            @block.gpsimd
            def _(eng):
                run(eng, prog["pool"])

            @block.tensor
            def _(eng):
                run(eng, prog["pe"])


def pipeline(gens, W):
    gens = list(gens)
    active = []
    nxt = 0
    while active or nxt < len(gens):
        while len(active) < W and nxt < len(gens):
            active.append(gens[nxt])
            nxt += 1
        for g in list(active):
            try:
                next(g)
            except StopIteration:
                active.remove(g)


class Arena:
    def __init__(self, ap):
        self.ap = ap
        self.off = 0
        self.cap = ap.shape[1]
        self.peak = 0

    def alloc(self, n, dtype=F32):
        ne = n * (2 if dtype in (F32, I32) else 1)
        ne = (ne + 15) // 16 * 16
        a = self.off
        self.off += ne
        assert self.off <= self.cap, ("arena overflow", self.off, self.cap)
        self.peak = max(self.peak, self.off)
        v = self.ap[:, a:a + n * (2 if dtype in (F32, I32) else 1)]
        if dtype == F32:
            v = v.bitcast(F32)
        elif dtype == I32:
            v = v.bitcast(I32)
        return v


def build(debug=None):
    nc = bass.Bass("TRN2", target_bir_lowering=False)

    def din(name, shape, dt=F32):
        return nc.dram_tensor(name, list(shape), dt, kind="ExternalInput").ap()

    x_d = din("x", [S_LEN, D])
    ccol_d = din("ccol", [128, 16])
    pos_d = din("pos", [1, S_LEN], I32)
    wada_d = din("w_ada", [D, 6 * D])
    bada_d = din("b_ada", [1, 6 * D])
    n1g_d = din("norm1_gc", [128, 16])
    win_d = din("w_in", [D, IN_COLS])
    lbl_d = din("lb_logits", [2, 1024])
    hon_d = din("hgrn_onorm_g", [1, 128])
    qag_d = din("q_a_gc", [128, 4])
    wqu_d = din("w_q_up", [512, 1536])
    kvg_d = din("kv_a_gc", [128, 2])
    wkv_d = din("w_kv_up", [256, 2048])
    qng_d = din("q_norm_gc", [128, 4])
    kng_d = din("k_norm_gc", [128, 4])
    aon_d = din("attn_onorm_g", [1, 128])
    wout_d = din("w_out", [D, D])
    n2g_d = din("norm2_g", [1, D])
    wgr_d = din("w_gr", [D, 36])
    bgr_d = din("b_gr", [1, 36])
    wg_d = din("w_gate", [32 * 128, 16 * 512])
    wu_d = din("w_up", [32 * 128, 16 * 512])
    wd_d = din("w_down", [32 * 128, 4 * 2048])
    cst_d = din("consts", [128, 1024])
    invf_d = din("invf", [64, 1])
    metai_d = din("meta_init", [NSLOT, 32], BF16)
    out_d = nc.dram_tensor("out", [S_LEN + 128, D], F32, kind="ExternalOutput").ap()
    xbuf_d = nc.dram_tensor("xbuf", [NSLOT, D + 32], BF16).ap()
    meta_d = nc.dram_tensor("metabuf", [NSLOT, 16], F32).ap()
    dbg = {}

    def dout(name, shape, dt=F32):
        dbg[name] = nc.dram_tensor("dbg_" + name, list(shape), dt, kind="ExternalOutput").ap()
        return dbg[name]

    S = Sync(nc)
    import contextlib
    es = contextlib.ExitStack()
    arena_t = es.enter_context(nc.sbuf_tensor("arena", [128, 103 * 1024], BF16))
    A = Arena(arena_t[:])
    banks = [es.enter_context(nc.psum_tensor("pb%d" % i, [128, 512], F32)) for i in range(8)]
    PB = [b[:] for b in banks]
    PBH = [b[:].bitcast(BF16) for b in banks]
    PK = ["pb%d" % i for i in range(8)]

    def MM(out, lhsT, rhs, start, stop, r, w):
        return S.op("pe", lambda e: e.matmul(out, lhsT=lhsT, rhs=rhs, start=start, stop=stop,
                                             skip_group_check=True), r, w)

    def TR(out, in_, ident, r, w):
        return S.op("pe", lambda e: e.transpose(out=out, in_=in_, identity=ident), r, w)

    def ACT(out, in_, func, r, w, scale=1.0, bias=0.0, accum=None):
        if accum is None:
            return S.op("act", lambda e: e.activation(out=out, in_=in_, func=func, bias=bias, scale=scale), r, w)
        return S.op("act", lambda e: e.activation(out=out, in_=in_, func=func, bias=bias, scale=scale,
                                                  accum_out=accum), r, w)

    def TS(eng, out, in0, s1, s2, op0, op1, r, w):
        if s2 is None:
            return S.op(eng, lambda e: e.tensor_scalar(out, in0, s1, None, op0), r, w)
        return S.op(eng, lambda e: e.tensor_scalar(out, in0, s1, s2, op0, op1), r, w)

    def TT(eng, out, in0, in1, op, r, w):
        return S.op(eng, lambda e: e.tensor_tensor(out, in0, in1, op), r, w)

    def STT(eng, out, in0, sc, in1, op0, op1, r, w, accum=None):
        if accum is None:
            return S.op(eng, lambda e: e.scalar_tensor_tensor(out, in0, sc, in1, op0, op1), r, w)
        return S.op(eng, lambda e: e.scalar_tensor_tensor(out, in0, sc, in1, op0, op1, accum_out=accum), r, w)

    def CP(eng, out, in_, r, w):
        return S.op(eng, lambda e: e.tensor_copy(out, in_), r, w)

    def RED(eng, out, in_, op, r, w):
        return S.op(eng, lambda e: e.tensor_reduce(out, in_, AX.X, op), r, w)

    def RCP(out, in_, r, w):
        return S.op("dve", lambda e: e.reciprocal(out, in_), r, w)

    def DMA(q, out, in_, r, w):
        return S.dma(q, lambda e: e.dma_start(out=out, in_=in_), r, w)

    _regs = {}

    def breg(e, val):
        if val not in _regs:
            _regs[val] = e.to_reg(val)
        return _regs[val]

    def rstd_col(out, ssq, n, r, w, tmpk):
        ACT(out, ssq, AF.Ln, r, [tmpk], scale=1.0 / n, bias=epsc)
        ACT(out, out, AF.Exp, [tmpk], w, scale=-0.5)

    cst = A.alloc(1024, F32)
    identf = cst[:, 0:128]
    triu = cst[:, 128:256]
    M1 = cst[:, 256:384]
    M2 = cst[:, 384:512]
    tril_strict = cst[:, 512:640]
    iota_p = cst[:, 640:641]
    thr16 = cst[:, 656:672]
    thr64 = cst[:, 672:736]
    eidx = cst[:, 736:768]
    DMA("sp", cst, cst_d, [], ["cst"])
    cb = A.alloc(512, BF16)
    identb = cb[:, 0:128]
    onesb = cb[:, 128:256]
    triub = cb[:, 256:384]
    trilsb = cb[:, 384:512]
    CP("dve", identb, identf, ["cst"], ["cb"])
    CP("dve", triub, triu, ["cst"], ["cb"])
    CP("dve", trilsb, tril_strict, ["cst"], ["cb"])
    S.op("pool", lambda e: e.memset(onesb, 1.0), [], ["cb"])
    small = A.alloc(256, F32)
    epsc = small[:, 0:1]
    onec = small[:, 1:2]
    S.op("pool", lambda e: e.memset(epsc, EPS), [], ["epsc"])
    S.op("pool", lambda e: e.memset(onec, 1.0), [], ["epsc"])
    _sc = [2]

    def col(n=1):
        a = _sc[0]
        _sc[0] += n
        assert _sc[0] <= 256
        return small[:, a:a + n]

    modc = A.alloc(96, F32)
    A1c = A.alloc(16, F32)
    modrow_d = nc.dram_tensor("modrow_d", [1, 6 * D], F32).ap()

    mark = A.off
    ccol = A.alloc(16, F32)
    cact = A.alloc(16, BF16)
    tmp16 = A.alloc(16, F32)
    modrow = A.alloc(6 * D, F32)
    DMA("sp", ccol, ccol_d, [], ["ccol"])
    DMA("sp", modrow[0:1, :], bada_d, [], ["modrow_b"])
    ACT(tmp16, ccol, AF.Exp, ["ccol"], ["tmp16"], scale=-1.0)
    TS("dve", tmp16, tmp16, 1.0, None, ALU.add, None, ["tmp16"], ["tmp16"])
    RCP(tmp16, tmp16, ["tmp16"], ["tmp16"])
    TT("dve", cact, ccol, tmp16, ALU.mult, ["tmp16", "ccol"], ["cact"])
    wa = [A.alloc(16 * 512, BF16).rearrange("p (k n) -> p k n", k=16) for _ in range(2)]
    biasrow = modrow
    for jg in range(24):
        wt = wa[jg % 2]
        wk = "wa%d" % (jg % 2)
        S.dma("pool", (lambda wt=wt, jg=jg: (lambda e: e.dma_start(
            out=wt, in_=wada_d[:, jg * 512:(jg + 1) * 512].rearrange("(k p) n -> p k n", p=128))))(),
            [], [wk])
        pbi = jg % 2
        for k in range(16):
            MM(PB[pbi][0:1, :], cact[:, k:k + 1], wt[:, k, :], k == 0, k == 15, [wk, "cact"], [PK[pbi]])
        TT("dve", modrow[0:1, jg * 512:(jg + 1) * 512], PB[pbi][0:1, :], modrow[0:1, jg * 512:(jg + 1) * 512],
           ALU.add, ["modrow_b"], [PK[pbi], "modrow%d" % jg])
    allrow = ["modrow%d" % j for j in range(24)]
    if debug == "A":
        d = dout("mod", [1, 6 * D])
        DMA("sp", d, modrow[0:1, :], allrow, ["dbg"])
    for j in range(96):
        MM(PB[2][:, j:j + 1], modrow[0:1, j * 128:(j + 1) * 128], onec[0:1, 0:1], True, True, allrow + ["epsc"], [PK[2]])
    CP("dve", modc, PB[2][:, 0:96], [], [PK[2], "modc"])
    DMA("sp", modrow_d, modrow[0:1, :], allrow, ["modrow_d"])
    n1gc = A.alloc(16, F32)
    DMA("sp", n1gc, n1g_d, [], ["n1gc"])
    STT("dve", A1c, modc[:, 16:32], 1.0, n1gc, ALU.add, ALU.mult, ["modc", "n1gc"], ["A1c"])
    B1c = modc[:, 0:16]
    S.barrier()
    A.off = mark

    if debug == "A":
        d2 = dout("modc", [128, 96])
        DMA("sp", d2, modc, ["modc"], ["dbg"])
        S.emit()
        S.close()
        es.close()
        return nc, dbg

    markMix = A.off
    mixT = A.alloc(16 * S_LEN, BF16).rearrange("p (k t) -> p k t", k=16)
    markHT = A.off
    hT = A.alloc(16 * S_LEN, BF16).rearrange("p (k t) -> p k t", k=16)
    markB = A.off
    xt = [A.alloc(D, F32) for _ in range(2)]
    xn = [A.alloc(D, BF16) for _ in range(2)]
    tmod = [A.alloc(1024, F32) for _ in range(2)]
    ssq1 = col(16)
    rs1 = col(16)
    for i in range(NT):
        sl = i % 2
        DMA("sp", xt[sl], x_d[i * 128:(i + 1) * 128, :], [], ["xt%d" % sl])
        ACT(xn[sl], xt[sl], AF.Square, ["xt%d" % sl], ["xn%d" % sl, "ssq1_%d" % i], accum=ssq1[:, i:i + 1])
        rstd_col(rs1[:, i:i + 1], ssq1[:, i:i + 1], D, ["ssq1_%d" % i, "epsc"], ["rs1_%d" % i], "rs1t_%d" % i)
        ACT(xn[sl], xt[sl], AF.Identity, ["xt%d" % sl, "rs1_%d" % i], ["xn%d" % sl], scale=rs1[:, i:i + 1])
        for hf in range(2):
            for kk in range(8):
                k = hf * 8 + kk
                TR(PBH[hf][:, kk * 128:(kk + 1) * 128], xn[sl][:, k * 128:(k + 1) * 128], identb,
                   ["xn%d" % sl, "cb"], [PK[hf]])
            src = PBH[hf].rearrange("p (k t) -> p k t", k=8)
            tm = tmod[hf].rearrange("p (k t) -> p k t", k=8)
            a1 = A1c[:, hf * 8:(hf + 1) * 8].unsqueeze(2).to_broadcast([128, 8, 128])
            b1 = B1c[:, hf * 8:(hf + 1) * 8].unsqueeze(2).to_broadcast([128, 8, 128])
            TT("dve", tm, src, a1, ALU.mult, ["A1c"], [PK[hf], "tmod%d" % hf])
            TT("pool", hT[:, hf * 8:(hf + 1) * 8, i * 128:(i + 1) * 128], tm, b1, ALU.add,
               ["tmod%d" % hf, "modc"], ["hT%d" % i])
    hTall = ["hT%d" % i for i in range(NT)]
    S.barrier()
    A.off = markB
    if debug == "B":
        d = dout("hT", [128, 16 * S_LEN], BF16)
        DMA("sp", d, hT.rearrange("p k t -> p (k t)"), hTall, ["dbg"])
        S.emit(); S.close(); es.close()
        return nc, dbg

    markC = A.off
    wb = [A.alloc(16 * 512, BF16).rearrange("p (k n) -> p k n", k=16) for _ in range(2)]
    markC1 = A.off
    wsec = [wb[0], wb[1],
            mixT[:, 8:12, :].rearrange("p a t -> p (a t)").rearrange("p (k n) -> p k n", k=16),
            mixT[:, 12:16, :].rearrange("p a t -> p (a t)").rearrange("p (k n) -> p k n", k=16)]
    lbb = A.alloc(1024, F32)
    omlb = A.alloc(1024, F32)
    honb = A.alloc(128, F32)
    DMA("sp", lbb, lbl_d[0:1, :].partition_broadcast(128), [], ["lbb"])
    DMA("sp", omlb, lbl_d[1:2, :].partition_broadcast(128), [], ["omlb"])
    DMA("sp", honb, hon_d.partition_broadcast(128), [], ["honb"])
    TT("dve", omlb, omlb, lbb, ALU.subtract, ["lbb"], ["omlb"])
    ACT(omlb, omlb, AF.Exp, [], ["omlb"])
    TS("dve", omlb, omlb, 1.0, None, ALU.add, None, [], ["omlb"])
    RCP(lbb, omlb, ["omlb"], ["lbb"])
    TS("dve", omlb, lbb, -1.0, 1.0, ALU.mult, ALU.add, ["lbb"], ["omlb"])
    W4 = 512
    en_t, f_t, lf_t, kk_t = A.alloc(W4), A.alloc(W4), A.alloc(W4), A.alloc(W4)
    E1_t, E2_t = A.alloc(W4), A.alloc(W4)
    qin_t, qout_t, kin_t, kout_t, v_t = (A.alloc(W4, BF16) for _ in range(5))
    eng_t, sil_t = A.alloc(W4), A.alloc(W4)
    E3_t, E1n_t = eng_t, f_t
    trq_t, trk_t, tro_t = A.alloc(W4, BF16), A.alloc(W4, BF16), A.alloc(W4, BF16)
    am_t = A.alloc(W4, BF16)
    on_t = en_t
    og_t = A.alloc(W4, BF16)
    Sst = A.alloc(W4)
    Sbf = A.alloc(W4, BF16)
    deccol = col(4)
    ssqo = col(4)
    rso = col(4)
    QSC = 128.0 ** -0.5
    v4 = lambda t: t.rearrange("p (h d) -> p h d", h=4)
    triu4 = triu.unsqueeze(1).to_broadcast([128, 4, 128])
    for hgp in range(2):
        for sec in range(4):
            c0 = sec * 1024 + hgp * 512
            S.dma("pool", (lambda sec=sec, c0=c0: (lambda e: e.dma_start(
                out=wsec[sec], in_=win_d[:, c0:c0 + 512].rearrange("(k p) n -> p k n", p=128))))(), [], ["wsec%d" % sec])
        hs = slice(hgp * 512, (hgp + 1) * 512)
        for i in range(NT):
            tsl = slice(i * 128, (i + 1) * 128)
            for sec in range(4):
                for k in range(16):
                    MM(PB[sec], hT[:, k, tsl], wsec[sec][:, k, :], k == 0, k == 15, ["wsec%d" % sec, "hT%d" % i], [PK[sec]])
            hq, hf_, hi_, hg = PB[0], PB[1], PB[2], PB[3]
            ACT(en_t, hf_, AF.Exp, [], [PK[1], "en"], scale=-1.0)
            ACT(v_t, hi_, AF.Copy, [], [PK[2], "v"])
            ACT(eng_t, hg, AF.Exp, [], [PK[3], "eng"], scale=-1.0)
            ACT(en_t, en_t, AF.Ln, [], ["en"], bias=onec)
            ACT(en_t, en_t, AF.Exp, [], ["en"], scale=-1.0)
            TT("dve", f_t, en_t, omlb[:, hs], ALU.mult, ["en", "omlb"], ["f"])
            TT("dve", f_t, f_t, lbb[:, hs], ALU.add, ["lbb"], ["f"])
            ACT(lf_t, f_t, AF.Ln, ["f"], ["lf"])
            TS("pool", kk_t, f_t, -1.0, 1.0, ALU.mult, ALU.add, ["f"], ["kk"])
            ACT(eng_t, eng_t, AF.Ln, [], ["eng"], bias=onec)
            ACT(eng_t, eng_t, AF.Exp, [], ["eng"], scale=-1.0)
            TT("dve", sil_t, hg, eng_t, ALU.mult, ["eng"], [PK[3], "sil"])
            MM(PB[4], M1, lf_t, True, True, ["cst", "lf"], [PK[4]])
            MM(PB[5], M2, lf_t, True, True, ["cst", "lf"], [PK[5]])
            MM(PB[6], triu, lf_t, True, True, ["cst", "lf"], [PK[6]])
            for hh in range(4):
                MM(PB[7][:, hh:hh + 1], lf_t[:, hh * 128:(hh + 1) * 128], onec, True, True, ["epsc", "lf"], [PK[7]])
            ACT(E1_t, PB[4], AF.Exp, [], [PK[4], "E1"])
            ACT(E1n_t, PB[4], AF.Exp, [], [PK[4], "f"], scale=-1.0)
            ACT(E2_t, PB[5], AF.Exp, [], [PK[5], "E2"])
            ACT(E3_t, PB[6], AF.Exp, [], [PK[6], "eng"])
            ACT(deccol, PB[7][:, 0:4], AF.Exp, [], [PK[7], "dec"])
            STT("dve", qin_t, hq, QSC, E1_t, ALU.mult, ALU.mult, ["E1"], [PK[0], "qin"])
            STT("dve", qout_t, hq, QSC, E3_t, ALU.mult, ALU.mult, ["eng"], [PK[0], "qout"])
            TT("pool", kin_t, kk_t, E1n_t, ALU.mult, ["kk", "f"], ["kin"])
            TT("pool", kout_t, kk_t, E2_t, ALU.mult, ["kk", "E2"], ["kout"])
            for hh in range(4):
                hsl = slice(hh * 128, (hh + 1) * 128)
                TR(PBH[4][:, hsl], qin_t[:, hsl], identb, ["qin", "cb"], [PK[4]])
                TR(PBH[5][:, hsl], kin_t[:, hsl], identb, ["kin", "cb"], [PK[5]])
                TR(PBH[6][:, hsl], qout_t[:, hsl], identb, ["qout", "cb"], [PK[6]])
            CP("dve", trq_t, PBH[4][:, 0:512], [], [PK[4], "trq"])
            ACT(trk_t, PBH[5][:, 0:512], AF.Copy, [], [PK[5], "trk"])
            CP("dve", tro_t, PBH[6][:, 0:512], [], [PK[6], "tro"])
            for hh in range(4):
                hsl = slice(hh * 128, (hh + 1) * 128)
                MM(PB[0][:, hsl], trk_t[:, hsl], trq_t[:, hsl], True, True, ["trk", "trq"], [PK[0]])
            TT("dve", v4(am_t), v4(PB[0]), triu4, ALU.mult, ["cst"], [PK[0], "am"])
            for hh in range(4):
                hsl = slice(hh * 128, (hh + 1) * 128)
                if i == 0:
                    MM(PB[1][:, hsl], am_t[:, hsl], v_t[:, hsl], True, True, ["am", "v"], [PK[1]])
                else:
                    MM(PB[1][:, hsl], am_t[:, hsl], v_t[:, hsl], True, False, ["am", "v"], [PK[1]])
                    MM(PB[1][:, hsl], tro_t[:, hsl], Sbf[:, hsl], False, True, ["tro", "Sbf"], [PK[1]])
            for hh in range(4):
                hsl = slice(hh * 128, (hh + 1) * 128)
                MM(PB[2][:, hsl], kout_t[:, hsl], v_t[:, hsl], True, True, ["kout", "v"], [PK[2]])
            if i == 0:
                CP("dve", Sst, PB[2], [], [PK[2], "S"])
            else:
                TT("pool", v4(Sst), v4(Sst), deccol.unsqueeze(2).to_broadcast([128, 4, 128]), ALU.mult, ["dec"], ["S"])
                TT("dve", Sst, Sst, PB[2], ALU.add, [], [PK[2], "S"])
            if i < NT - 1:
                ACT(Sbf, Sst, AF.Copy, ["S"], ["Sbf"])
            ACT(on_t, PB[1], AF.Square, [], [PK[1], "en"])
            RED("dve", ssqo, v4(on_t), ALU.add, ["en"], ["ssqo"])
            ACT(rso, ssqo, AF.Ln, ["ssqo", "epsc"], ["rso"], scale=1.0 / 128, bias=epsc)
            ACT(rso, rso, AF.Exp, [], ["rso"], scale=-0.5)
            TT("dve", v4(on_t), v4(PB[1]), rso.unsqueeze(2).to_broadcast([128, 4, 128]), ALU.mult, ["rso"], [PK[1], "en"])
            TT("pool", v4(on_t), v4(on_t), honb.unsqueeze(1).to_broadcast([128, 4, 128]), ALU.mult, ["honb"], ["en"])
            TT("dve", og_t, on_t, sil_t, ALU.mult, ["en", "sil"], ["og"])
            for hh in range(4):
                hsl = slice(hh * 128, (hh + 1) * 128)
                TR(PBH[3][:, hsl], og_t[:, hsl], identb, ["og", "cb"], [PK[3]])
            ACT(mixT[:, hgp * 4:(hgp + 1) * 4, tsl], PBH[3][:, 0:512].rearrange("p (h t) -> p h t", h=4), AF.Copy, [],
                [PK[3], "mixT%d_%d" % (hgp, i)])
    S.barrier()
    A.off = markC1
    if debug == "C1":
        d = dout("mixT", [128, 16 * S_LEN], BF16)
        DMA("sp", d, mixT.rearrange("p k t -> p (k t)"), [], ["dbg"])
        S.emit(); S.close(); es.close()
        return nc, dbg

    A.off = markC + 16 * 512
    mla_d = nc.dram_tensor("mla_scratch", [128, 9 * S_LEN], BF16).ap()

    def alloc_mla():
        qaT = A.alloc(4 * S_LEN, BF16).rearrange("p (k t) -> p k t", k=4)
        kvaT = A.alloc(2 * S_LEN, BF16).rearrange("p (k t) -> p k t", k=2)
        return qaT, kvaT, A.alloc(S_LEN, BF16), A.alloc(S_LEN, BF16), A.alloc(S_LEN, BF16)
    mla0 = A.off
    qaT, kvaT, kpeT, kpesT, SQR = alloc_mla()
    mla_all = A.ap[:, mla0:mla0 + 9 * S_LEN]
    qagc = A.alloc(4, F32)
    kvgc = A.alloc(2, F32)
    qng = A.alloc(4, F32)
    kng = A.alloc(4, F32)
    DMA("sp", qagc, qag_d, [], ["qagc"])
    DMA("sp", kvgc, kvg_d, [], ["kvgc"])
    DMA("sp", qng, qng_d, [], ["qng"])
    DMA("sp", kng, kng_d, [], ["kng"])
    TT("dve", qng[:, 2:3], qng[:, 2:3], qng[:, 3:4], ALU.mult, [], ["qng"])
    TT("dve", kng[:, 2:3], kng[:, 2:3], kng[:, 3:4], ALU.mult, [], ["kng"])
    sq_t = [A.alloc(512, BF16) for _ in range(2)]
    rsb_t = [A.alloc(512, F32) for _ in range(2)]
    S.op("pool", lambda e: e.memset(SQR, 0.0), [], ["SQR"])
    wA, wB = wb[0], wb[0]
    S.dma("pool", lambda e: e.dma_start(out=wA, in_=win_d[:, 4096:4608].rearrange("(k p) n -> p k n", p=128)), [], ["wb0"])

    def lowrank(wt, wk, nch, dstT, gcol, gk, nfeat, dk):
        for tg in range(4):
            tsl = slice(tg * 512, (tg + 1) * 512)
            for c in range(nch):
                pbi = c % 2
                for k in range(16):
                    MM(PB[pbi], wt[:, k, c * 128:(c + 1) * 128], hT[:, k, tsl], k == 0, k == 15,
                       [wk] + hTall, [PK[pbi]])
                ACT(sq_t[pbi], PB[pbi], AF.Square, [], [PK[pbi], "sq%d" % pbi])
                TS("dve", dstT[:, c, tsl], PB[pbi], gcol[:, c:c + 1], None, ALU.mult, None, [gk],
                   [PK[pbi], dk + "%d_%d" % (c, tg)])
                MM(PB[2], onesb, sq_t[pbi], c == 0, c == nch - 1, ["cb", "sq%d" % pbi], [PK[2]])
            rb = rsb_t[tg % 2]
            rk = "rsb%d" % (tg % 2)
            ACT(rb, PB[2], AF.Ln, ["epsc"], [PK[2], rk], scale=1.0 / nfeat, bias=epsc)
            ACT(rb, rb, AF.Exp, [], [rk], scale=-0.5)
            for c in range(nch):
                TT("pool" if c % 2 else "dve", dstT[:, c, tsl], dstT[:, c, tsl], rb, ALU.mult, [rk],
                   [dk + "%d_%d" % (c, tg)])
    lowrank(wA, "wb0", 4, qaT, qagc, "qagc", 512, "qaT")
    S.op("pool", lambda e: e.memset(wB[:, :, 256:512], 0.0), [], ["wb0"])
    S.dma("pool", lambda e: e.dma_start(out=wB[:, :, 0:256], in_=win_d[:, 4608:4864].rearrange("(k p) n -> p k n", p=128)), [], ["wb0"])
    S.dma("pool", lambda e: e.dma_start(out=wB[:, :, 256:320], in_=win_d[:, 4864:4928].rearrange("(k p) n -> p k n", p=128)), [], ["wb0"])
    S.dma("pool", lambda e: e.dma_start(out=wB[:, :, 384:416], in_=win_d[:, 4896:4928].rearrange("(k p) n -> p k n", p=128)), [], ["wb0"])
    S.dma("pool", lambda e: e.dma_start(out=wB[:, :, 416:448], in_=win_d[:, 4864:4896].rearrange("(k p) n -> p k n", p=128)), [], ["wb0"])
    lowrank(wB, "wb0", 2, kvaT, kvgc, "kvgc", 256, "kvaT")
    for tg in range(4):
        tsl = slice(tg * 512, (tg + 1) * 512)
        for k in range(16):
            MM(PB[3], wB[:, k, 256:384], hT[:, k, tsl], k == 0, k == 15, ["wb0"] + hTall, [PK[3]])
        for k in range(16):
            MM(PB[4], wB[:, k, 384:512], hT[:, k, tsl], k == 0, k == 15, ["wb0"] + hTall, [PK[4]])
        ACT(SQR[0:64, tsl], PB[3][0:64, :], AF.Square, [], [PK[3], "SQR"])
        TS("dve", kpeT[0:64, tsl], PB[3][0:64, :], kng[0:64, 1:2], None, ALU.mult, None, ["kng"], [PK[3], "kpeT"])
        TS("dve", kpesT[0:64, tsl], PB[4][0:64, :], kng[0:64, 2:3], None, ALU.mult, None, ["kng"], [PK[4], "kpesT"])
    qaT_keys = ["qaT%d_%d" % (c, tg) for c in range(4) for tg in range(4)]
    kvaT_keys = ["kvaT%d_%d" % (c, tg) for c in range(2) for tg in range(4)]
    S.barrier()
    if debug == "C2":
        d = dout("qaT", [128, 4 * S_LEN], BF16)
        DMA("sp", d, qaT.rearrange("p k t -> p (k t)"), [], ["dbg"])
        d = dout("kvaT", [128, 2 * S_LEN], BF16)
        DMA("sp", d, kvaT.rearrange("p k t -> p (k t)"), [], ["dbg"])
        d = dout("kpeT", [64, S_LEN], BF16)
        DMA("sp", d, kpeT[0:64, :], [], ["dbg"])
        d = dout("kpesT", [64, S_LEN], BF16)
        DMA("sp", d, kpesT[0:64, :], [], ["dbg"])
        S.emit(); S.close(); es.close()
        return nc, dbg
    qngp = col(4)
    kngp = col(4)
    CP("dve", qngp, qng, ["qng"], ["qngp"])
    CP("dve", kngp, kng, ["kng"], ["kngp"])
    S.barrier()
    topD = mla0 + 9 * S_LEN
    A.off = markHT
    A.cap = mla0
    if debug == "D00":
        d = dout("qaT", [128, 4 * S_LEN], BF16)
        DMA("sp", d, qaT.rearrange("p k t -> p (k t)"), [], ["dbg"])
        d = dout("kvaT", [128, 2 * S_LEN], BF16)
        DMA("sp", d, kvaT.rearrange("p k t -> p (k t)"), [], ["dbg"])
        d = dout("kpeT", [64, S_LEN], BF16)
        DMA("sp", d, kpeT[0:64, :], [], ["dbg"])
        d = dout("kpesT", [64, S_LEN], BF16)
        DMA("sp", d, kpesT[0:64, :], [], ["dbg"])
        S.emit(); S.close(); es.close()
        return nc, dbg
    PI = float(np.pi)
    cosT = A.alloc(S_LEN, F32)
    sinT = A.alloc(S_LEN, F32)
    RT = A.alloc(S_LEN, BF16)
    aonb = A.alloc(128, F32)
    import os
    SK = os.environ.get("SKIPD", "")
    if "a" not in SK:
        DMA("sp", aonb, aon_d.partition_broadcast(128), [], ["aonb"])
    markD0 = A.off
    posi = A.alloc(S_LEN, I32)
    ang = A.alloc(S_LEN, F32)
    kq = A.alloc(S_LEN, F32)
    kqi = A.alloc(S_LEN, I32)
    msk = A.alloc(S_LEN, F32)
    invf = col(1)
    if "i" not in SK:
        DMA("sp", invf[0:64, :], invf_d, [], ["invf"])
    if "p" not in SK:
        DMA("sp", posi[0:64, :], pos_d.partition_broadcast(64), [], ["posi"])
    def dump_kva(tag):
        if debug == tag:
            S.barrier()
            d = dout("kvaT2", [128, 2 * S_LEN], BF16); DMA("sp", d, kvaT.rearrange("p k t -> p (k t)"), [], ["dbg"])
            S.emit(); S.close(); es.close()
            return True
        return False
    if dump_kva("X1"):
        return nc, dbg
    for (dst, shift, key) in ((sinT, 0.0, "sinT"), (cosT, PI / 2, "cosT")):
        a_, q_, qi_, m_ = ang[0:64, :], kq[0:64, :], kqi[0:64, :], msk[0:64, :]
        CP("dve", a_, posi[0:64, :], ["posi"], ["ang"])
        TS("dve", a_, a_, invf[0:64, :], shift, ALU.mult, ALU.add, ["invf"], ["ang"])
        TS("dve", q_, a_, 1.0 / (2 * PI), None, ALU.mult, None, ["ang"], ["kq"])
        CP("dve", qi_, q_, ["kq"], ["kqi"])
        CP("dve", q_, qi_, ["kqi"], ["kq"])
        if key == "sinT" and dump_kva("X2"):
            return nc, dbg
        STT("dve", a_, q_, -2 * PI, a_, ALU.mult, ALU.add, ["kq"], ["ang"])
        TS("dve", m_, a_, PI, None, ALU.is_gt, None, ["ang"], ["msk"])
        STT("dve", a_, m_, -2 * PI, a_, ALU.mult, ALU.add, ["msk"], ["ang"])
        TS("dve", m_, a_, -PI, None, ALU.is_lt, None, ["ang"], ["msk"])
        STT("dve", a_, m_, 2 * PI, a_, ALU.mult, ALU.add, ["msk"], ["ang"])
        if key == "sinT" and dump_kva("X3"):
            return nc, dbg
        ACT(dst[0:64, :], a_, AF.Sin, ["ang"], [key])
        if key == "sinT" and dump_kva("X4"):
            return nc, dbg
    TT("dve", ang[0:64, :], kpeT[0:64, :], cosT[0:64, :], ALU.mult, ["kpeT", "cosT"], ["ang"])
    TT("dve", kq[0:64, :], kpesT[0:64, :], sinT[0:64, :], ALU.mult, ["kpesT", "sinT"], ["kq"])
    TT("dve", RT[0:64, :], ang[0:64, :], kq[0:64, :], ALU.add, ["kq", "ang"], ["RT"])
    S.barrier()
    A.off = markD0
    if debug == "D0":
        d = dout("cosT", [64, S_LEN]); DMA("sp", d, cosT[0:64, :], [], ["dbg"])
        d = dout("sinT", [64, S_LEN]); DMA("sp", d, sinT[0:64, :], [], ["dbg"])
        d = dout("RT", [64, S_LEN], BF16); DMA("sp", d, RT[0:64, :], [], ["dbg"])
        d = dout("kvaT2", [128, 2 * S_LEN], BF16); DMA("sp", d, kvaT.rearrange("p k t -> p (k t)"), [], ["dbg"])
        print("offsets", markHT, mla0, markD0, A.off)
        S.emit(); S.close(); es.close()
        return nc, dbg

    wq_t = [A.alloc(4 * 384, BF16).rearrange("p (k n) -> p k n", k=4) for _ in range(2)]
    wkv_t = [A.alloc(2 * 256, BF16).rearrange("p (k n) -> p k n", k=2) for _ in range(2)]
    QTn_t = [A.alloc(S_LEN, BF16) for _ in range(2)]
    QTr_t = [A.alloc(S_LEN, BF16) for _ in range(2)]
    KTn_t = [A.alloc(S_LEN, BF16) for _ in range(2)]
    KTr_t = [A.alloc(S_LEN, BF16) for _ in range(2)]
    V_t = [A.alloc(16 * 130, BF16).rearrange("p (t v) -> p t v", t=16) for _ in range(2)]
    for sl in range(2):
        S.op("pool", (lambda sl=sl: (lambda e: e.memset(V_t[sl][:, :, 128:130], 1.0)))(), [], ["V%d" % sl])
        S.op("pool", (lambda sl=sl: (lambda e: e.memset(wq_t[sl], 0.0)))(), [], ["wq%d" % sl])
        S.op("pool", (lambda sl=sl: (lambda e: e.memset(QTr_t[sl], 0.0)))(), [], ["QTr%d" % sl])
        S.op("pool", (lambda sl=sl: (lambda e: e.memset(KTr_t[sl], 0.0)))(), [], ["KTr%d" % sl])
    lowtop = A.off
    A.off = topD
    A.cap = A.ap.shape[1]
    sqn_t = [A.alloc(512, BF16) for _ in range(2)]
    sqr_t = [A.alloc(512, BF16) for _ in range(2)]
    rb_t = [A.alloc(512, F32) for _ in range(2)]
    t1_t = [A.alloc(512, F32) for _ in range(2)]
    t2_t = [A.alloc(512, F32) for _ in range(2)]
    A.off = lowtop
    A.cap = mla0
    PT_t = [A.alloc(512, BF16) for _ in range(3)]
    ob_t = [A.alloc(128, BF16) for _ in range(2)]
    junk3 = A.alloc(128, F32)
    ocol = col(8)
    SM_SCALE = 192.0 ** -0.5
    nev = 0
    npt = 0
    for h in range(8):
        sl = h % 2
        s_ = str(sl)
        wq, wkv = wq_t[sl], wkv_t[sl]
        QTn, QTr, KTn, KTr, V = QTn_t[sl], QTr_t[sl], KTn_t[sl], KTr_t[sl], V_t[sl]
        b0 = h * 192
        for (a, bnd, c0, n) in ((0, 128, b0, 128), (128, 192, b0 + 128, 64), (256, 288, b0 + 160, 32), (288, 320, b0 + 128, 32)):
            S.dma("pool", (lambda wq=wq, a=a, bnd=bnd, c0=c0, n=n: (lambda e: e.dma_start(
                out=wq[:, :, a:bnd], in_=wqu_d[:, c0:c0 + n].rearrange("(k p) n -> p k n", p=128))))(), [], ["wq" + s_])
        S.dma("pool", (lambda wkv=wkv, h=h: (lambda e: e.dma_start(
            out=wkv, in_=wkv_d[:, h * 256:(h + 1) * 256].rearrange("(k p) n -> p k n", p=128))))(), [], ["wkv" + s_])
        def proj_body(tg, sl=sl, s_=s_, wq=wq, wkv=wkv, QTn=QTn, QTr=QTr, KTn=KTn, KTr=KTr, V=V):
            tsl = slice(tg * 512, (tg + 1) * 512)
            g2 = tg % 2
            gs = str(g2)
            bA, bB, bC, bD = (0, 1, 2, 3) if g2 == 0 else (4, 5, 6, 7)
            for k in range(4):
                MM(PB[bA], wq[:, k, 0:128], qaT[:, k, tsl], k == 0, k == 3, ["wq" + s_] + qaT_keys, [PK[bA]])
            for k in range(4):
                MM(PB[bB], wq[:, k, 128:256], qaT[:, k, tsl], k == 0, k == 3, ["wq" + s_] + qaT_keys, [PK[bB]])
            for k in range(4):
                MM(PB[bC], wq[:, k, 256:384], qaT[:, k, tsl], k == 0, k == 3, ["wq" + s_] + qaT_keys, [PK[bC]])
            yield
            ACT(sqn_t[g2], PB[bA], AF.Square, [], [PK[bA], "sqn" + gs])
            ACT(sqr_t[g2], PB[bB], AF.Square, [], [PK[bB], "sqr" + gs])
            yield
            MM(PB[bD], onesb, sqn_t[g2], True, False, ["cb", "sqn" + gs], [PK[bD]])
            MM(PB[bD], onesb, sqr_t[g2], False, True, ["cb", "sqr" + gs], [PK[bD]])
            yield
            rb = rb_t[g2]
            ACT(rb, PB[bD], AF.Ln, ["epsc"], [PK[bD], "rb" + gs], scale=1.0 / 192, bias=epsc)
            ACT(rb, rb, AF.Exp, [], ["rb" + gs], scale=-0.5)
            yield
            STT("dve", QTn[:, tsl], PB[bA], qngp[:, 0:1], rb, ALU.mult, ALU.mult, ["qngp", "rb" + gs], [PK[bA], "QTn" + s_])
            STT("dve", t1_t[g2][0:64, :], PB[bB][0:64, :], qngp[0:64, 1:2], cosT[0:64, tsl], ALU.mult, ALU.mult,
                ["qngp", "cosT"], [PK[bB], "t1" + gs])
            STT("dve", t2_t[g2][0:64, :], PB[bC][0:64, :], qngp[0:64, 2:3], sinT[0:64, tsl], ALU.mult, ALU.mult,
                ["qngp", "sinT"], [PK[bC], "t2" + gs])
            yield
            TT("pool", t1_t[g2][0:64, :], t1_t[g2][0:64, :], t2_t[g2][0:64, :], ALU.add, ["t2" + gs], ["t1" + gs])
            TT("pool", QTr[0:64, tsl], t1_t[g2][0:64, :], rb[0:64, :], ALU.mult, ["t1" + gs, "rb" + gs], ["QTr" + s_])
            for k in range(2):
                MM(PB[bA], wkv[:, k, 0:128], kvaT[:, k, tsl], k == 0, k == 1, ["wkv" + s_] + kvaT_keys, [PK[bA]])
            for j in range(4):
                t = tg * 4 + j
                for k in range(2):
                    MM(PB[bB][:, j * 128:(j + 1) * 128], kvaT[:, k, t * 128:(t + 1) * 128], wkv[:, k, 128:256],
                       k == 0, k == 1, ["wkv" + s_] + kvaT_keys, [PK[bB]])
            yield
            ACT(sqn_t[g2], PB[bA], AF.Square, [], [PK[bA], "sqn" + gs])
            ACT(V[:, tg * 4:(tg + 1) * 4, 0:128], PB[bB].rearrange("p (t v) -> p t v", t=4), AF.Copy, [],
                [PK[bB], "V" + s_])
            yield
            MM(PB[bD], onesb, sqn_t[g2], True, False, ["cb", "sqn" + gs], [PK[bD]])
            MM(PB[bD], onesb, SQR[:, tsl], False, True, ["cb", "SQR"], [PK[bD]])
            yield
            ACT(rb, PB[bD], AF.Ln, ["epsc"], [PK[bD], "rb" + gs], scale=1.0 / 192, bias=epsc)
            ACT(rb, rb, AF.Exp, [], ["rb" + gs], scale=-0.5)
            yield
            STT("dve", KTn[:, tsl], PB[bA], kngp[:, 0:1], rb, ALU.mult, ALU.mult, ["kngp", "rb" + gs], [PK[bA], "KTn" + s_])
            TT("pool", KTr[0:64, tsl], RT[0:64, tsl], rb[0:64, :], ALU.mult, ["RT", "rb" + gs], ["KTr" + s_])
            yield
        pipeline([proj_body(tg) for tg in range(4)], 2)
        steps = [(G, kt) for G in range(4) for kt in range(4 * G + 4)]

        def geom(G, kt):
            j0 = max(0, kt - 4 * G)
            return j0, (4 - j0) * 128, (4 * G + j0) * 128

        def emit_ST(n):
            G, kt = steps[n]
            j0, ncol, q0 = geom(G, kt)
            sb = n % 2
            MM(PB[sb][:, 0:ncol], KTn[:, kt * 128:(kt + 1) * 128], QTn[:, q0:q0 + ncol], True, False,
               ["KTn" + s_, "QTn" + s_], [PK[sb]])
            MM(PB[sb][:, 0:ncol], KTr[:, kt * 128:(kt + 1) * 128], QTr[:, q0:q0 + ncol], False, True,
               ["KTr" + s_, "QTr" + s_], [PK[sb]])
        emit_ST(0)
        for n in range(len(steps)):
            G, kt = steps[n]
            j0, ncol, q0 = geom(G, kt)
            sb = n % 2
            pt = PT_t[npt % 3]
            pk = "PT%d" % (npt % 3)
            npt += 1
            if n + 1 < len(steps):
                emit_ST(n + 1)
            ACT(pt[:, 0:ncol], PB[sb][:, 0:ncol], AF.Exp, [], [PK[sb], pk], scale=SM_SCALE)
            if kt >= 4 * G:
                TT("pool", pt[:, 0:128], pt[:, 0:128], triub, ALU.mult, ["cb"], [pk])
            for j in range(j0, 4):
                qt = 4 * G + j
                MM(PB[2 + j][:, 0:129], pt[:, (j - j0) * 128:(j - j0 + 1) * 128], V[:, kt, 0:129],
                   kt == 0, kt == qt, [pk, "V" + s_], [PK[2 + j]])
                if kt == qt:
                    e2 = nev % 2
                    es_ = str(e2)
                    nev += 1
                    O = PB[2 + j]
                    ssq = ocol[:, e2 * 4:e2 * 4 + 1]
                    tt1 = ocol[:, e2 * 4 + 1:e2 * 4 + 2]
                    tt2 = ocol[:, e2 * 4 + 2:e2 * 4 + 3]
                    den = ocol[:, e2 * 4 + 3:e2 * 4 + 4]
                    ACT(junk3, O[:, 0:128], AF.Square, [], [PK[2 + j], "junk3", "ossq" + es_], accum=ssq)
                    CP("dve", den, O[:, 128:129], [], [PK[2 + j], "oden" + es_])
                    STT("dve", tt1, den, EPS, den, ALU.mult, ALU.mult, ["oden" + es_], ["ott1" + es_])
                    STT("dve", tt2, ssq, 1.0 / 128, tt1, ALU.mult, ALU.add, ["ossq" + es_, "ott1" + es_], ["ott2" + es_])
                    ACT(tt2, tt2, AF.Ln, [], ["ott2" + es_])
                    ACT(tt2, tt2, AF.Exp, [], ["ott2" + es_], scale=-0.5)
                    STT("dve", ob_t[e2], O[:, 0:128], tt2, aonb, ALU.mult, ALU.mult, ["ott2" + es_, "aonb"],
                        [PK[2 + j], "ob" + es_])
                    TR(PBH[6][:, 0:128], ob_t[e2], identb, ["ob" + es_, "cb"], [PK[6]])
                    ACT(mixT[:, 8 + h, qt * 128:(qt + 1) * 128], PBH[6][:, 0:128], AF.Copy, [],
                        [PK[6], "mixT%d_%d" % (8 + h, qt)])
    S.barrier()
    A.off = markHT
    A.cap = A.ap.shape[1]
    if debug == "D":
        d = dout("mixT", [128, 16 * S_LEN], BF16)
        DMA("sp", d, mixT.rearrange("p k t -> p (k t)"), [], ["dbg"])
        S.emit(); S.close(); es.close()
        return nc, dbg

    markE = A.off
    wo = A.alloc(16 * D, BF16).rearrange("p (k n) -> p k n", k=16)
    g1b = A.alloc(D, F32)
    xt = [A.alloc(D, F32) for _ in range(2)]
    tmpe = [A.alloc(512, F32) for _ in range(2)]
    for n in range(4):
        S.dma("pool", (lambda n=n: (lambda e: e.dma_start(
            out=wo[:, :, n * 512:(n + 1) * 512],
            in_=wout_d[:, n * 512:(n + 1) * 512].rearrange("(k p) n -> p k n", p=128))))(), [], ["wo%d" % n])
    DMA("sp", g1b, modrow_d[0:1, 2 * D:3 * D].partition_broadcast(128), ["modrow_d"], ["g1b"])
    mix_keys = []
    for i in range(NT):
        sl = i % 2
        DMA("sp", xt[sl], x_d[i * 128:(i + 1) * 128, :], [], ["xt%d" % sl])
        for n in range(4):
            pbi = (i * 4 + n) % 4
            for k in range(16):
                MM(PB[pbi], mixT[:, k, i * 128:(i + 1) * 128], wo[:, k, n * 512:(n + 1) * 512], k == 0, k == 15,
                   ["wo%d" % n], [PK[pbi]])
            tp = tmpe[n % 2]
            TT("dve", tp, PB[pbi], g1b[:, n * 512:(n + 1) * 512], ALU.mult, ["g1b"], [PK[pbi], "tmpe%d" % (n % 2)])
            TT("pool", xt[sl][:, n * 512:(n + 1) * 512], xt[sl][:, n * 512:(n + 1) * 512], tp, ALU.add,
               ["tmpe%d" % (n % 2)], ["xt%d" % sl])
        DMA("sp", out_d[i * 128:(i + 1) * 128, :], xt[sl], ["xt%d" % sl], ["out"])
    S.barrier()
    A.off = markMix
    if debug == "E1":
        S.emit(); S.close(); es.close()
        return nc, dbg

    h2_d = nc.dram_tensor("h2_scratch", [S_LEN, D], BF16).ap()
    A2b = A.alloc(D, F32)
    B2b = A.alloc(D, F32)
    g2b = A.alloc(D, F32)
    LG = A.alloc(16 * 36, F32).rearrange("p (t n) -> p t n", t=16)
    IDXW = A.alloc(64, I32)
    markE2 = A.off
    n2gb = A.alloc(D, F32)
    wgr = A.alloc(16 * 36, F32).rearrange("p (k n) -> p k n", k=16)
    bgrb = A.alloc(36, F32)
    DMA("sp", A2b, modrow_d[0:1, 4 * D:5 * D].partition_broadcast(128), ["modrow_d"], ["A2b"])
    DMA("sp", B2b, modrow_d[0:1, 3 * D:4 * D].partition_broadcast(128), ["modrow_d"], ["B2b"])
    DMA("sp", g2b, modrow_d[0:1, 5 * D:6 * D].partition_broadcast(128), ["modrow_d"], ["g2b"])
    DMA("sp", n2gb, n2g_d.partition_broadcast(128), [], ["n2gb"])
    DMA("sp", wgr, wgr_d.rearrange("(k p) n -> p k n", p=128), [], ["wgr"])
    DMA("sp", bgrb, bgr_d.partition_broadcast(128), [], ["bgrb"])
    STT("dve", A2b, A2b, 1.0, n2gb, ALU.add, ALU.mult, ["n2gb"], ["A2b"])
    xt = [A.alloc(D, F32) for _ in range(2)]
    h2f = A.alloc(D, F32)
    h2b = [A.alloc(D, BF16) for _ in range(2)]
    h2T = A.alloc(16 * 128, F32).rearrange("p (k t) -> p k t", k=16)
    ssq2 = col(16)
    rs2 = col(16)
    for i in range(NT):
        sl = i % 2
        DMA("sp", xt[sl], out_d[i * 128:(i + 1) * 128, :], ["out"], ["xt%d" % sl])
        ACT(h2f, xt[sl], AF.Square, ["xt%d" % sl], ["h2f", "ssq2_%d" % i], accum=ssq2[:, i:i + 1])
        rstd_col(rs2[:, i:i + 1], ssq2[:, i:i + 1], D, ["ssq2_%d" % i, "epsc"], ["rs2_%d" % i], "rs2t_%d" % i)
        STT("dve", h2f, xt[sl], rs2[:, i:i + 1], A2b, ALU.mult, ALU.mult, ["rs2_%d" % i, "A2b"], ["h2f"])
        TT("pool", h2f, h2f, B2b, ALU.add, ["B2b"], ["h2f"])
        ACT(h2b[sl], h2f, AF.Copy, ["h2f"], ["h2b%d" % sl])
        DMA("sp", h2_d[i * 128:(i + 1) * 128, :], h2b[sl], ["h2b%d" % sl], ["h2_d%d" % i])
        for q in range(4):
            for kk in range(4):
                k = q * 4 + kk
                TR(PB[q][:, kk * 128:(kk + 1) * 128], h2f[:, k * 128:(k + 1) * 128], identf, ["h2f", "cst"], [PK[q]])
            CP("dve" if q % 2 else "act", h2T[:, q * 4:(q + 1) * 4, :], PB[q].rearrange("p (k t) -> p k t", k=4),
               [], [PK[q], "h2T"]) if q % 2 else ACT(h2T[:, q * 4:(q + 1) * 4, :],
               PB[q].rearrange("p (k t) -> p k t", k=4), AF.Copy, [], [PK[q], "h2T"])
        for k in range(16):
            MM(PB[4][:, 0:36], h2T[:, k, :], wgr[:, k, :], k == 0, k == 15, ["h2T", "wgr"], [PK[4]])
        TT("dve", LG[:, i, :], PB[4][:, 0:36], bgrb, ALU.add, ["bgrb"], [PK[4], "LG"])
    S.barrier()
    A.off = markE2
    if debug == "E2":
        d = dout("LG", [128, 16 * 36]); DMA("sp", d, LG.rearrange("p t n -> p (t n)"), [], ["dbg"])
        d = dout("h2", [S_LEN, D], BF16); DMA("sp", d, h2_d, [], ["dbg"])
        S.emit(); S.close(); es.close()
        return nc, dbg

    BIG = 1.0e30

    def T3(n, m):
        t = A.alloc(16 * n * m, F32)
        return t.rearrange("p (t n) -> p t n", t=16) if m == 1 else t.rearrange("p (t n m) -> p t n m", t=16, n=n)
    def bc(ap2, shape):
        v = ap2
        for ax in range(2, len(shape)):
            v = v.unsqueeze(ax)
        return v.to_broadcast(shape)
    G = LG[:, :, 0:4]
    EL = LG[:, :, 4:36]
    gmax = A.alloc(16, F32)
    RED("dve", gmax, G, ALU.max, ["LG"], ["gmax"])
    gone = T3(4, 1)
    TT("dve", gone, G, bc(gmax, [128, 16, 4]), ALU.is_equal, ["gmax", "LG"], ["gone"])
    gd = T3(4, 1)
    TT("dve", gd, G, bc(gmax, [128, 16, 4]), ALU.subtract, ["gmax", "LG"], ["gd"])
    ACT(gd, gd, AF.Exp, [], ["gd"])
    pg = A.alloc(16, F32)
    RED("dve", pg, gd, ALU.add, ["gd"], ["pg"])
    RCP(pg, pg, [], ["pg"])
    pen = T3(4, 1)
    TS("dve", pen, gone, -1.0, BIG, ALU.add, ALU.mult, ["gone"], ["pen"])
    EM = T3(32, 1)
    TT("dve", EM.rearrange("p t (g e) -> p t g e", g=4), EL.rearrange("p t (g e) -> p t g e", g=4),
       pen.unsqueeze(3).to_broadcast([128, 16, 4, 8]), ALU.add, ["pen", "LG"], ["EM"])
    v1 = A.alloc(16, F32)
    RED("dve", v1, EM, ALU.max, ["EM"], ["v1"])
    M1 = T3(32, 1)
    TT("dve", M1, EM, bc(v1, [128, 16, 32]), ALU.is_equal, ["EM", "v1"], ["M1"])
    EM2 = T3(32, 1)
    STT("dve", EM2, M1, -BIG, EM, ALU.mult, ALU.add, ["M1", "EM"], ["EM2"])
    v2 = A.alloc(16, F32)
    RED("dve", v2, EM2, ALU.max, ["EM2"], ["v2"])
    M2 = T3(32, 1)
    TT("dve", M2, EM2, bc(v2, [128, 16, 32]), ALU.is_equal, ["EM2", "v2"], ["M2"])
    e21 = A.alloc(16, F32)
    TT("dve", e21, v2, v1, ALU.subtract, ["v1", "v2"], ["e21"])
    ACT(e21, e21, AF.Exp, [], ["e21"])
    w1 = A.alloc(16, F32)
    w2 = A.alloc(16, F32)
    TS("dve", w1, e21, 1.0, None, ALU.add, None, ["e21"], ["w1"])
    RCP(w1, w1, [], ["w1"])
    TT("dve", w1, w1, pg, ALU.mult, ["pg"], ["w1"])
    TT("dve", w2, w1, e21, ALU.mult, ["w1", "e21"], ["w2"])
    Mb = A.alloc(16 * 32, BF16).rearrange("p (t n) -> p t n", t=16)
    TT("dve", Mb, M1, M2, ALU.add, ["M1", "M2"], ["Mb"])
    for i in range(NT):
        MM(PB[0][:, i * 32:(i + 1) * 32], trilsb, Mb[:, i, :], True, i == 0, ["cb", "Mb"], [PK[0]])
        for j in range(i):
            MM(PB[0][:, i * 32:(i + 1) * 32], onesb, Mb[:, j, :], False, j == i - 1, ["cb", "Mb"], [PK[0]])
    POS = T3(32, 1)
    CP("dve", POS.rearrange("p t n -> p (t n)"), PB[0], [], [PK[0], "POS"])
    for j in range(NT):
        MM(PB[1][:, 0:32], onesb, Mb[:, j, :], j == 0, j == NT - 1, ["cb", "Mb"], [PK[1]])
    cnt = A.alloc(32, F32)
    CP("dve", cnt, PB[1][:, 0:32], [], [PK[1], "cnt"])
    cmp1 = A.alloc(32 * 16, F32).rearrange("p (e m) -> p e m", e=32)
    TT("dve", cmp1, cnt.unsqueeze(2).to_broadcast([128, 32, 16]), thr16.unsqueeze(1).to_broadcast([128, 32, 16]),
       ALU.is_gt, ["cnt", "cst"], ["cmp1"])
    padded = A.alloc(32, F32)
    RED("dve", padded, cmp1, ALU.add, ["cmp1"], ["padded"])
    TS("dve", padded, padded, 128.0, None, ALU.mult, None, [], ["padded"])
    cs = [A.alloc(32, F32) for _ in range(2)]
    CP("dve", cs[0], padded, ["padded"], ["cs0"])
    cur = 0
    for sh in (1, 2, 4, 8, 16):
        nx = 1 - cur
        CP("dve", cs[nx][:, 0:sh], cs[cur][:, 0:sh], ["cs%d" % cur], ["cs%d" % nx])
        TT("dve", cs[nx][:, sh:32], cs[cur][:, sh:32], cs[cur][:, 0:32 - sh], ALU.add, ["cs%d" % cur], ["cs%d" % nx])
        cur = nx
    pad_end = cs[cur]
    pek = "cs%d" % cur
    pad_start = A.alloc(32, F32)
    TT("dve", pad_start, pad_end, padded, ALU.subtract, [pek, "padded"], ["pad_start"])
    cmp2 = A.alloc(64 * 32, F32).rearrange("p (j e) -> p j e", j=64)
    TT("dve", cmp2, pad_end.unsqueeze(1).to_broadcast([128, 64, 32]), thr64.unsqueeze(2).to_broadcast([128, 64, 32]),
       ALU.is_le, [pek, "cst"], ["cmp2"])
    blke = A.alloc(64, F32)
    RED("dve", blke, cmp2, ALU.add, ["cmp2"], ["blke"])
    TS("dve", blke, blke, 31.0, None, ALU.min, None, [], ["blke"])
    same = A.alloc(64, F32)
    S.op("pool", lambda e: e.memset(same, 0.0), [], ["same"])
    TT("dve", same[:, 2:64], blke[:, 2:64], blke[:, 0:62], ALU.is_equal, ["blke"], ["same"])
    TS("dve", blke, blke, 128.0, iota_p, ALU.mult, ALU.add, ["cst"], ["blke"])
    STT("dve", blke, same, 8192.0, blke, ALU.mult, ALU.add, ["same"], ["blke"])
    CP("dve", IDXW, blke, ["blke"], ["IDXW"])
    Tt = T3(32, 1)
    TT("dve", Tt, POS, pad_start.unsqueeze(1).to_broadcast([128, 16, 32]), ALU.add, ["POS", "pad_start"], ["Tt"])
    prod = T3(32, 1)
    dstf = A.alloc(32, F32).rearrange("p (t k) -> p t k", t=16)
    TT("dve", prod, M1, Tt, ALU.mult, ["M1", "Tt"], ["prod"])
    RED("dve", dstf[:, :, 0], prod, ALU.add, ["prod"], ["dstf"])
    TT("dve", prod, M2, Tt, ALU.mult, ["M2", "Tt"], ["prod"])
    RED("dve", dstf[:, :, 1], prod, ALU.add, ["prod"], ["dstf"])
    DI = A.alloc(32, I32)
    CP("dve", DI, dstf.rearrange("p t k -> p (t k)"), ["dstf"], ["DI"])
    tokf = A.alloc(16, F32)
    TS("dve", tokf, thr16, iota_p, None, ALU.add, None, ["cst"], ["tokf"])
    with nc.allow_non_contiguous_dma(reason="64B meta tails"):
        pass
    DMA("sp", xbuf_d[:, D:D + 32], metai_d, [], ["xbuf"])
    if debug == "E3":
        S.barrier()
        d = dout("LG", [128, 16 * 36]); DMA("sp", d, LG.rearrange("p t n -> p (t n)"), [], ["dbg"])
        d = dout("IDXW", [128, 64], I32); DMA("sp", d, IDXW, [], ["dbg"])
        d = dout("DI", [128, 32], I32); DMA("sp", d, DI, [], ["dbg"])
        d = dout("w1", [128, 16]); DMA("sp", d, w1, [], ["dbg"])
        d = dout("w2", [128, 16]); DMA("sp", d, w2, [], ["dbg"])
        d = dout("cnt", [128, 32]); DMA("sp", d, cnt, [], ["dbg"])
        d = dout("M1", [128, 512]); DMA("sp", d, M1.rearrange("p t n -> p (t n)"), [], ["dbg"])
        d = dout("M2", [128, 512]); DMA("sp", d, M2.rearrange("p t n -> p (t n)"), [], ["dbg"])
        S.emit(); S.close(); es.close()
        return nc, dbg
    hbx = [[A.alloc(D + 32, BF16) for _ in range(2)] for _ in range(2)]
    for i in range(NT):
        sl = i % 2
        for k in range(2):
            c = i * 2 + k
            hb = hbx[k][sl]
            hk = "hb%d_%d" % (k, sl)
            DMA("sp", hb[:, 0:D], h2_d[i * 128:(i + 1) * 128, :], ["h2_d%d" % i], [hk])
            tailF = hb[:, D:D + 32].bitcast(F32)
            tailI = hb[:, D:D + 32].bitcast(I32)
            CP("dve", tailI[:, 0:1], tokf[:, i:i + 1], ["tokf"], [hk])
            CP("dve", tailF[:, 1:2], (w1, w2)[k][:, i:i + 1], ["w1", "w2"], [hk])
            S.dma("pool", (lambda hb=hb, c=c: (lambda e: e.indirect_dma_start(
                out=xbuf_d, out_offset=bass.IndirectOffsetOnAxis(ap=DI[:, c:c + 1], axis=0),
                in_=hb, in_offset=None, bounds_check=breg(e, NSLOT - 1), oob_is_err=False)))(),
                [hk, "DI"], ["xbuf"])
    S.barrier()
    A.off = markE2
    markF = A.off

    wg_t = [A.alloc(16 * 512, BF16) for _ in range(2)]
    wu_t = [A.alloc(16 * 512, BF16) for _ in range(2)]
    wd_t = [A.alloc(4 * D, BF16) for _ in range(2)]
    xb_t = [A.alloc(D + 32, BF16) for _ in range(2)]
    XT_t = [A.alloc(16 * 128, BF16).rearrange("p (k s) -> p k s", k=16) for _ in range(2)]
    en_f = [A.alloc(512, F32) for _ in range(2)]
    tg_f = [A.alloc(512, F32) for _ in range(2)]
    act_b = [A.alloc(512, BF16) for _ in range(2)]
    actT = [A.alloc(4 * 128, BF16).rearrange("p (k s) -> p k s", k=4) for _ in range(2)]
    Y_t = [A.alloc(D, F32) for _ in range(2)]

    def pf(j, which):
        sl = j % 2
        s_ = str(sl)
        lst = []
        if "g" in which:
            lst += [(wg_t[sl], wg_d, "wg"), (wu_t[sl], wu_d, "wu")]
        if "d" in which:
            lst += [(wd_t[sl], wd_d, "wd")]
        for (dst, src, key) in lst:
            S.dma("pool", (lambda dst=dst, src=src, j=j: (lambda e: e.indirect_dma_start(
                out=dst, out_offset=None, in_=src, in_offset=bass.IndirectOffsetOnAxis(ap=IDXW[:, j:j + 1], axis=0),
                bounds_check=breg(e, 32 * 128 - 1), oob_is_err=False)))(), ["IDXW"], [key + s_])
        if "d" in which:
            DMA("sp", xb_t[sl], xbuf_d[j * 128:(j + 1) * 128, :], ["xbuf"], ["xb" + s_])

    def stage_A(j):
        sl = j % 2
        s_ = str(sl)
        bG, bU = 2 * sl, 2 * sl + 1
        xb, XT = xb_t[sl], XT_t[sl]
        xbv = xb[:, 0:D].rearrange("p (f k) -> p k f", k=16)
        for hf, bb in ((0, 4), (1, 5)):
            for kk in range(8):
                TR(PBH[bb][:, kk * 128:(kk + 1) * 128], xbv[:, hf * 8 + kk, :], identb, ["xb" + s_, "cb"], [PK[bb]])
        CP("dve", XT[:, 0:8, :], PBH[4].rearrange("p (k s) -> p k s", k=8), [], [PK[4], "XT" + s_])
        ACT(XT[:, 8:16, :], PBH[5].rearrange("p (k s) -> p k s", k=8), AF.Copy, [], [PK[5], "XT" + s_])
        wg, wu = wg_t[sl], wu_t[sl]
        for k in range(16):
            MM(PB[bG], XT[:, k, :], wg[:, k * 512:(k + 1) * 512], k == 0, k == 15, ["XT" + s_, "wg" + s_], [PK[bG]])
        for k in range(16):
            MM(PB[bU], XT[:, k, :], wu[:, k * 512:(k + 1) * 512], k == 0, k == 15, ["XT" + s_, "wu" + s_], [PK[bU]])

    def stage_B1(j):
        sl = j % 2
        s_ = str(sl)
        bG, bU = 2 * sl, 2 * sl + 1
        ACT(en_f[sl], PB[bG], AF.Exp, [], [PK[bG], "en_f" + s_], scale=-1.0)
        ACT(en_f[sl], en_f[sl], AF.Ln, [], ["en_f" + s_], bias=onec)
        ACT(en_f[sl], en_f[sl], AF.Exp, [], ["en_f" + s_], scale=-1.0)
        TT("dve", tg_f[sl], PB[bG], en_f[sl], ALU.mult, ["en_f" + s_], [PK[bG], "tg_f" + s_])
        TT("dve", act_b[sl], tg_f[sl], PB[bU], ALU.mult, ["tg_f" + s_], [PK[bU], "act_b" + s_])
        abv = act_b[sl].rearrange("p (j k) -> p k j", k=4)
        for kk in range(4):
            TR(PBH[6][:, kk * 128:(kk + 1) * 128], abv[:, kk, :], identb, ["act_b" + s_, "cb"], [PK[6]])
        ACT(actT[sl], PBH[6][:, 0:512].rearrange("p (k s) -> p k s", k=4), AF.Copy, [], [PK[6], "actT" + s_])

    def stage_B2(j):
        sl = j % 2
        s_ = str(sl)
        bG, bU = 2 * sl, 2 * sl + 1
        wd = wd_t[sl]
        xb = xb_t[sl]
        Y = Y_t[sl]
        wcol = xb[:, D:D + 32].bitcast(F32)[:, 1:2]
        for n in range(4):
            pbi = (bG, bU, 7, bG)[n] if False else (bG if n % 2 == 0 else bU)
            for kk in range(4):
                MM(PB[pbi], actT[sl][:, kk, :], wd[:, kk * D + n * 512: kk * D + (n + 1) * 512], kk == 0, kk == 3,
                   ["actT" + s_, "wd" + s_], [PK[pbi]])
            STT("dve", Y[:, n * 512:(n + 1) * 512], PB[pbi], wcol, g2b[:, n * 512:(n + 1) * 512],
                ALU.mult, ALU.mult, ["xb" + s_, "g2b"], [PK[pbi], "Y" + s_])
        S.dma("pool", (lambda sl=sl, Y=Y: (lambda e: e.indirect_dma_start(
            out=out_d, out_offset=bass.IndirectOffsetOnAxis(ap=xb_t[sl][:, D:D + 32].bitcast(I32)[:, 0:1], axis=0),
            in_=Y, in_offset=None, bounds_check=breg(e, S_LEN + 127), oob_is_err=True, compute_op=ALU.add)))(),
            ["Y" + s_, "xb" + s_], ["out"])
    pf(0, "gd")
    pf(1, "gd")
    stage_A(0)
    pf(2, "g")
    for j in range(NBLK):
        stage_B1(j)
        stage_B2(j)
        if j + 2 < NBLK:
            pf(j + 2, "d")
        if j + 1 < NBLK:
            stage_A(j + 1)
        if j + 3 < NBLK:
            pf(j + 3, "g")
    S.emit()
    S.close()
    es.close()
    return nc, dbg


def host_inputs(inputs, b):
    f = np.float32
    m = {}
    m["x"] = np.ascontiguousarray(inputs["x"][b])
    m["ccol"] = np.ascontiguousarray(inputs["c"][b].reshape(16, 128).T)
    m["pos"] = np.ascontiguousarray(inputs["positions"][b].reshape(1, S_LEN)).astype(np.int32)
    m["w_ada"] = inputs["w_ada"][0]
    m["b_ada"] = inputs["b_ada"][0].reshape(1, -1)
    m["norm1_gc"] = np.ascontiguousarray(inputs["norm1_g"][0].reshape(16, 128).T)
    m["w_in"] = inputs["w_in"][0]
    m["lb_logits"] = inputs["hgrn_lb_logits"]
    m["hgrn_onorm_g"] = inputs["hgrn_onorm_g"][0].reshape(1, 128)
    m["q_a_gc"] = np.ascontiguousarray(inputs["q_a_norm_g"][0].reshape(4, 128).T)
    m["w_q_up"] = inputs["w_q_up"][0]
    m["kv_a_gc"] = np.ascontiguousarray(inputs["kv_a_norm_g"][0].reshape(2, 128).T)
    m["w_kv_up"] = inputs["w_kv_up"][0]

    def qk_cols(g):
        o = np.zeros((128, 4), f)
        o[:, 0] = g[0:128]
        o[0:64, 1] = g[128:192]
        o[0:32, 2] = g[160:192]
        o[32:64, 2] = g[128:160]
        o[0:32, 3] = -1.0
        o[32:64, 3] = 1.0
        return o
    m["q_norm_gc"] = qk_cols(inputs["q_norm_g"][0])
    m["k_norm_gc"] = qk_cols(inputs["k_norm_g"][0])
    m["attn_onorm_g"] = inputs["attn_onorm_g"][0].reshape(1, 128)
    m["w_out"] = inputs["w_out"][0]
    m["norm2_g"] = inputs["norm2_g"][0].reshape(1, D)
    m["w_gr"] = np.ascontiguousarray(np.concatenate([inputs["w_group"][0], inputs["w_router"][0]], axis=1))
    m["b_gr"] = np.concatenate([inputs["b_group"][0], inputs["b_router"][0]]).reshape(1, 36)
    m["w_gate"] = inputs["w_gate"][0].reshape(32 * 128, 16 * 512)
    m["w_up"] = inputs["w_up"][0].reshape(32 * 128, 16 * 512)
    m["w_down"] = inputs["w_down"][0].reshape(32 * 128, 4 * 2048)
    m["consts"] = CONSTS
    m["invf"] = INVF
    m["meta_init"] = META_INIT.view(ml_dtypes.bfloat16)
    return m


def _consts():
    c = np.zeros((128, 1024), np.float32)
    s = np.arange(128)[:, None]
    t = np.arange(128)[None, :]
    c[:, 0:128] = np.eye(128)
    c[:, 128:256] = (s <= t)
    c[:, 256:384] = (s <= t).astype(np.float32) - (s <= 63).astype(np.float32)
    c[:, 384:512] = (s > t)
    c[:, 512:640] = (s < t)
    c[:, 640] = np.arange(128)
    c[:, 656:672] = np.arange(16) * 128
    c[:, 672:736] = np.arange(64) * 128
    c[:, 736:768] = np.arange(32)
    return c


CONSTS = _consts()
INVF = (10000.0 ** (-(np.arange(64) % 32).astype(np.float32) * 2 / 64)).astype(np.float32).reshape(64, 1)
META_INIT = np.zeros((NSLOT, 16), np.int32)
META_INIT[:, 0] = 2048 + (np.arange(NSLOT) % 128)

_NC = None


def kernel(**inputs):
    global _NC
    if _NC is None:
        _NC = build()[0]
    inputs = {k: np.asarray(v) for k, v in inputs.items()}
    in_maps = [host_inputs(inputs, b) for b in range(8)]
    res = run_bass_kernel_spmd(_NC, in_maps, core_ids=list(range(8)))
    out = np.stack([np.asarray(r["out"])[:S_LEN] for r in res.results], axis=0)
    return out.astype(np.float32)
```

```python
import numpy as np
import ml_dtypes
import concourse.bass as bass
import concourse.mybir as mybir
from concourse.bass_utils import run_bass_kernel_spmd

F32 = mybir.dt.float32
BF16 = mybir.dt.bfloat16
I32 = mybir.dt.int32
AF = mybir.ActivationFunctionType
ALU = mybir.AluOpType
AX = mybir.AxisListType

D = 2048
S_LEN = 2048
NT = 16
EPS = 1e-6
IN_COLS = 4928
BLK = 128
NBLK = 64
NSLOT = NBLK * BLK
DEBUG = None


class Sync:
    def __init__(self, nc, n_dma_sems=32):
        self.nc = nc
        self.eng = {"pe": nc.tensor, "dve": nc.vector, "act": nc.scalar,
                    "pool": nc.gpsimd, "sp": nc.sync}
        self.sem = {}
        self.cnt = {}
        self._ctx = []
        for e in self.eng:
            cm = nc.semaphore("s_" + e)
            self.sem[e] = cm.__enter__()
            self._ctx.append(cm)
            self.cnt[e] = 0
        self.dma_sems = []
        self.dma_pool = {"sp": [], "pool": [], "act": []}
        self.dma_rr = {"sp": 0, "pool": 0, "act": 0}
        for q, n in (("sp", n_dma_sems // 2), ("pool", n_dma_sems // 2), ("act", 2)):
            for i in range(n):
                cm = nc.semaphore("d%s%d" % (q, i))
                slot = [cm.__enter__(), 0, None]
                self.dma_sems.append(slot)
                self.dma_pool[q].append(slot)
                self._ctx.append(cm)
        self.waited = {}
        self.last_w = {}
        self.readers = {}
        self.prog = {e: [] for e in self.eng}

    def close(self):
        for cm in reversed(self._ctx):
            cm.__exit__(None, None, None)

    def _wait(self, e, tok):
        if tok is None:
            return
        sem, sid, val, src = tok
        if src == e and e == "pe":
            return
        k = (e, sid)
        if self.waited.get(k, 0) >= val:
            return
        self.waited[k] = val
        self.prog[e].append(("w", sem, val))

    def _deps(self, e, reads, writes, skip_same_war=True):
        for r in reads:
            self._wait(e, self.last_w.get(r))
        for w in writes:
            self._wait(e, self.last_w.get(w))
            for tok in self.readers.get(w, ()):
                if skip_same_war and tok[3] == e and e != "pool":
                    continue
                self._wait(e, tok)

    def _commit(self, tok, reads, writes):
        for w in writes:
            self.last_w[w] = tok
            self.readers[w] = []
        for r in reads:
            self.readers.setdefault(r, []).append(tok)

    def op(self, e, fn, reads=(), writes=()):
        self._deps(e, reads, writes)
        self.cnt[e] += 1
        self.prog[e].append(("i", fn, self.sem[e], 1))
        tok = (self.sem[e], e, self.cnt[e], e)
        self._commit(tok, reads, writes)
        return tok

    def dma(self, e, fn, reads=(), writes=()):
        pool = self.dma_pool[e]
        slot = pool[self.dma_rr[e]]
        self.dma_rr[e] = (self.dma_rr[e] + 1) % len(pool)
        self._wait(e, slot[2])
        self._deps(e, reads, writes, skip_same_war=False)
        slot[1] += 16
        self.prog[e].append(("i", fn, slot[0], 16))
        tok = (slot[0], id(slot), slot[1], None)
        slot[2] = tok
        self._commit(tok, reads, writes)
        return tok

    def barrier(self):
        toks = [(self.sem[e], e, self.cnt[e], e) for e in self.eng if self.cnt[e] > 0]
        toks += [s[2] for s in self.dma_sems if s[2] is not None]
        for e in self.eng:
            for t in toks:
                if t[3] == e:
                    continue
                self._wait(e, t)

    def emit(self):
        nc = self.nc
        self.barrier()
        prog = self.prog

        def run(engine, lst):
            for it in lst:
                if it[0] == "w":
                    engine.wait_ge(it[1], it[2])
                else:
                    it[1](engine).then_inc(it[2], it[3])

        with nc.Block() as block:
            @block.sync
            def _(eng):
                run(eng, prog["sp"])

            @block.scalar
            def _(eng):
                run(eng, prog["act"])

            @block.vector
            def _(eng):
                run(eng, prog["dve"])

            @block.gpsimd
            def _(eng):
                run(eng, prog["pool"])

            @block.tensor
            def _(eng):
                run(eng, prog["pe"])


def pipeline(gens, W):
    gens = list(gens)
    active = []
    nxt = 0
    while active or nxt < len(gens):
        while len(active) < W and nxt < len(gens):
            active.append(gens[nxt])
            nxt += 1
        for g in list(active):
            try:
                next(g)
            except StopIteration:
                active.remove(g)


class Arena:
    def __init__(self, ap):
        self.ap = ap
        self.off = 0
        self.cap = ap.shape[1]
        self.peak = 0

    def alloc(self, n, dtype=F32):
        ne = n * (2 if dtype in (F32, I32) else 1)
        ne = (ne + 15) // 16 * 16
        a = self.off
        self.off += ne
        assert self.off <= self.cap, ("arena overflow", self.off, self.cap)
        self.peak = max(self.peak, self.off)
        v = self.ap[:, a:a + n * (2 if dtype in (F32, I32) else 1)]
        if dtype == F32:
            v = v.bitcast(F32)
        elif dtype == I32:
            v = v.bitcast(I32)
        return v


def build(debug=None):
    nc = bass.Bass("TRN2", target_bir_lowering=False)

    def din(name, shape, dt=F32):
        return nc.dram_tensor(name, list(shape), dt, kind="ExternalInput").ap()

    x_d = din("x", [S_LEN, D])
    ccol_d = din("ccol", [128, 16])
    pos_d = din("pos", [1, S_LEN], I32)
    wada_d = din("w_ada", [D, 6 * D])
    bada_d = din("b_ada", [1, 6 * D])
    n1g_d = din("norm1_gc", [128, 16])
    win_d = din("w_in", [D, IN_COLS])
    lbl_d = din("lb_logits", [2, 1024])
    hon_d = din("hgrn_onorm_g", [1, 128])
    qag_d = din("q_a_gc", [128, 4])
    wqu_d = din("w_q_up", [512, 1536])
    kvg_d = din("kv_a_gc", [128, 2])
    wkv_d = din("w_kv_up", [256, 2048])
    qng_d = din("q_norm_gc", [128, 4])
    kng_d = din("k_norm_gc", [128, 4])
    aon_d = din("attn_onorm_g", [1, 128])
    wout_d = din("w_out", [D, D])
    n2g_d = din("norm2_g", [1, D])
    wgr_d = din("w_gr", [D, 36])
    bgr_d = din("b_gr", [1, 36])
    wg_d = din("w_gate", [32 * 128, 16 * 512])
    wu_d = din("w_up", [32 * 128, 16 * 512])
    wd_d = din("w_down", [32 * 128, 4 * 2048])
    cst_d = din("consts", [128, 1024])
    invf_d = din("invf", [64, 1])
    metai_d = din("meta_init", [NSLOT, 32], BF16)
    out_d = nc.dram_tensor("out", [S_LEN + 128, D], F32, kind="ExternalOutput").ap()
    xbuf_d = nc.dram_tensor("xbuf", [NSLOT, D + 32], BF16).ap()
    meta_d = nc.dram_tensor("metabuf", [NSLOT, 16], F32).ap()
    dbg = {}

    def dout(name, shape, dt=F32):
        dbg[name] = nc.dram_tensor("dbg_" + name, list(shape), dt, kind="ExternalOutput").ap()
        return dbg[name]

    S = Sync(nc)
    import contextlib
    es = contextlib.ExitStack()
    arena_t = es.enter_context(nc.sbuf_tensor("arena", [128, 103 * 1024], BF16))
    A = Arena(arena_t[:])
    banks = [es.enter_context(nc.psum_tensor("pb%d" % i, [128, 512], F32)) for i in range(8)]
    PB = [b[:] for b in banks]
    PBH = [b[:].bitcast(BF16) for b in banks]
    PK = ["pb%d" % i for i in range(8)]

    def MM(out, lhsT, rhs, start, stop, r, w):
        return S.op("pe", lambda e: e.matmul(out, lhsT=lhsT, rhs=rhs, start=start, stop=stop,
                                             skip_group_check=True), r, w)

    def TR(out, in_, ident, r, w):
        return S.op("pe", lambda e: e.transpose(out=out, in_=in_, identity=ident), r, w)

    def ACT(out, in_, func, r, w, scale=1.0, bias=0.0, accum=None):
        if accum is None:
            return S.op("act", lambda e: e.activation(out=out, in_=in_, func=func, bias=bias, scale=scale), r, w)
        return S.op("act", lambda e: e.activation(out=out, in_=in_, func=func, bias=bias, scale=scale,
                                                  accum_out=accum), r, w)

    def TS(eng, out, in0, s1, s2, op0, op1, r, w):
        if s2 is None:
            return S.op(eng, lambda e: e.tensor_scalar(out, in0, s1, None, op0), r, w)
        return S.op(eng, lambda e: e.tensor_scalar(out, in0, s1, s2, op0, op1), r, w)

    def TT(eng, out, in0, in1, op, r, w):
        return S.op(eng, lambda e: e.tensor_tensor(out, in0, in1, op), r, w)

    def STT(eng, out, in0, sc, in1, op0, op1, r, w, accum=None):
        if accum is None:
            return S.op(eng, lambda e: e.scalar_tensor_tensor(out, in0, sc, in1, op0, op1), r, w)
        return S.op(eng, lambda e: e.scalar_tensor_tensor(out, in0, sc, in1, op0, op1, accum_out=accum), r, w)

    def CP(eng, out, in_, r, w):
        return S.op(eng, lambda e: e.tensor_copy(out, in_), r, w)

    def RED(eng, out, in_, op, r, w):
        return S.op(eng, lambda e: e.tensor_reduce(out, in_, AX.X, op), r, w)

    def RCP(out, in_, r, w):
        return S.op("dve", lambda e: e.reciprocal(out, in_), r, w)

    def DMA(q, out, in_, r, w):
        return S.dma(q, lambda e: e.dma_start(out=out, in_=in_), r, w)

    _regs = {}

    def breg(e, val):
        if val not in _regs:
            _regs[val] = e.to_reg(val)
        return _regs[val]

    def rstd_col(out, ssq, n, r, w, tmpk):
        ACT(out, ssq, AF.Ln, r, [tmpk], scale=1.0 / n, bias=epsc)
        ACT(out, out, AF.Exp, [tmpk], w, scale=-0.5)

    cst = A.alloc(1024, F32)
    identf = cst[:, 0:128]
    triu = cst[:, 128:256]
    M1 = cst[:, 256:384]
    M2 = cst[:, 384:512]
    tril_strict = cst[:, 512:640]
    iota_p = cst[:, 640:641]
    thr16 = cst[:, 656:672]
    thr64 = cst[:, 672:736]
    eidx = cst[:, 736:768]
    DMA("sp", cst, cst_d, [], ["cst"])
    cb = A.alloc(512, BF16)
    identb = cb[:, 0:128]
    onesb = cb[:, 128:256]
    triub = cb[:, 256:384]
    trilsb = cb[:, 384:512]
    CP("dve", identb, identf, ["cst"], ["cb"])
    CP("dve", triub, triu, ["cst"], ["cb"])
    CP("dve", trilsb, tril_strict, ["cst"], ["cb"])
    S.op("pool", lambda e: e.memset(onesb, 1.0), [], ["cb"])
    small = A.alloc(256, F32)
    epsc = small[:, 0:1]
    onec = small[:, 1:2]
    S.op("pool", lambda e: e.memset(epsc, EPS), [], ["epsc"])
    S.op("pool", lambda e: e.memset(onec, 1.0), [], ["epsc"])
    _sc = [2]

    def col(n=1):
        a = _sc[0]
        _sc[0] += n
        assert _sc[0] <= 256
        return small[:, a:a + n]

    modc = A.alloc(96, F32)
    A1c = A.alloc(16, F32)
    modrow_d = nc.dram_tensor("modrow_d", [1, 6 * D], F32).ap()

    mark = A.off
    ccol = A.alloc(16, F32)
    cact = A.alloc(16, BF16)
    tmp16 = A.alloc(16, F32)
    modrow = A.alloc(6 * D, F32)
    DMA("sp", ccol, ccol_d, [], ["ccol"])
    DMA("sp", modrow[0:1, :], bada_d, [], ["modrow_b"])
    ACT(tmp16, ccol, AF.Exp, ["ccol"], ["tmp16"], scale=-1.0)
    TS("dve", tmp16, tmp16, 1.0, None, ALU.add, None, ["tmp16"], ["tmp16"])
    RCP(tmp16, tmp16, ["tmp16"], ["tmp16"])
    TT("dve", cact, ccol, tmp16, ALU.mult, ["tmp16", "ccol"], ["cact"])
    wa = [A.alloc(16 * 512, BF16).rearrange("p (k n) -> p k n", k=16) for _ in range(2)]
    biasrow = modrow
    for jg in range(8):
        wt = wa[jg % 2]
        wk = "wa%d" % (jg % 2)
        S.dma("pool", (lambda wt=wt, jg=jg: (lambda e: e.dma_start(
            out=wt, in_=wada_d[:, jg * 512:(jg + 1) * 512].rearrange("(k p) n -> p k n", p=128))))(),
            [], [wk])
        pbi = jg % 2
        for k in range(16):
            MM(PB[pbi][0:1, :], cact[:, k:k + 1], wt[:, k, :], k == 0, k == 15, [wk, "cact"], [PK[pbi]])
        TT("dve", modrow[0:1, jg * 512:(jg + 1) * 512], PB[pbi][0:1, :], modrow[0:1, jg * 512:(jg + 1) * 512],
           ALU.add, ["modrow_b"], [PK[pbi], "modrow%d" % jg])
    allrow = ["modrow%d" % j for j in range(8)]
    if debug == "A":
        d = dout("mod", [1, 6 * D])
        DMA("sp", d, modrow[0:1, :], allrow, ["dbg"])
    for j in range(32):
        MM(PB[2][:, j:j + 1], modrow[0:1, j * 128:(j + 1) * 128], onec[0:1, 0:1], True, True, allrow + ["epsc"], [PK[2]])
    CP("dve", modc[:, 0:32], PB[2][:, 0:32], [], [PK[2], "modc"])
    cactp = col(16)
    cactb = cactp.bitcast(BF16)[:, 0:16]
    CP("dve", cactb, cact, ["cact"], ["cactb"])
    n1gc = A.alloc(16, F32)
    DMA("sp", n1gc, n1g_d, [], ["n1gc"])
    STT("dve", A1c, modc[:, 16:32], 1.0, n1gc, ALU.add, ALU.mult, ["modc", "n1gc"], ["A1c"])
    B1c = modc[:, 0:16]
    S.barrier()
    A.off = mark

    if debug == "A":
        d2 = dout("modc", [128, 96])
        DMA("sp", d2, modc, ["modc"], ["dbg"])
        S.emit()
        S.close()
        es.close()
        return nc, dbg

    markMix = A.off
    mixT = A.alloc(16 * S_LEN, BF16).rearrange("p (k t) -> p k t", k=16)
    markHT = A.off
    hT = A.alloc(16 * S_LEN, BF16).rearrange("p (k t) -> p k t", k=16)
    markB = A.off
    xt = [A.alloc(D, F32) for _ in range(2)]
    xn = [A.alloc(D, BF16) for _ in range(2)]
    tmod = [A.alloc(1024, F32) for _ in range(2)]
    ssq1 = col(16)
    rs1 = col(16)
    for i in range(NT):
        sl = i % 2
        DMA("sp", xt[sl], x_d[i * 128:(i + 1) * 128, :], [], ["xt%d" % sl])
        ACT(xn[sl], xt[sl], AF.Square, ["xt%d" % sl], ["xn%d" % sl, "ssq1_%d" % i], accum=ssq1[:, i:i + 1])
        rstd_col(rs1[:, i:i + 1], ssq1[:, i:i + 1], D, ["ssq1_%d" % i, "epsc"], ["rs1_%d" % i], "rs1t_%d" % i)
        ACT(xn[sl], xt[sl], AF.Identity, ["xt%d" % sl, "rs1_%d" % i], ["xn%d" % sl], scale=rs1[:, i:i + 1])
        for hf in range(2):
            for kk in range(8):
                k = hf * 8 + kk
                TR(PBH[hf][:, kk * 128:(kk + 1) * 128], xn[sl][:, k * 128:(k + 1) * 128], identb,
                   ["xn%d" % sl, "cb"], [PK[hf]])
            src = PBH[hf].rearrange("p (k t) -> p k t", k=8)
            tm = tmod[hf].rearrange("p (k t) -> p k t", k=8)
            a1 = A1c[:, hf * 8:(hf + 1) * 8].unsqueeze(2).to_broadcast([128, 8, 128])
            b1 = B1c[:, hf * 8:(hf + 1) * 8].unsqueeze(2).to_broadcast([128, 8, 128])
            TT("dve", tm, src, a1, ALU.mult, ["A1c"], [PK[hf], "tmod%d" % hf])
            TT("pool", hT[:, hf * 8:(hf + 1) * 8, i * 128:(i + 1) * 128], tm, b1, ALU.add,
               ["tmod%d" % hf, "modc"], ["hT%d" % i])
    hTall = ["hT%d" % i for i in range(NT)]
    S.barrier()
    A.off = markB
    if debug == "B":
        d = dout("hT", [128, 16 * S_LEN], BF16)
        DMA("sp", d, hT.rearrange("p k t -> p (k t)"), hTall, ["dbg"])
        S.emit(); S.close(); es.close()
        return nc, dbg

    markC = A.off
    wb = [A.alloc(16 * 512, BF16).rearrange("p (k n) -> p k n", k=16) for _ in range(2)]
    markC1 = A.off
    wsec = [wb[0], wb[1],
            mixT[:, 8:12, :].rearrange("p a t -> p (a t)").rearrange("p (k n) -> p k n", k=16),
            mixT[:, 12:16, :].rearrange("p a t -> p (a t)").rearrange("p (k n) -> p k n", k=16)]
    lbb = A.alloc(1024, F32)
    omlb = A.alloc(1024, F32)
    honb = A.alloc(128, F32)
    DMA("sp", lbb, lbl_d[0:1, :].partition_broadcast(128), [], ["lbb"])
    DMA("sp", omlb, lbl_d[1:2, :].partition_broadcast(128), [], ["omlb"])
    DMA("sp", honb, hon_d.partition_broadcast(128), [], ["honb"])
    TT("dve", omlb, omlb, lbb, ALU.subtract, ["lbb"], ["omlb"])
    ACT(omlb, omlb, AF.Exp, [], ["omlb"])
    TS("dve", omlb, omlb, 1.0, None, ALU.add, None, [], ["omlb"])
    RCP(lbb, omlb, ["omlb"], ["lbb"])
    TS("dve", omlb, lbb, -1.0, 1.0, ALU.mult, ALU.add, ["lbb"], ["omlb"])
    W4 = 512
    en_t, f_t, lf_t, kk_t = A.alloc(W4), A.alloc(W4), A.alloc(W4), A.alloc(W4)
    E1_t, E2_t = A.alloc(W4), A.alloc(W4)
    qin_t, qout_t, kin_t, kout_t, v_t = (A.alloc(W4, BF16) for _ in range(5))
    eng_t, sil_t = A.alloc(W4), A.alloc(W4)
    E3_t, E1n_t = eng_t, f_t
    trq_t, trk_t, tro_t = A.alloc(W4, BF16), A.alloc(W4, BF16), A.alloc(W4, BF16)
    am_t = A.alloc(W4, BF16)
    on_t = en_t
    og_t = A.alloc(W4, BF16)
    Sst = A.alloc(W4)
    Sbf = A.alloc(W4, BF16)
    deccol = col(4)
    ssqo = col(4)
    rso = col(4)
    QSC = 128.0 ** -0.5
    v4 = lambda t: t.rearrange("p (h d) -> p h d", h=4)
    triu4 = triu.unsqueeze(1).to_broadcast([128, 4, 128])
    for hgp in range(2):
        for sec in range(4):
            c0 = sec * 1024 + hgp * 512
            S.dma("pool", (lambda sec=sec, c0=c0: (lambda e: e.dma_start(
                out=wsec[sec], in_=win_d[:, c0:c0 + 512].rearrange("(k p) n -> p k n", p=128))))(), [], ["wsec%d" % sec])
        hs = slice(hgp * 512, (hgp + 1) * 512)
        for i in range(NT):
            tsl = slice(i * 128, (i + 1) * 128)
            for sec in range(4):
                for k in range(16):
                    MM(PB[sec], hT[:, k, tsl], wsec[sec][:, k, :], k == 0, k == 15, ["wsec%d" % sec, "hT%d" % i], [PK[sec]])
            hq, hf_, hi_, hg = PB[0], PB[1], PB[2], PB[3]
            ACT(en_t, hf_, AF.Exp, [], [PK[1], "en"], scale=-1.0)
            ACT(v_t, hi_, AF.Copy, [], [PK[2], "v"])
            ACT(eng_t, hg, AF.Exp, [], [PK[3], "eng"], scale=-1.0)
            ACT(en_t, en_t, AF.Ln, [], ["en"], bias=onec)
            ACT(en_t, en_t, AF.Exp, [], ["en"], scale=-1.0)
            TT("dve", f_t, en_t, omlb[:, hs], ALU.mult, ["en", "omlb"], ["f"])
            TT("dve", f_t, f_t, lbb[:, hs], ALU.add, ["lbb"], ["f"])
            ACT(lf_t, f_t, AF.Ln, ["f"], ["lf"])
            TS("pool", kk_t, f_t, -1.0, 1.0, ALU.mult, ALU.add, ["f"], ["kk"])
            ACT(eng_t, eng_t, AF.Ln, [], ["eng"], bias=onec)
            ACT(eng_t, eng_t, AF.Exp, [], ["eng"], scale=-1.0)
            TT("dve", sil_t, hg, eng_t, ALU.mult, ["eng"], [PK[3], "sil"])
            MM(PB[4], M1, lf_t, True, True, ["cst", "lf"], [PK[4]])
            MM(PB[5], M2, lf_t, True, True, ["cst", "lf"], [PK[5]])
            MM(PB[6], triu, lf_t, True, True, ["cst", "lf"], [PK[6]])
            for hh in range(4):
                MM(PB[7][:, hh:hh + 1], lf_t[:, hh * 128:(hh + 1) * 128], onec, True, True, ["epsc", "lf"], [PK[7]])
            ACT(E1_t, PB[4], AF.Exp, [], [PK[4], "E1"])
            ACT(E1n_t, PB[4], AF.Exp, [], [PK[4], "f"], scale=-1.0)
            ACT(E2_t, PB[5], AF.Exp, [], [PK[5], "E2"])
            ACT(E3_t, PB[6], AF.Exp, [], [PK[6], "eng"])
            ACT(deccol, PB[7][:, 0:4], AF.Exp, [], [PK[7], "dec"])
            STT("dve", qin_t, hq, QSC, E1_t, ALU.mult, ALU.mult, ["E1"], [PK[0], "qin"])
            STT("dve", qout_t, hq, QSC, E3_t, ALU.mult, ALU.mult, ["eng"], [PK[0], "qout"])
            TT("pool", kin_t, kk_t, E1n_t, ALU.mult, ["kk", "f"], ["kin"])
            TT("pool", kout_t, kk_t, E2_t, ALU.mult, ["kk", "E2"], ["kout"])
            for hh in range(4):
                hsl = slice(hh * 128, (hh + 1) * 128)
                TR(PBH[4][:, hsl], qin_t[:, hsl], identb, ["qin", "cb"], [PK[4]])
                TR(PBH[5][:, hsl], kin_t[:, hsl], identb, ["kin", "cb"], [PK[5]])
                TR(PBH[6][:, hsl], qout_t[:, hsl], identb, ["qout", "cb"], [PK[6]])
            CP("dve", trq_t, PBH[4][:, 0:512], [], [PK[4], "trq"])
            ACT(trk_t, PBH[5][:, 0:512], AF.Copy, [], [PK[5], "trk"])
            CP("dve", tro_t, PBH[6][:, 0:512], [], [PK[6], "tro"])
            for hh in range(4):
                hsl = slice(hh * 128, (hh + 1) * 128)
                MM(PB[0][:, hsl], trk_t[:, hsl], trq_t[:, hsl], True, True, ["trk", "trq"], [PK[0]])
            TT("dve", v4(am_t), v4(PB[0]), triu4, ALU.mult, ["cst"], [PK[0], "am"])
            for hh in range(4):
                hsl = slice(hh * 128, (hh + 1) * 128)
                if i == 0:
                    MM(PB[1][:, hsl], am_t[:, hsl], v_t[:, hsl], True, True, ["am", "v"], [PK[1]])
                else:
                    MM(PB[1][:, hsl], am_t[:, hsl], v_t[:, hsl], True, False, ["am", "v"], [PK[1]])
                    MM(PB[1][:, hsl], tro_t[:, hsl], Sbf[:, hsl], False, True, ["tro", "Sbf"], [PK[1]])
            for hh in range(4):
                hsl = slice(hh * 128, (hh + 1) * 128)
                MM(PB[2][:, hsl], kout_t[:, hsl], v_t[:, hsl], True, True, ["kout", "v"], [PK[2]])
            if i == 0:
                CP("dve", Sst, PB[2], [], [PK[2], "S"])
            else:
                TT("pool", v4(Sst), v4(Sst), deccol.unsqueeze(2).to_broadcast([128, 4, 128]), ALU.mult, ["dec"], ["S"])
                TT("dve", Sst, Sst, PB[2], ALU.add, [], [PK[2], "S"])
            if i < NT - 1:
                ACT(Sbf, Sst, AF.Copy, ["S"], ["Sbf"])
            ACT(on_t, PB[1], AF.Square, [], [PK[1], "en"])
            RED("dve", ssqo, v4(on_t), ALU.add, ["en"], ["ssqo"])
            ACT(rso, ssqo, AF.Ln, ["ssqo", "epsc"], ["rso"], scale=1.0 / 128, bias=epsc)
            ACT(rso, rso, AF.Exp, [], ["rso"], scale=-0.5)
            TT("dve", v4(on_t), v4(PB[1]), rso.unsqueeze(2).to_broadcast([128, 4, 128]), ALU.mult, ["rso"], [PK[1], "en"])
            TT("pool", v4(on_t), v4(on_t), honb.unsqueeze(1).to_broadcast([128, 4, 128]), ALU.mult, ["honb"], ["en"])
            TT("dve", og_t, on_t, sil_t, ALU.mult, ["en", "sil"], ["og"])
            for hh in range(4):
                hsl = slice(hh * 128, (hh + 1) * 128)
                TR(PBH[3][:, hsl], og_t[:, hsl], identb, ["og", "cb"], [PK[3]])
            ACT(mixT[:, hgp * 4:(hgp + 1) * 4, tsl], PBH[3][:, 0:512].rearrange("p (h t) -> p h t", h=4), AF.Copy, [],
                [PK[3], "mixT%d_%d" % (hgp, i)])
    S.barrier()
    A.off = markC1
    if debug == "C1":
        d = dout("mixT", [128, 16 * S_LEN], BF16)
        DMA("sp", d, mixT.rearrange("p k t -> p (k t)"), [], ["dbg"])
        S.emit(); S.close(); es.close()
        return nc, dbg

    A.off = markC + 16 * 512
    mla_d = nc.dram_tensor("mla_scratch", [128, 9 * S_LEN], BF16).ap()

    def alloc_mla():
        qaT = A.alloc(4 * S_LEN, BF16).rearrange("p (k t) -> p k t", k=4)
        kvaT = A.alloc(2 * S_LEN, BF16).rearrange("p (k t) -> p k t", k=2)
        return qaT, kvaT, A.alloc(S_LEN, BF16), A.alloc(S_LEN, BF16), A.alloc(S_LEN, BF16)
    mla0 = A.off
    qaT, kvaT, kpeT, kpesT, SQR = alloc_mla()
    mla_all = A.ap[:, mla0:mla0 + 9 * S_LEN]
    qagc = A.alloc(4, F32)
    kvgc = A.alloc(2, F32)
    qng = A.alloc(4, F32)
    kng = A.alloc(4, F32)
    DMA("sp", qagc, qag_d, [], ["qagc"])
    DMA("sp", kvgc, kvg_d, [], ["kvgc"])
    DMA("sp", qng, qng_d, [], ["qng"])
    DMA("sp", kng, kng_d, [], ["kng"])
    TT("dve", qng[:, 2:3], qng[:, 2:3], qng[:, 3:4], ALU.mult, [], ["qng"])
    TT("dve", kng[:, 2:3], kng[:, 2:3], kng[:, 3:4], ALU.mult, [], ["kng"])
    sq_t = [A.alloc(512, BF16) for _ in range(2)]
    rsb_t = [A.alloc(512, F32) for _ in range(2)]
    S.op("pool", lambda e: e.memset(SQR, 0.0), [], ["SQR"])
    wA, wB = wb[0], wb[0]
    S.dma("pool", lambda e: e.dma_start(out=wA, in_=win_d[:, 4096:4608].rearrange("(k p) n -> p k n", p=128)), [], ["wb0"])

    def lowrank(wt, wk, nch, dstT, gcol, gk, nfeat, dk):
        for tg in range(4):
            tsl = slice(tg * 512, (tg + 1) * 512)
            for c in range(nch):
                pbi = c % 2
                for k in range(16):
                    MM(PB[pbi], wt[:, k, c * 128:(c + 1) * 128], hT[:, k, tsl], k == 0, k == 15,
                       [wk] + hTall, [PK[pbi]])
                ACT(sq_t[pbi], PB[pbi], AF.Square, [], [PK[pbi], "sq%d" % pbi])
                TS("dve", dstT[:, c, tsl], PB[pbi], gcol[:, c:c + 1], None, ALU.mult, None, [gk],
                   [PK[pbi], dk + "%d_%d" % (c, tg)])
                MM(PB[2], onesb, sq_t[pbi], c == 0, c == nch - 1, ["cb", "sq%d" % pbi], [PK[2]])
            rb = rsb_t[tg % 2]
            rk = "rsb%d" % (tg % 2)
            ACT(rb, PB[2], AF.Ln, ["epsc"], [PK[2], rk], scale=1.0 / nfeat, bias=epsc)
            ACT(rb, rb, AF.Exp, [], [rk], scale=-0.5)
            for c in range(nch):
                TT("pool" if c % 2 else "dve", dstT[:, c, tsl], dstT[:, c, tsl], rb, ALU.mult, [rk],
                   [dk + "%d_%d" % (c, tg)])
    lowrank(wA, "wb0", 4, qaT, qagc, "qagc", 512, "qaT")
    S.op("pool", lambda e: e.memset(wB[:, :, 256:512], 0.0), [], ["wb0"])
    S.dma("pool", lambda e: e.dma_start(out=wB[:, :, 0:256], in_=win_d[:, 4608:4864].rearrange("(k p) n -> p k n", p=128)), [], ["wb0"])
    S.dma("pool", lambda e: e.dma_start(out=wB[:, :, 256:320], in_=win_d[:, 4864:4928].rearrange("(k p) n -> p k n", p=128)), [], ["wb0"])
    S.dma("pool", lambda e: e.dma_start(out=wB[:, :, 384:416], in_=win_d[:, 4896:4928].rearrange("(k p) n -> p k n", p=128)), [], ["wb0"])
    S.dma("pool", lambda e: e.dma_start(out=wB[:, :, 416:448], in_=win_d[:, 4864:4896].rearrange("(k p) n -> p k n", p=128)), [], ["wb0"])
    lowrank(wB, "wb0", 2, kvaT, kvgc, "kvgc", 256, "kvaT")
    for tg in range(4):
        tsl = slice(tg * 512, (tg + 1) * 512)
        for k in range(16):
            MM(PB[3], wB[:, k, 256:384], hT[:, k, tsl], k == 0, k == 15, ["wb0"] + hTall, [PK[3]])
        for k in range(16):
            MM(PB[4], wB[:, k, 384:512], hT[:, k, tsl], k == 0, k == 15, ["wb0"] + hTall, [PK[4]])
        ACT(SQR[0:64, tsl], PB[3][0:64, :], AF.Square, [], [PK[3], "SQR"])
        TS("dve", kpeT[0:64, tsl], PB[3][0:64, :], kng[0:64, 1:2], None, ALU.mult, None, ["kng"], [PK[3], "kpeT"])
        TS("dve", kpesT[0:64, tsl], PB[4][0:64, :], kng[0:64, 2:3], None, ALU.mult, None, ["kng"], [PK[4], "kpesT"])
    qaT_keys = ["qaT%d_%d" % (c, tg) for c in range(4) for tg in range(4)]
    kvaT_keys = ["kvaT%d_%d" % (c, tg) for c in range(2) for tg in range(4)]
    S.barrier()
    if debug == "C2":
        d = dout("qaT", [128, 4 * S_LEN], BF16)
        DMA("sp", d, qaT.rearrange("p k t -> p (k t)"), [], ["dbg"])
        d = dout("kvaT", [128, 2 * S_LEN], BF16)
        DMA("sp", d, kvaT.rearrange("p k t -> p (k t)"), [], ["dbg"])
        d = dout("kpeT", [64, S_LEN], BF16)
        DMA("sp", d, kpeT[0:64, :], [], ["dbg"])
        d = dout("kpesT", [64, S_LEN], BF16)
        DMA("sp", d, kpesT[0:64, :], [], ["dbg"])
        S.emit(); S.close(); es.close()
        return nc, dbg
    qngp = col(4)
    kngp = col(4)
    CP("dve", qngp, qng, ["qng"], ["qngp"])
    CP("dve", kngp, kng, ["kng"], ["kngp"])
    S.barrier()
    topD = mla0 + 9 * S_LEN
    A.off = markHT
    A.cap = mla0
    if debug == "D00":
        d = dout("qaT", [128, 4 * S_LEN], BF16)
        DMA("sp", d, qaT.rearrange("p k t -> p (k t)"), [], ["dbg"])
        d = dout("kvaT", [128, 2 * S_LEN], BF16)
        DMA("sp", d, kvaT.rearrange("p k t -> p (k t)"), [], ["dbg"])
        d = dout("kpeT", [64, S_LEN], BF16)
        DMA("sp", d, kpeT[0:64, :], [], ["dbg"])
        d = dout("kpesT", [64, S_LEN], BF16)
        DMA("sp", d, kpesT[0:64, :], [], ["dbg"])
        S.emit(); S.close(); es.close()
        return nc, dbg
    PI = float(np.pi)
    cosT = A.alloc(S_LEN, F32)
    sinT = A.alloc(S_LEN, F32)
    RT = A.alloc(S_LEN, BF16)
    aonb = A.alloc(128, F32)
    import os
    SK = os.environ.get("SKIPD", "")
    if "a" not in SK:
        DMA("sp", aonb, aon_d.partition_broadcast(128), [], ["aonb"])
    markD0 = A.off
    posi = A.alloc(S_LEN, I32)
    ang = A.alloc(S_LEN, F32)
    kq = A.alloc(S_LEN, F32)
    kqi = A.alloc(S_LEN, I32)
    msk = A.alloc(S_LEN, F32)
    invf = col(1)
    if "i" not in SK:
        DMA("sp", invf[0:64, :], invf_d, [], ["invf"])
    if "p" not in SK:
        DMA("sp", posi[0:64, :], pos_d.partition_broadcast(64), [], ["posi"])
    def dump_kva(tag):
        if debug == tag:
            S.barrier()
            d = dout("kvaT2", [128, 2 * S_LEN], BF16); DMA("sp", d, kvaT.rearrange("p k t -> p (k t)"), [], ["dbg"])
            S.emit(); S.close(); es.close()
            return True
        return False
    if dump_kva("X1"):
        return nc, dbg
    for (dst, shift, key) in ((sinT, 0.0, "sinT"), (cosT, PI / 2, "cosT")):
        a_, q_, qi_, m_ = ang[0:64, :], kq[0:64, :], kqi[0:64, :], msk[0:64, :]
        CP("dve", a_, posi[0:64, :], ["posi"], ["ang"])
        TS("dve", a_, a_, invf[0:64, :], shift, ALU.mult, ALU.add, ["invf"], ["ang"])
        TS("dve", q_, a_, 1.0 / (2 * PI), None, ALU.mult, None, ["ang"], ["kq"])
        CP("dve", qi_, q_, ["kq"], ["kqi"])
        CP("dve", q_, qi_, ["kqi"], ["kq"])
        if key == "sinT" and dump_kva("X2"):
            return nc, dbg
        STT("dve", a_, q_, -2 * PI, a_, ALU.mult, ALU.add, ["kq"], ["ang"])
        TS("dve", m_, a_, PI, None, ALU.is_gt, None, ["ang"], ["msk"])
        STT("dve", a_, m_, -2 * PI, a_, ALU.mult, ALU.add, ["msk"], ["ang"])
        TS("dve", m_, a_, -PI, None, ALU.is_lt, None, ["ang"], ["msk"])
        STT("dve", a_, m_, 2 * PI, a_, ALU.mult, ALU.add, ["msk"], ["ang"])
        if key == "sinT" and dump_kva("X3"):
            return nc, dbg
        ACT(dst[0:64, :], a_, AF.Sin, ["ang"], [key])
        if key == "sinT" and dump_kva("X4"):
            return nc, dbg
    TT("dve", ang[0:64, :], kpeT[0:64, :], cosT[0:64, :], ALU.mult, ["kpeT", "cosT"], ["ang"])
    TT("dve", kq[0:64, :], kpesT[0:64, :], sinT[0:64, :], ALU.mult, ["kpesT", "sinT"], ["kq"])
    TT("dve", RT[0:64, :], ang[0:64, :], kq[0:64, :], ALU.add, ["kq", "ang"], ["RT"])
    S.barrier()
    A.off = markD0
    if debug == "D0":
        d = dout("cosT", [64, S_LEN]); DMA("sp", d, cosT[0:64, :], [], ["dbg"])
        d = dout("sinT", [64, S_LEN]); DMA("sp", d, sinT[0:64, :], [], ["dbg"])
        d = dout("RT", [64, S_LEN], BF16); DMA("sp", d, RT[0:64, :], [], ["dbg"])
        d = dout("kvaT2", [128, 2 * S_LEN], BF16); DMA("sp", d, kvaT.rearrange("p k t -> p (k t)"), [], ["dbg"])
        print("offsets", markHT, mla0, markD0, A.off)
        S.emit(); S.close(); es.close()
        return nc, dbg

    wq_t = [A.alloc(4 * 384, BF16).rearrange("p (k n) -> p k n", k=4) for _ in range(2)]
    wkv_t = [A.alloc(2 * 256, BF16).rearrange("p (k n) -> p k n", k=2) for _ in range(2)]
    QTn_t = [A.alloc(S_LEN, BF16) for _ in range(2)]
    QTr_t = [A.alloc(S_LEN, BF16) for _ in range(2)]
    KTn_t = [A.alloc(S_LEN, BF16) for _ in range(2)]
    KTr_t = [A.alloc(S_LEN, BF16) for _ in range(2)]
    V_t = [A.alloc(16 * 130, BF16).rearrange("p (t v) -> p t v", t=16) for _ in range(2)]
    for sl in range(2):
        S.op("pool", (lambda sl=sl: (lambda e: e.memset(V_t[sl][:, :, 128:130], 1.0)))(), [], ["V%d" % sl])
        S.op("pool", (lambda sl=sl: (lambda e: e.memset(wq_t[sl], 0.0)))(), [], ["wq%d" % sl])
        S.op("pool", (lambda sl=sl: (lambda e: e.memset(QTr_t[sl], 0.0)))(), [], ["QTr%d" % sl])
        S.op("pool", (lambda sl=sl: (lambda e: e.memset(KTr_t[sl], 0.0)))(), [], ["KTr%d" % sl])
    lowtop = A.off
    A.off = topD
    A.cap = A.ap.shape[1]
    sqn_t = [A.alloc(512, BF16) for _ in range(2)]
    sqr_t = [A.alloc(512, BF16) for _ in range(2)]
    rb_t = [A.alloc(512, F32) for _ in range(2)]
    t1_t = [A.alloc(512, F32) for _ in range(2)]
    t2_t = [A.alloc(512, F32) for _ in range(2)]
    mrowt = A.alloc(256, F32)
    A.off = lowtop
    A.cap = mla0
    PT_t = [A.alloc(512, BF16) for _ in range(2)]
    wad = A.alloc(16 * 256, BF16).rearrange("p (k n) -> p k n", k=16)
    ada_next = [0]

    ada_pending = [None]

    def ada_group():
        if ada_pending[0] is not None:
            c0 = ada_pending[0]
            for k in range(16):
                MM(PB[7][0:1, 0:256], cactb[:, k:k + 1], wad[:, k, :], k == 0, k == 15, ["wad", "cactb"], [PK[7]])
            TT("dve", mrowt[0:1, :], PB[7][0:1, 0:256], mrowt[0:1, :], ALU.add, [], [PK[7], "mrowt"])
            DMA("sp", modrow_d[0:1, c0:c0 + 256], mrowt[0:1, :], ["mrowt"], ["modrow_d"])
            ada_pending[0] = None
        g = ada_next[0]
        if g >= 32:
            return
        ada_next[0] += 1
        c0 = 4096 + g * 256
        S.dma("pool", (lambda c0=c0: (lambda e: e.dma_start(
            out=wad, in_=wada_d[:, c0:c0 + 256].rearrange("(k p) n -> p k n", p=128))))(), [], ["wad"])
        DMA("sp", mrowt[0:1, :], bada_d[0:1, c0:c0 + 256], [], ["mrowt"])
        ada_pending[0] = c0
    ob_t = [A.alloc(128, BF16) for _ in range(2)]
    junk3 = A.alloc(128, F32)
    ocol = col(8)
    SM_SCALE = 192.0 ** -0.5
    nev = 0
    npt = 0
    for h in range(8):
        sl = h % 2
        s_ = str(sl)
        wq, wkv = wq_t[sl], wkv_t[sl]
        QTn, QTr, KTn, KTr, V = QTn_t[sl], QTr_t[sl], KTn_t[sl], KTr_t[sl], V_t[sl]
        b0 = h * 192
        for (a, bnd, c0, n) in ((0, 128, b0, 128), (128, 192, b0 + 128, 64), (256, 288, b0 + 160, 32), (288, 320, b0 + 128, 32)):
            S.dma("pool", (lambda wq=wq, a=a, bnd=bnd, c0=c0, n=n: (lambda e: e.dma_start(
                out=wq[:, :, a:bnd], in_=wqu_d[:, c0:c0 + n].rearrange("(k p) n -> p k n", p=128))))(), [], ["wq" + s_])
        S.dma("pool", (lambda wkv=wkv, h=h: (lambda e: e.dma_start(
            out=wkv, in_=wkv_d[:, h * 256:(h + 1) * 256].rearrange("(k p) n -> p k n", p=128))))(), [], ["wkv" + s_])
        def proj_body(tg, sl=sl, s_=s_, wq=wq, wkv=wkv, QTn=QTn, QTr=QTr, KTn=KTn, KTr=KTr, V=V):
            tsl = slice(tg * 512, (tg + 1) * 512)
            g2 = tg % 2
            gs = str(g2)
            bA, bB, bC, bD = (0, 1, 2, 3) if g2 == 0 else (4, 5, 6, 7)
            for k in range(4):
                MM(PB[bA], wq[:, k, 0:128], qaT[:, k, tsl], k == 0, k == 3, ["wq" + s_] + qaT_keys, [PK[bA]])
            for k in range(4):
                MM(PB[bB], wq[:, k, 128:256], qaT[:, k, tsl], k == 0, k == 3, ["wq" + s_] + qaT_keys, [PK[bB]])
            for k in range(4):
                MM(PB[bC], wq[:, k, 256:384], qaT[:, k, tsl], k == 0, k == 3, ["wq" + s_] + qaT_keys, [PK[bC]])
            yield
            ACT(sqn_t[g2], PB[bA], AF.Square, [], [PK[bA], "sqn" + gs])
            ACT(sqr_t[g2], PB[bB], AF.Square, [], [PK[bB], "sqr" + gs])
            yield
            MM(PB[bD], onesb, sqn_t[g2], True, False, ["cb", "sqn" + gs], [PK[bD]])
            MM(PB[bD], onesb, sqr_t[g2], False, True, ["cb", "sqr" + gs], [PK[bD]])
            yield
            rb = rb_t[g2]
            ACT(rb, PB[bD], AF.Ln, ["epsc"], [PK[bD], "rb" + gs], scale=1.0 / 192, bias=epsc)
            ACT(rb, rb, AF.Exp, [], ["rb" + gs], scale=-0.5)
            yield
            STT("dve", QTn[:, tsl], PB[bA], qngp[:, 0:1], rb, ALU.mult, ALU.mult, ["qngp", "rb" + gs], [PK[bA], "QTn" + s_])
            STT("dve", t1_t[g2][0:64, :], PB[bB][0:64, :], qngp[0:64, 1:2], cosT[0:64, tsl], ALU.mult, ALU.mult,
                ["qngp", "cosT"], [PK[bB], "t1" + gs])
            STT("dve", t2_t[g2][0:64, :], PB[bC][0:64, :], qngp[0:64, 2:3], sinT[0:64, tsl], ALU.mult, ALU.mult,
                ["qngp", "sinT"], [PK[bC], "t2" + gs])
            yield
            TT("pool", t1_t[g2][0:64, :], t1_t[g2][0:64, :], t2_t[g2][0:64, :], ALU.add, ["t2" + gs], ["t1" + gs])
            TT("pool", QTr[0:64, tsl], t1_t[g2][0:64, :], rb[0:64, :], ALU.mult, ["t1" + gs, "rb" + gs], ["QTr" + s_])
            for k in range(2):
                MM(PB[bA], wkv[:, k, 0:128], kvaT[:, k, tsl], k == 0, k == 1, ["wkv" + s_] + kvaT_keys, [PK[bA]])
            for j in range(4):
                t = tg * 4 + j
                for k in range(2):
                    MM(PB[bB][:, j * 128:(j + 1) * 128], kvaT[:, k, t * 128:(t + 1) * 128], wkv[:, k, 128:256],
                       k == 0, k == 1, ["wkv" + s_] + kvaT_keys, [PK[bB]])
            yield
            ACT(sqn_t[g2], PB[bA], AF.Square, [], [PK[bA], "sqn" + gs])
            ACT(V[:, tg * 4:(tg + 1) * 4, 0:128], PB[bB].rearrange("p (t v) -> p t v", t=4), AF.Copy, [],
                [PK[bB], "V" + s_])
            yield
            MM(PB[bD], onesb, sqn_t[g2], True, False, ["cb", "sqn" + gs], [PK[bD]])
            MM(PB[bD], onesb, SQR[:, tsl], False, True, ["cb", "SQR"], [PK[bD]])
            yield
            ACT(rb, PB[bD], AF.Ln, ["epsc"], [PK[bD], "rb" + gs], scale=1.0 / 192, bias=epsc)
            ACT(rb, rb, AF.Exp, [], ["rb" + gs], scale=-0.5)
            yield
            STT("dve", KTn[:, tsl], PB[bA], kngp[:, 0:1], rb, ALU.mult, ALU.mult, ["kngp", "rb" + gs], [PK[bA], "KTn" + s_])
            TT("pool", KTr[0:64, tsl], RT[0:64, tsl], rb[0:64, :], ALU.mult, ["RT", "rb" + gs], ["KTr" + s_])
            yield
        pipeline([proj_body(tg) for tg in range(4)], 2)
        steps = [(G, kt) for G in range(4) for kt in range(4 * G + 4)]

        def geom(G, kt):
            j0 = max(0, kt - 4 * G)
            return j0, (4 - j0) * 128, (4 * G + j0) * 128

        def emit_ST(n):
            G, kt = steps[n]
            j0, ncol, q0 = geom(G, kt)
            sb = n % 2
            MM(PB[sb][:, 0:ncol], KTn[:, kt * 128:(kt + 1) * 128], QTn[:, q0:q0 + ncol], True, False,
               ["KTn" + s_, "QTn" + s_], [PK[sb]])
            MM(PB[sb][:, 0:ncol], KTr[:, kt * 128:(kt + 1) * 128], QTr[:, q0:q0 + ncol], False, True,
               ["KTr" + s_, "QTr" + s_], [PK[sb]])
        emit_ST(0)
        for n in range(len(steps)):
            G, kt = steps[n]
            j0, ncol, q0 = geom(G, kt)
            sb = n % 2
            pt = PT_t[npt % 2]
            pk = "PT%d" % (npt % 2)
            if n % 10 == 5:
                ada_group()
            npt += 1
            if n + 1 < len(steps):
                emit_ST(n + 1)
            ACT(pt[:, 0:ncol], PB[sb][:, 0:ncol], AF.Exp, [], [PK[sb], pk], scale=SM_SCALE)
            if kt >= 4 * G:
                TT("pool", pt[:, 0:128], pt[:, 0:128], triub, ALU.mult, ["cb"], [pk])
            for j in range(j0, 4):
                qt = 4 * G + j
                MM(PB[2 + j][:, 0:129], pt[:, (j - j0) * 128:(j - j0 + 1) * 128], V[:, kt, 0:129],
                   kt == 0, kt == qt, [pk, "V" + s_], [PK[2 + j]])
                if kt == qt:
                    e2 = nev % 2
                    es_ = str(e2)
                    nev += 1
                    O = PB[2 + j]
                    ssq = ocol[:, e2 * 4:e2 * 4 + 1]
                    tt1 = ocol[:, e2 * 4 + 1:e2 * 4 + 2]
                    tt2 = ocol[:, e2 * 4 + 2:e2 * 4 + 3]
                    den = ocol[:, e2 * 4 + 3:e2 * 4 + 4]
                    ACT(junk3, O[:, 0:128], AF.Square, [], [PK[2 + j], "junk3", "ossq" + es_], accum=ssq)
                    CP("dve", den, O[:, 128:129], [], [PK[2 + j], "oden" + es_])
                    STT("dve", tt1, den, EPS, den, ALU.mult, ALU.mult, ["oden" + es_], ["ott1" + es_])
                    STT("dve", tt2, ssq, 1.0 / 128, tt1, ALU.mult, ALU.add, ["ossq" + es_, "ott1" + es_], ["ott2" + es_])
                    ACT(tt2, tt2, AF.Ln, [], ["ott2" + es_])
                    ACT(tt2, tt2, AF.Exp, [], ["ott2" + es_], scale=-0.5)
                    STT("dve", ob_t[e2], O[:, 0:128], tt2, aonb, ALU.mult, ALU.mult, ["ott2" + es_, "aonb"],
                        [PK[2 + j], "ob" + es_])
                    TR(PBH[6][:, 0:128], ob_t[e2], identb, ["ob" + es_, "cb"], [PK[6]])
                    ACT(mixT[:, 8 + h, qt * 128:(qt + 1) * 128], PBH[6][:, 0:128], AF.Copy, [],
                        [PK[6], "mixT%d_%d" % (8 + h, qt)])
    ada_group()
    assert ada_next[0] == 32 and ada_pending[0] is None
    S.barrier()
    A.off = markHT
    A.cap = A.ap.shape[1]
    if debug == "D":
        d = dout("mixT", [128, 16 * S_LEN], BF16)
        DMA("sp", d, mixT.rearrange("p k t -> p (k t)"), [], ["dbg"])
        S.emit(); S.close(); es.close()
        return nc, dbg

    markE = A.off
    wo = A.alloc(16 * D, BF16).rearrange("p (k n) -> p k n", k=16)
    g1b = A.alloc(D, F32)
    xt = [A.alloc(D, F32) for _ in range(2)]
    tmpe = [A.alloc(512, F32) for _ in range(2)]
    for n in range(4):
        S.dma("pool", (lambda n=n: (lambda e: e.dma_start(
            out=wo[:, :, n * 512:(n + 1) * 512],
            in_=wout_d[:, n * 512:(n + 1) * 512].rearrange("(k p) n -> p k n", p=128))))(), [], ["wo%d" % n])
    DMA("sp", g1b, modrow_d[0:1, 2 * D:3 * D].partition_broadcast(128), ["modrow_d"], ["g1b"])
    mix_keys = []
    for i in range(NT):
        sl = i % 2
        DMA("sp", xt[sl], x_d[i * 128:(i + 1) * 128, :], [], ["xt%d" % sl])
        for n in range(4):
            pbi = (i * 4 + n) % 4
            for k in range(16):
                MM(PB[pbi], mixT[:, k, i * 128:(i + 1) * 128], wo[:, k, n * 512:(n + 1) * 512], k == 0, k == 15,
                   ["wo%d" % n], [PK[pbi]])
            tp = tmpe[n % 2]
            TT("dve", tp, PB[pbi], g1b[:, n * 512:(n + 1) * 512], ALU.mult, ["g1b"], [PK[pbi], "tmpe%d" % (n % 2)])
            TT("pool", xt[sl][:, n * 512:(n + 1) * 512], xt[sl][:, n * 512:(n + 1) * 512], tp, ALU.add,
               ["tmpe%d" % (n % 2)], ["xt%d" % sl])
        DMA("sp", out_d[i * 128:(i + 1) * 128, :], xt[sl], ["xt%d" % sl], ["out"])
    S.barrier()
    A.off = markMix
    if debug == "E1":
        S.emit(); S.close(); es.close()
        return nc, dbg

    h2_d = nc.dram_tensor("h2_scratch", [S_LEN, D], BF16).ap()
    A2b = A.alloc(D, F32)
    B2b = A.alloc(D, F32)
    g2b = A.alloc(D, F32)
    LG = A.alloc(16 * 36, F32).rearrange("p (t n) -> p t n", t=16)
    IDXW = A.alloc(64, I32)
    markE2 = A.off
    n2gb = A.alloc(D, F32)
    wgr = A.alloc(16 * 36, F32).rearrange("p (k n) -> p k n", k=16)
    bgrb = A.alloc(36, F32)
    DMA("sp", A2b, modrow_d[0:1, 4 * D:5 * D].partition_broadcast(128), ["modrow_d"], ["A2b"])
    DMA("sp", B2b, modrow_d[0:1, 3 * D:4 * D].partition_broadcast(128), ["modrow_d"], ["B2b"])
    DMA("sp", g2b, modrow_d[0:1, 5 * D:6 * D].partition_broadcast(128), ["modrow_d"], ["g2b"])
    DMA("sp", n2gb, n2g_d.partition_broadcast(128), [], ["n2gb"])
    DMA("sp", wgr, wgr_d.rearrange("(k p) n -> p k n", p=128), [], ["wgr"])
    DMA("sp", bgrb, bgr_d.partition_broadcast(128), [], ["bgrb"])
    STT("dve", A2b, A2b, 1.0, n2gb, ALU.add, ALU.mult, ["n2gb"], ["A2b"])
    xt = [A.alloc(D, F32) for _ in range(2)]
    h2f = A.alloc(D, F32)
    h2b = [A.alloc(D, BF16) for _ in range(2)]
    h2T = A.alloc(16 * 128, F32).rearrange("p (k t) -> p k t", k=16)
    ssq2 = col(16)
    rs2 = col(16)
    for i in range(NT):
        sl = i % 2
        DMA("sp", xt[sl], out_d[i * 128:(i + 1) * 128, :], ["out"], ["xt%d" % sl])
        ACT(h2f, xt[sl], AF.Square, ["xt%d" % sl], ["h2f", "ssq2_%d" % i], accum=ssq2[:, i:i + 1])
        rstd_col(rs2[:, i:i + 1], ssq2[:, i:i + 1], D, ["ssq2_%d" % i, "epsc"], ["rs2_%d" % i], "rs2t_%d" % i)
        STT("dve", h2f, xt[sl], rs2[:, i:i + 1], A2b, ALU.mult, ALU.mult, ["rs2_%d" % i, "A2b"], ["h2f"])
        TT("pool", h2f, h2f, B2b, ALU.add, ["B2b"], ["h2f"])
        ACT(h2b[sl], h2f, AF.Copy, ["h2f"], ["h2b%d" % sl])
        DMA("sp", h2_d[i * 128:(i + 1) * 128, :], h2b[sl], ["h2b%d" % sl], ["h2_d%d" % i])
        for q in range(4):
            for kk in range(4):
                k = q * 4 + kk
                TR(PB[q][:, kk * 128:(kk + 1) * 128], h2f[:, k * 128:(k + 1) * 128], identf, ["h2f", "cst"], [PK[q]])
            CP("dve" if q % 2 else "act", h2T[:, q * 4:(q + 1) * 4, :], PB[q].rearrange("p (k t) -> p k t", k=4),
               [], [PK[q], "h2T"]) if q % 2 else ACT(h2T[:, q * 4:(q + 1) * 4, :],
               PB[q].rearrange("p (k t) -> p k t", k=4), AF.Copy, [], [PK[q], "h2T"])
        for k in range(16):
            MM(PB[4][:, 0:36], h2T[:, k, :], wgr[:, k, :], k == 0, k == 15, ["h2T", "wgr"], [PK[4]])
        TT("dve", LG[:, i, :], PB[4][:, 0:36], bgrb, ALU.add, ["bgrb"], [PK[4], "LG"])
    S.barrier()
    A.off = markE2
    if debug == "E2":
        d = dout("LG", [128, 16 * 36]); DMA("sp", d, LG.rearrange("p t n -> p (t n)"), [], ["dbg"])
        d = dout("h2", [S_LEN, D], BF16); DMA("sp", d, h2_d, [], ["dbg"])
        S.emit(); S.close(); es.close()
        return nc, dbg

    BIG = 1.0e30

    def T3(n, m):
        t = A.alloc(16 * n * m, F32)
        return t.rearrange("p (t n) -> p t n", t=16) if m == 1 else t.rearrange("p (t n m) -> p t n m", t=16, n=n)
    def bc(ap2, shape):
        v = ap2
        for ax in range(2, len(shape)):
            v = v.unsqueeze(ax)
        return v.to_broadcast(shape)
    G = LG[:, :, 0:4]
    EL = LG[:, :, 4:36]
    gmax = A.alloc(16, F32)
    RED("dve", gmax, G, ALU.max, ["LG"], ["gmax"])
    gone = T3(4, 1)
    TT("dve", gone, G, bc(gmax, [128, 16, 4]), ALU.is_equal, ["gmax", "LG"], ["gone"])
    gd = T3(4, 1)
    TT("dve", gd, G, bc(gmax, [128, 16, 4]), ALU.subtract, ["gmax", "LG"], ["gd"])
    ACT(gd, gd, AF.Exp, [], ["gd"])
    pg = A.alloc(16, F32)
    RED("dve", pg, gd, ALU.add, ["gd"], ["pg"])
    RCP(pg, pg, [], ["pg"])
    pen = T3(4, 1)
    TS("dve", pen, gone, -1.0, BIG, ALU.add, ALU.mult, ["gone"], ["pen"])
    EM = T3(32, 1)
    TT("dve", EM.rearrange("p t (g e) -> p t g e", g=4), EL.rearrange("p t (g e) -> p t g e", g=4),
       pen.unsqueeze(3).to_broadcast([128, 16, 4, 8]), ALU.add, ["pen", "LG"], ["EM"])
    v1 = A.alloc(16, F32)
    RED("dve", v1, EM, ALU.max, ["EM"], ["v1"])
    M1 = T3(32, 1)
    TT("dve", M1, EM, bc(v1, [128, 16, 32]), ALU.is_equal, ["EM", "v1"], ["M1"])
    EM2 = T3(32, 1)
    STT("dve", EM2, M1, -BIG, EM, ALU.mult, ALU.add, ["M1", "EM"], ["EM2"])
    v2 = A.alloc(16, F32)
    RED("dve", v2, EM2, ALU.max, ["EM2"], ["v2"])
    M2 = T3(32, 1)
    TT("dve", M2, EM2, bc(v2, [128, 16, 32]), ALU.is_equal, ["EM2", "v2"], ["M2"])
    e21 = A.alloc(16, F32)
    TT("dve", e21, v2, v1, ALU.subtract, ["v1", "v2"], ["e21"])
    ACT(e21, e21, AF.Exp, [], ["e21"])
    w1 = A.alloc(16, F32)
    w2 = A.alloc(16, F32)
    TS("dve", w1, e21, 1.0, None, ALU.add, None, ["e21"], ["w1"])
    RCP(w1, w1, [], ["w1"])
    TT("dve", w1, w1, pg, ALU.mult, ["pg"], ["w1"])
    TT("dve", w2, w1, e21, ALU.mult, ["w1", "e21"], ["w2"])
    Mb = A.alloc(16 * 32, BF16).rearrange("p (t n) -> p t n", t=16)
    TT("dve", Mb, M1, M2, ALU.add, ["M1", "M2"], ["Mb"])
    for i in range(NT):
        MM(PB[0][:, i * 32:(i + 1) * 32], trilsb, Mb[:, i, :], True, i == 0, ["cb", "Mb"], [PK[0]])
        for j in range(i):
            MM(PB[0][:, i * 32:(i + 1) * 32], onesb, Mb[:, j, :], False, j == i - 1, ["cb", "Mb"], [PK[0]])
    POS = T3(32, 1)
    CP("dve", POS.rearrange("p t n -> p (t n)"), PB[0], [], [PK[0], "POS"])
    for j in range(NT):
        MM(PB[1][:, 0:32], onesb, Mb[:, j, :], j == 0, j == NT - 1, ["cb", "Mb"], [PK[1]])
    cnt = A.alloc(32, F32)
    CP("dve", cnt, PB[1][:, 0:32], [], [PK[1], "cnt"])
    cmp1 = A.alloc(32 * 16, F32).rearrange("p (e m) -> p e m", e=32)
    TT("dve", cmp1, cnt.unsqueeze(2).to_broadcast([128, 32, 16]), thr16.unsqueeze(1).to_broadcast([128, 32, 16]),
       ALU.is_gt, ["cnt", "cst"], ["cmp1"])
    padded = A.alloc(32, F32)
    RED("dve", padded, cmp1, ALU.add, ["cmp1"], ["padded"])
    TS("dve", padded, padded, 128.0, None, ALU.mult, None, [], ["padded"])
    cs = [A.alloc(32, F32) for _ in range(2)]
    CP("dve", cs[0], padded, ["padded"], ["cs0"])
    cur = 0
    for sh in (1, 2, 4, 8, 16):
        nx = 1 - cur
        CP("dve", cs[nx][:, 0:sh], cs[cur][:, 0:sh], ["cs%d" % cur], ["cs%d" % nx])
        TT("dve", cs[nx][:, sh:32], cs[cur][:, sh:32], cs[cur][:, 0:32 - sh], ALU.add, ["cs%d" % cur], ["cs%d" % nx])
        cur = nx
    pad_end = cs[cur]
    pek = "cs%d" % cur
    pad_start = A.alloc(32, F32)
    TT("dve", pad_start, pad_end, padded, ALU.subtract, [pek, "padded"], ["pad_start"])
    cmp2 = A.alloc(64 * 32, F32).rearrange("p (j e) -> p j e", j=64)
    TT("dve", cmp2, pad_end.unsqueeze(1).to_broadcast([128, 64, 32]), thr64.unsqueeze(2).to_broadcast([128, 64, 32]),
       ALU.is_le, [pek, "cst"], ["cmp2"])
    blke = A.alloc(64, F32)
    RED("dve", blke, cmp2, ALU.add, ["cmp2"], ["blke"])
    TS("dve", blke, blke, 31.0, None, ALU.min, None, [], ["blke"])
    same = A.alloc(64, F32)
    S.op("pool", lambda e: e.memset(same, 0.0), [], ["same"])
    TT("dve", same[:, 2:64], blke[:, 2:64], blke[:, 0:62], ALU.is_equal, ["blke"], ["same"])
    TS("dve", blke, blke, 128.0, iota_p, ALU.mult, ALU.add, ["cst"], ["blke"])
    STT("dve", blke, same, 8192.0, blke, ALU.mult, ALU.add, ["same"], ["blke"])
    CP("dve", IDXW, blke, ["blke"], ["IDXW"])
    Tt = T3(32, 1)
    TT("dve", Tt, POS, pad_start.unsqueeze(1).to_broadcast([128, 16, 32]), ALU.add, ["POS", "pad_start"], ["Tt"])
    prod = T3(32, 1)
    dstf = A.alloc(32, F32).rearrange("p (t k) -> p t k", t=16)
    TT("dve", prod, M1, Tt, ALU.mult, ["M1", "Tt"], ["prod"])
    RED("dve", dstf[:, :, 0], prod, ALU.add, ["prod"], ["dstf"])
    TT("dve", prod, M2, Tt, ALU.mult, ["M2", "Tt"], ["prod"])
    RED("dve", dstf[:, :, 1], prod, ALU.add, ["prod"], ["dstf"])
    DI = A.alloc(32, I32)
    CP("dve", DI, dstf.rearrange("p t k -> p (t k)"), ["dstf"], ["DI"])
    tokf = A.alloc(16, F32)
    TS("dve", tokf, thr16, iota_p, None, ALU.add, None, ["cst"], ["tokf"])
    with nc.allow_non_contiguous_dma(reason="64B meta tails"):
        pass
    DMA("sp", xbuf_d[:, D:D + 32], metai_d, [], ["xbuf"])
    if debug == "E3":
        S.barrier()
        d = dout("LG", [128, 16 * 36]); DMA("sp", d, LG.rearrange("p t n -> p (t n)"), [], ["dbg"])
        d = dout("IDXW", [128, 64], I32); DMA("sp", d, IDXW, [], ["dbg"])
        d = dout("DI", [128, 32], I32); DMA("sp", d, DI, [], ["dbg"])
        d = dout("w1", [128, 16]); DMA("sp", d, w1, [], ["dbg"])
        d = dout("w2", [128, 16]); DMA("sp", d, w2, [], ["dbg"])
        d = dout("cnt", [128, 32]); DMA("sp", d, cnt, [], ["dbg"])
        d = dout("M1", [128, 512]); DMA("sp", d, M1.rearrange("p t n -> p (t n)"), [], ["dbg"])
        d = dout("M2", [128, 512]); DMA("sp", d, M2.rearrange("p t n -> p (t n)"), [], ["dbg"])
        S.emit(); S.close(); es.close()
        return nc, dbg
    hbx = [[A.alloc(D + 32, BF16) for _ in range(2)] for _ in range(2)]
    for i in range(NT):
        sl = i % 2
        for k in range(2):
            c = i * 2 + k
            hb = hbx[k][sl]
            hk = "hb%d_%d" % (k, sl)
            DMA("sp", hb[:, 0:D], h2_d[i * 128:(i + 1) * 128, :], ["h2_d%d" % i], [hk])
            tailF = hb[:, D:D + 32].bitcast(F32)
            tailI = hb[:, D:D + 32].bitcast(I32)
            CP("dve", tailI[:, 0:1], tokf[:, i:i + 1], ["tokf"], [hk])
            CP("dve", tailF[:, 1:2], (w1, w2)[k][:, i:i + 1], ["w1", "w2"], [hk])
            S.dma("pool", (lambda hb=hb, c=c: (lambda e: e.indirect_dma_start(
                out=xbuf_d, out_offset=bass.IndirectOffsetOnAxis(ap=DI[:, c:c + 1], axis=0),
                in_=hb, in_offset=None, bounds_check=breg(e, NSLOT - 1), oob_is_err=False)))(),
                [hk, "DI"], ["xbuf"])
    S.barrier()
    A.off = markE2
    markF = A.off

    wg_t = [A.alloc(16 * 512, BF16) for _ in range(2)]
    wu_t = [A.alloc(16 * 512, BF16) for _ in range(2)]
    wd_t = [A.alloc(4 * D, BF16) for _ in range(2)]
    xb_t = [A.alloc(D + 32, BF16) for _ in range(2)]
    XT_t = [A.alloc(16 * 128, BF16).rearrange("p (k s) -> p k s", k=16) for _ in range(2)]
    en_f = [A.alloc(512, F32) for _ in range(2)]
    tg_f = [A.alloc(512, F32) for _ in range(2)]
    act_b = [A.alloc(512, BF16) for _ in range(2)]
    actT = [A.alloc(4 * 128, BF16).rearrange("p (k s) -> p k s", k=4) for _ in range(2)]
    Y_t = [A.alloc(D, F32) for _ in range(2)]

    def pf(j, which):
        sl = j % 2
        s_ = str(sl)
        lst = []
        if "g" in which:
            lst += [(wg_t[sl], wg_d, "wg"), (wu_t[sl], wu_d, "wu")]
        if "d" in which:
            lst += [(wd_t[sl], wd_d, "wd")]
        for (dst, src, key) in lst:
            S.dma("pool", (lambda dst=dst, src=src, j=j: (lambda e: e.indirect_dma_start(
                out=dst, out_offset=None, in_=src, in_offset=bass.IndirectOffsetOnAxis(ap=IDXW[:, j:j + 1], axis=0),
                bounds_check=breg(e, 32 * 128 - 1), oob_is_err=False)))(), ["IDXW"], [key + s_])
        if "d" in which:
            DMA("sp", xb_t[sl], xbuf_d[j * 128:(j + 1) * 128, :], ["xbuf"], ["xb" + s_])

    def stage_A(j):
        sl = j % 2
        s_ = str(sl)
        bG, bU = 2 * sl, 2 * sl + 1
        xb, XT = xb_t[sl], XT_t[sl]
        xbv = xb[:, 0:D].rearrange("p (f k) -> p k f", k=16)
        for hf, bb in ((0, 4), (1, 5)):
            for kk in range(8):
                TR(PBH[bb][:, kk * 128:(kk + 1) * 128], xbv[:, hf * 8 + kk, :], identb, ["xb" + s_, "cb"], [PK[bb]])
        CP("dve", XT[:, 0:8, :], PBH[4].rearrange("p (k s) -> p k s", k=8), [], [PK[4], "XT" + s_])
        ACT(XT[:, 8:16, :], PBH[5].rearrange("p (k s) -> p k s", k=8), AF.Copy, [], [PK[5], "XT" + s_])
        wg, wu = wg_t[sl], wu_t[sl]
        for k in range(16):
            MM(PB[bG], XT[:, k, :], wg[:, k * 512:(k + 1) * 512], k == 0, k == 15, ["XT" + s_, "wg" + s_], [PK[bG]])
        for k in range(16):
            MM(PB[bU], XT[:, k, :], wu[:, k * 512:(k + 1) * 512], k == 0, k == 15, ["XT" + s_, "wu" + s_], [PK[bU]])

    def stage_B1(j):
        sl = j % 2
        s_ = str(sl)
        bG, bU = 2 * sl, 2 * sl + 1
        ACT(en_f[sl], PB[bG], AF.Exp, [], [PK[bG], "en_f" + s_], scale=-1.0)
        ACT(en_f[sl], en_f[sl], AF.Ln, [], ["en_f" + s_], bias=onec)
        ACT(en_f[sl], en_f[sl], AF.Exp, [], ["en_f" + s_], scale=-1.0)
        TT("dve", tg_f[sl], PB[bG], en_f[sl], ALU.mult, ["en_f" + s_], [PK[bG], "tg_f" + s_])
        TT("dve", act_b[sl], tg_f[sl], PB[bU], ALU.mult, ["tg_f" + s_], [PK[bU], "act_b" + s_])
        abv = act_b[sl].rearrange("p (j k) -> p k j", k=4)
        for kk in range(4):
            TR(PBH[6][:, kk * 128:(kk + 1) * 128], abv[:, kk, :], identb, ["act_b" + s_, "cb"], [PK[6]])
        ACT(actT[sl], PBH[6][:, 0:512].rearrange("p (k s) -> p k s", k=4), AF.Copy, [], [PK[6], "actT" + s_])

    def stage_B2(j):
        sl = j % 2
        s_ = str(sl)
        bG, bU = 2 * sl, 2 * sl + 1
        wd = wd_t[sl]
        xb = xb_t[sl]
        Y = Y_t[sl]
        wcol = xb[:, D:D + 32].bitcast(F32)[:, 1:2]
        for n in range(4):
            pbi = (bG, bU, 7, bG)[n] if False else (bG if n % 2 == 0 else bU)
            for kk in range(4):
                MM(PB[pbi], actT[sl][:, kk, :], wd[:, kk * D + n * 512: kk * D + (n + 1) * 512], kk == 0, kk == 3,
                   ["actT" + s_, "wd" + s_], [PK[pbi]])
            STT("dve", Y[:, n * 512:(n + 1) * 512], PB[pbi], wcol, g2b[:, n * 512:(n + 1) * 512],
                ALU.mult, ALU.mult, ["xb" + s_, "g2b"], [PK[pbi], "Y" + s_])
        S.dma("pool", (lambda sl=sl, Y=Y: (lambda e: e.indirect_dma_start(
            out=out_d, out_offset=bass.IndirectOffsetOnAxis(ap=xb_t[sl][:, D:D + 32].bitcast(I32)[:, 0:1], axis=0),
            in_=Y, in_offset=None, bounds_check=breg(e, S_LEN + 127), oob_is_err=True, compute_op=ALU.add)))(),
            ["Y" + s_, "xb" + s_], ["out"])
    pf(0, "gd")
    pf(1, "gd")
    stage_A(0)
    pf(2, "g")
    for j in range(NBLK):
        stage_B1(j)
        stage_B2(j)
        if j + 2 < NBLK:
            pf(j + 2, "d")
        if j + 1 < NBLK:
            stage_A(j + 1)
        if j + 3 < NBLK:
            pf(j + 3, "g")
    S.emit()
    S.close()
    es.close()
    return nc, dbg


def host_inputs(inputs, b):
    f = np.float32
    m = {}
    m["x"] = np.ascontiguousarray(inputs["x"][b])
    m["ccol"] = np.ascontiguousarray(inputs["c"][b].reshape(16, 128).T)
    m["pos"] = np.ascontiguousarray(inputs["positions"][b].reshape(1, S_LEN)).astype(np.int32)
    m["w_ada"] = inputs["w_ada"][0]
    m["b_ada"] = inputs["b_ada"][0].reshape(1, -1)
    m["norm1_gc"] = np.ascontiguousarray(inputs["norm1_g"][0].reshape(16, 128).T)
    m["w_in"] = inputs["w_in"][0]
    m["lb_logits"] = inputs["hgrn_lb_logits"]
    m["hgrn_onorm_g"] = inputs["hgrn_onorm_g"][0].reshape(1, 128)
    m["q_a_gc"] = np.ascontiguousarray(inputs["q_a_norm_g"][0].reshape(4, 128).T)
    m["w_q_up"] = inputs["w_q_up"][0]
    m["kv_a_gc"] = np.ascontiguousarray(inputs["kv_a_norm_g"][0].reshape(2, 128).T)
    m["w_kv_up"] = inputs["w_kv_up"][0]

    def qk_cols(g):
        o = np.zeros((128, 4), f)
        o[:, 0] = g[0:128]
        o[0:64, 1] = g[128:192]
        o[0:32, 2] = g[160:192]
        o[32:64, 2] = g[128:160]
        o[0:32, 3] = -1.0
        o[32:64, 3] = 1.0
        return o
    m["q_norm_gc"] = qk_cols(inputs["q_norm_g"][0])
    m["k_norm_gc"] = qk_cols(inputs["k_norm_g"][0])
    m["attn_onorm_g"] = inputs["attn_onorm_g"][0].reshape(1, 128)
    m["w_out"] = inputs["w_out"][0]
    m["norm2_g"] = inputs["norm2_g"][0].reshape(1, D)
    m["w_gr"] = np.ascontiguousarray(np.concatenate([inputs["w_group"][0], inputs["w_router"][0]], axis=1))
    m["b_gr"] = np.concatenate([inputs["b_group"][0], inputs["b_router"][0]]).reshape(1, 36)
    m["w_gate"] = inputs["w_gate"][0].reshape(32 * 128, 16 * 512)
    m["w_up"] = inputs["w_up"][0].reshape(32 * 128, 16 * 512)
    m["w_down"] = inputs["w_down"][0].reshape(32 * 128, 4 * 2048)
    m["consts"] = CONSTS
    m["invf"] = INVF
    m["meta_init"] = META_INIT.view(ml_dtypes.bfloat16)
    return m


def _consts():
    c = np.zeros((128, 1024), np.float32)
    s = np.arange(128)[:, None]
    t = np.arange(128)[None, :]
    c[:, 0:128] = np.eye(128)
    c[:, 128:256] = (s <= t)
    c[:, 256:384] = (s <= t).astype(np.float32) - (s <= 63).astype(np.float32)
    c[:, 384:512] = (s > t)
    c[:, 512:640] = (s < t)
    c[:, 640] = np.arange(128)
    c[:, 656:672] = np.arange(16) * 128
    c[:, 672:736] = np.arange(64) * 128
    c[:, 736:768] = np.arange(32)
    return c


CONSTS = _consts()
INVF = (10000.0 ** (-(np.arange(64) % 32).astype(np.float32) * 2 / 64)).astype(np.float32).reshape(64, 1)
META_INIT = np.zeros((NSLOT, 16), np.int32)
META_INIT[:, 0] = 2048 + (np.arange(NSLOT) % 128)

_NC = None


def kernel(**inputs):
    global _NC
    if _NC is None:
        _NC = build()[0]
    inputs = {k: np.asarray(v) for k, v in inputs.items()}
    in_maps = [host_inputs(inputs, b) for b in range(8)]
    res = run_bass_kernel_spmd(_NC, in_maps, core_ids=list(range(8)))
    out = np.stack([np.asarray(r["out"])[:S_LEN] for r in res.results], axis=0)
    return out.astype(np.float32)
```

```python
import numpy as np
import ml_dtypes
import concourse.bass as bass
import concourse.mybir as mybir
from concourse.bass_utils import run_bass_kernel_spmd

F32 = mybir.dt.float32
BF16 = mybir.dt.bfloat16
I32 = mybir.dt.int32
AF = mybir.ActivationFunctionType
ALU = mybir.AluOpType
AX = mybir.AxisListType

D = 2048
S_LEN = 2048
NT = 16
EPS = 1e-6
IN_COLS = 4928
BLK = 128
NBLK = 64
NSLOT = NBLK * BLK
DEBUG = None


class Sync:
    def __init__(self, nc, n_dma_sems=32):
        self.nc = nc
        self.eng = {"pe": nc.tensor, "dve": nc.vector, "act": nc.scalar,
                    "pool": nc.gpsimd, "sp": nc.sync}
        self.sem = {}
        self.cnt = {}
        self._ctx = []
        for e in self.eng:
            cm = nc.semaphore("s_" + e)
            self.sem[e] = cm.__enter__()
            self._ctx.append(cm)
            self.cnt[e] = 0
        self.dma_sems = []
        self.dma_pool = {"sp": [], "pool": [], "act": []}
        self.dma_rr = {"sp": 0, "pool": 0, "act": 0}
        for q, n in (("sp", n_dma_sems // 2), ("pool", n_dma_sems // 2), ("act", 2)):
            for i in range(n):
                cm = nc.semaphore("d%s%d" % (q, i))
                slot = [cm.__enter__(), 0, None]
                self.dma_sems.append(slot)
                self.dma_pool[q].append(slot)
                self._ctx.append(cm)
        self.waited = {}
        self.last_w = {}
        self.readers = {}
        self.prog = {e: [] for e in self.eng}

    def close(self):
        for cm in reversed(self._ctx):
            cm.__exit__(None, None, None)

    def _wait(self, e, tok):
        if tok is None:
            return
        sem, sid, val, src = tok
        if src == e and e == "pe":
            return
        k = (e, sid)
        if self.waited.get(k, 0) >= val:
            return
        self.waited[k] = val
        self.prog[e].append(("w", sem, val))

    def _deps(self, e, reads, writes, skip_same_war=True):
        for r in reads:
            self._wait(e, self.last_w.get(r))
        for w in writes:
            self._wait(e, self.last_w.get(w))
            for tok in self.readers.get(w, ()):
                if skip_same_war and tok[3] == e and e != "pool":
                    continue
                self._wait(e, tok)

    def _commit(self, tok, reads, writes):
        for w in writes:
            self.last_w[w] = tok
            self.readers[w] = []
        for r in reads:
            self.readers.setdefault(r, []).append(tok)

    def op(self, e, fn, reads=(), writes=()):
        self._deps(e, reads, writes)
        self.cnt[e] += 1
        self.prog[e].append(("i", fn, self.sem[e], 1))
        tok = (self.sem[e], e, self.cnt[e], e)
        self._commit(tok, reads, writes)
        return tok

    def dma(self, e, fn, reads=(), writes=()):
        pool = self.dma_pool[e]
        slot = pool[self.dma_rr[e]]
        self.dma_rr[e] = (self.dma_rr[e] + 1) % len(pool)
        self._wait(e, slot[2])
        self._deps(e, reads, writes, skip_same_war=False)
        slot[1] += 16
        self.prog[e].append(("i", fn, slot[0], 16))
        tok = (slot[0], id(slot), slot[1], None)
        slot[2] = tok
        self._commit(tok, reads, writes)
        return tok

    def barrier(self):
        toks = [(self.sem[e], e, self.cnt[e], e) for e in self.eng if self.cnt[e] > 0]
        toks += [s[2] for s in self.dma_sems if s[2] is not None]
        for e in self.eng:
            for t in toks:
                if t[3] == e and e == "pe":
                    continue
                self._wait(e, t)

    def emit(self):
        nc = self.nc
        self.barrier()
        prog = self.prog

        def run(engine, lst):
            for it in lst:
                if it[0] == "w":
                    engine.wait_ge(it[1], it[2])
                else:
                    it[1](engine).then_inc(it[2], it[3])

        with nc.Block() as block:
            @block.sync
            def _(eng):
                run(eng, prog["sp"])

            @block.scalar
            def _(eng):
                run(eng, prog["act"])

            @block.vector
            def _(eng):
                run(eng, prog["dve"])

            @block.gpsimd
            def _(eng):
                run(eng, prog["pool"])

            @block.tensor
            def _(eng):
                run(eng, prog["pe"])


def pipeline(gens, W):
    gens = list(gens)
    active = []
    nxt = 0
    while active or nxt < len(gens):
        while len(active) < W and nxt < len(gens):
            active.append(gens[nxt])
            nxt += 1
        for g in list(active):
            try:
                next(g)
            except StopIteration:
                active.remove(g)


class Arena:
    def __init__(self, ap):
        self.ap = ap
        self.off = 0
        self.cap = ap.shape[1]
        self.peak = 0

    def alloc(self, n, dtype=F32):
        ne = n * (2 if dtype in (F32, I32) else 1)
        ne = (ne + 15) // 16 * 16
        a = self.off
        self.off += ne
        assert self.off <= self.cap, ("arena overflow", self.off, self.cap)
        self.peak = max(self.peak, self.off)
        v = self.ap[:, a:a + n * (2 if dtype in (F32, I32) else 1)]
        if dtype == F32:
            v = v.bitcast(F32)
        elif dtype == I32:
            v = v.bitcast(I32)
        return v


def build(debug=None):
    nc = bass.Bass("TRN2", target_bir_lowering=False)

    def din(name, shape, dt=F32):
        return nc.dram_tensor(name, list(shape), dt, kind="ExternalInput").ap()

    x_d = din("x", [S_LEN, D])
    ccol_d = din("ccol", [128, 16])
    pos_d = din("pos", [1, S_LEN], I32)
    wada_d = din("w_ada", [D, 6 * D])
    bada_d = din("b_ada", [1, 6 * D])
    n1g_d = din("norm1_gc", [128, 16])
    win_d = din("w_in", [D, IN_COLS])
    lbl_d = din("lb_logits", [2, 1024])
    hon_d = din("hgrn_onorm_g", [1, 128])
    qag_d = din("q_a_gc", [128, 4])
    wqu_d = din("w_q_up", [512, 1536])
    kvg_d = din("kv_a_gc", [128, 2])
    wkv_d = din("w_kv_up", [256, 2048])
    qng_d = din("q_norm_gc", [128, 4])
    kng_d = din("k_norm_gc", [128, 4])
    aon_d = din("attn_onorm_g", [1, 128])
    wout_d = din("w_out", [D, D])
    n2g_d = din("norm2_g", [1, D])
    wgr_d = din("w_gr", [D, 36])
    bgr_d = din("b_gr", [1, 36])
    wg_d = din("w_gate", [32 * 128, 16 * 512])
    wu_d = din("w_up", [32 * 128, 16 * 512])
    wd_d = din("w_down", [32 * 128, 4 * 2048])
    cst_d = din("consts", [128, 1024])
    invf_d = din("invf", [64, 1])
    metai_d = din("meta_init", [NSLOT, 32], BF16)
    out_d = nc.dram_tensor("out", [S_LEN + 128, D], F32, kind="ExternalOutput").ap()
    xbuf_d = nc.dram_tensor("xbuf", [NSLOT, D + 32], BF16).ap()
    meta_d = nc.dram_tensor("metabuf", [NSLOT, 16], F32).ap()
    dbg = {}

    def dout(name, shape, dt=F32):
        dbg[name] = nc.dram_tensor("dbg_" + name, list(shape), dt, kind="ExternalOutput").ap()
        return dbg[name]

    S = Sync(nc)
    import contextlib
    es = contextlib.ExitStack()
    arena_t = es.enter_context(nc.sbuf_tensor("arena", [128, 103 * 1024], BF16))
    A = Arena(arena_t[:])
    banks = [es.enter_context(nc.psum_tensor("pb%d" % i, [128, 512], F32)) for i in range(8)]
    PB = [b[:] for b in banks]
    PBH = [b[:].bitcast(BF16) for b in banks]
    PK = ["pb%d" % i for i in range(8)]

    def MM(out, lhsT, rhs, start, stop, r, w):
        return S.op("pe", lambda e: e.matmul(out, lhsT=lhsT, rhs=rhs, start=start, stop=stop,
                                             skip_group_check=True), r, w)

    def TR(out, in_, ident, r, w):
        return S.op("pe", lambda e: e.transpose(out=out, in_=in_, identity=ident), r, w)

    def ACT(out, in_, func, r, w, scale=1.0, bias=0.0, accum=None):
        if accum is None:
            return S.op("act", lambda e: e.activation(out=out, in_=in_, func=func, bias=bias, scale=scale), r, w)
        return S.op("act", lambda e: e.activation(out=out, in_=in_, func=func, bias=bias, scale=scale,
                                                  accum_out=accum), r, w)

    def TS(eng, out, in0, s1, s2, op0, op1, r, w):
        if s2 is None:
            return S.op(eng, lambda e: e.tensor_scalar(out, in0, s1, None, op0), r, w)
        return S.op(eng, lambda e: e.tensor_scalar(out, in0, s1, s2, op0, op1), r, w)

    def TT(eng, out, in0, in1, op, r, w):
        return S.op(eng, lambda e: e.tensor_tensor(out, in0, in1, op), r, w)

    def STT(eng, out, in0, sc, in1, op0, op1, r, w, accum=None):
        if accum is None:
            return S.op(eng, lambda e: e.scalar_tensor_tensor(out, in0, sc, in1, op0, op1), r, w)
        return S.op(eng, lambda e: e.scalar_tensor_tensor(out, in0, sc, in1, op0, op1, accum_out=accum), r, w)

    def CP(eng, out, in_, r, w):
        return S.op(eng, lambda e: e.tensor_copy(out, in_), r, w)

    def RED(eng, out, in_, op, r, w):
        return S.op(eng, lambda e: e.tensor_reduce(out, in_, AX.X, op), r, w)

    def RCP(out, in_, r, w):
        return S.op("dve", lambda e: e.reciprocal(out, in_), r, w)

    def DMA(q, out, in_, r, w):
        return S.dma(q, lambda e: e.dma_start(out=out, in_=in_), r, w)

    _regs = {}

    def breg(e, val):
        if val not in _regs:
            _regs[val] = e.to_reg(val)
        return _regs[val]

    def rstd_col(out, ssq, n, r, w, tmpk):
        ACT(out, ssq, AF.Ln, r, [tmpk], scale=1.0 / n, bias=epsc)
        ACT(out, out, AF.Exp, [tmpk], w, scale=-0.5)

    cst = A.alloc(1024, F32)
    identf = cst[:, 0:128]
    triu = cst[:, 128:256]
    M1 = cst[:, 256:384]
    M2 = cst[:, 384:512]
    tril_strict = cst[:, 512:640]
    iota_p = cst[:, 640:641]
    thr16 = cst[:, 656:672]
    thr64 = cst[:, 672:736]
    eidx = cst[:, 736:768]
    DMA("sp", cst, cst_d, [], ["cst"])
    cb = A.alloc(512, BF16)
    identb = cb[:, 0:128]
    onesb = cb[:, 128:256]
    triub = cb[:, 256:384]
    trilsb = cb[:, 384:512]
    CP("dve", identb, identf, ["cst"], ["cb"])
    CP("dve", triub, triu, ["cst"], ["cb"])
    CP("dve", trilsb, tril_strict, ["cst"], ["cb"])
    S.op("pool", lambda e: e.memset(onesb, 1.0), [], ["cb"])
    small = A.alloc(256, F32)
    epsc = small[:, 0:1]
    onec = small[:, 1:2]
    S.op("pool", lambda e: e.memset(epsc, EPS), [], ["epsc"])
    S.op("pool", lambda e: e.memset(onec, 1.0), [], ["epsc"])
    _sc = [2]

    def col(n=1):
        a = _sc[0]
        _sc[0] += n
        assert _sc[0] <= 256
        return small[:, a:a + n]

    modc = A.alloc(96, F32)
    A1c = A.alloc(16, F32)
    modrow_d = nc.dram_tensor("modrow_d", [1, 6 * D], F32).ap()

    mark = A.off
    ccol = A.alloc(16, F32)
    cact = A.alloc(16, BF16)
    tmp16 = A.alloc(16, F32)
    modrow = A.alloc(6 * D, F32)
    DMA("sp", ccol, ccol_d, [], ["ccol"])
    DMA("sp", modrow[0:1, :], bada_d, [], ["modrow_b"])
    ACT(tmp16, ccol, AF.Exp, ["ccol"], ["tmp16"], scale=-1.0)
    TS("dve", tmp16, tmp16, 1.0, None, ALU.add, None, ["tmp16"], ["tmp16"])
    RCP(tmp16, tmp16, ["tmp16"], ["tmp16"])
    TT("dve", cact, ccol, tmp16, ALU.mult, ["tmp16", "ccol"], ["cact"])
    wa = [A.alloc(16 * 512, BF16).rearrange("p (k n) -> p k n", k=16) for _ in range(2)]
    biasrow = modrow
    for jg in range(8):
        wt = wa[jg % 2]
        wk = "wa%d" % (jg % 2)
        S.dma("pool", (lambda wt=wt, jg=jg: (lambda e: e.dma_start(
            out=wt, in_=wada_d[:, jg * 512:(jg + 1) * 512].rearrange("(k p) n -> p k n", p=128))))(),
            [], [wk])
        pbi = jg % 2
        for k in range(16):
            MM(PB[pbi][0:1, :], cact[:, k:k + 1], wt[:, k, :], k == 0, k == 15, [wk, "cact"], [PK[pbi]])
        TT("dve", modrow[0:1, jg * 512:(jg + 1) * 512], PB[pbi][0:1, :], modrow[0:1, jg * 512:(jg + 1) * 512],
           ALU.add, ["modrow_b"], [PK[pbi], "modrow%d" % jg])
    allrow = ["modrow%d" % j for j in range(8)]
    if debug == "A":
        d = dout("mod", [1, 6 * D])
        DMA("sp", d, modrow[0:1, :], allrow, ["dbg"])
    for j in range(32):
        MM(PB[2][:, j:j + 1], modrow[0:1, j * 128:(j + 1) * 128], onec[0:1, 0:1], True, True, allrow + ["epsc"], [PK[2]])
    CP("dve", modc[:, 0:32], PB[2][:, 0:32], [], [PK[2], "modc"])
    cactp = col(16)
    cactb = cactp.bitcast(BF16)[:, 0:16]
    CP("dve", cactb, cact, ["cact"], ["cactb"])
    n1gc = A.alloc(16, F32)
    DMA("sp", n1gc, n1g_d, [], ["n1gc"])
    STT("dve", A1c, modc[:, 16:32], 1.0, n1gc, ALU.add, ALU.mult, ["modc", "n1gc"], ["A1c"])
    B1c = modc[:, 0:16]
    S.barrier()
    A.off = mark

    if debug == "A":
        d2 = dout("modc", [128, 96])
        DMA("sp", d2, modc, ["modc"], ["dbg"])
        S.emit()
        S.close()
        es.close()
        return nc, dbg

    markMix = A.off
    mixT = A.alloc(16 * S_LEN, BF16).rearrange("p (k t) -> p k t", k=16)
    markHT = A.off
    hT = A.alloc(16 * S_LEN, BF16).rearrange("p (k t) -> p k t", k=16)
    markB = A.off
    xt = [A.alloc(D, F32) for _ in range(2)]
    xn = [A.alloc(D, BF16) for _ in range(2)]
    tmod = [A.alloc(1024, F32) for _ in range(2)]
    ssq1 = col(16)
    rs1 = col(16)
    for i in range(NT):
        sl = i % 2
        DMA("sp", xt[sl], x_d[i * 128:(i + 1) * 128, :], [], ["xt%d" % sl])
        ACT(xn[sl], xt[sl], AF.Square, ["xt%d" % sl], ["xn%d" % sl, "ssq1_%d" % i], accum=ssq1[:, i:i + 1])
        rstd_col(rs1[:, i:i + 1], ssq1[:, i:i + 1], D, ["ssq1_%d" % i, "epsc"], ["rs1_%d" % i], "rs1t_%d" % i)
        ACT(xn[sl], xt[sl], AF.Identity, ["xt%d" % sl, "rs1_%d" % i], ["xn%d" % sl], scale=rs1[:, i:i + 1])
        for hf in range(2):
            for kk in range(8):
                k = hf * 8 + kk
                TR(PBH[hf][:, kk * 128:(kk + 1) * 128], xn[sl][:, k * 128:(k + 1) * 128], identb,
                   ["xn%d" % sl, "cb"], [PK[hf]])
            src = PBH[hf].rearrange("p (k t) -> p k t", k=8)
            tm = tmod[hf].rearrange("p (k t) -> p k t", k=8)
            a1 = A1c[:, hf * 8:(hf + 1) * 8].unsqueeze(2).to_broadcast([128, 8, 128])
            b1 = B1c[:, hf * 8:(hf + 1) * 8].unsqueeze(2).to_broadcast([128, 8, 128])
            TT("dve", tm, src, a1, ALU.mult, ["A1c"], [PK[hf], "tmod%d" % hf])
            TT("pool", hT[:, hf * 8:(hf + 1) * 8, i * 128:(i + 1) * 128], tm, b1, ALU.add,
               ["tmod%d" % hf, "modc"], ["hT%d" % i])
    hTall = ["hT%d" % i for i in range(NT)]
    S.barrier()
    A.off = markB
    if debug == "B":
        d = dout("hT", [128, 16 * S_LEN], BF16)
        DMA("sp", d, hT.rearrange("p k t -> p (k t)"), hTall, ["dbg"])
        S.emit(); S.close(); es.close()
        return nc, dbg

    markC = A.off
    wb = [A.alloc(16 * 512, BF16).rearrange("p (k n) -> p k n", k=16) for _ in range(2)]
    markC1 = A.off
    wsec = [wb[0], wb[1],
            mixT[:, 8:12, :].rearrange("p a t -> p (a t)").rearrange("p (k n) -> p k n", k=16),
            mixT[:, 12:16, :].rearrange("p a t -> p (a t)").rearrange("p (k n) -> p k n", k=16)]
    lbb = A.alloc(1024, F32)
    omlb = A.alloc(1024, F32)
    honb = A.alloc(128, F32)
    DMA("sp", lbb, lbl_d[0:1, :].partition_broadcast(128), [], ["lbb"])
    DMA("sp", omlb, lbl_d[1:2, :].partition_broadcast(128), [], ["omlb"])
    DMA("sp", honb, hon_d.partition_broadcast(128), [], ["honb"])
    TT("dve", omlb, omlb, lbb, ALU.subtract, ["lbb"], ["omlb"])
    ACT(omlb, omlb, AF.Exp, [], ["omlb"])
    TS("dve", omlb, omlb, 1.0, None, ALU.add, None, [], ["omlb"])
    RCP(lbb, omlb, ["omlb"], ["lbb"])
    TS("dve", omlb, lbb, -1.0, 1.0, ALU.mult, ALU.add, ["lbb"], ["omlb"])
    W4 = 512
    en_t, f_t, lf_t, kk_t = A.alloc(W4), A.alloc(W4), A.alloc(W4), A.alloc(W4)
    E1_t, E2_t = A.alloc(W4), A.alloc(W4)
    qin_t, qout_t, kin_t, kout_t, v_t = (A.alloc(W4, BF16) for _ in range(5))
    eng_t, sil_t = A.alloc(W4), A.alloc(W4)
    E3_t, E1n_t = eng_t, f_t
    trq_t, trk_t, tro_t = A.alloc(W4, BF16), A.alloc(W4, BF16), A.alloc(W4, BF16)
    am_t = A.alloc(W4, BF16)
    on_t = en_t
    og_t = A.alloc(W4, BF16)
    Sst = A.alloc(W4)
    Sbf = A.alloc(W4, BF16)
    deccol = col(4)
    ssqo = col(4)
    rso = col(4)
    QSC = 128.0 ** -0.5
    v4 = lambda t: t.rearrange("p (h d) -> p h d", h=4)
    triu4 = triu.unsqueeze(1).to_broadcast([128, 4, 128])
    for hgp in range(2):
        for sec in range(4):
            c0 = sec * 1024 + hgp * 512
            S.dma("pool", (lambda sec=sec, c0=c0: (lambda e: e.dma_start(
                out=wsec[sec], in_=win_d[:, c0:c0 + 512].rearrange("(k p) n -> p k n", p=128))))(), [], ["wsec%d" % sec])
        hs = slice(hgp * 512, (hgp + 1) * 512)
        for i in range(NT):
            tsl = slice(i * 128, (i + 1) * 128)

            def emit_proj(ii):
                for sec in range(4):
                    for k in range(16):
                        MM(PB[sec], hT[:, k, ii * 128:(ii + 1) * 128], wsec[sec][:, k, :], k == 0, k == 15,
                           ["wsec%d" % sec, "hT%d" % ii], [PK[sec]])
            if i == 0:
                emit_proj(0)
            hq, hf_, hi_, hg = PB[0], PB[1], PB[2], PB[3]
            ACT(en_t, hf_, AF.Exp, [], [PK[1], "en"], scale=-1.0)
            ACT(v_t, hi_, AF.Copy, [], [PK[2], "v"])
            ACT(eng_t, hg, AF.Exp, [], [PK[3], "eng"], scale=-1.0)
            ACT(en_t, en_t, AF.Ln, [], ["en"], bias=onec)
            ACT(en_t, en_t, AF.Exp, [], ["en"], scale=-1.0)
            TT("dve", f_t, en_t, omlb[:, hs], ALU.mult, ["en", "omlb"], ["f"])
            TT("dve", f_t, f_t, lbb[:, hs], ALU.add, ["lbb"], ["f"])
            ACT(lf_t, f_t, AF.Ln, ["f"], ["lf"])
            ACT(kk_t, f_t, AF.Identity, ["f"], ["kk"], scale=-1.0, bias=onec)
            ACT(eng_t, eng_t, AF.Ln, [], ["eng"], bias=onec)
            ACT(eng_t, eng_t, AF.Exp, [], ["eng"], scale=-1.0)
            TT("dve", sil_t, hg, eng_t, ALU.mult, ["eng"], [PK[3], "sil"])
            TT("pool", v4(sil_t), v4(sil_t), honb.unsqueeze(1).to_broadcast([128, 4, 128]), ALU.mult, ["honb"], ["sil"])
            MM(PB[4], M1, lf_t, True, True, ["cst", "lf"], [PK[4]])
            MM(PB[5], M2, lf_t, True, True, ["cst", "lf"], [PK[5]])
            MM(PB[6], triu, lf_t, True, True, ["cst", "lf"], [PK[6]])
            for hh in range(4):
                MM(PB[7][:, hh:hh + 1], lf_t[:, hh * 128:(hh + 1) * 128], onec, True, True, ["epsc", "lf"], [PK[7]])
            ACT(E1_t, PB[4], AF.Exp, [], [PK[4], "E1"])
            ACT(E1n_t, PB[4], AF.Exp, [], [PK[4], "f"], scale=-1.0)
            ACT(E2_t, PB[5], AF.Exp, [], [PK[5], "E2"])
            ACT(E3_t, PB[6], AF.Exp, [], [PK[6], "eng"])
            ACT(deccol, PB[7][:, 0:4], AF.Exp, [], [PK[7], "dec"])
            STT("dve", qin_t, hq, QSC, E1_t, ALU.mult, ALU.mult, ["E1"], [PK[0], "qin"])
            STT("dve", qout_t, hq, QSC, E3_t, ALU.mult, ALU.mult, ["eng"], [PK[0], "qout"])
            TT("pool", kin_t, kk_t, E1n_t, ALU.mult, ["kk", "f"], ["kin"])
            TT("pool", kout_t, kk_t, E2_t, ALU.mult, ["kk", "E2"], ["kout"])
            for hh in range(4):
                hsl = slice(hh * 128, (hh + 1) * 128)
                TR(PBH[4][:, hsl], qin_t[:, hsl], identb, ["qin", "cb"], [PK[4]])
                TR(PBH[5][:, hsl], kin_t[:, hsl], identb, ["kin", "cb"], [PK[5]])
                TR(PBH[6][:, hsl], qout_t[:, hsl], identb, ["qout", "cb"], [PK[6]])
            CP("dve", trq_t, PBH[4][:, 0:512], [], [PK[4], "trq"])
            ACT(trk_t, PBH[5][:, 0:512], AF.Copy, [], [PK[5], "trk"])
            CP("dve", tro_t, PBH[6][:, 0:512], [], [PK[6], "tro"])
            for hh in range(4):
                hsl = slice(hh * 128, (hh + 1) * 128)
                MM(PB[4][:, hsl], trk_t[:, hsl], trq_t[:, hsl], True, True, ["trk", "trq"], [PK[4]])
            if i + 1 < NT:
                emit_proj(i + 1)
            TT("dve", v4(am_t), v4(PB[4]), triu4, ALU.mult, ["cst"], [PK[4], "am"])
            for hh in range(4):
                hsl = slice(hh * 128, (hh + 1) * 128)
                if i == 0:
                    MM(PB[5][:, hsl], am_t[:, hsl], v_t[:, hsl], True, True, ["am", "v"], [PK[5]])
                else:
                    MM(PB[5][:, hsl], am_t[:, hsl], v_t[:, hsl], True, False, ["am", "v"], [PK[5]])
                    MM(PB[5][:, hsl], tro_t[:, hsl], Sbf[:, hsl], False, True, ["tro", "Sbf"], [PK[5]])
            for hh in range(4):
                hsl = slice(hh * 128, (hh + 1) * 128)
                MM(PB[6][:, hsl], kout_t[:, hsl], v_t[:, hsl], True, True, ["kout", "v"], [PK[6]])
            if i == 0:
                CP("dve", Sst, PB[6], [], [PK[6], "S"])
            else:
                TT("pool", v4(Sst), v4(Sst), deccol.unsqueeze(2).to_broadcast([128, 4, 128]), ALU.mult, ["dec"], ["S"])
                TT("dve", Sst, Sst, PB[6], ALU.add, [], [PK[6], "S"])
            if i < NT - 1:
                ACT(Sbf, Sst, AF.Copy, ["S"], ["Sbf"])
            ACT(on_t, PB[5], AF.Square, [], [PK[5], "en"])
            RED("dve", ssqo, v4(on_t), ALU.add, ["en"], ["ssqo"])
            ACT(rso, ssqo, AF.Ln, ["ssqo", "epsc"], ["rso"], scale=1.0 / 128, bias=epsc)
            ACT(rso, rso, AF.Exp, [], ["rso"], scale=-0.5)
            TT("dve", v4(on_t), v4(PB[5]), rso.unsqueeze(2).to_broadcast([128, 4, 128]), ALU.mult, ["rso"], [PK[5], "en"])
            TT("dve", og_t, on_t, sil_t, ALU.mult, ["en", "sil"], ["og"])
            for hh in range(4):
                hsl = slice(hh * 128, (hh + 1) * 128)
                TR(PBH[7][:, hsl], og_t[:, hsl], identb, ["og", "cb"], [PK[7]])
            ACT(mixT[:, hgp * 4:(hgp + 1) * 4, tsl], PBH[7][:, 0:512].rearrange("p (h t) -> p h t", h=4), AF.Copy, [],
                [PK[7], "mixT%d_%d" % (hgp, i)])
    S.barrier()
    A.off = markC1
    if debug == "C1":
        d = dout("mixT", [128, 16 * S_LEN], BF16)
        DMA("sp", d, mixT.rearrange("p k t -> p (k t)"), [], ["dbg"])
        S.emit(); S.close(); es.close()
        return nc, dbg

    A.off = markC + 16 * 512
    mla_d = nc.dram_tensor("mla_scratch", [128, 9 * S_LEN], BF16).ap()

    def alloc_mla():
        qaT = A.alloc(4 * S_LEN, BF16).rearrange("p (k t) -> p k t", k=4)
        kvaT = A.alloc(2 * S_LEN, BF16).rearrange("p (k t) -> p k t", k=2)
        return qaT, kvaT, A.alloc(S_LEN, BF16), A.alloc(S_LEN, BF16), A.alloc(S_LEN, BF16)
    mla0 = A.off
    qaT, kvaT, kpeT, kpesT, SQR = alloc_mla()
    mla_all = A.ap[:, mla0:mla0 + 9 * S_LEN]
    qagc = A.alloc(4, F32)
    kvgc = A.alloc(2, F32)
    qng = A.alloc(4, F32)
    kng = A.alloc(4, F32)
    DMA("sp", qagc, qag_d, [], ["qagc"])
    DMA("sp", kvgc, kvg_d, [], ["kvgc"])
    DMA("sp", qng, qng_d, [], ["qng"])
    DMA("sp", kng, kng_d, [], ["kng"])
    TT("dve", qng[:, 2:3], qng[:, 2:3], qng[:, 3:4], ALU.mult, [], ["qng"])
    TT("dve", kng[:, 2:3], kng[:, 2:3], kng[:, 3:4], ALU.mult, [], ["kng"])
    sq_t = [A.alloc(512, BF16) for _ in range(2)]
    rsb_t = [A.alloc(512, F32) for _ in range(2)]
    S.op("pool", lambda e: e.memset(SQR, 0.0), [], ["SQR"])
    wA, wB = wb[0], wb[0]
    S.dma("pool", lambda e: e.dma_start(out=wA, in_=win_d[:, 4096:4608].rearrange("(k p) n -> p k n", p=128)), [], ["wb0"])

    def lowrank(wt, wk, nch, dstT, gcol, gk, nfeat, dk):
        for tg in range(4):
            tsl = slice(tg * 512, (tg + 1) * 512)
            for c in range(nch):
                pbi = c % 2
                for k in range(16):
                    MM(PB[pbi], wt[:, k, c * 128:(c + 1) * 128], hT[:, k, tsl], k == 0, k == 15,
                       [wk] + hTall, [PK[pbi]])
                ACT(sq_t[pbi], PB[pbi], AF.Square, [], [PK[pbi], "sq%d" % pbi])
                TS("dve", dstT[:, c, tsl], PB[pbi], gcol[:, c:c + 1], None, ALU.mult, None, [gk],
                   [PK[pbi], dk + "%d_%d" % (c, tg)])
                MM(PB[2], onesb, sq_t[pbi], c == 0, c == nch - 1, ["cb", "sq%d" % pbi], [PK[2]])
            rb = rsb_t[tg % 2]
            rk = "rsb%d" % (tg % 2)
            ACT(rb, PB[2], AF.Ln, ["epsc"], [PK[2], rk], scale=1.0 / nfeat, bias=epsc)
            ACT(rb, rb, AF.Exp, [], [rk], scale=-0.5)
            for c in range(nch):
                TT("pool" if c % 2 else "dve", dstT[:, c, tsl], dstT[:, c, tsl], rb, ALU.mult, [rk],
                   [dk + "%d_%d" % (c, tg)])
    lowrank(wA, "wb0", 4, qaT, qagc, "qagc", 512, "qaT")
    S.op("pool", lambda e: e.memset(wB[:, :, 256:512], 0.0), [], ["wb0"])
    S.dma("pool", lambda e: e.dma_start(out=wB[:, :, 0:256], in_=win_d[:, 4608:4864].rearrange("(k p) n -> p k n", p=128)), [], ["wb0"])
    S.dma("pool", lambda e: e.dma_start(out=wB[:, :, 256:320], in_=win_d[:, 4864:4928].rearrange("(k p) n -> p k n", p=128)), [], ["wb0"])
    S.dma("pool", lambda e: e.dma_start(out=wB[:, :, 384:416], in_=win_d[:, 4896:4928].rearrange("(k p) n -> p k n", p=128)), [], ["wb0"])
    S.dma("pool", lambda e: e.dma_start(out=wB[:, :, 416:448], in_=win_d[:, 4864:4896].rearrange("(k p) n -> p k n", p=128)), [], ["wb0"])
    lowrank(wB, "wb0", 2, kvaT, kvgc, "kvgc", 256, "kvaT")
    for tg in range(4):
        tsl = slice(tg * 512, (tg + 1) * 512)
        for k in range(16):
            MM(PB[3], wB[:, k, 256:384], hT[:, k, tsl], k == 0, k == 15, ["wb0"] + hTall, [PK[3]])
        for k in range(16):
            MM(PB[4], wB[:, k, 384:512], hT[:, k, tsl], k == 0, k == 15, ["wb0"] + hTall, [PK[4]])
        ACT(SQR[0:64, tsl], PB[3][0:64, :], AF.Square, [], [PK[3], "SQR"])
        TS("dve", kpeT[0:64, tsl], PB[3][0:64, :], kng[0:64, 1:2], None, ALU.mult, None, ["kng"], [PK[3], "kpeT"])
        TS("dve", kpesT[0:64, tsl], PB[4][0:64, :], kng[0:64, 2:3], None, ALU.mult, None, ["kng"], [PK[4], "kpesT"])
    qaT_keys = ["qaT%d_%d" % (c, tg) for c in range(4) for tg in range(4)]
    kvaT_keys = ["kvaT%d_%d" % (c, tg) for c in range(2) for tg in range(4)]
    S.barrier()
    if debug == "C2":
        d = dout("qaT", [128, 4 * S_LEN], BF16)
        DMA("sp", d, qaT.rearrange("p k t -> p (k t)"), [], ["dbg"])
        d = dout("kvaT", [128, 2 * S_LEN], BF16)
        DMA("sp", d, kvaT.rearrange("p k t -> p (k t)"), [], ["dbg"])
        d = dout("kpeT", [64, S_LEN], BF16)
        DMA("sp", d, kpeT[0:64, :], [], ["dbg"])
        d = dout("kpesT", [64, S_LEN], BF16)
        DMA("sp", d, kpesT[0:64, :], [], ["dbg"])
        S.emit(); S.close(); es.close()
        return nc, dbg
    qngp = col(4)
    kngp = col(4)
    CP("dve", qngp, qng, ["qng"], ["qngp"])
    CP("dve", kngp, kng, ["kng"], ["kngp"])
    S.barrier()
    topD = mla0 + 9 * S_LEN
    A.off = markHT
    A.cap = mla0
    if debug == "D00":
        d = dout("qaT", [128, 4 * S_LEN], BF16)
        DMA("sp", d, qaT.rearrange("p k t -> p (k t)"), [], ["dbg"])
        d = dout("kvaT", [128, 2 * S_LEN], BF16)
        DMA("sp", d, kvaT.rearrange("p k t -> p (k t)"), [], ["dbg"])
        d = dout("kpeT", [64, S_LEN], BF16)
        DMA("sp", d, kpeT[0:64, :], [], ["dbg"])
        d = dout("kpesT", [64, S_LEN], BF16)
        DMA("sp", d, kpesT[0:64, :], [], ["dbg"])
        S.emit(); S.close(); es.close()
        return nc, dbg
    PI = float(np.pi)
    cosT = A.alloc(S_LEN, F32)
    sinT = A.alloc(S_LEN, F32)
    RT = A.alloc(S_LEN, BF16)
    aonb = A.alloc(128, F32)
    import os
    SK = os.environ.get("SKIPD", "")
    if "a" not in SK:
        DMA("sp", aonb, aon_d.partition_broadcast(128), [], ["aonb"])
    markD0 = A.off
    posi = A.alloc(S_LEN, I32)
    ang = A.alloc(S_LEN, F32)
    kq = A.alloc(S_LEN, F32)
    kqi = A.alloc(S_LEN, I32)
    msk = A.alloc(S_LEN, F32)
    invf = col(1)
    if "i" not in SK:
        DMA("sp", invf[0:64, :], invf_d, [], ["invf"])
    if "p" not in SK:
        DMA("sp", posi[0:64, :], pos_d.partition_broadcast(64), [], ["posi"])
    def dump_kva(tag):
        if debug == tag:
            S.barrier()
            d = dout("kvaT2", [128, 2 * S_LEN], BF16); DMA("sp", d, kvaT.rearrange("p k t -> p (k t)"), [], ["dbg"])
            S.emit(); S.close(); es.close()
            return True
        return False
    if dump_kva("X1"):
        return nc, dbg
    for (dst, shift, key) in ((sinT, 0.0, "sinT"), (cosT, PI / 2, "cosT")):
        a_, q_, qi_, m_ = ang[0:64, :], kq[0:64, :], kqi[0:64, :], msk[0:64, :]
        CP("dve", a_, posi[0:64, :], ["posi"], ["ang"])
        TS("dve", a_, a_, invf[0:64, :], shift, ALU.mult, ALU.add, ["invf"], ["ang"])
        TS("dve", q_, a_, 1.0 / (2 * PI), None, ALU.mult, None, ["ang"], ["kq"])
        CP("dve", qi_, q_, ["kq"], ["kqi"])
        CP("dve", q_, qi_, ["kqi"], ["kq"])
        if key == "sinT" and dump_kva("X2"):
            return nc, dbg
        STT("dve", a_, q_, -2 * PI, a_, ALU.mult, ALU.add, ["kq"], ["ang"])
        TS("dve", m_, a_, PI, None, ALU.is_gt, None, ["ang"], ["msk"])
        STT("dve", a_, m_, -2 * PI, a_, ALU.mult, ALU.add, ["msk"], ["ang"])
        TS("dve", m_, a_, -PI, None, ALU.is_lt, None, ["ang"], ["msk"])
        STT("dve", a_, m_, 2 * PI, a_, ALU.mult, ALU.add, ["msk"], ["ang"])
        if key == "sinT" and dump_kva("X3"):
            return nc, dbg
        ACT(dst[0:64, :], a_, AF.Sin, ["ang"], [key])
        if key == "sinT" and dump_kva("X4"):
            return nc, dbg
    TT("dve", ang[0:64, :], kpeT[0:64, :], cosT[0:64, :], ALU.mult, ["kpeT", "cosT"], ["ang"])
    TT("dve", kq[0:64, :], kpesT[0:64, :], sinT[0:64, :], ALU.mult, ["kpesT", "sinT"], ["kq"])
    TT("dve", RT[0:64, :], ang[0:64, :], kq[0:64, :], ALU.add, ["kq", "ang"], ["RT"])
    S.barrier()
    A.off = markD0
    if debug == "D0":
        d = dout("cosT", [64, S_LEN]); DMA("sp", d, cosT[0:64, :], [], ["dbg"])
        d = dout("sinT", [64, S_LEN]); DMA("sp", d, sinT[0:64, :], [], ["dbg"])
        d = dout("RT", [64, S_LEN], BF16); DMA("sp", d, RT[0:64, :], [], ["dbg"])
        d = dout("kvaT2", [128, 2 * S_LEN], BF16); DMA("sp", d, kvaT.rearrange("p k t -> p (k t)"), [], ["dbg"])
        print("offsets", markHT, mla0, markD0, A.off)
        S.emit(); S.close(); es.close()
        return nc, dbg

    wq_t = [A.alloc(4 * 384, BF16).rearrange("p (k n) -> p k n", k=4) for _ in range(2)]
    wkv_t = [A.alloc(2 * 256, BF16).rearrange("p (k n) -> p k n", k=2) for _ in range(2)]
    QTn_t = [A.alloc(S_LEN, BF16) for _ in range(2)]
    QTr_t = [A.alloc(S_LEN, BF16) for _ in range(2)]
    KTn_t = [A.alloc(S_LEN, BF16) for _ in range(2)]
    KTr_t = [A.alloc(S_LEN, BF16) for _ in range(2)]
    V_t = [A.alloc(16 * 130, BF16).rearrange("p (t v) -> p t v", t=16) for _ in range(2)]
    for sl in range(2):
        S.op("pool", (lambda sl=sl: (lambda e: e.memset(V_t[sl][:, :, 128:130], 1.0)))(), [], ["V%d" % sl])
        S.op("pool", (lambda sl=sl: (lambda e: e.memset(wq_t[sl], 0.0)))(), [], ["wq%d" % sl])
        S.op("pool", (lambda sl=sl: (lambda e: e.memset(QTr_t[sl], 0.0)))(), [], ["QTr%d" % sl])
        S.op("pool", (lambda sl=sl: (lambda e: e.memset(KTr_t[sl], 0.0)))(), [], ["KTr%d" % sl])
    lowtop = A.off
    A.off = topD
    A.cap = A.ap.shape[1]
    sqn_t = [A.alloc(512, BF16) for _ in range(2)]
    sqr_t = [A.alloc(512, BF16) for _ in range(2)]
    rb_t = [A.alloc(512, F32) for _ in range(2)]
    t1_t = [A.alloc(512, F32) for _ in range(2)]
    t2_t = [A.alloc(512, F32) for _ in range(2)]
    mrowt = A.alloc(256, F32)
    A.off = lowtop
    A.cap = mla0
    PT_t = [A.alloc(512, BF16) for _ in range(2)]
    wad = A.alloc(16 * 256, BF16).rearrange("p (k n) -> p k n", k=16)
    ada_next = [0]

    ada_pending = [None]

    def ada_group():
        if ada_pending[0] is not None:
            c0 = ada_pending[0]
            for k in range(16):
                MM(PB[7][0:1, 0:256], cactb[:, k:k + 1], wad[:, k, :], k == 0, k == 15, ["wad", "cactb"], [PK[7]])
            TT("dve", mrowt[0:1, :], PB[7][0:1, 0:256], mrowt[0:1, :], ALU.add, [], [PK[7], "mrowt"])
            DMA("sp", modrow_d[0:1, c0:c0 + 256], mrowt[0:1, :], ["mrowt"], ["modrow_d"])
            ada_pending[0] = None
        g = ada_next[0]
        if g >= 32:
            return
        ada_next[0] += 1
        c0 = 4096 + g * 256
        S.dma("pool", (lambda c0=c0: (lambda e: e.dma_start(
            out=wad, in_=wada_d[:, c0:c0 + 256].rearrange("(k p) n -> p k n", p=128))))(), [], ["wad"])
        DMA("sp", mrowt[0:1, :], bada_d[0:1, c0:c0 + 256], [], ["mrowt"])
        ada_pending[0] = c0
    ob_t = [A.alloc(128, BF16) for _ in range(2)]
    junk3 = A.alloc(128, F32)
    ocol = col(8)
    SM_SCALE = 192.0 ** -0.5
    nev = 0
    npt = 0
    for h in range(8):
        sl = h % 2
        s_ = str(sl)
        wq, wkv = wq_t[sl], wkv_t[sl]
        QTn, QTr, KTn, KTr, V = QTn_t[sl], QTr_t[sl], KTn_t[sl], KTr_t[sl], V_t[sl]
        b0 = h * 192
        for (a, bnd, c0, n) in ((0, 128, b0, 128), (128, 192, b0 + 128, 64), (256, 288, b0 + 160, 32), (288, 320, b0 + 128, 32)):
            S.dma("pool", (lambda wq=wq, a=a, bnd=bnd, c0=c0, n=n: (lambda e: e.dma_start(
                out=wq[:, :, a:bnd], in_=wqu_d[:, c0:c0 + n].rearrange("(k p) n -> p k n", p=128))))(), [], ["wq" + s_])
        S.dma("pool", (lambda wkv=wkv, h=h: (lambda e: e.dma_start(
            out=wkv, in_=wkv_d[:, h * 256:(h + 1) * 256].rearrange("(k p) n -> p k n", p=128))))(), [], ["wkv" + s_])
        def proj_body(tg, sl=sl, s_=s_, wq=wq, wkv=wkv, QTn=QTn, QTr=QTr, KTn=KTn, KTr=KTr, V=V):
            tsl = slice(tg * 512, (tg + 1) * 512)
            g2 = tg % 2
            gs = str(g2)
            bA, bB, bC, bD = (0, 1, 2, 3) if g2 == 0 else (4, 5, 6, 7)
            for k in range(4):
                MM(PB[bA], wq[:, k, 0:128], qaT[:, k, tsl], k == 0, k == 3, ["wq" + s_] + qaT_keys, [PK[bA]])
            for k in range(4):
                MM(PB[bB], wq[:, k, 128:256], qaT[:, k, tsl], k == 0, k == 3, ["wq" + s_] + qaT_keys, [PK[bB]])
            for k in range(4):
                MM(PB[bC], wq[:, k, 256:384], qaT[:, k, tsl], k == 0, k == 3, ["wq" + s_] + qaT_keys, [PK[bC]])
            yield
            ACT(sqn_t[g2], PB[bA], AF.Square, [], [PK[bA], "sqn" + gs])
            ACT(sqr_t[g2], PB[bB], AF.Square, [], [PK[bB], "sqr" + gs])
            yield
            MM(PB[bD], onesb, sqn_t[g2], True, False, ["cb", "sqn" + gs], [PK[bD]])
            MM(PB[bD], onesb, sqr_t[g2], False, True, ["cb", "sqr" + gs], [PK[bD]])
            yield
            rb = rb_t[g2]
            ACT(rb, PB[bD], AF.Ln, ["epsc"], [PK[bD], "rb" + gs], scale=1.0 / 192, bias=epsc)
            ACT(rb, rb, AF.Exp, [], ["rb" + gs], scale=-0.5)
            yield
            STT("dve", QTn[:, tsl], PB[bA], qngp[:, 0:1], rb, ALU.mult, ALU.mult, ["qngp", "rb" + gs], [PK[bA], "QTn" + s_])
            STT("dve", t1_t[g2][0:64, :], PB[bB][0:64, :], qngp[0:64, 1:2], cosT[0:64, tsl], ALU.mult, ALU.mult,
                ["qngp", "cosT"], [PK[bB], "t1" + gs])
            STT("dve", t2_t[g2][0:64, :], PB[bC][0:64, :], qngp[0:64, 2:3], sinT[0:64, tsl], ALU.mult, ALU.mult,
                ["qngp", "sinT"], [PK[bC], "t2" + gs])
            yield
            TT("pool", t1_t[g2][0:64, :], t1_t[g2][0:64, :], t2_t[g2][0:64, :], ALU.add, ["t2" + gs], ["t1" + gs])
            TT("pool", QTr[0:64, tsl], t1_t[g2][0:64, :], rb[0:64, :], ALU.mult, ["t1" + gs, "rb" + gs], ["QTr" + s_])
            for k in range(2):
                MM(PB[bA], wkv[:, k, 0:128], kvaT[:, k, tsl], k == 0, k == 1, ["wkv" + s_] + kvaT_keys, [PK[bA]])
            for j in range(4):
                t = tg * 4 + j
                for k in range(2):
                    MM(PB[bB][:, j * 128:(j + 1) * 128], kvaT[:, k, t * 128:(t + 1) * 128], wkv[:, k, 128:256],
                       k == 0, k == 1, ["wkv" + s_] + kvaT_keys, [PK[bB]])
            yield
            ACT(sqn_t[g2], PB[bA], AF.Square, [], [PK[bA], "sqn" + gs])
            ACT(V[:, tg * 4:(tg + 1) * 4, 0:128], PB[bB].rearrange("p (t v) -> p t v", t=4), AF.Copy, [],
                [PK[bB], "V" + s_])
            yield
            MM(PB[bD], onesb, sqn_t[g2], True, False, ["cb", "sqn" + gs], [PK[bD]])
            MM(PB[bD], onesb, SQR[:, tsl], False, True, ["cb", "SQR"], [PK[bD]])
            yield
            ACT(rb, PB[bD], AF.Ln, ["epsc"], [PK[bD], "rb" + gs], scale=1.0 / 192, bias=epsc)
            ACT(rb, rb, AF.Exp, [], ["rb" + gs], scale=-0.5)
            yield
            STT("dve", KTn[:, tsl], PB[bA], kngp[:, 0:1], rb, ALU.mult, ALU.mult, ["kngp", "rb" + gs], [PK[bA], "KTn" + s_])
            TT("pool", KTr[0:64, tsl], RT[0:64, tsl], rb[0:64, :], ALU.mult, ["RT", "rb" + gs], ["KTr" + s_])
            yield
        pipeline([proj_body(tg) for tg in range(4)], 2)
        steps = [(G, kt) for G in range(4) for kt in range(4 * G + 4)]

        def geom(G, kt):
            j0 = max(0, kt - 4 * G)
            return j0, (4 - j0) * 128, (4 * G + j0) * 128

        def emit_ST(n):
            G, kt = steps[n]
            j0, ncol, q0 = geom(G, kt)
            sb = n % 2
            MM(PB[sb][:, 0:ncol], KTn[:, kt * 128:(kt + 1) * 128], QTn[:, q0:q0 + ncol], True, False,
               ["KTn" + s_, "QTn" + s_], [PK[sb]])
            MM(PB[sb][:, 0:ncol], KTr[:, kt * 128:(kt + 1) * 128], QTr[:, q0:q0 + ncol], False, True,
               ["KTr" + s_, "QTr" + s_], [PK[sb]])
        emit_ST(0)
        for n in range(len(steps)):
            G, kt = steps[n]
            j0, ncol, q0 = geom(G, kt)
            sb = n % 2
            pt = PT_t[npt % 2]
            pk = "PT%d" % (npt % 2)
            if n % 10 == 5:
                ada_group()
            npt += 1
            if n + 1 < len(steps):
                emit_ST(n + 1)
            ACT(pt[:, 0:ncol], PB[sb][:, 0:ncol], AF.Exp, [], [PK[sb], pk], scale=SM_SCALE)
            if kt >= 4 * G:
                TT("pool", pt[:, 0:128], pt[:, 0:128], triub, ALU.mult, ["cb"], [pk])
            for j in range(j0, 4):
                qt = 4 * G + j
                MM(PB[2 + j][:, 0:129], pt[:, (j - j0) * 128:(j - j0 + 1) * 128], V[:, kt, 0:129],
                   kt == 0, kt == qt, [pk, "V" + s_], [PK[2 + j]])
                if kt == qt:
                    e2 = nev % 2
                    es_ = str(e2)
                    nev += 1
                    O = PB[2 + j]
                    ssq = ocol[:, e2 * 4:e2 * 4 + 1]
                    tt1 = ocol[:, e2 * 4 + 1:e2 * 4 + 2]
                    tt2 = ocol[:, e2 * 4 + 2:e2 * 4 + 3]
                    den = ocol[:, e2 * 4 + 3:e2 * 4 + 4]
                    ACT(junk3, O[:, 0:128], AF.Square, [], [PK[2 + j], "junk3", "ossq" + es_], accum=ssq)
                    CP("dve", den, O[:, 128:129], [], [PK[2 + j], "oden" + es_])
                    STT("dve", tt1, den, EPS, den, ALU.mult, ALU.mult, ["oden" + es_], ["ott1" + es_])
                    STT("dve", tt2, ssq, 1.0 / 128, tt1, ALU.mult, ALU.add, ["ossq" + es_, "ott1" + es_], ["ott2" + es_])
                    ACT(tt2, tt2, AF.Ln, [], ["ott2" + es_])
                    ACT(tt2, tt2, AF.Exp, [], ["ott2" + es_], scale=-0.5)
                    STT("dve", ob_t[e2], O[:, 0:128], tt2, aonb, ALU.mult, ALU.mult, ["ott2" + es_, "aonb"],
                        [PK[2 + j], "ob" + es_])
                    TR(PBH[6][:, 0:128], ob_t[e2], identb, ["ob" + es_, "cb"], [PK[6]])
                    ACT(mixT[:, 8 + h, qt * 128:(qt + 1) * 128], PBH[6][:, 0:128], AF.Copy, [],
                        [PK[6], "mixT%d_%d" % (8 + h, qt)])
    ada_group()
    assert ada_next[0] == 32 and ada_pending[0] is None
    S.barrier()
    A.off = markHT
    A.cap = A.ap.shape[1]
    if debug == "D":
        d = dout("mixT", [128, 16 * S_LEN], BF16)
        DMA("sp", d, mixT.rearrange("p k t -> p (k t)"), [], ["dbg"])
        S.emit(); S.close(); es.close()
        return nc, dbg

    markE = A.off
    wo = A.alloc(16 * D, BF16).rearrange("p (k n) -> p k n", k=16)
    g1b = A.alloc(D, F32)
    xt = [A.alloc(D, F32) for _ in range(2)]
    tmpe = [A.alloc(512, F32) for _ in range(2)]
    for n in range(4):
        S.dma("pool", (lambda n=n: (lambda e: e.dma_start(
            out=wo[:, :, n * 512:(n + 1) * 512],
            in_=wout_d[:, n * 512:(n + 1) * 512].rearrange("(k p) n -> p k n", p=128))))(), [], ["wo%d" % n])
    DMA("sp", g1b, modrow_d[0:1, 2 * D:3 * D].partition_broadcast(128), ["modrow_d"], ["g1b"])
    mix_keys = []
    for i in range(NT):
        sl = i % 2
        DMA("sp", xt[sl], x_d[i * 128:(i + 1) * 128, :], [], ["xt%d" % sl])
        for n in range(4):
            pbi = (i * 4 + n) % 4
            for k in range(16):
                MM(PB[pbi], mixT[:, k, i * 128:(i + 1) * 128], wo[:, k, n * 512:(n + 1) * 512], k == 0, k == 15,
                   ["wo%d" % n], [PK[pbi]])
            tp = tmpe[n % 2]
            TT("dve", tp, PB[pbi], g1b[:, n * 512:(n + 1) * 512], ALU.mult, ["g1b"], [PK[pbi], "tmpe%d" % (n % 2)])
            TT("pool", xt[sl][:, n * 512:(n + 1) * 512], xt[sl][:, n * 512:(n + 1) * 512], tp, ALU.add,
               ["tmpe%d" % (n % 2)], ["xt%d" % sl])
        DMA("sp", out_d[i * 128:(i + 1) * 128, :], xt[sl], ["xt%d" % sl], ["out"])
    S.barrier()
    A.off = markMix
    if debug == "E1":
        S.emit(); S.close(); es.close()
        return nc, dbg

    h2_d = nc.dram_tensor("h2_scratch", [S_LEN, D], BF16).ap()
    A2b = A.alloc(D, F32)
    B2b = A.alloc(D, F32)
    g2b = A.alloc(D, F32)
    LG = A.alloc(16 * 36, F32).rearrange("p (t n) -> p t n", t=16)
    IDXW = A.alloc(64, I32)
    markE2 = A.off
    n2gb = A.alloc(D, F32)
    wgr = A.alloc(16 * 36, F32).rearrange("p (k n) -> p k n", k=16)
    bgrb = A.alloc(36, F32)
    DMA("sp", A2b, modrow_d[0:1, 4 * D:5 * D].partition_broadcast(128), ["modrow_d"], ["A2b"])
    DMA("sp", B2b, modrow_d[0:1, 3 * D:4 * D].partition_broadcast(128), ["modrow_d"], ["B2b"])
    DMA("sp", g2b, modrow_d[0:1, 5 * D:6 * D].partition_broadcast(128), ["modrow_d"], ["g2b"])
    DMA("sp", n2gb, n2g_d.partition_broadcast(128), [], ["n2gb"])
    DMA("sp", wgr, wgr_d.rearrange("(k p) n -> p k n", p=128), [], ["wgr"])
    DMA("sp", bgrb, bgr_d.partition_broadcast(128), [], ["bgrb"])
    STT("dve", A2b, A2b, 1.0, n2gb, ALU.add, ALU.mult, ["n2gb"], ["A2b"])
    xt = [A.alloc(D, F32) for _ in range(2)]
    h2f_t = [A.alloc(D, F32) for _ in range(2)]
    h2b = [A.alloc(D, BF16) for _ in range(2)]
    h2T_t = [A.alloc(16 * 128, F32).rearrange("p (k t) -> p k t", k=16) for _ in range(2)]
    ssq2 = col(16)
    rs2 = col(16)

    def e2_body(i):
        sl = i % 2
        s_ = str(sl)
        h2f, h2T = h2f_t[sl], h2T_t[sl]
        b0 = 4 * sl
        DMA("sp", xt[sl], out_d[i * 128:(i + 1) * 128, :], ["out"], ["xt" + s_])
        ACT(h2f, xt[sl], AF.Square, ["xt" + s_], ["h2f" + s_, "ssq2_%d" % i], accum=ssq2[:, i:i + 1])
        yield
        rstd_col(rs2[:, i:i + 1], ssq2[:, i:i + 1], D, ["ssq2_%d" % i, "epsc"], ["rs2_%d" % i], "rs2t_%d" % i)
        yield
        STT("dve", h2f, xt[sl], rs2[:, i:i + 1], A2b, ALU.mult, ALU.mult, ["rs2_%d" % i, "A2b"], ["h2f" + s_])
        yield
        TT("dve", h2f, h2f, B2b, ALU.add, ["B2b"], ["h2f" + s_])
        yield
        ACT(h2b[sl], h2f, AF.Copy, ["h2f" + s_], ["h2b" + s_])
        for q in range(4):
            for kk in range(4):
                k = q * 4 + kk
                TR(PB[b0 + q][:, kk * 128:(kk + 1) * 128], h2f[:, k * 128:(k + 1) * 128], identf, ["h2f" + s_, "cst"],
                   [PK[b0 + q]])
        yield
        DMA("sp", h2_d[i * 128:(i + 1) * 128, :], h2b[sl], ["h2b" + s_], ["h2_d%d" % i])
        for q in range(4):
            if q % 2:
                CP("dve", h2T[:, q * 4:(q + 1) * 4, :], PB[b0 + q].rearrange("p (k t) -> p k t", k=4), [],
                   [PK[b0 + q], "h2T" + s_])
            else:
                ACT(h2T[:, q * 4:(q + 1) * 4, :], PB[b0 + q].rearrange("p (k t) -> p k t", k=4), AF.Copy, [],
                    [PK[b0 + q], "h2T" + s_])
        yield
        for k in range(16):
            MM(PB[b0][:, 0:36], h2T[:, k, :], wgr[:, k, :], k == 0, k == 15, ["h2T" + s_, "wgr"], [PK[b0]])
        yield
        TT("dve", LG[:, i, :], PB[b0][:, 0:36], bgrb, ALU.add, ["bgrb"], [PK[b0], "LG"])
        yield
    pipeline([e2_body(i) for i in range(NT)], 2)
    S.barrier()
    A.off = markE2
    if debug == "E2":
        d = dout("LG", [128, 16 * 36]); DMA("sp", d, LG.rearrange("p t n -> p (t n)"), [], ["dbg"])
        d = dout("h2", [S_LEN, D], BF16); DMA("sp", d, h2_d, [], ["dbg"])
        S.emit(); S.close(); es.close()
        return nc, dbg

    BIG = 1.0e30

    def T3(n, m):
        t = A.alloc(16 * n * m, F32)
        return t.rearrange("p (t n) -> p t n", t=16) if m == 1 else t.rearrange("p (t n m) -> p t n m", t=16, n=n)
    def bc(ap2, shape):
        v = ap2
        for ax in range(2, len(shape)):
            v = v.unsqueeze(ax)
        return v.to_broadcast(shape)
    G = LG[:, :, 0:4]
    EL = LG[:, :, 4:36]
    gmax = A.alloc(16, F32)
    RED("dve", gmax, G, ALU.max, ["LG"], ["gmax"])
    gone = T3(4, 1)
    TT("dve", gone, G, bc(gmax, [128, 16, 4]), ALU.is_equal, ["gmax", "LG"], ["gone"])
    gd = T3(4, 1)
    TT("dve", gd, G, bc(gmax, [128, 16, 4]), ALU.subtract, ["gmax", "LG"], ["gd"])
    ACT(gd, gd, AF.Exp, [], ["gd"])
    pg = A.alloc(16, F32)
    RED("dve", pg, gd, ALU.add, ["gd"], ["pg"])
    RCP(pg, pg, [], ["pg"])
    pen = T3(4, 1)
    TS("dve", pen, gone, -1.0, BIG, ALU.add, ALU.mult, ["gone"], ["pen"])
    EM = T3(32, 1)
    TT("dve", EM.rearrange("p t (g e) -> p t g e", g=4), EL.rearrange("p t (g e) -> p t g e", g=4),
       pen.unsqueeze(3).to_broadcast([128, 16, 4, 8]), ALU.add, ["pen", "LG"], ["EM"])
    v1 = A.alloc(16, F32)
    RED("dve", v1, EM, ALU.max, ["EM"], ["v1"])
    M1 = T3(32, 1)
    TT("dve", M1, EM, bc(v1, [128, 16, 32]), ALU.is_equal, ["EM", "v1"], ["M1"])
    EM2 = T3(32, 1)
    STT("dve", EM2, M1, -BIG, EM, ALU.mult, ALU.add, ["M1", "EM"], ["EM2"])
    v2 = A.alloc(16, F32)
    RED("dve", v2, EM2, ALU.max, ["EM2"], ["v2"])
    M2 = T3(32, 1)
    TT("dve", M2, EM2, bc(v2, [128, 16, 32]), ALU.is_equal, ["EM2", "v2"], ["M2"])
    e21 = A.alloc(16, F32)
    TT("dve", e21, v2, v1, ALU.subtract, ["v1", "v2"], ["e21"])
    ACT(e21, e21, AF.Exp, [], ["e21"])
    w1 = A.alloc(16, F32)
    w2 = A.alloc(16, F32)
    TS("dve", w1, e21, 1.0, None, ALU.add, None, ["e21"], ["w1"])
    RCP(w1, w1, [], ["w1"])
    TT("dve", w1, w1, pg, ALU.mult, ["pg"], ["w1"])
    TT("dve", w2, w1, e21, ALU.mult, ["w1", "e21"], ["w2"])
    Mb = A.alloc(16 * 32, BF16).rearrange("p (t n) -> p t n", t=16)
    TT("dve", Mb, M1, M2, ALU.add, ["M1", "M2"], ["Mb"])
    for i in range(NT):
        MM(PB[0][:, i * 32:(i + 1) * 32], trilsb, Mb[:, i, :], True, i == 0, ["cb", "Mb"], [PK[0]])
        for j in range(i):
            MM(PB[0][:, i * 32:(i + 1) * 32], onesb, Mb[:, j, :], False, j == i - 1, ["cb", "Mb"], [PK[0]])
    POS = T3(32, 1)
    CP("dve", POS.rearrange("p t n -> p (t n)"), PB[0], [], [PK[0], "POS"])
    for j in range(NT):
        MM(PB[1][:, 0:32], onesb, Mb[:, j, :], j == 0, j == NT - 1, ["cb", "Mb"], [PK[1]])
    cnt = A.alloc(32, F32)
    CP("dve", cnt, PB[1][:, 0:32], [], [PK[1], "cnt"])
    cmp1 = A.alloc(32 * 16, F32).rearrange("p (e m) -> p e m", e=32)
    TT("dve", cmp1, cnt.unsqueeze(2).to_broadcast([128, 32, 16]), thr16.unsqueeze(1).to_broadcast([128, 32, 16]),
       ALU.is_gt, ["cnt", "cst"], ["cmp1"])
    padded = A.alloc(32, F32)
    RED("dve", padded, cmp1, ALU.add, ["cmp1"], ["padded"])
    TS("dve", padded, padded, 128.0, None, ALU.mult, None, [], ["padded"])
    cs = [A.alloc(32, F32) for _ in range(2)]
    CP("dve", cs[0], padded, ["padded"], ["cs0"])
    cur = 0
    for sh in (1, 2, 4, 8, 16):
        nx = 1 - cur
        CP("dve", cs[nx][:, 0:sh], cs[cur][:, 0:sh], ["cs%d" % cur], ["cs%d" % nx])
        TT("dve", cs[nx][:, sh:32], cs[cur][:, sh:32], cs[cur][:, 0:32 - sh], ALU.add, ["cs%d" % cur], ["cs%d" % nx])
        cur = nx
    pad_end = cs[cur]
    pek = "cs%d" % cur
    pad_start = A.alloc(32, F32)
    TT("dve", pad_start, pad_end, padded, ALU.subtract, [pek, "padded"], ["pad_start"])
    cmp2 = A.alloc(64 * 32, F32).rearrange("p (j e) -> p j e", j=64)
    TT("dve", cmp2, pad_end.unsqueeze(1).to_broadcast([128, 64, 32]), thr64.unsqueeze(2).to_broadcast([128, 64, 32]),
       ALU.is_le, [pek, "cst"], ["cmp2"])
    blke = A.alloc(64, F32)
    RED("dve", blke, cmp2, ALU.add, ["cmp2"], ["blke"])
    TS("dve", blke, blke, 31.0, None, ALU.min, None, [], ["blke"])
    same = A.alloc(64, F32)
    S.op("pool", lambda e: e.memset(same, 0.0), [], ["same"])
    TT("dve", same[:, 2:64], blke[:, 2:64], blke[:, 0:62], ALU.is_equal, ["blke"], ["same"])
    TS("dve", blke, blke, 128.0, iota_p, ALU.mult, ALU.add, ["cst"], ["blke"])
    STT("dve", blke, same, 8192.0, blke, ALU.mult, ALU.add, ["same"], ["blke"])
    CP("dve", IDXW, blke, ["blke"], ["IDXW"])
    Tt = T3(32, 1)
    TT("dve", Tt, POS, pad_start.unsqueeze(1).to_broadcast([128, 16, 32]), ALU.add, ["POS", "pad_start"], ["Tt"])
    prod = T3(32, 1)
    dstf = A.alloc(32, F32).rearrange("p (t k) -> p t k", t=16)
    TT("dve", prod, M1, Tt, ALU.mult, ["M1", "Tt"], ["prod"])
    RED("dve", dstf[:, :, 0], prod, ALU.add, ["prod"], ["dstf"])
    TT("dve", prod, M2, Tt, ALU.mult, ["M2", "Tt"], ["prod"])
    RED("dve", dstf[:, :, 1], prod, ALU.add, ["prod"], ["dstf"])
    DI = A.alloc(32, I32)
    CP("dve", DI, dstf.rearrange("p t k -> p (t k)"), ["dstf"], ["DI"])
    tokf = A.alloc(16, F32)
    TS("dve", tokf, thr16, iota_p, None, ALU.add, None, ["cst"], ["tokf"])
    with nc.allow_non_contiguous_dma(reason="64B meta tails"):
        pass
    DMA("sp", xbuf_d[:, D:D + 32], metai_d, [], ["xbuf"])
    if debug == "E3":
        S.barrier()
        d = dout("LG", [128, 16 * 36]); DMA("sp", d, LG.rearrange("p t n -> p (t n)"), [], ["dbg"])
        d = dout("IDXW", [128, 64], I32); DMA("sp", d, IDXW, [], ["dbg"])
        d = dout("DI", [128, 32], I32); DMA("sp", d, DI, [], ["dbg"])
        d = dout("w1", [128, 16]); DMA("sp", d, w1, [], ["dbg"])
        d = dout("w2", [128, 16]); DMA("sp", d, w2, [], ["dbg"])
        d = dout("cnt", [128, 32]); DMA("sp", d, cnt, [], ["dbg"])
        d = dout("M1", [128, 512]); DMA("sp", d, M1.rearrange("p t n -> p (t n)"), [], ["dbg"])
        d = dout("M2", [128, 512]); DMA("sp", d, M2.rearrange("p t n -> p (t n)"), [], ["dbg"])
        S.emit(); S.close(); es.close()
        return nc, dbg
    hbx = [[A.alloc(D + 32, BF16) for _ in range(2)] for _ in range(2)]
    for i in range(NT):
        sl = i % 2
        for k in range(2):
            c = i * 2 + k
            hb = hbx[k][sl]
            hk = "hb%d_%d" % (k, sl)
            DMA("sp", hb[:, 0:D], h2_d[i * 128:(i + 1) * 128, :], ["h2_d%d" % i], [hk])
            tailF = hb[:, D:D + 32].bitcast(F32)
            tailI = hb[:, D:D + 32].bitcast(I32)
            CP("dve", tailI[:, 0:1], tokf[:, i:i + 1], ["tokf"], [hk])
            CP("dve", tailF[:, 1:2], (w1, w2)[k][:, i:i + 1], ["w1", "w2"], [hk])
            S.dma("pool", (lambda hb=hb, c=c: (lambda e: e.indirect_dma_start(
                out=xbuf_d, out_offset=bass.IndirectOffsetOnAxis(ap=DI[:, c:c + 1], axis=0),
                in_=hb, in_offset=None, bounds_check=breg(e, NSLOT - 1), oob_is_err=False)))(),
                [hk, "DI"], ["xbuf"])
    S.barrier()
    A.off = markE2
    markF = A.off

    wg_t = [A.alloc(16 * 512, BF16) for _ in range(2)]
    wu_t = [A.alloc(16 * 512, BF16) for _ in range(2)]
    wd_t = [A.alloc(4 * D, BF16) for _ in range(2)]
    xb_t = [A.alloc(D + 32, BF16) for _ in range(2)]
    XT_t = [A.alloc(16 * 128, BF16).rearrange("p (k s) -> p k s", k=16) for _ in range(2)]
    en_f = [A.alloc(512, F32) for _ in range(2)]
    tg_f = [A.alloc(512, F32) for _ in range(2)]
    act_b = [A.alloc(512, BF16) for _ in range(2)]
    actT = [A.alloc(4 * 128, BF16).rearrange("p (k s) -> p k s", k=4) for _ in range(2)]
    Y_t = [A.alloc(D, F32) for _ in range(2)]

    def pf(j, which):
        sl = j % 2
        s_ = str(sl)
        lst = []
        if "g" in which:
            lst += [(wg_t[sl], wg_d, "wg"), (wu_t[sl], wu_d, "wu")]
        if "d" in which:
            lst += [(wd_t[sl], wd_d, "wd")]
        for (dst, src, key) in lst:
            S.dma("pool", (lambda dst=dst, src=src, j=j: (lambda e: e.indirect_dma_start(
                out=dst, out_offset=None, in_=src, in_offset=bass.IndirectOffsetOnAxis(ap=IDXW[:, j:j + 1], axis=0),
                bounds_check=breg(e, 32 * 128 - 1), oob_is_err=False)))(), ["IDXW"], [key + s_])
        if "d" in which:
            DMA("sp", xb_t[sl], xbuf_d[j * 128:(j + 1) * 128, :], ["xbuf"], ["xb" + s_])

    def stage_A(j):
        sl = j % 2
        s_ = str(sl)
        bG, bU = 2 * sl, 2 * sl + 1
        xb, XT = xb_t[sl], XT_t[sl]
        xbv = xb[:, 0:D].rearrange("p (f k) -> p k f", k=16)
        for hf, bb in ((0, 4), (1, 5)):
            for kk in range(8):
                TR(PBH[bb][:, kk * 128:(kk + 1) * 128], xbv[:, hf * 8 + kk, :], identb, ["xb" + s_, "cb"], [PK[bb]])
        CP("dve", XT[:, 0:8, :], PBH[4].rearrange("p (k s) -> p k s", k=8), [], [PK[4], "XT" + s_])
        ACT(XT[:, 8:16, :], PBH[5].rearrange("p (k s) -> p k s", k=8), AF.Copy, [], [PK[5], "XT" + s_])
        wg, wu = wg_t[sl], wu_t[sl]
        for k in range(16):
            MM(PB[bG], XT[:, k, :], wg[:, k * 512:(k + 1) * 512], k == 0, k == 15, ["XT" + s_, "wg" + s_], [PK[bG]])
        for k in range(16):
            MM(PB[bU], XT[:, k, :], wu[:, k * 512:(k + 1) * 512], k == 0, k == 15, ["XT" + s_, "wu" + s_], [PK[bU]])

    def stage_B1(j):
        sl = j % 2
        s_ = str(sl)
        bG, bU = 2 * sl, 2 * sl + 1
        ACT(en_f[sl], PB[bG], AF.Exp, [], [PK[bG], "en_f" + s_], scale=-1.0)
        ACT(en_f[sl], en_f[sl], AF.Ln, [], ["en_f" + s_], bias=onec)
        ACT(en_f[sl], en_f[sl], AF.Exp, [], ["en_f" + s_], scale=-1.0)
        TT("dve", tg_f[sl], PB[bG], en_f[sl], ALU.mult, ["en_f" + s_], [PK[bG], "tg_f" + s_])
        TT("dve", act_b[sl], tg_f[sl], PB[bU], ALU.mult, ["tg_f" + s_], [PK[bU], "act_b" + s_])
        abv = act_b[sl].rearrange("p (j k) -> p k j", k=4)
        for kk in range(4):
            TR(PBH[6][:, kk * 128:(kk + 1) * 128], abv[:, kk, :], identb, ["act_b" + s_, "cb"], [PK[6]])
        ACT(actT[sl], PBH[6][:, 0:512].rearrange("p (k s) -> p k s", k=4), AF.Copy, [], [PK[6], "actT" + s_])

    def stage_B2(j):
        sl = j % 2
        s_ = str(sl)
        bG, bU = 2 * sl, 2 * sl + 1
        wd = wd_t[sl]
        xb = xb_t[sl]
        Y = Y_t[sl]
        wcol = xb[:, D:D + 32].bitcast(F32)[:, 1:2]
        for n in range(4):
            pbi = (bG, bU, 7, bG)[n] if False else (bG if n % 2 == 0 else bU)
            for kk in range(4):
                MM(PB[pbi], actT[sl][:, kk, :], wd[:, kk * D + n * 512: kk * D + (n + 1) * 512], kk == 0, kk == 3,
                   ["actT" + s_, "wd" + s_], [PK[pbi]])
            STT("dve", Y[:, n * 512:(n + 1) * 512], PB[pbi], wcol, g2b[:, n * 512:(n + 1) * 512],
                ALU.mult, ALU.mult, ["xb" + s_, "g2b"], [PK[pbi], "Y" + s_])
        S.dma("pool", (lambda sl=sl, Y=Y: (lambda e: e.indirect_dma_start(
            out=out_d, out_offset=bass.IndirectOffsetOnAxis(ap=xb_t[sl][:, D:D + 32].bitcast(I32)[:, 0:1], axis=0),
            in_=Y, in_offset=None, bounds_check=breg(e, S_LEN + 127), oob_is_err=True, compute_op=ALU.add)))(),
            ["Y" + s_, "xb" + s_], ["out"])
    pf(0, "gd")
    pf(1, "gd")
    stage_A(0)
    pf(2, "g")
    for j in range(NBLK):
        stage_B1(j)
        stage_B2(j)
        if j + 2 < NBLK:
            pf(j + 2, "d")
        if j + 1 < NBLK:
            stage_A(j + 1)
        if j + 3 < NBLK:
            pf(j + 3, "g")
    S.emit()
    S.close()
    es.close()
    return nc, dbg


def host_inputs(inputs, b):
    f = np.float32
    m = {}
    m["x"] = np.ascontiguousarray(inputs["x"][b])
    m["ccol"] = np.ascontiguousarray(inputs["c"][b].reshape(16, 128).T)
    m["pos"] = np.ascontiguousarray(inputs["positions"][b].reshape(1, S_LEN)).astype(np.int32)
    m["w_ada"] = inputs["w_ada"][0]
    m["b_ada"] = inputs["b_ada"][0].reshape(1, -1)
    m["norm1_gc"] = np.ascontiguousarray(inputs["norm1_g"][0].reshape(16, 128).T)
    m["w_in"] = inputs["w_in"][0]
    m["lb_logits"] = inputs["hgrn_lb_logits"]
    m["hgrn_onorm_g"] = inputs["hgrn_onorm_g"][0].reshape(1, 128)
    m["q_a_gc"] = np.ascontiguousarray(inputs["q_a_norm_g"][0].reshape(4, 128).T)
    m["w_q_up"] = inputs["w_q_up"][0]
    m["kv_a_gc"] = np.ascontiguousarray(inputs["kv_a_norm_g"][0].reshape(2, 128).T)
    m["w_kv_up"] = inputs["w_kv_up"][0]

    def qk_cols(g):
        o = np.zeros((128, 4), f)
        o[:, 0] = g[0:128]
        o[0:64, 1] = g[128:192]
        o[0:32, 2] = g[160:192]
        o[32:64, 2] = g[128:160]
        o[0:32, 3] = -1.0
        o[32:64, 3] = 1.0
        return o
    m["q_norm_gc"] = qk_cols(inputs["q_norm_g"][0])
    m["k_norm_gc"] = qk_cols(inputs["k_norm_g"][0])
    m["attn_onorm_g"] = inputs["attn_onorm_g"][0].reshape(1, 128)
    m["w_out"] = inputs["w_out"][0]
    m["norm2_g"] = inputs["norm2_g"][0].reshape(1, D)
    m["w_gr"] = np.ascontiguousarray(np.concatenate([inputs["w_group"][0], inputs["w_router"][0]], axis=1))
    m["b_gr"] = np.concatenate([inputs["b_group"][0], inputs["b_router"][0]]).reshape(1, 36)
    m["w_gate"] = inputs["w_gate"][0].reshape(32 * 128, 16 * 512)
    m["w_up"] = inputs["w_up"][0].reshape(32 * 128, 16 * 512)
    m["w_down"] = inputs["w_down"][0].reshape(32 * 128, 4 * 2048)
    m["consts"] = CONSTS
    m["invf"] = INVF
    m["meta_init"] = META_INIT.view(ml_dtypes.bfloat16)
    return m


def _consts():
    c = np.zeros((128, 1024), np.float32)
    s = np.arange(128)[:, None]
    t = np.arange(128)[None, :]
    c[:, 0:128] = np.eye(128)
    c[:, 128:256] = (s <= t)
    c[:, 256:384] = (s <= t).astype(np.float32) - (s <= 63).astype(np.float32)
    c[:, 384:512] = (s > t)
    c[:, 512:640] = (s < t)
    c[:, 640] = np.arange(128)
    c[:, 656:672] = np.arange(16) * 128
    c[:, 672:736] = np.arange(64) * 128
    c[:, 736:768] = np.arange(32)
    return c


CONSTS = _consts()
INVF = (10000.0 ** (-(np.arange(64) % 32).astype(np.float32) * 2 / 64)).astype(np.float32).reshape(64, 1)
META_INIT = np.zeros((NSLOT, 16), np.int32)
META_INIT[:, 0] = 2048 + (np.arange(NSLOT) % 128)

_NC = None


def kernel(**inputs):
    global _NC
    if _NC is None:
        _NC = build()[0]
    inputs = {k: np.asarray(v) for k, v in inputs.items()}
    in_maps = [host_inputs(inputs, b) for b in range(8)]
    res = run_bass_kernel_spmd(_NC, in_maps, core_ids=list(range(8)))
    out = np.stack([np.asarray(r["out"])[:S_LEN] for r in res.results], axis=0)
    return out.astype(np.float32)
```

```python
import numpy as np
import ml_dtypes
import concourse.bass as bass
import concourse.mybir as mybir
from concourse.bass_utils import run_bass_kernel_spmd

F32 = mybir.dt.float32
BF16 = mybir.dt.bfloat16
I32 = mybir.dt.int32
AF = mybir.ActivationFunctionType
ALU = mybir.AluOpType
AX = mybir.AxisListType

D = 2048
S_LEN = 2048
NT = 16
EPS = 1e-6
IN_COLS = 4928
BLK = 128
NBLK = 64
NSLOT = NBLK * BLK
DEBUG = None


class Sync:
    def __init__(self, nc, n_dma_sems=32):
        self.nc = nc
        self.eng = {"pe": nc.tensor, "dve": nc.vector, "act": nc.scalar,
                    "pool": nc.gpsimd, "sp": nc.sync}
        self.sem = {}
        self.cnt = {}
        self._ctx = []
        for e in self.eng:
            cm = nc.semaphore("s_" + e)
            self.sem[e] = cm.__enter__()
            self._ctx.append(cm)
            self.cnt[e] = 0
        self.dma_sems = []
        self.dma_pool = {"sp": [], "pool": [], "act": []}
        self.dma_rr = {"sp": 0, "pool": 0, "act": 0}
        for q, n in (("sp", n_dma_sems // 2), ("pool", n_dma_sems // 2), ("act", 2)):
            for i in range(n):
                cm = nc.semaphore("d%s%d" % (q, i))
                slot = [cm.__enter__(), 0, None]
                self.dma_sems.append(slot)
                self.dma_pool[q].append(slot)
                self._ctx.append(cm)
        self.waited = {}
        self.last_w = {}
        self.readers = {}
        self.prog = {e: [] for e in self.eng}

    def close(self):
        for cm in reversed(self._ctx):
            cm.__exit__(None, None, None)

    def _wait(self, e, tok):
        if tok is None:
            return
        sem, sid, val, src = tok
        if src == e and e == "pe":
            return
        k = (e, sid)
        if self.waited.get(k, 0) >= val:
            return
        self.waited[k] = val
        self.prog[e].append(("w", sem, val))

    def _deps(self, e, reads, writes, skip_same_war=True):
        for r in reads:
            self._wait(e, self.last_w.get(r))
        for w in writes:
            self._wait(e, self.last_w.get(w))
            for tok in self.readers.get(w, ()):
                if skip_same_war and tok[3] == e and e == "pe":
                    continue
                self._wait(e, tok)

    def _commit(self, tok, reads, writes):
        for w in writes:
            self.last_w[w] = tok
            self.readers[w] = []
        for r in reads:
            self.readers.setdefault(r, []).append(tok)

    def op(self, e, fn, reads=(), writes=()):
        self._deps(e, reads, writes)
        self.cnt[e] += 1
        self.prog[e].append(("i", fn, self.sem[e], 1))
        tok = (self.sem[e], e, self.cnt[e], e)
        self._commit(tok, reads, writes)
        return tok

    def dma(self, e, fn, reads=(), writes=()):
        pool = self.dma_pool[e]
        slot = pool[self.dma_rr[e]]
        self.dma_rr[e] = (self.dma_rr[e] + 1) % len(pool)
        self._wait(e, slot[2])
        self._deps(e, reads, writes, skip_same_war=False)
        slot[1] += 16
        self.prog[e].append(("i", fn, slot[0], 16))
        tok = (slot[0], id(slot), slot[1], None)
        slot[2] = tok
        self._commit(tok, reads, writes)
        return tok

    def barrier(self):
        toks = [(self.sem[e], e, self.cnt[e], e) for e in self.eng if self.cnt[e] > 0]
        toks += [s[2] for s in self.dma_sems if s[2] is not None]
        for e in self.eng:
            for t in toks:
                if t[3] == e and e == "pe":
                    continue
                self._wait(e, t)

    def emit(self):
        nc = self.nc
        self.barrier()
        prog = self.prog

        def run(engine, lst):
            for it in lst:
                if it[0] == "w":
                    engine.wait_ge(it[1], it[2])
                else:
                    it[1](engine).then_inc(it[2], it[3])

        with nc.Block() as block:
            @block.sync
            def _(eng):
                run(eng, prog["sp"])

            @block.scalar
            def _(eng):
                run(eng, prog["act"])

            @block.vector
            def _(eng):
                run(eng, prog["dve"])

            @block.gpsimd
            def _(eng):
                run(eng, prog["pool"])

            @block.tensor
            def _(eng):
                run(eng, prog["pe"])


def pipeline(gens, W):
    gens = list(gens)
    active = []
    nxt = 0
    while active or nxt < len(gens):
        while len(active) < W and nxt < len(gens):
            active.append(gens[nxt])
            nxt += 1
        for g in list(active):
            try:
                next(g)
            except StopIteration:
                active.remove(g)


class Arena:
    def __init__(self, ap):
        self.ap = ap
        self.off = 0
        self.cap = ap.shape[1]
        self.peak = 0

    def alloc(self, n, dtype=F32):
        ne = n * (2 if dtype in (F32, I32) else 1)
        ne = (ne + 15) // 16 * 16
        a = self.off
        self.off += ne
        assert self.off <= self.cap, ("arena overflow", self.off, self.cap)
        self.peak = max(self.peak, self.off)
        v = self.ap[:, a:a + n * (2 if dtype in (F32, I32) else 1)]
        if dtype == F32:
            v = v.bitcast(F32)
        elif dtype == I32:
            v = v.bitcast(I32)
        return v


def build(debug=None):
    nc = bass.Bass("TRN2", target_bir_lowering=False)

    def din(name, shape, dt=F32):
        return nc.dram_tensor(name, list(shape), dt, kind="ExternalInput").ap()

    x_d = din("x", [S_LEN, D])
    ccol_d = din("ccol", [128, 16])
    pos_d = din("pos", [1, S_LEN], I32)
    wada_d = din("w_ada", [D, 6 * D])
    bada_d = din("b_ada", [1, 6 * D])
    n1g_d = din("norm1_gc", [128, 16])
    win_d = din("w_in", [D, IN_COLS])
    lbl_d = din("lb_logits", [2, 1024])
    hon_d = din("hgrn_onorm_g", [1, 128])
    qag_d = din("q_a_gc", [128, 4])
    wqu_d = din("w_q_up", [512, 1536])
    kvg_d = din("kv_a_gc", [128, 2])
    wkv_d = din("w_kv_up", [256, 2048])
    qng_d = din("q_norm_gc", [128, 4])
    kng_d = din("k_norm_gc", [128, 4])
    aon_d = din("attn_onorm_g", [1, 128])
    wout_d = din("w_out", [D, D])
    n2g_d = din("norm2_g", [1, D])
    wgr_d = din("w_gr", [D, 36])
    bgr_d = din("b_gr", [1, 36])
    wg_d = din("w_gate", [32 * 128, 16 * 512])
    wu_d = din("w_up", [32 * 128, 16 * 512])
    wd_d = din("w_down", [32 * 128, 4 * 2048])
    cst_d = din("consts", [128, 1024])
    invf_d = din("invf", [64, 1])
    metai_d = din("meta_init", [NSLOT, 32], BF16)
    out_d = nc.dram_tensor("out", [S_LEN + 128, D], F32, kind="ExternalOutput").ap()
    xbuf_d = nc.dram_tensor("xbuf", [NSLOT, D + 32], BF16).ap()
    meta_d = nc.dram_tensor("metabuf", [NSLOT, 16], F32).ap()
    dbg = {}

    def dout(name, shape, dt=F32):
        dbg[name] = nc.dram_tensor("dbg_" + name, list(shape), dt, kind="ExternalOutput").ap()
        return dbg[name]

    S = Sync(nc)
    import contextlib
    es = contextlib.ExitStack()
    arena_t = es.enter_context(nc.sbuf_tensor("arena", [128, 103 * 1024], BF16))
    A = Arena(arena_t[:])
    banks = [es.enter_context(nc.psum_tensor("pb%d" % i, [128, 512], F32)) for i in range(8)]
    PB = [b[:] for b in banks]
    PBH = [b[:].bitcast(BF16) for b in banks]
    PK = ["pb%d" % i for i in range(8)]

    def MM(out, lhsT, rhs, start, stop, r, w):
        return S.op("pe", lambda e: e.matmul(out, lhsT=lhsT, rhs=rhs, start=start, stop=stop,
                                             skip_group_check=True), r, w)

    def TR(out, in_, ident, r, w):
        return S.op("pe", lambda e: e.transpose(out=out, in_=in_, identity=ident), r, w)

    def ACT(out, in_, func, r, w, scale=1.0, bias=0.0, accum=None):
        if accum is None:
            return S.op("act", lambda e: e.activation(out=out, in_=in_, func=func, bias=bias, scale=scale), r, w)
        return S.op("act", lambda e: e.activation(out=out, in_=in_, func=func, bias=bias, scale=scale,
                                                  accum_out=accum), r, w)

    def TS(eng, out, in0, s1, s2, op0, op1, r, w):
        if s2 is None:
            return S.op(eng, lambda e: e.tensor_scalar(out, in0, s1, None, op0), r, w)
        return S.op(eng, lambda e: e.tensor_scalar(out, in0, s1, s2, op0, op1), r, w)

    def TT(eng, out, in0, in1, op, r, w):
        return S.op(eng, lambda e: e.tensor_tensor(out, in0, in1, op), r, w)

    def STT(eng, out, in0, sc, in1, op0, op1, r, w, accum=None):
        if accum is None:
            return S.op(eng, lambda e: e.scalar_tensor_tensor(out, in0, sc, in1, op0, op1), r, w)
        return S.op(eng, lambda e: e.scalar_tensor_tensor(out, in0, sc, in1, op0, op1, accum_out=accum), r, w)

    def CP(eng, out, in_, r, w):
        return S.op(eng, lambda e: e.tensor_copy(out, in_), r, w)

    def RED(eng, out, in_, op, r, w):
        return S.op(eng, lambda e: e.tensor_reduce(out, in_, AX.X, op), r, w)

    def RCP(out, in_, r, w):
        return S.op("dve", lambda e: e.reciprocal(out, in_), r, w)

    def DMA(q, out, in_, r, w):
        return S.dma(q, lambda e: e.dma_start(out=out, in_=in_), r, w)

    _regs = {}

    def breg(e, val):
        if val not in _regs:
            _regs[val] = e.to_reg(val)
        return _regs[val]

    def rstd_col(out, ssq, n, r, w, tmpk):
        ACT(out, ssq, AF.Ln, r, [tmpk], scale=1.0 / n, bias=epsc)
        ACT(out, out, AF.Exp, [tmpk], w, scale=-0.5)

    cst = A.alloc(1024, F32)
    identf = cst[:, 0:128]
    triu = cst[:, 128:256]
    M1 = cst[:, 256:384]
    M2 = cst[:, 384:512]
    tril_strict = cst[:, 512:640]
    iota_p = cst[:, 640:641]
    thr16 = cst[:, 656:672]
    thr64 = cst[:, 672:736]
    eidx = cst[:, 736:768]
    DMA("sp", cst, cst_d, [], ["cst"])
    cb = A.alloc(512, BF16)
    identb = cb[:, 0:128]
    onesb = cb[:, 128:256]
    triub = cb[:, 256:384]
    trilsb = cb[:, 384:512]
    CP("dve", identb, identf, ["cst"], ["cb"])
    CP("dve", triub, triu, ["cst"], ["cb"])
    CP("dve", trilsb, tril_strict, ["cst"], ["cb"])
    S.op("pool", lambda e: e.memset(onesb, 1.0), [], ["cb"])
    small = A.alloc(256, F32)
    epsc = small[:, 0:1]
    onec = small[:, 1:2]
    S.op("pool", lambda e: e.memset(epsc, EPS), [], ["epsc"])
    S.op("pool", lambda e: e.memset(onec, 1.0), [], ["epsc"])
    _sc = [2]

    def col(n=1):
        a = _sc[0]
        _sc[0] += n
        assert _sc[0] <= 256
        return small[:, a:a + n]

    modc = A.alloc(96, F32)
    A1c = A.alloc(16, F32)
    modrow_d = nc.dram_tensor("modrow_d", [1, 6 * D], F32).ap()

    mark = A.off
    ccol = A.alloc(16, F32)
    cact = A.alloc(16, BF16)
    tmp16 = A.alloc(16, F32)
    modrow = A.alloc(6 * D, F32)
    DMA("sp", ccol, ccol_d, [], ["ccol"])
    DMA("sp", modrow[0:1, :], bada_d, [], ["modrow_b"])
    ACT(tmp16, ccol, AF.Exp, ["ccol"], ["tmp16"], scale=-1.0)
    TS("dve", tmp16, tmp16, 1.0, None, ALU.add, None, ["tmp16"], ["tmp16"])
    RCP(tmp16, tmp16, ["tmp16"], ["tmp16"])
    TT("dve", cact, ccol, tmp16, ALU.mult, ["tmp16", "ccol"], ["cact"])
    wa = [A.alloc(16 * 512, BF16).rearrange("p (k n) -> p k n", k=16) for _ in range(2)]
    biasrow = modrow
    for jg in range(8):
        wt = wa[jg % 2]
        wk = "wa%d" % (jg % 2)
        S.dma("pool", (lambda wt=wt, jg=jg: (lambda e: e.dma_start(
            out=wt, in_=wada_d[:, jg * 512:(jg + 1) * 512].rearrange("(k p) n -> p k n", p=128))))(),
            [], [wk])
        pbi = jg % 2
        for k in range(16):
            MM(PB[pbi][0:1, :], cact[:, k:k + 1], wt[:, k, :], k == 0, k == 15, [wk, "cact"], [PK[pbi]])
        TT("dve", modrow[0:1, jg * 512:(jg + 1) * 512], PB[pbi][0:1, :], modrow[0:1, jg * 512:(jg + 1) * 512],
           ALU.add, ["modrow_b"], [PK[pbi], "modrow%d" % jg])
    allrow = ["modrow%d" % j for j in range(8)]
    if debug == "A":
        d = dout("mod", [1, 6 * D])
        DMA("sp", d, modrow[0:1, :], allrow, ["dbg"])
    for j in range(32):
        MM(PB[2][:, j:j + 1], modrow[0:1, j * 128:(j + 1) * 128], onec[0:1, 0:1], True, True, allrow + ["epsc"], [PK[2]])
    CP("dve", modc[:, 0:32], PB[2][:, 0:32], [], [PK[2], "modc"])
    cactp = col(16)
    cactb = cactp.bitcast(BF16)[:, 0:16]
    CP("dve", cactb, cact, ["cact"], ["cactb"])
    n1gc = A.alloc(16, F32)
    DMA("sp", n1gc, n1g_d, [], ["n1gc"])
    STT("dve", A1c, modc[:, 16:32], 1.0, n1gc, ALU.add, ALU.mult, ["modc", "n1gc"], ["A1c"])
    B1c = modc[:, 0:16]
    S.barrier()
    A.off = mark

    if debug == "A":
        d2 = dout("modc", [128, 96])
        DMA("sp", d2, modc, ["modc"], ["dbg"])
        S.emit()
        S.close()
        es.close()
        return nc, dbg

    markMix = A.off
    mixT = A.alloc(16 * S_LEN, BF16).rearrange("p (k t) -> p k t", k=16)
    markHT = A.off
    hT = A.alloc(16 * S_LEN, BF16).rearrange("p (k t) -> p k t", k=16)
    markB = A.off
    xt = [A.alloc(D, F32) for _ in range(2)]
    xn = [A.alloc(D, BF16) for _ in range(2)]
    tmod = [A.alloc(1024, F32) for _ in range(2)]
    ssq1 = col(16)
    rs1 = col(16)
    for i in range(NT):
        sl = i % 2
        DMA("sp", xt[sl], x_d[i * 128:(i + 1) * 128, :], [], ["xt%d" % sl])
        ACT(xn[sl], xt[sl], AF.Square, ["xt%d" % sl], ["xn%d" % sl, "ssq1_%d" % i], accum=ssq1[:, i:i + 1])
        rstd_col(rs1[:, i:i + 1], ssq1[:, i:i + 1], D, ["ssq1_%d" % i, "epsc"], ["rs1_%d" % i], "rs1t_%d" % i)
        ACT(xn[sl], xt[sl], AF.Identity, ["xt%d" % sl, "rs1_%d" % i], ["xn%d" % sl], scale=rs1[:, i:i + 1])
        for hf in range(2):
            for kk in range(8):
                k = hf * 8 + kk
                TR(PBH[hf][:, kk * 128:(kk + 1) * 128], xn[sl][:, k * 128:(k + 1) * 128], identb,
                   ["xn%d" % sl, "cb"], [PK[hf]])
            src = PBH[hf].rearrange("p (k t) -> p k t", k=8)
            tm = tmod[hf].rearrange("p (k t) -> p k t", k=8)
            a1 = A1c[:, hf * 8:(hf + 1) * 8].unsqueeze(2).to_broadcast([128, 8, 128])
            b1 = B1c[:, hf * 8:(hf + 1) * 8].unsqueeze(2).to_broadcast([128, 8, 128])
            TT("dve", tm, src, a1, ALU.mult, ["A1c"], [PK[hf], "tmod%d" % hf])
            TT("pool", hT[:, hf * 8:(hf + 1) * 8, i * 128:(i + 1) * 128], tm, b1, ALU.add,
               ["tmod%d" % hf, "modc"], ["hT%d" % i])
    hTall = ["hT%d" % i for i in range(NT)]
    S.barrier()
    A.off = markB
    if debug == "B":
        d = dout("hT", [128, 16 * S_LEN], BF16)
        DMA("sp", d, hT.rearrange("p k t -> p (k t)"), hTall, ["dbg"])
        S.emit(); S.close(); es.close()
        return nc, dbg

    markC = A.off
    wb = [A.alloc(16 * 512, BF16).rearrange("p (k n) -> p k n", k=16) for _ in range(2)]
    markC1 = A.off
    wsec = [wb[0], wb[1],
            mixT[:, 8:12, :].rearrange("p a t -> p (a t)").rearrange("p (k n) -> p k n", k=16),
            mixT[:, 12:16, :].rearrange("p a t -> p (a t)").rearrange("p (k n) -> p k n", k=16)]
    lbb = A.alloc(1024, F32)
    omlb = A.alloc(1024, F32)
    honb = A.alloc(128, F32)
    DMA("sp", lbb, lbl_d[0:1, :].partition_broadcast(128), [], ["lbb"])
    DMA("sp", omlb, lbl_d[1:2, :].partition_broadcast(128), [], ["omlb"])
    DMA("sp", honb, hon_d.partition_broadcast(128), [], ["honb"])
    TT("dve", omlb, omlb, lbb, ALU.subtract, ["lbb"], ["omlb"])
    ACT(omlb, omlb, AF.Exp, [], ["omlb"])
    TS("dve", omlb, omlb, 1.0, None, ALU.add, None, [], ["omlb"])
    RCP(lbb, omlb, ["omlb"], ["lbb"])
    TS("dve", omlb, lbb, -1.0, 1.0, ALU.mult, ALU.add, ["lbb"], ["omlb"])
    W4 = 512
    en_t, f_t, lf_t, kk_t = A.alloc(W4), A.alloc(W4), A.alloc(W4), A.alloc(W4)
    E1_t, E2_t = A.alloc(W4), A.alloc(W4)
    qin_t, qout_t, kin_t, kout_t, v_t = (A.alloc(W4, BF16) for _ in range(5))
    eng_t, sil_t = A.alloc(W4), A.alloc(W4)
    E3_t, E1n_t = eng_t, f_t
    trq_t, trk_t, tro_t = A.alloc(W4, BF16), A.alloc(W4, BF16), A.alloc(W4, BF16)
    am_t = A.alloc(W4, BF16)
    on_t = en_t
    og_t = A.alloc(W4, BF16)
    Sst = A.alloc(W4)
    Sbf = A.alloc(W4, BF16)
    deccol = col(4)
    ssqo = col(4)
    rso = col(4)
    QSC = 128.0 ** -0.5
    v4 = lambda t: t.rearrange("p (h d) -> p h d", h=4)
    triu4 = triu.unsqueeze(1).to_broadcast([128, 4, 128])
    for hgp in range(2):
        for sec in range(4):
            c0 = sec * 1024 + hgp * 512
            S.dma("pool", (lambda sec=sec, c0=c0: (lambda e: e.dma_start(
                out=wsec[sec], in_=win_d[:, c0:c0 + 512].rearrange("(k p) n -> p k n", p=128))))(), [], ["wsec%d" % sec])
        hs = slice(hgp * 512, (hgp + 1) * 512)
        for i in range(NT):
            tsl = slice(i * 128, (i + 1) * 128)

            def emit_proj(ii):
                for sec in range(4):
                    for k in range(16):
                        MM(PB[sec], hT[:, k, ii * 128:(ii + 1) * 128], wsec[sec][:, k, :], k == 0, k == 15,
                           ["wsec%d" % sec, "hT%d" % ii], [PK[sec]])
            if i == 0:
                emit_proj(0)
            hq, hf_, hi_, hg = PB[0], PB[1], PB[2], PB[3]
            ACT(en_t, hf_, AF.Exp, [], [PK[1], "en"], scale=-1.0)
            ACT(v_t, hi_, AF.Copy, [], [PK[2], "v"])
            ACT(eng_t, hg, AF.Exp, [], [PK[3], "eng"], scale=-1.0)
            ACT(en_t, en_t, AF.Ln, [], ["en"], bias=onec)
            ACT(en_t, en_t, AF.Exp, [], ["en"], scale=-1.0)
            TT("dve", f_t, en_t, omlb[:, hs], ALU.mult, ["en", "omlb"], ["f"])
            TT("dve", f_t, f_t, lbb[:, hs], ALU.add, ["lbb"], ["f"])
            ACT(lf_t, f_t, AF.Ln, ["f"], ["lf"])
            ACT(kk_t, f_t, AF.Identity, ["f"], ["kk"], scale=-1.0, bias=onec)
            ACT(eng_t, eng_t, AF.Ln, [], ["eng"], bias=onec)
            ACT(eng_t, eng_t, AF.Exp, [], ["eng"], scale=-1.0)
            TT("dve", sil_t, hg, eng_t, ALU.mult, ["eng"], [PK[3], "sil"])
            TT("pool", v4(sil_t), v4(sil_t), honb.unsqueeze(1).to_broadcast([128, 4, 128]), ALU.mult, ["honb"], ["sil"])
            MM(PB[4], M1, lf_t, True, True, ["cst", "lf"], [PK[4]])
            MM(PB[5], M2, lf_t, True, True, ["cst", "lf"], [PK[5]])
            MM(PB[6], triu, lf_t, True, True, ["cst", "lf"], [PK[6]])
            for hh in range(4):
                MM(PB[7][:, hh:hh + 1], lf_t[:, hh * 128:(hh + 1) * 128], onec, True, True, ["epsc", "lf"], [PK[7]])
            ACT(E1_t, PB[4], AF.Exp, [], [PK[4], "E1"])
            ACT(E1n_t, PB[4], AF.Exp, [], [PK[4], "f"], scale=-1.0)
            ACT(E2_t, PB[5], AF.Exp, [], [PK[5], "E2"])
            ACT(E3_t, PB[6], AF.Exp, [], [PK[6], "eng"])
            ACT(deccol, PB[7][:, 0:4], AF.Exp, [], [PK[7], "dec"])
            STT("dve", qin_t, hq, QSC, E1_t, ALU.mult, ALU.mult, ["E1"], [PK[0], "qin"])
            STT("dve", qout_t, hq, QSC, E3_t, ALU.mult, ALU.mult, ["eng"], [PK[0], "qout"])
            TT("pool", kin_t, kk_t, E1n_t, ALU.mult, ["kk", "f"], ["kin"])
            TT("pool", kout_t, kk_t, E2_t, ALU.mult, ["kk", "E2"], ["kout"])
            for hh in range(4):
                hsl = slice(hh * 128, (hh + 1) * 128)
                TR(PBH[4][:, hsl], qin_t[:, hsl], identb, ["qin", "cb"], [PK[4]])
                TR(PBH[5][:, hsl], kin_t[:, hsl], identb, ["kin", "cb"], [PK[5]])
                TR(PBH[6][:, hsl], qout_t[:, hsl], identb, ["qout", "cb"], [PK[6]])
            CP("dve", trq_t, PBH[4][:, 0:512], [], [PK[4], "trq"])
            ACT(trk_t, PBH[5][:, 0:512], AF.Copy, [], [PK[5], "trk"])
            CP("dve", tro_t, PBH[6][:, 0:512], [], [PK[6], "tro"])
            for hh in range(4):
                hsl = slice(hh * 128, (hh + 1) * 128)
                MM(PB[4][:, hsl], trk_t[:, hsl], trq_t[:, hsl], True, True, ["trk", "trq"], [PK[4]])
            if i + 1 < NT:
                emit_proj(i + 1)
            TT("dve", v4(am_t), v4(PB[4]), triu4, ALU.mult, ["cst"], [PK[4], "am"])
            for hh in range(4):
                hsl = slice(hh * 128, (hh + 1) * 128)
                if i == 0:
                    MM(PB[5][:, hsl], am_t[:, hsl], v_t[:, hsl], True, True, ["am", "v"], [PK[5]])
                else:
                    MM(PB[5][:, hsl], am_t[:, hsl], v_t[:, hsl], True, False, ["am", "v"], [PK[5]])
                    MM(PB[5][:, hsl], tro_t[:, hsl], Sbf[:, hsl], False, True, ["tro", "Sbf"], [PK[5]])
            for hh in range(4):
                hsl = slice(hh * 128, (hh + 1) * 128)
                MM(PB[6][:, hsl], kout_t[:, hsl], v_t[:, hsl], True, True, ["kout", "v"], [PK[6]])
            if i == 0:
                CP("dve", Sst, PB[6], [], [PK[6], "S"])
            else:
                TT("pool", v4(Sst), v4(Sst), deccol.unsqueeze(2).to_broadcast([128, 4, 128]), ALU.mult, ["dec"], ["S"])
                TT("dve", Sst, Sst, PB[6], ALU.add, [], [PK[6], "S"])
            if i < NT - 1:
                ACT(Sbf, Sst, AF.Copy, ["S"], ["Sbf"])
            ACT(on_t, PB[5], AF.Square, [], [PK[5], "en"])
            RED("dve", ssqo, v4(on_t), ALU.add, ["en"], ["ssqo"])
            ACT(rso, ssqo, AF.Ln, ["ssqo", "epsc"], ["rso"], scale=1.0 / 128, bias=epsc)
            ACT(rso, rso, AF.Exp, [], ["rso"], scale=-0.5)
            TT("dve", v4(on_t), v4(PB[5]), rso.unsqueeze(2).to_broadcast([128, 4, 128]), ALU.mult, ["rso"], [PK[5], "en"])
            TT("dve", og_t, on_t, sil_t, ALU.mult, ["en", "sil"], ["og"])
            for hh in range(4):
                hsl = slice(hh * 128, (hh + 1) * 128)
                TR(PBH[7][:, hsl], og_t[:, hsl], identb, ["og", "cb"], [PK[7]])
            ACT(mixT[:, hgp * 4:(hgp + 1) * 4, tsl], PBH[7][:, 0:512].rearrange("p (h t) -> p h t", h=4), AF.Copy, [],
                [PK[7], "mixT%d_%d" % (hgp, i)])
    S.barrier()
    A.off = markC1
    if debug == "C1":
        d = dout("mixT", [128, 16 * S_LEN], BF16)
        DMA("sp", d, mixT.rearrange("p k t -> p (k t)"), [], ["dbg"])
        S.emit(); S.close(); es.close()
        return nc, dbg

    A.off = markC + 16 * 512
    mla_d = nc.dram_tensor("mla_scratch", [128, 9 * S_LEN], BF16).ap()

    def alloc_mla():
        qaT = A.alloc(4 * S_LEN, BF16).rearrange("p (k t) -> p k t", k=4)
        kvaT = A.alloc(2 * S_LEN, BF16).rearrange("p (k t) -> p k t", k=2)
        return qaT, kvaT, A.alloc(S_LEN, BF16), A.alloc(S_LEN, BF16), A.alloc(S_LEN, BF16)
    mla0 = A.off
    qaT, kvaT, kpeT, kpesT, SQR = alloc_mla()
    mla_all = A.ap[:, mla0:mla0 + 9 * S_LEN]
    qagc = A.alloc(4, F32)
    kvgc = A.alloc(2, F32)
    qng = A.alloc(4, F32)
    kng = A.alloc(4, F32)
    DMA("sp", qagc, qag_d, [], ["qagc"])
    DMA("sp", kvgc, kvg_d, [], ["kvgc"])
    DMA("sp", qng, qng_d, [], ["qng"])
    DMA("sp", kng, kng_d, [], ["kng"])
    TT("dve", qng[:, 2:3], qng[:, 2:3], qng[:, 3:4], ALU.mult, [], ["qng"])
    TT("dve", kng[:, 2:3], kng[:, 2:3], kng[:, 3:4], ALU.mult, [], ["kng"])
    sq_t = [A.alloc(512, BF16) for _ in range(2)]
    rsb_t = [A.alloc(512, F32) for _ in range(2)]
    S.op("pool", lambda e: e.memset(SQR, 0.0), [], ["SQR"])
    wA, wB = wb[0], wb[0]
    S.dma("pool", lambda e: e.dma_start(out=wA, in_=win_d[:, 4096:4608].rearrange("(k p) n -> p k n", p=128)), [], ["wb0"])

    def lowrank(wt, wk, nch, dstT, gcol, gk, nfeat, dk):
        for tg in range(4):
            tsl = slice(tg * 512, (tg + 1) * 512)
            for c in range(nch):
                pbi = c % 2
                for k in range(16):
                    MM(PB[pbi], wt[:, k, c * 128:(c + 1) * 128], hT[:, k, tsl], k == 0, k == 15,
                       [wk] + hTall, [PK[pbi]])
                ACT(sq_t[pbi], PB[pbi], AF.Square, [], [PK[pbi], "sq%d" % pbi])
                TS("dve", dstT[:, c, tsl], PB[pbi], gcol[:, c:c + 1], None, ALU.mult, None, [gk],
                   [PK[pbi], dk + "%d_%d" % (c, tg)])
                MM(PB[2], onesb, sq_t[pbi], c == 0, c == nch - 1, ["cb", "sq%d" % pbi], [PK[2]])
            rb = rsb_t[tg % 2]
            rk = "rsb%d" % (tg % 2)
            ACT(rb, PB[2], AF.Ln, ["epsc"], [PK[2], rk], scale=1.0 / nfeat, bias=epsc)
            ACT(rb, rb, AF.Exp, [], [rk], scale=-0.5)
            for c in range(nch):
                TT("pool" if c % 2 else "dve", dstT[:, c, tsl], dstT[:, c, tsl], rb, ALU.mult, [rk],
                   [dk + "%d_%d" % (c, tg)])
    lowrank(wA, "wb0", 4, qaT, qagc, "qagc", 512, "qaT")
    S.op("pool", lambda e: e.memset(wB[:, :, 256:512], 0.0), [], ["wb0"])
    S.dma("pool", lambda e: e.dma_start(out=wB[:, :, 0:256], in_=win_d[:, 4608:4864].rearrange("(k p) n -> p k n", p=128)), [], ["wb0"])
    S.dma("pool", lambda e: e.dma_start(out=wB[:, :, 256:320], in_=win_d[:, 4864:4928].rearrange("(k p) n -> p k n", p=128)), [], ["wb0"])
    S.dma("pool", lambda e: e.dma_start(out=wB[:, :, 384:416], in_=win_d[:, 4896:4928].rearrange("(k p) n -> p k n", p=128)), [], ["wb0"])
    S.dma("pool", lambda e: e.dma_start(out=wB[:, :, 416:448], in_=win_d[:, 4864:4896].rearrange("(k p) n -> p k n", p=128)), [], ["wb0"])
    lowrank(wB, "wb0", 2, kvaT, kvgc, "kvgc", 256, "kvaT")
    for tg in range(4):
        tsl = slice(tg * 512, (tg + 1) * 512)
        for k in range(16):
            MM(PB[3], wB[:, k, 256:384], hT[:, k, tsl], k == 0, k == 15, ["wb0"] + hTall, [PK[3]])
        for k in range(16):
            MM(PB[4], wB[:, k, 384:512], hT[:, k, tsl], k == 0, k == 15, ["wb0"] + hTall, [PK[4]])
        ACT(SQR[0:64, tsl], PB[3][0:64, :], AF.Square, [], [PK[3], "SQR"])
        TS("dve", kpeT[0:64, tsl], PB[3][0:64, :], kng[0:64, 1:2], None, ALU.mult, None, ["kng"], [PK[3], "kpeT"])
        TS("dve", kpesT[0:64, tsl], PB[4][0:64, :], kng[0:64, 2:3], None, ALU.mult, None, ["kng"], [PK[4], "kpesT"])
    qaT_keys = ["qaT%d_%d" % (c, tg) for c in range(4) for tg in range(4)]
    kvaT_keys = ["kvaT%d_%d" % (c, tg) for c in range(2) for tg in range(4)]
    S.barrier()
    if debug == "C2":
        d = dout("qaT", [128, 4 * S_LEN], BF16)
        DMA("sp", d, qaT.rearrange("p k t -> p (k t)"), [], ["dbg"])
        d = dout("kvaT", [128, 2 * S_LEN], BF16)
        DMA("sp", d, kvaT.rearrange("p k t -> p (k t)"), [], ["dbg"])
        d = dout("kpeT", [64, S_LEN], BF16)
        DMA("sp", d, kpeT[0:64, :], [], ["dbg"])
        d = dout("kpesT", [64, S_LEN], BF16)
        DMA("sp", d, kpesT[0:64, :], [], ["dbg"])
        S.emit(); S.close(); es.close()
        return nc, dbg
    qngp = col(4)
    kngp = col(4)
    CP("dve", qngp, qng, ["qng"], ["qngp"])
    CP("dve", kngp, kng, ["kng"], ["kngp"])
    S.barrier()
    topD = mla0 + 9 * S_LEN
    A.off = markHT
    A.cap = mla0
    if debug == "D00":
        d = dout("qaT", [128, 4 * S_LEN], BF16)
        DMA("sp", d, qaT.rearrange("p k t -> p (k t)"), [], ["dbg"])
        d = dout("kvaT", [128, 2 * S_LEN], BF16)
        DMA("sp", d, kvaT.rearrange("p k t -> p (k t)"), [], ["dbg"])
        d = dout("kpeT", [64, S_LEN], BF16)
        DMA("sp", d, kpeT[0:64, :], [], ["dbg"])
        d = dout("kpesT", [64, S_LEN], BF16)
        DMA("sp", d, kpesT[0:64, :], [], ["dbg"])
        S.emit(); S.close(); es.close()
        return nc, dbg
    PI = float(np.pi)
    cosT = A.alloc(S_LEN, F32)
    sinT = A.alloc(S_LEN, F32)
    RT = A.alloc(S_LEN, BF16)
    aonb = A.alloc(128, F32)
    import os
    SK = os.environ.get("SKIPD", "")
    if "a" not in SK:
        DMA("sp", aonb, aon_d.partition_broadcast(128), [], ["aonb"])
    markD0 = A.off
    posi = A.alloc(S_LEN, I32)
    ang = A.alloc(S_LEN, F32)
    kq = A.alloc(S_LEN, F32)
    kqi = A.alloc(S_LEN, I32)
    msk = A.alloc(S_LEN, F32)
    invf = col(1)
    if "i" not in SK:
        DMA("sp", invf[0:64, :], invf_d, [], ["invf"])
    if "p" not in SK:
        DMA("sp", posi[0:64, :], pos_d.partition_broadcast(64), [], ["posi"])
    def dump_kva(tag):
        if debug == tag:
            S.barrier()
            d = dout("kvaT2", [128, 2 * S_LEN], BF16); DMA("sp", d, kvaT.rearrange("p k t -> p (k t)"), [], ["dbg"])
            S.emit(); S.close(); es.close()
            return True
        return False
    if dump_kva("X1"):
        return nc, dbg
    for (dst, shift, key) in ((sinT, 0.0, "sinT"), (cosT, PI / 2, "cosT")):
        a_, q_, qi_, m_ = ang[0:64, :], kq[0:64, :], kqi[0:64, :], msk[0:64, :]
        CP("dve", a_, posi[0:64, :], ["posi"], ["ang"])
        TS("dve", a_, a_, invf[0:64, :], shift, ALU.mult, ALU.add, ["invf"], ["ang"])
        TS("dve", q_, a_, 1.0 / (2 * PI), None, ALU.mult, None, ["ang"], ["kq"])
        CP("dve", qi_, q_, ["kq"], ["kqi"])
        CP("dve", q_, qi_, ["kqi"], ["kq"])
        if key == "sinT" and dump_kva("X2"):
            return nc, dbg
        STT("dve", a_, q_, -2 * PI, a_, ALU.mult, ALU.add, ["kq"], ["ang"])
        TS("dve", m_, a_, PI, None, ALU.is_gt, None, ["ang"], ["msk"])
        STT("dve", a_, m_, -2 * PI, a_, ALU.mult, ALU.add, ["msk"], ["ang"])
        TS("dve", m_, a_, -PI, None, ALU.is_lt, None, ["ang"], ["msk"])
        STT("dve", a_, m_, 2 * PI, a_, ALU.mult, ALU.add, ["msk"], ["ang"])
        if key == "sinT" and dump_kva("X3"):
            return nc, dbg
        ACT(dst[0:64, :], a_, AF.Sin, ["ang"], [key])
        if key == "sinT" and dump_kva("X4"):
            return nc, dbg
    TT("dve", ang[0:64, :], kpeT[0:64, :], cosT[0:64, :], ALU.mult, ["kpeT", "cosT"], ["ang"])
    TT("dve", kq[0:64, :], kpesT[0:64, :], sinT[0:64, :], ALU.mult, ["kpesT", "sinT"], ["kq"])
    TT("dve", RT[0:64, :], ang[0:64, :], kq[0:64, :], ALU.add, ["kq", "ang"], ["RT"])
    S.barrier()
    A.off = markD0
    if debug == "D0":
        d = dout("cosT", [64, S_LEN]); DMA("sp", d, cosT[0:64, :], [], ["dbg"])
        d = dout("sinT", [64, S_LEN]); DMA("sp", d, sinT[0:64, :], [], ["dbg"])
        d = dout("RT", [64, S_LEN], BF16); DMA("sp", d, RT[0:64, :], [], ["dbg"])
        d = dout("kvaT2", [128, 2 * S_LEN], BF16); DMA("sp", d, kvaT.rearrange("p k t -> p (k t)"), [], ["dbg"])
        print("offsets", markHT, mla0, markD0, A.off)
        S.emit(); S.close(); es.close()
        return nc, dbg

    wq_t = [A.alloc(4 * 384, BF16).rearrange("p (k n) -> p k n", k=4) for _ in range(2)]
    wkv_t = [A.alloc(2 * 256, BF16).rearrange("p (k n) -> p k n", k=2) for _ in range(2)]
    QTn_t = [A.alloc(S_LEN, BF16) for _ in range(2)]
    QTr_t = [A.alloc(S_LEN, BF16) for _ in range(2)]
    KTn_t = [A.alloc(S_LEN, BF16) for _ in range(2)]
    KTr_t = [A.alloc(S_LEN, BF16) for _ in range(2)]
    V_t = [A.alloc(16 * 130, BF16).rearrange("p (t v) -> p t v", t=16) for _ in range(2)]
    for sl in range(2):
        S.op("pool", (lambda sl=sl: (lambda e: e.memset(V_t[sl][:, :, 128:130], 1.0)))(), [], ["V%d" % sl])
        S.op("pool", (lambda sl=sl: (lambda e: e.memset(wq_t[sl], 0.0)))(), [], ["wq%d" % sl])
        S.op("pool", (lambda sl=sl: (lambda e: e.memset(QTr_t[sl], 0.0)))(), [], ["QTr%d" % sl])
        S.op("pool", (lambda sl=sl: (lambda e: e.memset(KTr_t[sl], 0.0)))(), [], ["KTr%d" % sl])
    lowtop = A.off
    A.off = topD
    A.cap = A.ap.shape[1]
    sqn_t = [A.alloc(512, BF16) for _ in range(2)]
    sqr_t = [A.alloc(512, BF16) for _ in range(2)]
    rb_t = [A.alloc(512, F32) for _ in range(2)]
    t1_t = [A.alloc(512, F32) for _ in range(2)]
    t2_t = [A.alloc(512, F32) for _ in range(2)]
    mrowt = A.alloc(256, F32)
    ob_t = [A.alloc(128, BF16) for _ in range(4)]
    A.off = lowtop
    A.cap = mla0
    PT_t = [A.alloc(512, BF16) for _ in range(2)]
    wad = A.alloc(16 * 256, BF16).rearrange("p (k n) -> p k n", k=16)
    ada_next = [0]

    ada_pending = [None]

    def ada_group():
        if ada_pending[0] is not None:
            c0 = ada_pending[0]
            for k in range(16):
                MM(PB[7][0:1, 0:256], cactb[:, k:k + 1], wad[:, k, :], k == 0, k == 15, ["wad", "cactb"], [PK[7]])
            TT("dve", mrowt[0:1, :], PB[7][0:1, 0:256], mrowt[0:1, :], ALU.add, [], [PK[7], "mrowt"])
            DMA("sp", modrow_d[0:1, c0:c0 + 256], mrowt[0:1, :], ["mrowt"], ["modrow_d"])
            ada_pending[0] = None
        g = ada_next[0]
        if g >= 32:
            return
        ada_next[0] += 1
        c0 = 4096 + g * 256
        S.dma("pool", (lambda c0=c0: (lambda e: e.dma_start(
            out=wad, in_=wada_d[:, c0:c0 + 256].rearrange("(k p) n -> p k n", p=128))))(), [], ["wad"])
        DMA("sp", mrowt[0:1, :], bada_d[0:1, c0:c0 + 256], [], ["mrowt"])
        ada_pending[0] = c0
    junk3 = A.alloc(128, F32)
    ocol = col(16)
    SM_SCALE = 192.0 ** -0.5
    nev = 0
    npt = 0
    for h in range(8):
        sl = h % 2
        s_ = str(sl)
        wq, wkv = wq_t[sl], wkv_t[sl]
        QTn, QTr, KTn, KTr, V = QTn_t[sl], QTr_t[sl], KTn_t[sl], KTr_t[sl], V_t[sl]
        b0 = h * 192
        for (a, bnd, c0, n) in ((0, 128, b0, 128), (128, 192, b0 + 128, 64), (256, 288, b0 + 160, 32), (288, 320, b0 + 128, 32)):
            S.dma("pool", (lambda wq=wq, a=a, bnd=bnd, c0=c0, n=n: (lambda e: e.dma_start(
                out=wq[:, :, a:bnd], in_=wqu_d[:, c0:c0 + n].rearrange("(k p) n -> p k n", p=128))))(), [], ["wq" + s_])
        S.dma("pool", (lambda wkv=wkv, h=h: (lambda e: e.dma_start(
            out=wkv, in_=wkv_d[:, h * 256:(h + 1) * 256].rearrange("(k p) n -> p k n", p=128))))(), [], ["wkv" + s_])
        def proj_body(tg, sl=sl, s_=s_, wq=wq, wkv=wkv, QTn=QTn, QTr=QTr, KTn=KTn, KTr=KTr, V=V):
            tsl = slice(tg * 512, (tg + 1) * 512)
            g2 = tg % 2
            gs = str(g2)
            bA, bB, bC, bD = (0, 1, 2, 3) if g2 == 0 else (4, 5, 6, 7)
            for k in range(4):
                MM(PB[bA], wq[:, k, 0:128], qaT[:, k, tsl], k == 0, k == 3, ["wq" + s_] + qaT_keys, [PK[bA]])
            for k in range(4):
                MM(PB[bB], wq[:, k, 128:256], qaT[:, k, tsl], k == 0, k == 3, ["wq" + s_] + qaT_keys, [PK[bB]])
            for k in range(4):
                MM(PB[bC], wq[:, k, 256:384], qaT[:, k, tsl], k == 0, k == 3, ["wq" + s_] + qaT_keys, [PK[bC]])
            yield
            ACT(sqn_t[g2], PB[bA], AF.Square, [], [PK[bA], "sqn" + gs])
            ACT(sqr_t[g2], PB[bB], AF.Square, [], [PK[bB], "sqr" + gs])
            yield
            MM(PB[bD], onesb, sqn_t[g2], True, False, ["cb", "sqn" + gs], [PK[bD]])
            MM(PB[bD], onesb, sqr_t[g2], False, True, ["cb", "sqr" + gs], [PK[bD]])
            yield
            rb = rb_t[g2]
            ACT(rb, PB[bD], AF.Ln, ["epsc"], [PK[bD], "rb" + gs], scale=1.0 / 192, bias=epsc)
            ACT(rb, rb, AF.Exp, [], ["rb" + gs], scale=-0.5)
            yield
            STT("dve", QTn[:, tsl], PB[bA], qngp[:, 0:1], rb, ALU.mult, ALU.mult, ["qngp", "rb" + gs], [PK[bA], "QTn" + s_])
            STT("dve", t1_t[g2][0:64, :], PB[bB][0:64, :], qngp[0:64, 1:2], cosT[0:64, tsl], ALU.mult, ALU.mult,
                ["qngp", "cosT"], [PK[bB], "t1" + gs])
            STT("dve", t2_t[g2][0:64, :], PB[bC][0:64, :], qngp[0:64, 2:3], sinT[0:64, tsl], ALU.mult, ALU.mult,
                ["qngp", "sinT"], [PK[bC], "t2" + gs])
            yield
            TT("pool", t1_t[g2][0:64, :], t1_t[g2][0:64, :], t2_t[g2][0:64, :], ALU.add, ["t2" + gs], ["t1" + gs])
            TT("pool", QTr[0:64, tsl], t1_t[g2][0:64, :], rb[0:64, :], ALU.mult, ["t1" + gs, "rb" + gs], ["QTr" + s_])
            for k in range(2):
                MM(PB[bA], wkv[:, k, 0:128], kvaT[:, k, tsl], k == 0, k == 1, ["wkv" + s_] + kvaT_keys, [PK[bA]])
            for j in range(4):
                t = tg * 4 + j
                for k in range(2):
                    MM(PB[bB][:, j * 128:(j + 1) * 128], kvaT[:, k, t * 128:(t + 1) * 128], wkv[:, k, 128:256],
                       k == 0, k == 1, ["wkv" + s_] + kvaT_keys, [PK[bB]])
            yield
            ACT(sqn_t[g2], PB[bA], AF.Square, [], [PK[bA], "sqn" + gs])
            ACT(V[:, tg * 4:(tg + 1) * 4, 0:128], PB[bB].rearrange("p (t v) -> p t v", t=4), AF.Copy, [],
                [PK[bB], "V" + s_])
            yield
            MM(PB[bD], onesb, sqn_t[g2], True, False, ["cb", "sqn" + gs], [PK[bD]])
            MM(PB[bD], onesb, SQR[:, tsl], False, True, ["cb", "SQR"], [PK[bD]])
            yield
            ACT(rb, PB[bD], AF.Ln, ["epsc"], [PK[bD], "rb" + gs], scale=1.0 / 192, bias=epsc)
            ACT(rb, rb, AF.Exp, [], ["rb" + gs], scale=-0.5)
            yield
            STT("dve", KTn[:, tsl], PB[bA], kngp[:, 0:1], rb, ALU.mult, ALU.mult, ["kngp", "rb" + gs], [PK[bA], "KTn" + s_])
            TT("pool", KTr[0:64, tsl], RT[0:64, tsl], rb[0:64, :], ALU.mult, ["RT", "rb" + gs], ["KTr" + s_])
            yield
        pipeline([proj_body(tg) for tg in range(4)], 2)
        steps = [(G, kt) for G in range(4) for kt in range(4 * G + 4)]

        def geom(G, kt):
            j0 = max(0, kt - 4 * G)
            return j0, (4 - j0) * 128, (4 * G + j0) * 128

        def emit_ST(n):
            G, kt = steps[n]
            j0, ncol, q0 = geom(G, kt)
            sb = n % 2
            MM(PB[sb][:, 0:ncol], KTn[:, kt * 128:(kt + 1) * 128], QTn[:, q0:q0 + ncol], True, False,
               ["KTn" + s_, "QTn" + s_], [PK[sb]])
            MM(PB[sb][:, 0:ncol], KTr[:, kt * 128:(kt + 1) * 128], QTr[:, q0:q0 + ncol], False, True,
               ["KTr" + s_, "QTr" + s_], [PK[sb]])
        emit_ST(0)
        pending = []
        for n in range(len(steps)):
            G, kt = steps[n]
            j0, ncol, q0 = geom(G, kt)
            sb = n % 2
            pt = PT_t[npt % 2]
            pk = "PT%d" % (npt % 2)
            if n % 10 == 5:
                ada_group()
            npt += 1
            if n + 1 < len(steps):
                emit_ST(n + 1)
            ACT(pt[:, 0:ncol], PB[sb][:, 0:ncol], AF.Exp, [], [PK[sb], pk], scale=SM_SCALE)
            if kt >= 4 * G:
                TT("pool", pt[:, 0:128], pt[:, 0:128], triub, ALU.mult, ["cb"], [pk])
            for j in range(j0, 4):
                qt = 4 * G + j
                MM(PB[2 + j][:, 0:129], pt[:, (j - j0) * 128:(j - j0 + 1) * 128], V[:, kt, 0:129],
                   kt == 0, kt == qt, [pk, "V" + s_], [PK[2 + j]])
                if kt == qt:
                    e2 = nev % 4
                    nev += 1

                    def mk(e2=e2, j=j, qt=qt, h=h):
                        es_ = str(e2)
                        O = PB[2 + j]
                        ssq = ocol[:, e2 * 4:e2 * 4 + 1]
                        tt1 = ocol[:, e2 * 4 + 1:e2 * 4 + 2]
                        tt2 = ocol[:, e2 * 4 + 2:e2 * 4 + 3]
                        den = ocol[:, e2 * 4 + 3:e2 * 4 + 4]

                        def s1():
                            ACT(junk3, O[:, 0:128], AF.Square, [], [PK[2 + j], "junk3", "ossq" + es_], accum=ssq)
                            CP("dve", den, O[:, 128:129], [], [PK[2 + j], "oden" + es_])
                            STT("dve", tt1, den, EPS, den, ALU.mult, ALU.mult, ["oden" + es_], ["ott1" + es_])
                            STT("dve", tt2, ssq, 1.0 / 128, tt1, ALU.mult, ALU.add, ["ossq" + es_, "ott1" + es_], ["ott2" + es_])

                        def s2():
                            ACT(tt2, tt2, AF.Ln, [], ["ott2" + es_])
                            ACT(tt2, tt2, AF.Exp, [], ["ott2" + es_], scale=-0.5)

                        def s3():
                            STT("dve", ob_t[e2], O[:, 0:128], tt2, aonb, ALU.mult, ALU.mult, ["ott2" + es_, "aonb"],
                                [PK[2 + j], "ob" + es_])
                            TR(PBH[6][:, 0:128], ob_t[e2], identb, ["ob" + es_, "cb"], [PK[6]])

                        def s4():
                            ACT(mixT[:, 8 + h, qt * 128:(qt + 1) * 128], PBH[6][:, 0:128], AF.Copy, [],
                                [PK[6], "mixT%d_%d" % (8 + h, qt)])
                        return [s1, s2, s3, s4]
                    st = mk()
                    for d_, fn in enumerate(st):
                        pending.append([n + d_, fn])
            last_of_group = (kt == 4 * G + 3)
            keep = []
            for item in pending:
                if item[0] <= n or last_of_group and False:
                    item[1]()
                else:
                    keep.append(item)
            pending[:] = keep
            if last_of_group:
                for item in sorted(pending, key=lambda it: it[0]):
                    item[1]()
                pending[:] = []
    ada_group()
    assert ada_next[0] == 32 and ada_pending[0] is None
    S.barrier()
    A.off = markHT
    A.cap = A.ap.shape[1]
    if debug == "D":
        d = dout("mixT", [128, 16 * S_LEN], BF16)
        DMA("sp", d, mixT.rearrange("p k t -> p (k t)"), [], ["dbg"])
        S.emit(); S.close(); es.close()
        return nc, dbg

    markE = A.off
    wo = A.alloc(16 * D, BF16).rearrange("p (k n) -> p k n", k=16)
    g1b = A.alloc(D, F32)
    xt = [A.alloc(D, F32) for _ in range(2)]
    tmpe = [A.alloc(512, F32) for _ in range(2)]
    for n in range(4):
        S.dma("pool", (lambda n=n: (lambda e: e.dma_start(
            out=wo[:, :, n * 512:(n + 1) * 512],
            in_=wout_d[:, n * 512:(n + 1) * 512].rearrange("(k p) n -> p k n", p=128))))(), [], ["wo%d" % n])
    DMA("sp", g1b, modrow_d[0:1, 2 * D:3 * D].partition_broadcast(128), ["modrow_d"], ["g1b"])
    mix_keys = []
    for i in range(NT):
        sl = i % 2
        DMA("sp", xt[sl], x_d[i * 128:(i + 1) * 128, :], [], ["xt%d" % sl])
        for n in range(4):
            pbi = (i * 4 + n) % 4
            for k in range(16):
                MM(PB[pbi], mixT[:, k, i * 128:(i + 1) * 128], wo[:, k, n * 512:(n + 1) * 512], k == 0, k == 15,
                   ["wo%d" % n], [PK[pbi]])
            tp = tmpe[n % 2]
            TT("dve", tp, PB[pbi], g1b[:, n * 512:(n + 1) * 512], ALU.mult, ["g1b"], [PK[pbi], "tmpe%d" % (n % 2)])
            TT("pool", xt[sl][:, n * 512:(n + 1) * 512], xt[sl][:, n * 512:(n + 1) * 512], tp, ALU.add,
               ["tmpe%d" % (n % 2)], ["xt%d" % sl])
        DMA("sp", out_d[i * 128:(i + 1) * 128, :], xt[sl], ["xt%d" % sl], ["out"])
    S.barrier()
    A.off = markMix
    if debug == "E1":
        S.emit(); S.close(); es.close()
        return nc, dbg

    h2_d = nc.dram_tensor("h2_scratch", [S_LEN, D], BF16).ap()
    A2b = A.alloc(D, F32)
    B2b = A.alloc(D, F32)
    g2b = A.alloc(D, F32)
    LG = A.alloc(16 * 36, F32).rearrange("p (t n) -> p t n", t=16)
    IDXW = A.alloc(64, I32)
    zt = A.alloc(D + 32, BF16)
    S.op("pool", lambda e: e.memset(zt, 0.0), [], ["zt"])
    for j in range(NBLK):
        DMA("pool", xbuf_d[j * 128:(j + 1) * 128, :], zt, ["zt"], ["xbz%d" % j])
    xbz_keys = ["xbz%d" % j for j in range(NBLK)]
    markE2 = A.off
    n2gb = A.alloc(D, F32)
    wgr = A.alloc(16 * 36, F32).rearrange("p (k n) -> p k n", k=16)
    bgrb = A.alloc(36, F32)
    DMA("sp", A2b, modrow_d[0:1, 4 * D:5 * D].partition_broadcast(128), ["modrow_d"], ["A2b"])
    DMA("sp", B2b, modrow_d[0:1, 3 * D:4 * D].partition_broadcast(128), ["modrow_d"], ["B2b"])
    DMA("sp", g2b, modrow_d[0:1, 5 * D:6 * D].partition_broadcast(128), ["modrow_d"], ["g2b"])
    DMA("sp", n2gb, n2g_d.partition_broadcast(128), [], ["n2gb"])
    DMA("sp", wgr, wgr_d.rearrange("(k p) n -> p k n", p=128), [], ["wgr"])
    DMA("sp", bgrb, bgr_d.partition_broadcast(128), [], ["bgrb"])
    STT("dve", A2b, A2b, 1.0, n2gb, ALU.add, ALU.mult, ["n2gb"], ["A2b"])
    xt = [A.alloc(D, F32) for _ in range(2)]
    h2f_t = [A.alloc(D, F32) for _ in range(2)]
    h2b = [A.alloc(D, BF16) for _ in range(2)]
    h2T_t = [A.alloc(16 * 128, F32).rearrange("p (k t) -> p k t", k=16) for _ in range(2)]
    ssq2 = col(16)
    rs2 = col(16)

    def e2_body(i):
        sl = i % 2
        s_ = str(sl)
        h2f, h2T = h2f_t[sl], h2T_t[sl]
        b0 = 4 * sl
        DMA("sp", xt[sl], out_d[i * 128:(i + 1) * 128, :], ["out"], ["xt" + s_])
        ACT(h2f, xt[sl], AF.Square, ["xt" + s_], ["h2f" + s_, "ssq2_%d" % i], accum=ssq2[:, i:i + 1])
        yield
        rstd_col(rs2[:, i:i + 1], ssq2[:, i:i + 1], D, ["ssq2_%d" % i, "epsc"], ["rs2_%d" % i], "rs2t_%d" % i)
        yield
        STT("dve", h2f, xt[sl], rs2[:, i:i + 1], A2b, ALU.mult, ALU.mult, ["rs2_%d" % i, "A2b"], ["h2f" + s_])
        yield
        TT("dve", h2f, h2f, B2b, ALU.add, ["B2b"], ["h2f" + s_])
        yield
        ACT(h2b[sl], h2f, AF.Copy, ["h2f" + s_], ["h2b" + s_])
        for q in range(4):
            for kk in range(4):
                k = q * 4 + kk
                TR(PB[b0 + q][:, kk * 128:(kk + 1) * 128], h2f[:, k * 128:(k + 1) * 128], identf, ["h2f" + s_, "cst"],
                   [PK[b0 + q]])
        yield
        DMA("sp", h2_d[i * 128:(i + 1) * 128, :], h2b[sl], ["h2b" + s_], ["h2_d%d" % i])
        for q in range(4):
            if q % 2:
                CP("dve", h2T[:, q * 4:(q + 1) * 4, :], PB[b0 + q].rearrange("p (k t) -> p k t", k=4), [],
                   [PK[b0 + q], "h2T" + s_])
            else:
                ACT(h2T[:, q * 4:(q + 1) * 4, :], PB[b0 + q].rearrange("p (k t) -> p k t", k=4), AF.Copy, [],
                    [PK[b0 + q], "h2T" + s_])
        yield
        for k in range(16):
            MM(PB[b0][:, 0:36], h2T[:, k, :], wgr[:, k, :], k == 0, k == 15, ["h2T" + s_, "wgr"], [PK[b0]])
        yield
        TT("dve", LG[:, i, :], PB[b0][:, 0:36], bgrb, ALU.add, ["bgrb"], [PK[b0], "LG"])
        yield
    pipeline([e2_body(i) for i in range(NT)], 2)
    S.barrier()
    A.off = markE2
    if debug == "E2":
        d = dout("LG", [128, 16 * 36]); DMA("sp", d, LG.rearrange("p t n -> p (t n)"), [], ["dbg"])
        d = dout("h2", [S_LEN, D], BF16); DMA("sp", d, h2_d, [], ["dbg"])
        S.emit(); S.close(); es.close()
        return nc, dbg

    BIG = 1.0e30

    def T3(n, m):
        t = A.alloc(16 * n * m, F32)
        return t.rearrange("p (t n) -> p t n", t=16) if m == 1 else t.rearrange("p (t n m) -> p t n m", t=16, n=n)
    def bc(ap2, shape):
        v = ap2
        for ax in range(2, len(shape)):
            v = v.unsqueeze(ax)
        return v.to_broadcast(shape)
    G = LG[:, :, 0:4]
    EL = LG[:, :, 4:36]
    gmax = A.alloc(16, F32)
    RED("dve", gmax, G, ALU.max, ["LG"], ["gmax"])
    gone = T3(4, 1)
    TT("dve", gone, G, bc(gmax, [128, 16, 4]), ALU.is_equal, ["gmax", "LG"], ["gone"])
    gd = T3(4, 1)
    TT("dve", gd, G, bc(gmax, [128, 16, 4]), ALU.subtract, ["gmax", "LG"], ["gd"])
    ACT(gd, gd, AF.Exp, [], ["gd"])
    pg = A.alloc(16, F32)
    RED("dve", pg, gd, ALU.add, ["gd"], ["pg"])
    RCP(pg, pg, [], ["pg"])
    pen = T3(4, 1)
    TS("dve", pen, gone, -1.0, BIG, ALU.add, ALU.mult, ["gone"], ["pen"])
    EM = T3(32, 1)
    TT("dve", EM.rearrange("p t (g e) -> p t g e", g=4), EL.rearrange("p t (g e) -> p t g e", g=4),
       pen.unsqueeze(3).to_broadcast([128, 16, 4, 8]), ALU.add, ["pen", "LG"], ["EM"])
    v1 = A.alloc(16, F32)
    RED("dve", v1, EM, ALU.max, ["EM"], ["v1"])
    M1 = T3(32, 1)
    TT("dve", M1, EM, bc(v1, [128, 16, 32]), ALU.is_equal, ["EM", "v1"], ["M1"])
    EM2 = T3(32, 1)
    STT("dve", EM2, M1, -BIG, EM, ALU.mult, ALU.add, ["M1", "EM"], ["EM2"])
    v2 = A.alloc(16, F32)
    RED("dve", v2, EM2, ALU.max, ["EM2"], ["v2"])
    M2 = T3(32, 1)
    TT("dve", M2, EM2, bc(v2, [128, 16, 32]), ALU.is_equal, ["EM2", "v2"], ["M2"])
    e21 = A.alloc(16, F32)
    TT("dve", e21, v2, v1, ALU.subtract, ["v1", "v2"], ["e21"])
    ACT(e21, e21, AF.Exp, [], ["e21"])
    w1 = A.alloc(16, F32)
    w2 = A.alloc(16, F32)
    TS("dve", w1, e21, 1.0, None, ALU.add, None, ["e21"], ["w1"])
    RCP(w1, w1, [], ["w1"])
    TT("dve", w1, w1, pg, ALU.mult, ["pg"], ["w1"])
    TT("dve", w2, w1, e21, ALU.mult, ["w1", "e21"], ["w2"])
    Mb = A.alloc(16 * 32, BF16).rearrange("p (t n) -> p t n", t=16)
    TT("dve", Mb, M1, M2, ALU.add, ["M1", "M2"], ["Mb"])
    for i in range(NT):
        MM(PB[0][:, i * 32:(i + 1) * 32], trilsb, Mb[:, i, :], True, i == 0, ["cb", "Mb"], [PK[0]])
        for j in range(i):
            MM(PB[0][:, i * 32:(i + 1) * 32], onesb, Mb[:, j, :], False, j == i - 1, ["cb", "Mb"], [PK[0]])
    POS = T3(32, 1)
    CP("dve", POS.rearrange("p t n -> p (t n)"), PB[0], [], [PK[0], "POS"])
    for j in range(NT):
        MM(PB[1][:, 0:32], onesb, Mb[:, j, :], j == 0, j == NT - 1, ["cb", "Mb"], [PK[1]])
    cnt = A.alloc(32, F32)
    CP("dve", cnt, PB[1][:, 0:32], [], [PK[1], "cnt"])
    cmp1 = A.alloc(32 * 16, F32).rearrange("p (e m) -> p e m", e=32)
    TT("dve", cmp1, cnt.unsqueeze(2).to_broadcast([128, 32, 16]), thr16.unsqueeze(1).to_broadcast([128, 32, 16]),
       ALU.is_gt, ["cnt", "cst"], ["cmp1"])
    padded = A.alloc(32, F32)
    RED("dve", padded, cmp1, ALU.add, ["cmp1"], ["padded"])
    TS("dve", padded, padded, 128.0, None, ALU.mult, None, [], ["padded"])
    cs = [A.alloc(32, F32) for _ in range(2)]
    CP("dve", cs[0], padded, ["padded"], ["cs0"])
    cur = 0
    for sh in (1, 2, 4, 8, 16):
        nx = 1 - cur
        CP("dve", cs[nx][:, 0:sh], cs[cur][:, 0:sh], ["cs%d" % cur], ["cs%d" % nx])
        TT("dve", cs[nx][:, sh:32], cs[cur][:, sh:32], cs[cur][:, 0:32 - sh], ALU.add, ["cs%d" % cur], ["cs%d" % nx])
        cur = nx
    pad_end = cs[cur]
    pek = "cs%d" % cur
    pad_start = A.alloc(32, F32)
    TT("dve", pad_start, pad_end, padded, ALU.subtract, [pek, "padded"], ["pad_start"])
    cmp2 = A.alloc(64 * 32, F32).rearrange("p (j e) -> p j e", j=64)
    TT("dve", cmp2, pad_end.unsqueeze(1).to_broadcast([128, 64, 32]), thr64.unsqueeze(2).to_broadcast([128, 64, 32]),
       ALU.is_le, [pek, "cst"], ["cmp2"])
    blke = A.alloc(64, F32)
    RED("dve", blke, cmp2, ALU.add, ["cmp2"], ["blke"])
    TS("dve", blke, blke, 31.0, None, ALU.min, None, [], ["blke"])
    same = A.alloc(64, F32)
    S.op("pool", lambda e: e.memset(same, 0.0), [], ["same"])
    TT("dve", same[:, 2:64], blke[:, 2:64], blke[:, 0:62], ALU.is_equal, ["blke"], ["same"])
    TS("dve", blke, blke, 128.0, iota_p, ALU.mult, ALU.add, ["cst"], ["blke"])
    STT("dve", blke, same, 8192.0, blke, ALU.mult, ALU.add, ["same"], ["blke"])
    CP("dve", IDXW, blke, ["blke"], ["IDXW"])
    Tt = T3(32, 1)
    TT("dve", Tt, POS, pad_start.unsqueeze(1).to_broadcast([128, 16, 32]), ALU.add, ["POS", "pad_start"], ["Tt"])
    prod = T3(32, 1)
    dstf = A.alloc(32, F32).rearrange("p (t k) -> p t k", t=16)
    TT("dve", prod, M1, Tt, ALU.mult, ["M1", "Tt"], ["prod"])
    RED("dve", dstf[:, :, 0], prod, ALU.add, ["prod"], ["dstf"])
    TT("dve", prod, M2, Tt, ALU.mult, ["M2", "Tt"], ["prod"])
    RED("dve", dstf[:, :, 1], prod, ALU.add, ["prod"], ["dstf"])
    DI = A.alloc(32, I32)
    CP("dve", DI, dstf.rearrange("p t k -> p (t k)"), ["dstf"], ["DI"])
    tokf = A.alloc(16, F32)
    TS("dve", tokf, thr16, iota_p, None, ALU.add, None, ["cst"], ["tokf"])
    with nc.allow_non_contiguous_dma(reason="64B meta tails"):
        pass
    DMA("sp", xbuf_d[:, D:D + 32], metai_d, xbz_keys, ["xbuf"])
    if debug == "E3":
        S.barrier()
        d = dout("LG", [128, 16 * 36]); DMA("sp", d, LG.rearrange("p t n -> p (t n)"), [], ["dbg"])
        d = dout("IDXW", [128, 64], I32); DMA("sp", d, IDXW, [], ["dbg"])
        d = dout("DI", [128, 32], I32); DMA("sp", d, DI, [], ["dbg"])
        d = dout("w1", [128, 16]); DMA("sp", d, w1, [], ["dbg"])
        d = dout("w2", [128, 16]); DMA("sp", d, w2, [], ["dbg"])
        d = dout("cnt", [128, 32]); DMA("sp", d, cnt, [], ["dbg"])
        d = dout("M1", [128, 512]); DMA("sp", d, M1.rearrange("p t n -> p (t n)"), [], ["dbg"])
        d = dout("M2", [128, 512]); DMA("sp", d, M2.rearrange("p t n -> p (t n)"), [], ["dbg"])
        S.emit(); S.close(); es.close()
        return nc, dbg
    hbx = [[A.alloc(D + 32, BF16) for _ in range(2)] for _ in range(2)]
    for k in range(2):
        for sl in range(2):
            S.op("pool", (lambda k=k, sl=sl: (lambda e: e.memset(hbx[k][sl][:, D:D + 32], 0.0)))(), [], ["hb%d_%d" % (k, sl)])
    for i in range(NT):
        sl = i % 2
        for k in range(2):
            c = i * 2 + k
            hb = hbx[k][sl]
            hk = "hb%d_%d" % (k, sl)
            DMA("sp", hb[:, 0:D], h2_d[i * 128:(i + 1) * 128, :], ["h2_d%d" % i], [hk])
            tailF = hb[:, D:D + 32].bitcast(F32)
            tailI = hb[:, D:D + 32].bitcast(I32)
            CP("dve", tailI[:, 0:1], tokf[:, i:i + 1], ["tokf"], [hk])
            CP("dve", tailF[:, 1:2], (w1, w2)[k][:, i:i + 1], ["w1", "w2"], [hk])
            S.dma("pool", (lambda hb=hb, c=c: (lambda e: e.indirect_dma_start(
                out=xbuf_d, out_offset=bass.IndirectOffsetOnAxis(ap=DI[:, c:c + 1], axis=0),
                in_=hb, in_offset=None, bounds_check=breg(e, NSLOT - 1), oob_is_err=False)))(),
                [hk, "DI"], ["xbuf"])
    S.barrier()
    A.off = markE2
    markF = A.off

    wg_t = [A.alloc(16 * 512, BF16) for _ in range(2)]
    wu_t = [A.alloc(16 * 512, BF16) for _ in range(2)]
    wd_t = [A.alloc(4 * D, BF16) for _ in range(2)]
    xb_t = [A.alloc(D + 32, BF16) for _ in range(2)]
    XT_t = [A.alloc(16 * 128, BF16).rearrange("p (k s) -> p k s", k=16) for _ in range(2)]
    en_f = [A.alloc(512, F32) for _ in range(2)]
    tg_f = [A.alloc(512, F32) for _ in range(2)]
    act_b = [A.alloc(512, BF16) for _ in range(2)]
    actT = [A.alloc(4 * 128, BF16).rearrange("p (k s) -> p k s", k=4) for _ in range(2)]
    Y_t = [A.alloc(D, F32) for _ in range(2)]

    def pf(j, which):
        sl = j % 2
        s_ = str(sl)
        lst = []
        if "g" in which:
            lst += [(wg_t[sl], wg_d, "wg"), (wu_t[sl], wu_d, "wu")]
        if "d" in which:
            lst += [(wd_t[sl], wd_d, "wd")]
        for (dst, src, key) in lst:
            S.dma("pool", (lambda dst=dst, src=src, j=j: (lambda e: e.indirect_dma_start(
                out=dst, out_offset=None, in_=src, in_offset=bass.IndirectOffsetOnAxis(ap=IDXW[:, j:j + 1], axis=0),
                bounds_check=breg(e, 32 * 128 - 1), oob_is_err=False)))(), ["IDXW"], [key + s_])
        if "d" in which:
            DMA("sp", xb_t[sl], xbuf_d[j * 128:(j + 1) * 128, :], ["xbuf"], ["xb" + s_])

    def stage_A(j):
        sl = j % 2
        s_ = str(sl)
        bG, bU = 2 * sl, 2 * sl + 1
        xb, XT = xb_t[sl], XT_t[sl]
        xbv = xb[:, 0:D].rearrange("p (f k) -> p k f", k=16)
        for hf, bb in ((0, 4), (1, 5)):
            for kk in range(8):
                TR(PBH[bb][:, kk * 128:(kk + 1) * 128], xbv[:, hf * 8 + kk, :], identb, ["xb" + s_, "cb"], [PK[bb]])
        CP("dve", XT[:, 0:8, :], PBH[4].rearrange("p (k s) -> p k s", k=8), [], [PK[4], "XT" + s_])
        ACT(XT[:, 8:16, :], PBH[5].rearrange("p (k s) -> p k s", k=8), AF.Copy, [], [PK[5], "XT" + s_])
        wg, wu = wg_t[sl], wu_t[sl]
        for k in range(16):
            MM(PB[bG], XT[:, k, :], wg[:, k * 512:(k + 1) * 512], k == 0, k == 15, ["XT" + s_, "wg" + s_], [PK[bG]])
        for k in range(16):
            MM(PB[bU], XT[:, k, :], wu[:, k * 512:(k + 1) * 512], k == 0, k == 15, ["XT" + s_, "wu" + s_], [PK[bU]])

    def stage_B1(j):
        sl = j % 2
        s_ = str(sl)
        bG, bU = 2 * sl, 2 * sl + 1
        ACT(en_f[sl], PB[bG], AF.Exp, [], [PK[bG], "en_f" + s_], scale=-1.0)
        ACT(en_f[sl], en_f[sl], AF.Ln, [], ["en_f" + s_], bias=onec)
        ACT(en_f[sl], en_f[sl], AF.Exp, [], ["en_f" + s_], scale=-1.0)
        TT("dve", tg_f[sl], PB[bG], en_f[sl], ALU.mult, ["en_f" + s_], [PK[bG], "tg_f" + s_])
        TT("dve", act_b[sl], tg_f[sl], PB[bU], ALU.mult, ["tg_f" + s_], [PK[bU], "act_b" + s_])
        abv = act_b[sl].rearrange("p (j k) -> p k j", k=4)
        for kk in range(4):
            TR(PBH[6][:, kk * 128:(kk + 1) * 128], abv[:, kk, :], identb, ["act_b" + s_, "cb"], [PK[6]])
        ACT(actT[sl], PBH[6][:, 0:512].rearrange("p (k s) -> p k s", k=4), AF.Copy, [], [PK[6], "actT" + s_])

    def stage_B2(j):
        sl = j % 2
        s_ = str(sl)
        bG, bU = 2 * sl, 2 * sl + 1
        wd = wd_t[sl]
        xb = xb_t[sl]
        Y = Y_t[sl]
        wcol = xb[:, D:D + 32].bitcast(F32)[:, 1:2]
        for n in range(4):
            pbi = (bG, bU, 7, bG)[n] if False else (bG if n % 2 == 0 else bU)
            for kk in range(4):
                MM(PB[pbi], actT[sl][:, kk, :], wd[:, kk * D + n * 512: kk * D + (n + 1) * 512], kk == 0, kk == 3,
                   ["actT" + s_, "wd" + s_], [PK[pbi]])
            STT("dve", Y[:, n * 512:(n + 1) * 512], PB[pbi], wcol, g2b[:, n * 512:(n + 1) * 512],
                ALU.mult, ALU.mult, ["xb" + s_, "g2b"], [PK[pbi], "Y" + s_])
        S.dma("pool", (lambda sl=sl, Y=Y: (lambda e: e.indirect_dma_start(
            out=out_d, out_offset=bass.IndirectOffsetOnAxis(ap=xb_t[sl][:, D:D + 32].bitcast(I32)[:, 0:1], axis=0),
            in_=Y, in_offset=None, bounds_check=breg(e, S_LEN + 127), oob_is_err=True, compute_op=ALU.add)))(),
            ["Y" + s_, "xb" + s_], ["out"])
    pf(0, "gd")
    pf(1, "gd")
    stage_A(0)
    pf(2, "g")
    for j in range(NBLK):
        stage_B1(j)
        stage_B2(j)
        if j + 2 < NBLK:
            pf(j + 2, "d")
        if j + 1 < NBLK:
            stage_A(j + 1)
        if j + 3 < NBLK:
            pf(j + 3, "g")
    S.emit()
    S.close()
    es.close()
    return nc, dbg


def host_inputs(inputs, b):
    f = np.float32
    m = {}
    m["x"] = np.ascontiguousarray(inputs["x"][b])
    m["ccol"] = np.ascontiguousarray(inputs["c"][b].reshape(16, 128).T)
    m["pos"] = np.ascontiguousarray(inputs["positions"][b].reshape(1, S_LEN)).astype(np.int32)
    m["w_ada"] = inputs["w_ada"][0]
    m["b_ada"] = inputs["b_ada"][0].reshape(1, -1)
    m["norm1_gc"] = np.ascontiguousarray(inputs["norm1_g"][0].reshape(16, 128).T)
    m["w_in"] = inputs["w_in"][0]
    m["lb_logits"] = inputs["hgrn_lb_logits"]
    m["hgrn_onorm_g"] = inputs["hgrn_onorm_g"][0].reshape(1, 128)
    m["q_a_gc"] = np.ascontiguousarray(inputs["q_a_norm_g"][0].reshape(4, 128).T)
    m["w_q_up"] = inputs["w_q_up"][0]
    m["kv_a_gc"] = np.ascontiguousarray(inputs["kv_a_norm_g"][0].reshape(2, 128).T)
    m["w_kv_up"] = inputs["w_kv_up"][0]

    def qk_cols(g):
        o = np.zeros((128, 4), f)
        o[:, 0] = g[0:128]
        o[0:64, 1] = g[128:192]
        o[0:32, 2] = g[160:192]
        o[32:64, 2] = g[128:160]
        o[0:32, 3] = -1.0
        o[32:64, 3] = 1.0
        return o
    m["q_norm_gc"] = qk_cols(inputs["q_norm_g"][0])
    m["k_norm_gc"] = qk_cols(inputs["k_norm_g"][0])
    m["attn_onorm_g"] = inputs["attn_onorm_g"][0].reshape(1, 128)
    m["w_out"] = inputs["w_out"][0]
    m["norm2_g"] = inputs["norm2_g"][0].reshape(1, D)
    m["w_gr"] = np.ascontiguousarray(np.concatenate([inputs["w_group"][0], inputs["w_router"][0]], axis=1))
    m["b_gr"] = np.concatenate([inputs["b_group"][0], inputs["b_router"][0]]).reshape(1, 36)
    m["w_gate"] = inputs["w_gate"][0].reshape(32 * 128, 16 * 512)
    m["w_up"] = inputs["w_up"][0].reshape(32 * 128, 16 * 512)
    m["w_down"] = inputs["w_down"][0].reshape(32 * 128, 4 * 2048)
    m["consts"] = CONSTS
    m["invf"] = INVF
    m["meta_init"] = META_INIT.view(ml_dtypes.bfloat16)
    return m


def _consts():
    c = np.zeros((128, 1024), np.float32)
    s = np.arange(128)[:, None]
    t = np.arange(128)[None, :]
    c[:, 0:128] = np.eye(128)
    c[:, 128:256] = (s <= t)
    c[:, 256:384] = (s <= t).astype(np.float32) - (s <= 63).astype(np.float32)
    c[:, 384:512] = (s > t)
    c[:, 512:640] = (s < t)
    c[:, 640] = np.arange(128)
    c[:, 656:672] = np.arange(16) * 128
    c[:, 672:736] = np.arange(64) * 128
    c[:, 736:768] = np.arange(32)
    return c


CONSTS = _consts()
INVF = (10000.0 ** (-(np.arange(64) % 32).astype(np.float32) * 2 / 64)).astype(np.float32).reshape(64, 1)
META_INIT = np.zeros((NSLOT, 16), np.int32)
META_INIT[:, 0] = 2048 + (np.arange(NSLOT) % 128)

_NC = None


def kernel(**inputs):
    global _NC
    if _NC is None:
        _NC = build()[0]
    inputs = {k: np.asarray(v) for k, v in inputs.items()}
    in_maps = [host_inputs(inputs, b) for b in range(8)]
    res = run_bass_kernel_spmd(_NC, in_maps, core_ids=list(range(8)))
    out = np.stack([np.asarray(r["out"])[:S_LEN] for r in res.results], axis=0)
    return out.astype(np.float32)
```

```python
import numpy as np
import ml_dtypes
import concourse.bass as bass
import concourse.mybir as mybir
from concourse.bass_utils import run_bass_kernel_spmd

F32 = mybir.dt.float32
BF16 = mybir.dt.bfloat16
I32 = mybir.dt.int32
AF = mybir.ActivationFunctionType
ALU = mybir.AluOpType
AX = mybir.AxisListType

D = 2048
S_LEN = 2048
NT = 16
EPS = 1e-6
IN_COLS = 4928
BLK = 128
NBLK = 64
NSLOT = NBLK * BLK
DEBUG = None


class Sync:
    def __init__(self, nc, n_dma_sems=32):
        self.nc = nc
        self.eng = {"pe": nc.tensor, "dve": nc.vector, "act": nc.scalar,
                    "pool": nc.gpsimd, "sp": nc.sync}
        self.sem = {}
        self.cnt = {}
        self._ctx = []
        for e in self.eng:
            cm = nc.semaphore("s_" + e)
            self.sem[e] = cm.__enter__()
            self._ctx.append(cm)
            self.cnt[e] = 0
        self.dma_sems = []
        self.dma_pool = {"sp": [], "pool": [], "act": []}
        self.dma_rr = {"sp": 0, "pool": 0, "act": 0}
        for q, n in (("sp", n_dma_sems // 2), ("pool", n_dma_sems // 2), ("act", 2)):
            for i in range(n):
                cm = nc.semaphore("d%s%d" % (q, i))
                slot = [cm.__enter__(), 0, None]
                self.dma_sems.append(slot)
                self.dma_pool[q].append(slot)
                self._ctx.append(cm)
        self.waited = {}
        self.last_w = {}
        self.readers = {}
        self.prog = {e: [] for e in self.eng}

    def close(self):
        for cm in reversed(self._ctx):
            cm.__exit__(None, None, None)

    def _wait(self, e, tok):
        if tok is None:
            return
        sem, sid, val, src = tok
        if src == e and e == "pe":
            return
        k = (e, sid)
        if self.waited.get(k, 0) >= val:
            return
        self.waited[k] = val
        self.prog[e].append(("w", sem, val))

    def _deps(self, e, reads, writes, skip_same_war=True):
        for r in reads:
            self._wait(e, self.last_w.get(r))
        for w in writes:
            self._wait(e, self.last_w.get(w))
            for tok in self.readers.get(w, ()):
                if skip_same_war and tok[3] == e and e == "pe":
                    continue
                self._wait(e, tok)

    def _commit(self, tok, reads, writes):
        for w in writes:
            self.last_w[w] = tok
            self.readers[w] = []
        for r in reads:
            self.readers.setdefault(r, []).append(tok)

    def op(self, e, fn, reads=(), writes=()):
        self._deps(e, reads, writes)
        self.cnt[e] += 1
        self.prog[e].append(("i", fn, self.sem[e], 1))
        tok = (self.sem[e], e, self.cnt[e], e)
        self._commit(tok, reads, writes)
        return tok

    def dma(self, e, fn, reads=(), writes=()):
        pool = self.dma_pool[e]
        slot = pool[self.dma_rr[e]]
        self.dma_rr[e] = (self.dma_rr[e] + 1) % len(pool)
        self._wait(e, slot[2])
        self._deps(e, reads, writes, skip_same_war=False)
        slot[1] += 16
        self.prog[e].append(("i", fn, slot[0], 16))
        tok = (slot[0], id(slot), slot[1], None)
        slot[2] = tok
        self._commit(tok, reads, writes)
        return tok

    def barrier(self):
        toks = [(self.sem[e], e, self.cnt[e], e) for e in self.eng if self.cnt[e] > 0]
        toks += [s[2] for s in self.dma_sems if s[2] is not None]
        for e in self.eng:
            for t in toks:
                if t[3] == e and e == "pe":
                    continue
                self._wait(e, t)

    def emit(self):
        nc = self.nc
        self.barrier()
        prog = self.prog

        def run(engine, lst):
            for it in lst:
                if it[0] == "w":
                    engine.wait_ge(it[1], it[2])
                else:
                    it[1](engine).then_inc(it[2], it[3])

        with nc.Block() as block:
            @block.sync
            def _(eng):
                run(eng, prog["sp"])

            @block.scalar
            def _(eng):
                run(eng, prog["act"])

            @block.vector
            def _(eng):
                run(eng, prog["dve"])

            @block.gpsimd
            def _(eng):
                run(eng, prog["pool"])

            @block.tensor
            def _(eng):
                run(eng, prog["pe"])


def pipeline(gens, W):
    gens = list(gens)
    active = []
    nxt = 0
    while active or nxt < len(gens):
        while len(active) < W and nxt < len(gens):
            active.append(gens[nxt])
            nxt += 1
        for g in list(active):
            try:
                next(g)
            except StopIteration:
                active.remove(g)


class Arena:
    def __init__(self, ap):
        self.ap = ap
        self.off = 0
        self.cap = ap.shape[1]
        self.peak = 0

    def alloc(self, n, dtype=F32):
        ne = n * (2 if dtype in (F32, I32) else 1)
        ne = (ne + 15) // 16 * 16
        a = self.off
        self.off += ne
        assert self.off <= self.cap, ("arena overflow", self.off, self.cap)
        self.peak = max(self.peak, self.off)
        v = self.ap[:, a:a + n * (2 if dtype in (F32, I32) else 1)]
        if dtype == F32:
            v = v.bitcast(F32)
        elif dtype == I32:
            v = v.bitcast(I32)
        return v


def build(debug=None):
    nc = bass.Bass("TRN2", target_bir_lowering=False)

    def din(name, shape, dt=F32):
        return nc.dram_tensor(name, list(shape), dt, kind="ExternalInput").ap()

    x_d = din("x", [S_LEN, D])
    ccol_d = din("ccol", [128, 16])
    pos_d = din("pos", [1, S_LEN], I32)
    wada_d = din("w_ada", [D, 6 * D])
    bada_d = din("b_ada", [1, 6 * D])
    n1g_d = din("norm1_gc", [128, 16])
    win_d = din("w_in", [D, IN_COLS])
    lbl_d = din("lb_logits", [2, 1024])
    hon_d = din("hgrn_onorm_g", [1, 128])
    qag_d = din("q_a_gc", [128, 4])
    wqu_d = din("w_q_up", [512, 1536])
    kvg_d = din("kv_a_gc", [128, 2])
    wkv_d = din("w_kv_up", [256, 2048])
    qng_d = din("q_norm_gc", [128, 4])
    kng_d = din("k_norm_gc", [128, 4])
    aon_d = din("attn_onorm_g", [1, 128])
    wout_d = din("w_out", [D, D])
    n2g_d = din("norm2_g", [1, D])
    wgr_d = din("w_gr", [D, 36])
    bgr_d = din("b_gr", [1, 36])
    wg_d = din("w_gate", [32 * 128, 16 * 512])
    wu_d = din("w_up", [32 * 128, 16 * 512])
    wd_d = din("w_down", [32 * 128, 4 * 2048])
    cst_d = din("consts", [128, 1024])
    invf_d = din("invf", [64, 1])
    metai_d = din("meta_init", [NSLOT, 32], BF16)
    out_d = nc.dram_tensor("out", [S_LEN + 128, D], F32, kind="ExternalOutput").ap()
    xbuf_d = nc.dram_tensor("xbuf", [NSLOT, D + 32], BF16).ap()
    meta_d = nc.dram_tensor("metabuf", [NSLOT, 16], F32).ap()
    dbg = {}

    def dout(name, shape, dt=F32):
        dbg[name] = nc.dram_tensor("dbg_" + name, list(shape), dt, kind="ExternalOutput").ap()
        return dbg[name]

    S = Sync(nc)
    import contextlib
    es = contextlib.ExitStack()
    arena_t = es.enter_context(nc.sbuf_tensor("arena", [128, 103 * 1024], BF16))
    A = Arena(arena_t[:])
    banks = [es.enter_context(nc.psum_tensor("pb%d" % i, [128, 512], F32)) for i in range(8)]
    PB = [b[:] for b in banks]
    PBH = [b[:].bitcast(BF16) for b in banks]
    PK = ["pb%d" % i for i in range(8)]

    def MM(out, lhsT, rhs, start, stop, r, w):
        return S.op("pe", lambda e: e.matmul(out, lhsT=lhsT, rhs=rhs, start=start, stop=stop,
                                             skip_group_check=True), r, w)

    def TR(out, in_, ident, r, w):
        return S.op("pe", lambda e: e.transpose(out=out, in_=in_, identity=ident), r, w)

    def ACT(out, in_, func, r, w, scale=1.0, bias=0.0, accum=None):
        if accum is None:
            return S.op("act", lambda e: e.activation(out=out, in_=in_, func=func, bias=bias, scale=scale), r, w)
        return S.op("act", lambda e: e.activation(out=out, in_=in_, func=func, bias=bias, scale=scale,
                                                  accum_out=accum), r, w)

    def TS(eng, out, in0, s1, s2, op0, op1, r, w):
        if s2 is None:
            return S.op(eng, lambda e: e.tensor_scalar(out, in0, s1, None, op0), r, w)
        return S.op(eng, lambda e: e.tensor_scalar(out, in0, s1, s2, op0, op1), r, w)

    def TT(eng, out, in0, in1, op, r, w):
        return S.op(eng, lambda e: e.tensor_tensor(out, in0, in1, op), r, w)

    def STT(eng, out, in0, sc, in1, op0, op1, r, w, accum=None):
        if accum is None:
            return S.op(eng, lambda e: e.scalar_tensor_tensor(out, in0, sc, in1, op0, op1), r, w)
        return S.op(eng, lambda e: e.scalar_tensor_tensor(out, in0, sc, in1, op0, op1, accum_out=accum), r, w)

    def CP(eng, out, in_, r, w):
        return S.op(eng, lambda e: e.tensor_copy(out, in_), r, w)

    def RED(eng, out, in_, op, r, w):
        return S.op(eng, lambda e: e.tensor_reduce(out, in_, AX.X, op), r, w)

    def RCP(out, in_, r, w):
        return S.op("dve", lambda e: e.reciprocal(out, in_), r, w)

    def DMA(q, out, in_, r, w):
        return S.dma(q, lambda e: e.dma_start(out=out, in_=in_), r, w)

    _regs = {}

    def breg(e, val):
        if val not in _regs:
            _regs[val] = e.to_reg(val)
        return _regs[val]

    def rstd_col(out, ssq, n, r, w, tmpk):
        ACT(out, ssq, AF.Ln, r, [tmpk], scale=1.0 / n, bias=epsc)
        ACT(out, out, AF.Exp, [tmpk], w, scale=-0.5)

    cst = A.alloc(1024, F32)
    identf = cst[:, 0:128]
    triu = cst[:, 128:256]
    M1 = cst[:, 256:384]
    M2 = cst[:, 384:512]
    tril_strict = cst[:, 512:640]
    iota_p = cst[:, 640:641]
    thr16 = cst[:, 656:672]
    thr64 = cst[:, 672:736]
    eidx = cst[:, 736:768]
    DMA("sp", cst, cst_d, [], ["cst"])
    cb = A.alloc(512, BF16)
    identb = cb[:, 0:128]
    onesb = cb[:, 128:256]
    triub = cb[:, 256:384]
    trilsb = cb[:, 384:512]
    CP("dve", identb, identf, ["cst"], ["cb"])
    CP("dve", triub, triu, ["cst"], ["cb"])
    CP("dve", trilsb, tril_strict, ["cst"], ["cb"])
    S.op("pool", lambda e: e.memset(onesb, 1.0), [], ["cb"])
    small = A.alloc(256, F32)
    epsc = small[:, 0:1]
    onec = small[:, 1:2]
    S.op("pool", lambda e: e.memset(epsc, EPS), [], ["epsc"])
    S.op("pool", lambda e: e.memset(onec, 1.0), [], ["epsc"])
    _sc = [2]

    def col(n=1):
        a = _sc[0]
        _sc[0] += n
        assert _sc[0] <= 256
        return small[:, a:a + n]

    modc = A.alloc(96, F32)
    A1c = A.alloc(16, F32)
    modrow_d = nc.dram_tensor("modrow_d", [1, 6 * D], F32).ap()

    mark = A.off
    ccol = A.alloc(16, F32)
    cact = A.alloc(16, BF16)
    tmp16 = A.alloc(16, F32)
    modrow = A.alloc(6 * D, F32)
    DMA("sp", ccol, ccol_d, [], ["ccol"])
    DMA("sp", modrow[0:1, :], bada_d, [], ["modrow_b"])
    ACT(tmp16, ccol, AF.Exp, ["ccol"], ["tmp16"], scale=-1.0)
    TS("dve", tmp16, tmp16, 1.0, None, ALU.add, None, ["tmp16"], ["tmp16"])
    RCP(tmp16, tmp16, ["tmp16"], ["tmp16"])
    TT("dve", cact, ccol, tmp16, ALU.mult, ["tmp16", "ccol"], ["cact"])
    wa = [A.alloc(16 * 512, BF16).rearrange("p (k n) -> p k n", k=16) for _ in range(2)]
    biasrow = modrow
    for jg in range(8):
        wt = wa[jg % 2]
        wk = "wa%d" % (jg % 2)
        S.dma("pool", (lambda wt=wt, jg=jg: (lambda e: e.dma_start(
            out=wt, in_=wada_d[:, jg * 512:(jg + 1) * 512].rearrange("(k p) n -> p k n", p=128))))(),
            [], [wk])
        pbi = jg % 2
        for k in range(16):
            MM(PB[pbi][0:1, :], cact[:, k:k + 1], wt[:, k, :], k == 0, k == 15, [wk, "cact"], [PK[pbi]])
        TT("dve", modrow[0:1, jg * 512:(jg + 1) * 512], PB[pbi][0:1, :], modrow[0:1, jg * 512:(jg + 1) * 512],
           ALU.add, ["modrow_b"], [PK[pbi], "modrow%d" % jg])
    allrow = ["modrow%d" % j for j in range(8)]
    if debug == "A":
        d = dout("mod", [1, 6 * D])
        DMA("sp", d, modrow[0:1, :], allrow, ["dbg"])
    for j in range(32):
        MM(PB[2][:, j:j + 1], modrow[0:1, j * 128:(j + 1) * 128], onec[0:1, 0:1], True, True, allrow + ["epsc"], [PK[2]])
    CP("dve", modc[:, 0:32], PB[2][:, 0:32], [], [PK[2], "modc"])
    cactp = col(16)
    cactb = cactp.bitcast(BF16)[:, 0:16]
    CP("dve", cactb, cact, ["cact"], ["cactb"])
    n1gc = A.alloc(16, F32)
    DMA("sp", n1gc, n1g_d, [], ["n1gc"])
    STT("dve", A1c, modc[:, 16:32], 1.0, n1gc, ALU.add, ALU.mult, ["modc", "n1gc"], ["A1c"])
    B1c = modc[:, 0:16]
    S.barrier()
    A.off = mark

    if debug == "A":
        d2 = dout("modc", [128, 96])
        DMA("sp", d2, modc, ["modc"], ["dbg"])
        S.emit()
        S.close()
        es.close()
        return nc, dbg

    markMix = A.off
    mixT = A.alloc(16 * S_LEN, BF16).rearrange("p (k t) -> p k t", k=16)
    markHT = A.off
    hT = A.alloc(16 * S_LEN, BF16).rearrange("p (k t) -> p k t", k=16)
    markB = A.off
    xt = [A.alloc(D, F32) for _ in range(2)]
    xn = [A.alloc(D, BF16) for _ in range(2)]
    tmod = [A.alloc(1024, F32) for _ in range(2)]
    ssq1 = col(16)
    rs1 = col(16)
    for i in range(NT):
        sl = i % 2
        DMA("sp", xt[sl], x_d[i * 128:(i + 1) * 128, :], [], ["xt%d" % sl])
        ACT(xn[sl], xt[sl], AF.Square, ["xt%d" % sl], ["xn%d" % sl, "ssq1_%d" % i], accum=ssq1[:, i:i + 1])
        rstd_col(rs1[:, i:i + 1], ssq1[:, i:i + 1], D, ["ssq1_%d" % i, "epsc"], ["rs1_%d" % i], "rs1t_%d" % i)
        ACT(xn[sl], xt[sl], AF.Identity, ["xt%d" % sl, "rs1_%d" % i], ["xn%d" % sl], scale=rs1[:, i:i + 1])
        for hf in range(2):
            for kk in range(8):
                k = hf * 8 + kk
                TR(PBH[hf][:, kk * 128:(kk + 1) * 128], xn[sl][:, k * 128:(k + 1) * 128], identb,
                   ["xn%d" % sl, "cb"], [PK[hf]])
            src = PBH[hf].rearrange("p (k t) -> p k t", k=8)
            tm = tmod[hf].rearrange("p (k t) -> p k t", k=8)
            a1 = A1c[:, hf * 8:(hf + 1) * 8].unsqueeze(2).to_broadcast([128, 8, 128])
            b1 = B1c[:, hf * 8:(hf + 1) * 8].unsqueeze(2).to_broadcast([128, 8, 128])
            TT("dve", tm, src, a1, ALU.mult, ["A1c"], [PK[hf], "tmod%d" % hf])
            TT("pool", hT[:, hf * 8:(hf + 1) * 8, i * 128:(i + 1) * 128], tm, b1, ALU.add,
               ["tmod%d" % hf, "modc"], ["hT%d" % i])
    hTall = ["hT%d" % i for i in range(NT)]
    S.barrier()
    A.off = markB
    if debug == "B":
        d = dout("hT", [128, 16 * S_LEN], BF16)
        DMA("sp", d, hT.rearrange("p k t -> p (k t)"), hTall, ["dbg"])
        S.emit(); S.close(); es.close()
        return nc, dbg

    markC = A.off
    wb = [A.alloc(16 * 512, BF16).rearrange("p (k n) -> p k n", k=16) for _ in range(2)]
    markC1 = A.off
    wsec = [wb[0], wb[1],
            mixT[:, 8:12, :].rearrange("p a t -> p (a t)").rearrange("p (k n) -> p k n", k=16),
            mixT[:, 12:16, :].rearrange("p a t -> p (a t)").rearrange("p (k n) -> p k n", k=16)]
    lbb = A.alloc(1024, F32)
    omlb = A.alloc(1024, F32)
    honb = A.alloc(128, F32)
    DMA("sp", lbb, lbl_d[0:1, :].partition_broadcast(128), [], ["lbb"])
    DMA("sp", omlb, lbl_d[1:2, :].partition_broadcast(128), [], ["omlb"])
    DMA("sp", honb, hon_d.partition_broadcast(128), [], ["honb"])
    TT("dve", omlb, omlb, lbb, ALU.subtract, ["lbb"], ["omlb"])
    ACT(omlb, omlb, AF.Exp, [], ["omlb"])
    TS("dve", omlb, omlb, 1.0, None, ALU.add, None, [], ["omlb"])
    RCP(lbb, omlb, ["omlb"], ["lbb"])
    TS("dve", omlb, lbb, -1.0, 1.0, ALU.mult, ALU.add, ["lbb"], ["omlb"])
    W4 = 512
    en_t, f_t, lf_t, kk_t = A.alloc(W4), A.alloc(W4), A.alloc(W4), A.alloc(W4)
    E1_t, E2_t = A.alloc(W4), A.alloc(W4)
    qin_t, qout_t, kin_t, kout_t, v_t = (A.alloc(W4, BF16) for _ in range(5))
    eng_t, sil_t = A.alloc(W4), A.alloc(W4)
    E3_t, E1n_t = eng_t, f_t
    trq_t, trk_t, tro_t = A.alloc(W4, BF16), A.alloc(W4, BF16), A.alloc(W4, BF16)
    am_t = A.alloc(W4, BF16)
    on_t = en_t
    og_t = A.alloc(W4, BF16)
    Sst = A.alloc(W4)
    Sbf = A.alloc(W4, BF16)
    deccol = col(4)
    ssqo = col(4)
    rso = col(4)
    QSC = 128.0 ** -0.5
    v4 = lambda t: t.rearrange("p (h d) -> p h d", h=4)
    triu4 = triu.unsqueeze(1).to_broadcast([128, 4, 128])
    for hgp in range(2):
        for sec in range(4):
            c0 = sec * 1024 + hgp * 512
            S.dma("pool", (lambda sec=sec, c0=c0: (lambda e: e.dma_start(
                out=wsec[sec], in_=win_d[:, c0:c0 + 512].rearrange("(k p) n -> p k n", p=128))))(), [], ["wsec%d" % sec])
        hs = slice(hgp * 512, (hgp + 1) * 512)
        for i in range(NT):
            tsl = slice(i * 128, (i + 1) * 128)

            def emit_proj(ii):
                for sec in range(4):
                    for k in range(16):
                        MM(PB[sec], hT[:, k, ii * 128:(ii + 1) * 128], wsec[sec][:, k, :], k == 0, k == 15,
                           ["wsec%d" % sec, "hT%d" % ii], [PK[sec]])
            if i == 0:
                emit_proj(0)
            hq, hf_, hi_, hg = PB[0], PB[1], PB[2], PB[3]
            ACT(en_t, hf_, AF.Exp, [], [PK[1], "en"], scale=-1.0)
            ACT(v_t, hi_, AF.Copy, [], [PK[2], "v"])
            ACT(eng_t, hg, AF.Exp, [], [PK[3], "eng"], scale=-1.0)
            ACT(en_t, en_t, AF.Ln, [], ["en"], bias=onec)
            ACT(en_t, en_t, AF.Exp, [], ["en"], scale=-1.0)
            TT("dve", f_t, en_t, omlb[:, hs], ALU.mult, ["en", "omlb"], ["f"])
            TT("dve", f_t, f_t, lbb[:, hs], ALU.add, ["lbb"], ["f"])
            ACT(lf_t, f_t, AF.Ln, ["f"], ["lf"])
            ACT(kk_t, f_t, AF.Identity, ["f"], ["kk"], scale=-1.0, bias=onec)
            ACT(eng_t, eng_t, AF.Ln, [], ["eng"], bias=onec)
            ACT(eng_t, eng_t, AF.Exp, [], ["eng"], scale=-1.0)
            TT("dve", sil_t, hg, eng_t, ALU.mult, ["eng"], [PK[3], "sil"])
            TT("pool", v4(sil_t), v4(sil_t), honb.unsqueeze(1).to_broadcast([128, 4, 128]), ALU.mult, ["honb"], ["sil"])
            MM(PB[4], M1, lf_t, True, True, ["cst", "lf"], [PK[4]])
            MM(PB[5], M2, lf_t, True, True, ["cst", "lf"], [PK[5]])
            MM(PB[6], triu, lf_t, True, True, ["cst", "lf"], [PK[6]])
            for hh in range(4):
                MM(PB[7][:, hh:hh + 1], lf_t[:, hh * 128:(hh + 1) * 128], onec, True, True, ["epsc", "lf"], [PK[7]])
            ACT(E1_t, PB[4], AF.Exp, [], [PK[4], "E1"])
            ACT(E1n_t, PB[4], AF.Exp, [], [PK[4], "f"], scale=-1.0)
            ACT(E2_t, PB[5], AF.Exp, [], [PK[5], "E2"])
            ACT(E3_t, PB[6], AF.Exp, [], [PK[6], "eng"])
            ACT(deccol, PB[7][:, 0:4], AF.Exp, [], [PK[7], "dec"])
            STT("dve", qin_t, hq, QSC, E1_t, ALU.mult, ALU.mult, ["E1"], [PK[0], "qin"])
            STT("dve", qout_t, hq, QSC, E3_t, ALU.mult, ALU.mult, ["eng"], [PK[0], "qout"])
            TT("pool", kin_t, kk_t, E1n_t, ALU.mult, ["kk", "f"], ["kin"])
            TT("pool", kout_t, kk_t, E2_t, ALU.mult, ["kk", "E2"], ["kout"])
            for hh in range(4):
                hsl = slice(hh * 128, (hh + 1) * 128)
                TR(PBH[4][:, hsl], qin_t[:, hsl], identb, ["qin", "cb"], [PK[4]])
                TR(PBH[5][:, hsl], kin_t[:, hsl], identb, ["kin", "cb"], [PK[5]])
                TR(PBH[6][:, hsl], qout_t[:, hsl], identb, ["qout", "cb"], [PK[6]])
            CP("dve", trq_t, PBH[4][:, 0:512], [], [PK[4], "trq"])
            ACT(trk_t, PBH[5][:, 0:512], AF.Copy, [], [PK[5], "trk"])
            CP("dve", tro_t, PBH[6][:, 0:512], [], [PK[6], "tro"])
            for hh in range(4):
                hsl = slice(hh * 128, (hh + 1) * 128)
                MM(PB[4][:, hsl], trk_t[:, hsl], trq_t[:, hsl], True, True, ["trk", "trq"], [PK[4]])
            if i + 1 < NT:
                emit_proj(i + 1)
            TT("dve", v4(am_t), v4(PB[4]), triu4, ALU.mult, ["cst"], [PK[4], "am"])
            for hh in range(4):
                hsl = slice(hh * 128, (hh + 1) * 128)
                if i == 0:
                    MM(PB[5][:, hsl], am_t[:, hsl], v_t[:, hsl], True, True, ["am", "v"], [PK[5]])
                else:
                    MM(PB[5][:, hsl], am_t[:, hsl], v_t[:, hsl], True, False, ["am", "v"], [PK[5]])
                    MM(PB[5][:, hsl], tro_t[:, hsl], Sbf[:, hsl], False, True, ["tro", "Sbf"], [PK[5]])
            for hh in range(4):
                hsl = slice(hh * 128, (hh + 1) * 128)
                MM(PB[6][:, hsl], kout_t[:, hsl], v_t[:, hsl], True, True, ["kout", "v"], [PK[6]])
            if i == 0:
                CP("dve", Sst, PB[6], [], [PK[6], "S"])
            else:
                TT("pool", v4(Sst), v4(Sst), deccol.unsqueeze(2).to_broadcast([128, 4, 128]), ALU.mult, ["dec"], ["S"])
                TT("dve", Sst, Sst, PB[6], ALU.add, [], [PK[6], "S"])
            if i < NT - 1:
                ACT(Sbf, Sst, AF.Copy, ["S"], ["Sbf"])
            ACT(on_t, PB[5], AF.Square, [], [PK[5], "en"])
            RED("dve", ssqo, v4(on_t), ALU.add, ["en"], ["ssqo"])
            ACT(rso, ssqo, AF.Ln, ["ssqo", "epsc"], ["rso"], scale=1.0 / 128, bias=epsc)
            ACT(rso, rso, AF.Exp, [], ["rso"], scale=-0.5)
            TT("dve", v4(on_t), v4(PB[5]), rso.unsqueeze(2).to_broadcast([128, 4, 128]), ALU.mult, ["rso"], [PK[5], "en"])
            TT("dve", og_t, on_t, sil_t, ALU.mult, ["en", "sil"], ["og"])
            for hh in range(4):
                hsl = slice(hh * 128, (hh + 1) * 128)
                TR(PBH[7][:, hsl], og_t[:, hsl], identb, ["og", "cb"], [PK[7]])
            ACT(mixT[:, hgp * 4:(hgp + 1) * 4, tsl], PBH[7][:, 0:512].rearrange("p (h t) -> p h t", h=4), AF.Copy, [],
                [PK[7], "mixT%d_%d" % (hgp, i)])
    S.barrier()
    A.off = markC1
    if debug == "C1":
        d = dout("mixT", [128, 16 * S_LEN], BF16)
        DMA("sp", d, mixT.rearrange("p k t -> p (k t)"), [], ["dbg"])
        S.emit(); S.close(); es.close()
        return nc, dbg

    A.off = markC + 16 * 512
    mla_d = nc.dram_tensor("mla_scratch", [128, 9 * S_LEN], BF16).ap()

    def alloc_mla():
        qaT = A.alloc(4 * S_LEN, BF16).rearrange("p (k t) -> p k t", k=4)
        kvaT = A.alloc(2 * S_LEN, BF16).rearrange("p (k t) -> p k t", k=2)
        return qaT, kvaT, A.alloc(S_LEN, BF16), A.alloc(S_LEN, BF16), A.alloc(S_LEN, BF16)
    mla0 = A.off
    qaT, kvaT, kpeT, kpesT, SQR = alloc_mla()
    mla_all = A.ap[:, mla0:mla0 + 9 * S_LEN]
    qagc = A.alloc(4, F32)
    kvgc = A.alloc(2, F32)
    qng = A.alloc(4, F32)
    kng = A.alloc(4, F32)
    DMA("sp", qagc, qag_d, [], ["qagc"])
    DMA("sp", kvgc, kvg_d, [], ["kvgc"])
    DMA("sp", qng, qng_d, [], ["qng"])
    DMA("sp", kng, kng_d, [], ["kng"])
    TT("dve", qng[:, 2:3], qng[:, 2:3], qng[:, 3:4], ALU.mult, [], ["qng"])
    TT("dve", kng[:, 2:3], kng[:, 2:3], kng[:, 3:4], ALU.mult, [], ["kng"])
    sq_t = [A.alloc(512, BF16) for _ in range(2)]
    rsb_t = [A.alloc(512, F32) for _ in range(2)]
    S.op("pool", lambda e: e.memset(SQR, 0.0), [], ["SQR"])
    wA, wB = wb[0], wb[0]
    S.dma("pool", lambda e: e.dma_start(out=wA, in_=win_d[:, 4096:4608].rearrange("(k p) n -> p k n", p=128)), [], ["wb0"])

    def lowrank(wt, wk, nch, dstT, gcol, gk, nfeat, dk):
        for tg in range(4):
            tsl = slice(tg * 512, (tg + 1) * 512)
            for c in range(nch):
                pbi = c % 2
                for k in range(16):
                    MM(PB[pbi], wt[:, k, c * 128:(c + 1) * 128], hT[:, k, tsl], k == 0, k == 15,
                       [wk] + hTall, [PK[pbi]])
                ACT(sq_t[pbi], PB[pbi], AF.Square, [], [PK[pbi], "sq%d" % pbi])
                TS("dve", dstT[:, c, tsl], PB[pbi], gcol[:, c:c + 1], None, ALU.mult, None, [gk],
                   [PK[pbi], dk + "%d_%d" % (c, tg)])
                MM(PB[2], onesb, sq_t[pbi], c == 0, c == nch - 1, ["cb", "sq%d" % pbi], [PK[2]])
            rb = rsb_t[tg % 2]
            rk = "rsb%d" % (tg % 2)
            ACT(rb, PB[2], AF.Ln, ["epsc"], [PK[2], rk], scale=1.0 / nfeat, bias=epsc)
            ACT(rb, rb, AF.Exp, [], [rk], scale=-0.5)
            for c in range(nch):
                TT("pool" if c % 2 else "dve", dstT[:, c, tsl], dstT[:, c, tsl], rb, ALU.mult, [rk],
                   [dk + "%d_%d" % (c, tg)])
    lowrank(wA, "wb0", 4, qaT, qagc, "qagc", 512, "qaT")
    S.op("pool", lambda e: e.memset(wB[:, :, 256:512], 0.0), [], ["wb0"])
    S.dma("pool", lambda e: e.dma_start(out=wB[:, :, 0:256], in_=win_d[:, 4608:4864].rearrange("(k p) n -> p k n", p=128)), [], ["wb0"])
    S.dma("pool", lambda e: e.dma_start(out=wB[:, :, 256:320], in_=win_d[:, 4864:4928].rearrange("(k p) n -> p k n", p=128)), [], ["wb0"])
    S.dma("pool", lambda e: e.dma_start(out=wB[:, :, 384:416], in_=win_d[:, 4896:4928].rearrange("(k p) n -> p k n", p=128)), [], ["wb0"])
    S.dma("pool", lambda e: e.dma_start(out=wB[:, :, 416:448], in_=win_d[:, 4864:4896].rearrange("(k p) n -> p k n", p=128)), [], ["wb0"])
    lowrank(wB, "wb0", 2, kvaT, kvgc, "kvgc", 256, "kvaT")
    for tg in range(4):
        tsl = slice(tg * 512, (tg + 1) * 512)
        for k in range(16):
            MM(PB[3], wB[:, k, 256:384], hT[:, k, tsl], k == 0, k == 15, ["wb0"] + hTall, [PK[3]])
        for k in range(16):
            MM(PB[4], wB[:, k, 384:512], hT[:, k, tsl], k == 0, k == 15, ["wb0"] + hTall, [PK[4]])
        ACT(SQR[0:64, tsl], PB[3][0:64, :], AF.Square, [], [PK[3], "SQR"])
        TS("dve", kpeT[0:64, tsl], PB[3][0:64, :], kng[0:64, 1:2], None, ALU.mult, None, ["kng"], [PK[3], "kpeT"])
        TS("dve", kpesT[0:64, tsl], PB[4][0:64, :], kng[0:64, 2:3], None, ALU.mult, None, ["kng"], [PK[4], "kpesT"])
    qaT_keys = ["qaT%d_%d" % (c, tg) for c in range(4) for tg in range(4)]
    kvaT_keys = ["kvaT%d_%d" % (c, tg) for c in range(2) for tg in range(4)]
    S.barrier()
    if debug == "C2":
        d = dout("qaT", [128, 4 * S_LEN], BF16)
        DMA("sp", d, qaT.rearrange("p k t -> p (k t)"), [], ["dbg"])
        d = dout("kvaT", [128, 2 * S_LEN], BF16)
        DMA("sp", d, kvaT.rearrange("p k t -> p (k t)"), [], ["dbg"])
        d = dout("kpeT", [64, S_LEN], BF16)
        DMA("sp", d, kpeT[0:64, :], [], ["dbg"])
        d = dout("kpesT", [64, S_LEN], BF16)
        DMA("sp", d, kpesT[0:64, :], [], ["dbg"])
        S.emit(); S.close(); es.close()
        return nc, dbg
    qngp = col(4)
    kngp = col(4)
    CP("dve", qngp, qng, ["qng"], ["qngp"])
    CP("dve", kngp, kng, ["kng"], ["kngp"])
    S.barrier()
    topD = mla0 + 9 * S_LEN
    A.off = markHT
    A.cap = mla0
    if debug == "D00":
        d = dout("qaT", [128, 4 * S_LEN], BF16)
        DMA("sp", d, qaT.rearrange("p k t -> p (k t)"), [], ["dbg"])
        d = dout("kvaT", [128, 2 * S_LEN], BF16)
        DMA("sp", d, kvaT.rearrange("p k t -> p (k t)"), [], ["dbg"])
        d = dout("kpeT", [64, S_LEN], BF16)
        DMA("sp", d, kpeT[0:64, :], [], ["dbg"])
        d = dout("kpesT", [64, S_LEN], BF16)
        DMA("sp", d, kpesT[0:64, :], [], ["dbg"])
        S.emit(); S.close(); es.close()
        return nc, dbg
    PI = float(np.pi)
    cosT = A.alloc(S_LEN, F32)
    sinT = A.alloc(S_LEN, F32)
    RT = A.alloc(S_LEN, BF16)
    aonb = A.alloc(128, F32)
    import os
    SK = os.environ.get("SKIPD", "")
    if "a" not in SK:
        DMA("sp", aonb, aon_d.partition_broadcast(128), [], ["aonb"])
    markD0 = A.off
    posi = A.alloc(S_LEN, I32)
    ang = A.alloc(S_LEN, F32)
    kq = A.alloc(S_LEN, F32)
    kqi = A.alloc(S_LEN, I32)
    msk = A.alloc(S_LEN, F32)
    invf = col(1)
    if "i" not in SK:
        DMA("sp", invf[0:64, :], invf_d, [], ["invf"])
    if "p" not in SK:
        DMA("sp", posi[0:64, :], pos_d.partition_broadcast(64), [], ["posi"])
    def dump_kva(tag):
        if debug == tag:
            S.barrier()
            d = dout("kvaT2", [128, 2 * S_LEN], BF16); DMA("sp", d, kvaT.rearrange("p k t -> p (k t)"), [], ["dbg"])
            S.emit(); S.close(); es.close()
            return True
        return False
    if dump_kva("X1"):
        return nc, dbg
    for (dst, shift, key) in ((sinT, 0.0, "sinT"), (cosT, PI / 2, "cosT")):
        a_, q_, qi_, m_ = ang[0:64, :], kq[0:64, :], kqi[0:64, :], msk[0:64, :]
        CP("dve", a_, posi[0:64, :], ["posi"], ["ang"])
        TS("dve", a_, a_, invf[0:64, :], shift, ALU.mult, ALU.add, ["invf"], ["ang"])
        TS("dve", q_, a_, 1.0 / (2 * PI), None, ALU.mult, None, ["ang"], ["kq"])
        CP("dve", qi_, q_, ["kq"], ["kqi"])
        CP("dve", q_, qi_, ["kqi"], ["kq"])
        if key == "sinT" and dump_kva("X2"):
            return nc, dbg
        STT("dve", a_, q_, -2 * PI, a_, ALU.mult, ALU.add, ["kq"], ["ang"])
        TS("dve", m_, a_, PI, None, ALU.is_gt, None, ["ang"], ["msk"])
        STT("dve", a_, m_, -2 * PI, a_, ALU.mult, ALU.add, ["msk"], ["ang"])
        TS("dve", m_, a_, -PI, None, ALU.is_lt, None, ["ang"], ["msk"])
        STT("dve", a_, m_, 2 * PI, a_, ALU.mult, ALU.add, ["msk"], ["ang"])
        if key == "sinT" and dump_kva("X3"):
            return nc, dbg
        ACT(dst[0:64, :], a_, AF.Sin, ["ang"], [key])
        if key == "sinT" and dump_kva("X4"):
            return nc, dbg
    TT("dve", ang[0:64, :], kpeT[0:64, :], cosT[0:64, :], ALU.mult, ["kpeT", "cosT"], ["ang"])
    TT("dve", kq[0:64, :], kpesT[0:64, :], sinT[0:64, :], ALU.mult, ["kpesT", "sinT"], ["kq"])
    TT("dve", RT[0:64, :], ang[0:64, :], kq[0:64, :], ALU.add, ["kq", "ang"], ["RT"])
    S.barrier()
    A.off = markD0
    if debug == "D0":
        d = dout("cosT", [64, S_LEN]); DMA("sp", d, cosT[0:64, :], [], ["dbg"])
        d = dout("sinT", [64, S_LEN]); DMA("sp", d, sinT[0:64, :], [], ["dbg"])
        d = dout("RT", [64, S_LEN], BF16); DMA("sp", d, RT[0:64, :], [], ["dbg"])
        d = dout("kvaT2", [128, 2 * S_LEN], BF16); DMA("sp", d, kvaT.rearrange("p k t -> p (k t)"), [], ["dbg"])
        print("offsets", markHT, mla0, markD0, A.off)
        S.emit(); S.close(); es.close()
        return nc, dbg

    wq_t = [A.alloc(4 * 384, BF16).rearrange("p (k n) -> p k n", k=4) for _ in range(2)]
    wkv_t = [A.alloc(2 * 256, BF16).rearrange("p (k n) -> p k n", k=2) for _ in range(2)]
    QTn_t = [A.alloc(S_LEN, BF16) for _ in range(2)]
    QTr_t = [A.alloc(S_LEN, BF16) for _ in range(2)]
    KTn_t = [A.alloc(S_LEN, BF16) for _ in range(2)]
    KTr_t = [A.alloc(S_LEN, BF16) for _ in range(2)]
    V_t = [A.alloc(16 * 130, BF16).rearrange("p (t v) -> p t v", t=16) for _ in range(2)]
    for sl in range(2):
        S.op("pool", (lambda sl=sl: (lambda e: e.memset(V_t[sl][:, :, 128:130], 1.0)))(), [], ["V%d" % sl])
        S.op("pool", (lambda sl=sl: (lambda e: e.memset(wq_t[sl], 0.0)))(), [], ["wq%d" % sl])
        S.op("pool", (lambda sl=sl: (lambda e: e.memset(QTr_t[sl], 0.0)))(), [], ["QTr%d" % sl])
        S.op("pool", (lambda sl=sl: (lambda e: e.memset(KTr_t[sl], 0.0)))(), [], ["KTr%d" % sl])
    lowtop = A.off
    A.off = topD
    A.cap = A.ap.shape[1]
    sqn_t = [A.alloc(512, BF16) for _ in range(2)]
    sqr_t = [A.alloc(512, BF16) for _ in range(2)]
    rb_t = [A.alloc(512, F32) for _ in range(2)]
    t1_t = [A.alloc(512, F32) for _ in range(2)]
    t2_t = [A.alloc(512, F32) for _ in range(2)]
    mrowt = A.alloc(256, F32)
    ob_t = [A.alloc(128, BF16) for _ in range(4)]
    A.off = lowtop
    A.cap = mla0
    PT_t = [A.alloc(512, BF16) for _ in range(2)]
    wad = A.alloc(16 * 256, BF16).rearrange("p (k n) -> p k n", k=16)
    ada_next = [0]

    ada_pending = [None]

    def ada_group():
        if ada_pending[0] is not None:
            c0 = ada_pending[0]
            for k in range(16):
                MM(PB[7][0:1, 0:256], cactb[:, k:k + 1], wad[:, k, :], k == 0, k == 15, ["wad", "cactb"], [PK[7]])
            TT("dve", mrowt[0:1, :], PB[7][0:1, 0:256], mrowt[0:1, :], ALU.add, [], [PK[7], "mrowt"])
            DMA("sp", modrow_d[0:1, c0:c0 + 256], mrowt[0:1, :], ["mrowt"], ["modrow_d"])
            ada_pending[0] = None
        g = ada_next[0]
        if g >= 32:
            return
        ada_next[0] += 1
        c0 = 4096 + g * 256
        S.dma("pool", (lambda c0=c0: (lambda e: e.dma_start(
            out=wad, in_=wada_d[:, c0:c0 + 256].rearrange("(k p) n -> p k n", p=128))))(), [], ["wad"])
        DMA("sp", mrowt[0:1, :], bada_d[0:1, c0:c0 + 256], [], ["mrowt"])
        ada_pending[0] = c0
    junk3 = A.alloc(128, F32)
    ocol = col(16)
    SM_SCALE = 192.0 ** -0.5
    nev = 0
    npt = 0
    for h in range(8):
        sl = h % 2
        s_ = str(sl)
        wq, wkv = wq_t[sl], wkv_t[sl]
        QTn, QTr, KTn, KTr, V = QTn_t[sl], QTr_t[sl], KTn_t[sl], KTr_t[sl], V_t[sl]
        b0 = h * 192
        for (a, bnd, c0, n) in ((0, 128, b0, 128), (128, 192, b0 + 128, 64), (256, 288, b0 + 160, 32), (288, 320, b0 + 128, 32)):
            S.dma("pool", (lambda wq=wq, a=a, bnd=bnd, c0=c0, n=n: (lambda e: e.dma_start(
                out=wq[:, :, a:bnd], in_=wqu_d[:, c0:c0 + n].rearrange("(k p) n -> p k n", p=128))))(), [], ["wq" + s_])
        S.dma("pool", (lambda wkv=wkv, h=h: (lambda e: e.dma_start(
            out=wkv, in_=wkv_d[:, h * 256:(h + 1) * 256].rearrange("(k p) n -> p k n", p=128))))(), [], ["wkv" + s_])
        def proj_body(tg, sl=sl, s_=s_, wq=wq, wkv=wkv, QTn=QTn, QTr=QTr, KTn=KTn, KTr=KTr, V=V):
            tsl = slice(tg * 512, (tg + 1) * 512)
            g2 = tg % 2
            gs = str(g2)
            bA, bB, bC, bD = (0, 1, 2, 3) if g2 == 0 else (4, 5, 6, 7)
            for k in range(4):
                MM(PB[bA], wq[:, k, 0:128], qaT[:, k, tsl], k == 0, k == 3, ["wq" + s_] + qaT_keys, [PK[bA]])
            for k in range(4):
                MM(PB[bB], wq[:, k, 128:256], qaT[:, k, tsl], k == 0, k == 3, ["wq" + s_] + qaT_keys, [PK[bB]])
            for k in range(4):
                MM(PB[bC], wq[:, k, 256:384], qaT[:, k, tsl], k == 0, k == 3, ["wq" + s_] + qaT_keys, [PK[bC]])
            yield
            ACT(sqn_t[g2], PB[bA], AF.Square, [], [PK[bA], "sqn" + gs])
            ACT(sqr_t[g2], PB[bB], AF.Square, [], [PK[bB], "sqr" + gs])
            yield
            MM(PB[bD], onesb, sqn_t[g2], True, False, ["cb", "sqn" + gs], [PK[bD]])
            MM(PB[bD], onesb, sqr_t[g2], False, True, ["cb", "sqr" + gs], [PK[bD]])
            yield
            rb = rb_t[g2]
            ACT(rb, PB[bD], AF.Ln, ["epsc"], [PK[bD], "rb" + gs], scale=1.0 / 192, bias=epsc)
            ACT(rb, rb, AF.Exp, [], ["rb" + gs], scale=-0.5)
            yield
            STT("dve", QTn[:, tsl], PB[bA], qngp[:, 0:1], rb, ALU.mult, ALU.mult, ["qngp", "rb" + gs], [PK[bA], "QTn" + s_])
            STT("dve", t1_t[g2][0:64, :], PB[bB][0:64, :], qngp[0:64, 1:2], cosT[0:64, tsl], ALU.mult, ALU.mult,
                ["qngp", "cosT"], [PK[bB], "t1" + gs])
            STT("dve", t2_t[g2][0:64, :], PB[bC][0:64, :], qngp[0:64, 2:3], sinT[0:64, tsl], ALU.mult, ALU.mult,
                ["qngp", "sinT"], [PK[bC], "t2" + gs])
            yield
            TT("pool", t1_t[g2][0:64, :], t1_t[g2][0:64, :], t2_t[g2][0:64, :], ALU.add, ["t2" + gs], ["t1" + gs])
            TT("pool", QTr[0:64, tsl], t1_t[g2][0:64, :], rb[0:64, :], ALU.mult, ["t1" + gs, "rb" + gs], ["QTr" + s_])
            for k in range(2):
                MM(PB[bA], wkv[:, k, 0:128], kvaT[:, k, tsl], k == 0, k == 1, ["wkv" + s_] + kvaT_keys, [PK[bA]])
            for j in range(4):
                t = tg * 4 + j
                for k in range(2):
                    MM(PB[bB][:, j * 128:(j + 1) * 128], kvaT[:, k, t * 128:(t + 1) * 128], wkv[:, k, 128:256],
                       k == 0, k == 1, ["wkv" + s_] + kvaT_keys, [PK[bB]])
            yield
            ACT(sqn_t[g2], PB[bA], AF.Square, [], [PK[bA], "sqn" + gs])
            ACT(V[:, tg * 4:(tg + 1) * 4, 0:128], PB[bB].rearrange("p (t v) -> p t v", t=4), AF.Copy, [],
                [PK[bB], "V" + s_])
            yield
            MM(PB[bD], onesb, sqn_t[g2], True, False, ["cb", "sqn" + gs], [PK[bD]])
            MM(PB[bD], onesb, SQR[:, tsl], False, True, ["cb", "SQR"], [PK[bD]])
            yield
            ACT(rb, PB[bD], AF.Ln, ["epsc"], [PK[bD], "rb" + gs], scale=1.0 / 192, bias=epsc)
            ACT(rb, rb, AF.Exp, [], ["rb" + gs], scale=-0.5)
            yield
            STT("dve", KTn[:, tsl], PB[bA], kngp[:, 0:1], rb, ALU.mult, ALU.mult, ["kngp", "rb" + gs], [PK[bA], "KTn" + s_])
            TT("pool", KTr[0:64, tsl], RT[0:64, tsl], rb[0:64, :], ALU.mult, ["RT", "rb" + gs], ["KTr" + s_])
            yield
        pipeline([proj_body(tg) for tg in range(4)], 2)
        steps = [(G, kt) for G in range(4) for kt in range(4 * G + 4)]

        def geom(G, kt):
            j0 = max(0, kt - 4 * G)
            return j0, (4 - j0) * 128, (4 * G + j0) * 128

        def emit_ST(n):
            G, kt = steps[n]
            j0, ncol, q0 = geom(G, kt)
            sb = n % 2
            MM(PB[sb][:, 0:ncol], KTn[:, kt * 128:(kt + 1) * 128], QTn[:, q0:q0 + ncol], True, False,
               ["KTn" + s_, "QTn" + s_], [PK[sb]])
            MM(PB[sb][:, 0:ncol], KTr[:, kt * 128:(kt + 1) * 128], QTr[:, q0:q0 + ncol], False, True,
               ["KTr" + s_, "QTr" + s_], [PK[sb]])
        emit_ST(0)
        pending = []
        for n in range(len(steps)):
            G, kt = steps[n]
            j0, ncol, q0 = geom(G, kt)
            sb = n % 2
            pt = PT_t[npt % 2]
            pk = "PT%d" % (npt % 2)
            if n % 10 == 5:
                ada_group()
            npt += 1
            if n + 1 < len(steps):
                emit_ST(n + 1)
            ACT(pt[:, 0:ncol], PB[sb][:, 0:ncol], AF.Exp, [], [PK[sb], pk], scale=SM_SCALE)
            if kt >= 4 * G:
                TT("pool", pt[:, 0:128], pt[:, 0:128], triub, ALU.mult, ["cb"], [pk])
            for j in range(j0, 4):
                qt = 4 * G + j
                MM(PB[2 + j][:, 0:129], pt[:, (j - j0) * 128:(j - j0 + 1) * 128], V[:, kt, 0:129],
                   kt == 0, kt == qt, [pk, "V" + s_], [PK[2 + j]])
                if kt == qt:
                    e2 = nev % 4
                    nev += 1

                    def mk(e2=e2, j=j, qt=qt, h=h):
                        es_ = str(e2)
                        O = PB[2 + j]
                        ssq = ocol[:, e2 * 4:e2 * 4 + 1]
                        tt1 = ocol[:, e2 * 4 + 1:e2 * 4 + 2]
                        tt2 = ocol[:, e2 * 4 + 2:e2 * 4 + 3]
                        den = ocol[:, e2 * 4 + 3:e2 * 4 + 4]

                        def s1():
                            ACT(junk3, O[:, 0:128], AF.Square, [], [PK[2 + j], "junk3", "ossq" + es_], accum=ssq)
                            CP("dve", den, O[:, 128:129], [], [PK[2 + j], "oden" + es_])
                            STT("dve", tt1, den, EPS, den, ALU.mult, ALU.mult, ["oden" + es_], ["ott1" + es_])
                            STT("dve", tt2, ssq, 1.0 / 128, tt1, ALU.mult, ALU.add, ["ossq" + es_, "ott1" + es_], ["ott2" + es_])

                        def s2():
                            ACT(tt2, tt2, AF.Ln, [], ["ott2" + es_])
                            ACT(tt2, tt2, AF.Exp, [], ["ott2" + es_], scale=-0.5)

                        def s3():
                            STT("dve", ob_t[e2], O[:, 0:128], tt2, aonb, ALU.mult, ALU.mult, ["ott2" + es_, "aonb"],
                                [PK[2 + j], "ob" + es_])
                            TR(PBH[6][:, 0:128], ob_t[e2], identb, ["ob" + es_, "cb"], [PK[6]])

                        def s4():
                            ACT(mixT[:, 8 + h, qt * 128:(qt + 1) * 128], PBH[6][:, 0:128], AF.Copy, [],
                                [PK[6], "mixT%d_%d" % (8 + h, qt)])
                        return [s1, s2, s3, s4]
                    st = mk()
                    for d_, fn in enumerate(st):
                        pending.append([n + d_, fn])
            last_of_group = (kt == 4 * G + 3)
            keep = []
            for item in pending:
                if item[0] <= n or last_of_group and False:
                    item[1]()
                else:
                    keep.append(item)
            pending[:] = keep
            if last_of_group:
                for item in sorted(pending, key=lambda it: it[0]):
                    item[1]()
                pending[:] = []
    ada_group()
    assert ada_next[0] == 32 and ada_pending[0] is None
    S.barrier()
    A.off = markHT
    A.cap = A.ap.shape[1]
    if debug == "D":
        d = dout("mixT", [128, 16 * S_LEN], BF16)
        DMA("sp", d, mixT.rearrange("p k t -> p (k t)"), [], ["dbg"])
        S.emit(); S.close(); es.close()
        return nc, dbg

    markE = A.off
    wo = A.alloc(16 * D, BF16).rearrange("p (k n) -> p k n", k=16)
    g1b = A.alloc(D, F32)
    xt = [A.alloc(D, F32) for _ in range(2)]
    tmpe = [A.alloc(512, F32) for _ in range(2)]
    for n in range(4):
        S.dma("pool", (lambda n=n: (lambda e: e.dma_start(
            out=wo[:, :, n * 512:(n + 1) * 512],
            in_=wout_d[:, n * 512:(n + 1) * 512].rearrange("(k p) n -> p k n", p=128))))(), [], ["wo%d" % n])
    DMA("sp", g1b, modrow_d[0:1, 2 * D:3 * D].partition_broadcast(128), ["modrow_d"], ["g1b"])
    mix_keys = []
    for i in range(NT):
        sl = i % 2
        DMA("sp", xt[sl], x_d[i * 128:(i + 1) * 128, :], [], ["xt%d" % sl])
        for n in range(4):
            pbi = (i * 4 + n) % 4
            for k in range(16):
                MM(PB[pbi], mixT[:, k, i * 128:(i + 1) * 128], wo[:, k, n * 512:(n + 1) * 512], k == 0, k == 15,
                   ["wo%d" % n], [PK[pbi]])
            tp = tmpe[n % 2]
            TT("dve", tp, PB[pbi], g1b[:, n * 512:(n + 1) * 512], ALU.mult, ["g1b"], [PK[pbi], "tmpe%d" % (n % 2)])
            TT("pool", xt[sl][:, n * 512:(n + 1) * 512], xt[sl][:, n * 512:(n + 1) * 512], tp, ALU.add,
               ["tmpe%d" % (n % 2)], ["xt%d" % sl])
        DMA("sp", out_d[i * 128:(i + 1) * 128, :], xt[sl], ["xt%d" % sl], ["out"])
    S.barrier()
    A.off = markMix
    if debug == "E1":
        S.emit(); S.close(); es.close()
        return nc, dbg

    h2_d = nc.dram_tensor("h2_scratch", [S_LEN, D], BF16).ap()
    A2b = A.alloc(D, F32)
    B2b = A.alloc(D, F32)
    g2b = A.alloc(D, F32)
    LG = A.alloc(16 * 36, F32).rearrange("p (t n) -> p t n", t=16)
    IDXW = A.alloc(64, I32)
    zt = A.alloc(D + 32, BF16)
    S.op("pool", lambda e: e.memset(zt, 0.0), [], ["zt"])
    for j in range(NBLK):
        DMA("pool", xbuf_d[j * 128:(j + 1) * 128, :], zt, ["zt"], ["xbz%d" % j])
    xbz_keys = ["xbz%d" % j for j in range(NBLK)]
    markE2 = A.off
    n2gb = A.alloc(D, F32)
    wgr = A.alloc(16 * 36, F32).rearrange("p (k n) -> p k n", k=16)
    bgrb = A.alloc(36, F32)
    DMA("sp", A2b, modrow_d[0:1, 4 * D:5 * D].partition_broadcast(128), ["modrow_d"], ["A2b"])
    DMA("sp", B2b, modrow_d[0:1, 3 * D:4 * D].partition_broadcast(128), ["modrow_d"], ["B2b"])
    DMA("sp", g2b, modrow_d[0:1, 5 * D:6 * D].partition_broadcast(128), ["modrow_d"], ["g2b"])
    DMA("sp", n2gb, n2g_d.partition_broadcast(128), [], ["n2gb"])
    DMA("sp", wgr, wgr_d.rearrange("(k p) n -> p k n", p=128), [], ["wgr"])
    DMA("sp", bgrb, bgr_d.partition_broadcast(128), [], ["bgrb"])
    STT("dve", A2b, A2b, 1.0, n2gb, ALU.add, ALU.mult, ["n2gb"], ["A2b"])
    xt = [A.alloc(D, F32) for _ in range(2)]
    h2f_t = [A.alloc(D, F32) for _ in range(2)]
    h2b = [A.alloc(D, BF16) for _ in range(2)]
    h2T_t = [A.alloc(16 * 128, F32).rearrange("p (k t) -> p k t", k=16) for _ in range(2)]
    ssq2 = col(16)
    rs2 = col(16)

    def e2_body(i):
        sl = i % 2
        s_ = str(sl)
        h2f, h2T = h2f_t[sl], h2T_t[sl]
        b0 = 4 * sl
        DMA("sp", xt[sl], out_d[i * 128:(i + 1) * 128, :], ["out"], ["xt" + s_])
        ACT(h2f, xt[sl], AF.Square, ["xt" + s_], ["h2f" + s_, "ssq2_%d" % i], accum=ssq2[:, i:i + 1])
        yield
        rstd_col(rs2[:, i:i + 1], ssq2[:, i:i + 1], D, ["ssq2_%d" % i, "epsc"], ["rs2_%d" % i], "rs2t_%d" % i)
        yield
        STT("dve", h2f, xt[sl], rs2[:, i:i + 1], A2b, ALU.mult, ALU.mult, ["rs2_%d" % i, "A2b"], ["h2f" + s_])
        yield
        TT("dve", h2f, h2f, B2b, ALU.add, ["B2b"], ["h2f" + s_])
        yield
        ACT(h2b[sl], h2f, AF.Copy, ["h2f" + s_], ["h2b" + s_])
        for q in range(4):
            for kk in range(4):
                k = q * 4 + kk
                TR(PB[b0 + q][:, kk * 128:(kk + 1) * 128], h2f[:, k * 128:(k + 1) * 128], identf, ["h2f" + s_, "cst"],
                   [PK[b0 + q]])
        yield
        DMA("sp", h2_d[i * 128:(i + 1) * 128, :], h2b[sl], ["h2b" + s_], ["h2_d%d" % i])
        for q in range(4):
            if q % 2:
                CP("dve", h2T[:, q * 4:(q + 1) * 4, :], PB[b0 + q].rearrange("p (k t) -> p k t", k=4), [],
                   [PK[b0 + q], "h2T" + s_])
            else:
                ACT(h2T[:, q * 4:(q + 1) * 4, :], PB[b0 + q].rearrange("p (k t) -> p k t", k=4), AF.Copy, [],
                    [PK[b0 + q], "h2T" + s_])
        yield
        for k in range(16):
            MM(PB[b0][:, 0:36], h2T[:, k, :], wgr[:, k, :], k == 0, k == 15, ["h2T" + s_, "wgr"], [PK[b0]])
        yield
        TT("dve", LG[:, i, :], PB[b0][:, 0:36], bgrb, ALU.add, ["bgrb"], [PK[b0], "LG"])
        yield
    pipeline([e2_body(i) for i in range(NT)], 2)
    S.barrier()
    A.off = markE2
    if debug == "E2":
        d = dout("LG", [128, 16 * 36]); DMA("sp", d, LG.rearrange("p t n -> p (t n)"), [], ["dbg"])
        d = dout("h2", [S_LEN, D], BF16); DMA("sp", d, h2_d, [], ["dbg"])
        S.emit(); S.close(); es.close()
        return nc, dbg

    BIG = 1.0e30

    def T3(n, m):
        t = A.alloc(16 * n * m, F32)
        return t.rearrange("p (t n) -> p t n", t=16) if m == 1 else t.rearrange("p (t n m) -> p t n m", t=16, n=n)
    def bc(ap2, shape):
        v = ap2
        for ax in range(2, len(shape)):
            v = v.unsqueeze(ax)
        return v.to_broadcast(shape)
    G = LG[:, :, 0:4]
    EL = LG[:, :, 4:36]
    gmax = A.alloc(16, F32)
    RED("dve", gmax, G, ALU.max, ["LG"], ["gmax"])
    gone = T3(4, 1)
    TT("dve", gone, G, bc(gmax, [128, 16, 4]), ALU.is_equal, ["gmax", "LG"], ["gone"])
    gd = T3(4, 1)
    TT("dve", gd, G, bc(gmax, [128, 16, 4]), ALU.subtract, ["gmax", "LG"], ["gd"])
    ACT(gd, gd, AF.Exp, [], ["gd"])
    pg = A.alloc(16, F32)
    RED("dve", pg, gd, ALU.add, ["gd"], ["pg"])
    RCP(pg, pg, [], ["pg"])
    pen = T3(4, 1)
    TS("dve", pen, gone, -1.0, BIG, ALU.add, ALU.mult, ["gone"], ["pen"])
    EM = T3(32, 1)
    TT("dve", EM.rearrange("p t (g e) -> p t g e", g=4), EL.rearrange("p t (g e) -> p t g e", g=4),
       pen.unsqueeze(3).to_broadcast([128, 16, 4, 8]), ALU.add, ["pen", "LG"], ["EM"])
    v1 = A.alloc(16, F32)
    RED("dve", v1, EM, ALU.max, ["EM"], ["v1"])
    M1 = T3(32, 1)
    TT("dve", M1, EM, bc(v1, [128, 16, 32]), ALU.is_equal, ["EM", "v1"], ["M1"])
    EM2 = T3(32, 1)
    STT("dve", EM2, M1, -BIG, EM, ALU.mult, ALU.add, ["M1", "EM"], ["EM2"])
    v2 = A.alloc(16, F32)
    RED("dve", v2, EM2, ALU.max, ["EM2"], ["v2"])
    M2 = T3(32, 1)
    TT("dve", M2, EM2, bc(v2, [128, 16, 32]), ALU.is_equal, ["EM2", "v2"], ["M2"])
    e21 = A.alloc(16, F32)
    TT("dve", e21, v2, v1, ALU.subtract, ["v1", "v2"], ["e21"])
    ACT(e21, e21, AF.Exp, [], ["e21"])
    w1 = A.alloc(16, F32)
    w2 = A.alloc(16, F32)
    TS("dve", w1, e21, 1.0, None, ALU.add, None, ["e21"], ["w1"])
    RCP(w1, w1, [], ["w1"])
    TT("dve", w1, w1, pg, ALU.mult, ["pg"], ["w1"])
    TT("dve", w2, w1, e21, ALU.mult, ["w1", "e21"], ["w2"])
    Mb = A.alloc(16 * 32, BF16).rearrange("p (t n) -> p t n", t=16)
    TT("dve", Mb, M1, M2, ALU.add, ["M1", "M2"], ["Mb"])
    for i in range(NT):
        MM(PB[0][:, i * 32:(i + 1) * 32], trilsb, Mb[:, i, :], True, i == 0, ["cb", "Mb"], [PK[0]])
        for j in range(i):
            MM(PB[0][:, i * 32:(i + 1) * 32], onesb, Mb[:, j, :], False, j == i - 1, ["cb", "Mb"], [PK[0]])
    POS = T3(32, 1)
    CP("dve", POS.rearrange("p t n -> p (t n)"), PB[0], [], [PK[0], "POS"])
    for j in range(NT):
        MM(PB[1][:, 0:32], onesb, Mb[:, j, :], j == 0, j == NT - 1, ["cb", "Mb"], [PK[1]])
    cnt = A.alloc(32, F32)
    CP("dve", cnt, PB[1][:, 0:32], [], [PK[1], "cnt"])
    cmp1 = A.alloc(32 * 16, F32).rearrange("p (e m) -> p e m", e=32)
    TT("dve", cmp1, cnt.unsqueeze(2).to_broadcast([128, 32, 16]), thr16.unsqueeze(1).to_broadcast([128, 32, 16]),
       ALU.is_gt, ["cnt", "cst"], ["cmp1"])
    padded = A.alloc(32, F32)
    RED("dve", padded, cmp1, ALU.add, ["cmp1"], ["padded"])
    TS("dve", padded, padded, 128.0, None, ALU.mult, None, [], ["padded"])
    cs = [A.alloc(32, F32) for _ in range(2)]
    CP("dve", cs[0], padded, ["padded"], ["cs0"])
    cur = 0
    for sh in (1, 2, 4, 8, 16):
        nx = 1 - cur
        CP("dve", cs[nx][:, 0:sh], cs[cur][:, 0:sh], ["cs%d" % cur], ["cs%d" % nx])
        TT("dve", cs[nx][:, sh:32], cs[cur][:, sh:32], cs[cur][:, 0:32 - sh], ALU.add, ["cs%d" % cur], ["cs%d" % nx])
        cur = nx
    pad_end = cs[cur]
    pek = "cs%d" % cur
    pad_start = A.alloc(32, F32)
    TT("dve", pad_start, pad_end, padded, ALU.subtract, [pek, "padded"], ["pad_start"])
    cmp2 = A.alloc(64 * 32, F32).rearrange("p (j e) -> p j e", j=64)
    TT("dve", cmp2, pad_end.unsqueeze(1).to_broadcast([128, 64, 32]), thr64.unsqueeze(2).to_broadcast([128, 64, 32]),
       ALU.is_le, [pek, "cst"], ["cmp2"])
    blke = A.alloc(64, F32)
    RED("dve", blke, cmp2, ALU.add, ["cmp2"], ["blke"])
    TS("dve", blke, blke, 31.0, None, ALU.min, None, [], ["blke"])
    same = A.alloc(64, F32)
    S.op("pool", lambda e: e.memset(same, 0.0), [], ["same"])
    TT("dve", same[:, 2:64], blke[:, 2:64], blke[:, 0:62], ALU.is_equal, ["blke"], ["same"])
    TS("dve", blke, blke, 128.0, iota_p, ALU.mult, ALU.add, ["cst"], ["blke"])
    STT("dve", blke, same, 8192.0, blke, ALU.mult, ALU.add, ["same"], ["blke"])
    CP("dve", IDXW, blke, ["blke"], ["IDXW"])
    Tt = T3(32, 1)
    TT("dve", Tt, POS, pad_start.unsqueeze(1).to_broadcast([128, 16, 32]), ALU.add, ["POS", "pad_start"], ["Tt"])
    prod = T3(32, 1)
    dstf = A.alloc(32, F32).rearrange("p (t k) -> p t k", t=16)
    TT("dve", prod, M1, Tt, ALU.mult, ["M1", "Tt"], ["prod"])
    RED("dve", dstf[:, :, 0], prod, ALU.add, ["prod"], ["dstf"])
    TT("dve", prod, M2, Tt, ALU.mult, ["M2", "Tt"], ["prod"])
    RED("dve", dstf[:, :, 1], prod, ALU.add, ["prod"], ["dstf"])
    DI = A.alloc(32, I32)
    CP("dve", DI, dstf.rearrange("p t k -> p (t k)"), ["dstf"], ["DI"])
    tokf = A.alloc(16, F32)
    TS("dve", tokf, thr16, iota_p, None, ALU.add, None, ["cst"], ["tokf"])
    with nc.allow_non_contiguous_dma(reason="64B meta tails"):
        pass
    DMA("sp", xbuf_d[:, D:D + 32], metai_d, xbz_keys, ["xbuf"])
    if debug == "E3":
        S.barrier()
        d = dout("LG", [128, 16 * 36]); DMA("sp", d, LG.rearrange("p t n -> p (t n)"), [], ["dbg"])
        d = dout("IDXW", [128, 64], I32); DMA("sp", d, IDXW, [], ["dbg"])
        d = dout("DI", [128, 32], I32); DMA("sp", d, DI, [], ["dbg"])
        d = dout("w1", [128, 16]); DMA("sp", d, w1, [], ["dbg"])
        d = dout("w2", [128, 16]); DMA("sp", d, w2, [], ["dbg"])
        d = dout("cnt", [128, 32]); DMA("sp", d, cnt, [], ["dbg"])
        d = dout("M1", [128, 512]); DMA("sp", d, M1.rearrange("p t n -> p (t n)"), [], ["dbg"])
        d = dout("M2", [128, 512]); DMA("sp", d, M2.rearrange("p t n -> p (t n)"), [], ["dbg"])
        S.emit(); S.close(); es.close()
        return nc, dbg
    hbx = [[A.alloc(D + 32, BF16) for _ in range(2)] for _ in range(2)]
    whi = [A.alloc(16, BF16) for _ in range(2)]
    wlo = [A.alloc(16, BF16) for _ in range(2)]
    whf = A.alloc(16, F32)
    for k in range(2):
        wk_ = (w1, w2)[k]
        CP("dve", whi[k], wk_, ["w1", "w2"], ["whl"])
        CP("dve", whf, whi[k], ["whl"], ["whf"])
        TT("dve", whf, wk_, whf, ALU.subtract, ["w1", "w2"], ["whf"])
        CP("dve", wlo[k], whf, ["whf"], ["whl"])
    for k in range(2):
        for sl in range(2):
            S.op("pool", (lambda k=k, sl=sl: (lambda e: e.memset(hbx[k][sl][:, D:D + 32], 0.0)))(), [], ["hb%d_%d" % (k, sl)])
    for i in range(NT):
        sl = i % 2
        for k in range(2):
            c = i * 2 + k
            hb = hbx[k][sl]
            hk = "hb%d_%d" % (k, sl)
            DMA("sp", hb[:, 0:D], h2_d[i * 128:(i + 1) * 128, :], ["h2_d%d" % i], [hk])
            tailI = hb[:, D:D + 32].bitcast(I32)
            CP("dve", tailI[:, 0:1], tokf[:, i:i + 1], ["tokf"], [hk])
            CP("dve", hb[:, D + 2:D + 3], whi[k][:, i:i + 1], ["whl"], [hk])
            CP("dve", hb[:, D + 3:D + 4], wlo[k][:, i:i + 1], ["whl"], [hk])
            S.dma("pool", (lambda hb=hb, c=c: (lambda e: e.indirect_dma_start(
                out=xbuf_d, out_offset=bass.IndirectOffsetOnAxis(ap=DI[:, c:c + 1], axis=0),
                in_=hb, in_offset=None, bounds_check=breg(e, NSLOT - 1), oob_is_err=False)))(),
                [hk, "DI"], ["xbuf"])
    S.barrier()
    A.off = markE2
    markF = A.off

    wg_t = [A.alloc(16 * 512, BF16) for _ in range(2)]
    wu_t = [A.alloc(16 * 512, BF16) for _ in range(2)]
    wd_t = [A.alloc(4 * D, BF16) for _ in range(2)]
    xb_t = [A.alloc(D + 32, BF16) for _ in range(2)]
    XT_t = [A.alloc(16 * 128, BF16).rearrange("p (k s) -> p k s", k=16) for _ in range(2)]
    en_f = [A.alloc(512, F32) for _ in range(2)]
    tg_f = [A.alloc(512, F32) for _ in range(2)]
    act_b = [A.alloc(512, BF16) for _ in range(2)]
    actT = [A.alloc(4 * 128, BF16).rearrange("p (k s) -> p k s", k=4) for _ in range(2)]
    Y_t = [A.alloc(D, F32) for _ in range(2)]
    wc_t = [col(1) for _ in range(2)]

    def pf(j, which):
        sl = j % 2
        s_ = str(sl)
        lst = []
        if "g" in which:
            lst += [(wg_t[sl], wg_d, "wg"), (wu_t[sl], wu_d, "wu")]
        if "d" in which:
            lst += [(wd_t[sl], wd_d, "wd")]
        for (dst, src, key) in lst:
            S.dma("pool", (lambda dst=dst, src=src, j=j: (lambda e: e.indirect_dma_start(
                out=dst, out_offset=None, in_=src, in_offset=bass.IndirectOffsetOnAxis(ap=IDXW[:, j:j + 1], axis=0),
                bounds_check=breg(e, 32 * 128 - 1), oob_is_err=False)))(), ["IDXW"], [key + s_])
        if "d" in which:
            DMA("sp", xb_t[sl], xbuf_d[j * 128:(j + 1) * 128, :], ["xbuf"], ["xb" + s_])

    def stage_A(j):
        sl = j % 2
        s_ = str(sl)
        bG, bU = 2 * sl, 2 * sl + 1
        xb, XT = xb_t[sl], XT_t[sl]
        xbv = xb[:, 0:D].rearrange("p (f k) -> p k f", k=16)
        for hf, bb in ((0, 4), (1, 5)):
            for kk in range(8):
                TR(PBH[bb][:, kk * 128:(kk + 1) * 128], xbv[:, hf * 8 + kk, :], identb, ["xb" + s_, "cb"], [PK[bb]])
        CP("dve", XT[:, 0:8, :], PBH[4].rearrange("p (k s) -> p k s", k=8), [], [PK[4], "XT" + s_])
        ACT(XT[:, 8:16, :], PBH[5].rearrange("p (k s) -> p k s", k=8), AF.Copy, [], [PK[5], "XT" + s_])
        wg, wu = wg_t[sl], wu_t[sl]
        for k in range(16):
            MM(PB[bG], XT[:, k, :], wg[:, k * 512:(k + 1) * 512], k == 0, k == 15, ["XT" + s_, "wg" + s_], [PK[bG]])
        for k in range(16):
            MM(PB[bU], XT[:, k, :], wu[:, k * 512:(k + 1) * 512], k == 0, k == 15, ["XT" + s_, "wu" + s_], [PK[bU]])

    def stage_B1(j):
        sl = j % 2
        s_ = str(sl)
        bG, bU = 2 * sl, 2 * sl + 1
        ACT(en_f[sl], PB[bG], AF.Exp, [], [PK[bG], "en_f" + s_], scale=-1.0)
        ACT(en_f[sl], en_f[sl], AF.Ln, [], ["en_f" + s_], bias=onec)
        ACT(en_f[sl], en_f[sl], AF.Exp, [], ["en_f" + s_], scale=-1.0)
        TT("dve", tg_f[sl], PB[bG], en_f[sl], ALU.mult, ["en_f" + s_], [PK[bG], "tg_f" + s_])
        TT("dve", act_b[sl], tg_f[sl], PB[bU], ALU.mult, ["tg_f" + s_], [PK[bU], "act_b" + s_])
        abv = act_b[sl].rearrange("p (j k) -> p k j", k=4)
        for kk in range(4):
            TR(PBH[6][:, kk * 128:(kk + 1) * 128], abv[:, kk, :], identb, ["act_b" + s_, "cb"], [PK[6]])
        ACT(actT[sl], PBH[6][:, 0:512].rearrange("p (k s) -> p k s", k=4), AF.Copy, [], [PK[6], "actT" + s_])

    def stage_B2(j):
        sl = j % 2
        s_ = str(sl)
        bG, bU = 2 * sl, 2 * sl + 1
        wd = wd_t[sl]
        xb = xb_t[sl]
        Y = Y_t[sl]
        wcol = wc_t[sl]
        TT("dve", wcol, xb[:, D + 2:D + 3], xb[:, D + 3:D + 4], ALU.add, ["xb" + s_], ["wc" + s_])
        for n in range(4):
            pbi = (bG, bU, 7, bG)[n] if False else (bG if n % 2 == 0 else bU)
            for kk in range(4):
                MM(PB[pbi], actT[sl][:, kk, :], wd[:, kk * D + n * 512: kk * D + (n + 1) * 512], kk == 0, kk == 3,
                   ["actT" + s_, "wd" + s_], [PK[pbi]])
            STT("dve", Y[:, n * 512:(n + 1) * 512], PB[pbi], wcol, g2b[:, n * 512:(n + 1) * 512],
                ALU.mult, ALU.mult, ["wc" + s_, "g2b"], [PK[pbi], "Y" + s_])
        S.dma("pool", (lambda sl=sl, Y=Y: (lambda e: e.indirect_dma_start(
            out=out_d, out_offset=bass.IndirectOffsetOnAxis(ap=xb_t[sl][:, D:D + 32].bitcast(I32)[:, 0:1], axis=0),
            in_=Y, in_offset=None, bounds_check=breg(e, S_LEN + 127), oob_is_err=True, compute_op=ALU.add)))(),
            ["Y" + s_, "xb" + s_], ["out"])
    pf(0, "gd")
    pf(1, "gd")
    stage_A(0)
    pf(2, "g")
    for j in range(NBLK):
        stage_B1(j)
        stage_B2(j)
        if j + 2 < NBLK:
            pf(j + 2, "d")
        if j + 1 < NBLK:
            stage_A(j + 1)
        if j + 3 < NBLK:
            pf(j + 3, "g")
    S.emit()
    S.close()
    es.close()
    return nc, dbg


def host_inputs(inputs, b):
    f = np.float32
    m = {}
    m["x"] = np.ascontiguousarray(inputs["x"][b])
    m["ccol"] = np.ascontiguousarray(inputs["c"][b].reshape(16, 128).T)
    m["pos"] = np.ascontiguousarray(inputs["positions"][b].reshape(1, S_LEN)).astype(np.int32)
    m["w_ada"] = inputs["w_ada"][0]
    m["b_ada"] = inputs["b_ada"][0].reshape(1, -1)
    m["norm1_gc"] = np.ascontiguousarray(inputs["norm1_g"][0].reshape(16, 128).T)
    m["w_in"] = inputs["w_in"][0]
    m["lb_logits"] = inputs["hgrn_lb_logits"]
    m["hgrn_onorm_g"] = inputs["hgrn_onorm_g"][0].reshape(1, 128)
    m["q_a_gc"] = np.ascontiguousarray(inputs["q_a_norm_g"][0].reshape(4, 128).T)
    m["w_q_up"] = inputs["w_q_up"][0]
    m["kv_a_gc"] = np.ascontiguousarray(inputs["kv_a_norm_g"][0].reshape(2, 128).T)
    m["w_kv_up"] = inputs["w_kv_up"][0]

    def qk_cols(g):
        o = np.zeros((128, 4), f)
        o[:, 0] = g[0:128]
        o[0:64, 1] = g[128:192]
        o[0:32, 2] = g[160:192]
        o[32:64, 2] = g[128:160]
        o[0:32, 3] = -1.0
        o[32:64, 3] = 1.0
        return o
    m["q_norm_gc"] = qk_cols(inputs["q_norm_g"][0])
    m["k_norm_gc"] = qk_cols(inputs["k_norm_g"][0])
    m["attn_onorm_g"] = inputs["attn_onorm_g"][0].reshape(1, 128)
    m["w_out"] = inputs["w_out"][0]
    m["norm2_g"] = inputs["norm2_g"][0].reshape(1, D)
    m["w_gr"] = np.ascontiguousarray(np.concatenate([inputs["w_group"][0], inputs["w_router"][0]], axis=1))
    m["b_gr"] = np.concatenate([inputs["b_group"][0], inputs["b_router"][0]]).reshape(1, 36)
    m["w_gate"] = inputs["w_gate"][0].reshape(32 * 128, 16 * 512)
    m["w_up"] = inputs["w_up"][0].reshape(32 * 128, 16 * 512)
    m["w_down"] = inputs["w_down"][0].reshape(32 * 128, 4 * 2048)
    m["consts"] = CONSTS
    m["invf"] = INVF
    m["meta_init"] = META_INIT.view(ml_dtypes.bfloat16)
    return m


def _consts():
    c = np.zeros((128, 1024), np.float32)
    s = np.arange(128)[:, None]
    t = np.arange(128)[None, :]
    c[:, 0:128] = np.eye(128)
    c[:, 128:256] = (s <= t)
    c[:, 256:384] = (s <= t).astype(np.float32) - (s <= 63).astype(np.float32)
    c[:, 384:512] = (s > t)
    c[:, 512:640] = (s < t)
    c[:, 640] = np.arange(128)
    c[:, 656:672] = np.arange(16) * 128
    c[:, 672:736] = np.arange(64) * 128
    c[:, 736:768] = np.arange(32)
    return c


CONSTS = _consts()
INVF = (10000.0 ** (-(np.arange(64) % 32).astype(np.float32) * 2 / 64)).astype(np.float32).reshape(64, 1)
META_INIT = np.zeros((NSLOT, 16), np.int32)
META_INIT[:, 0] = 2048 + (np.arange(NSLOT) % 128)

_NC = None


def kernel(**inputs):
    global _NC
    if _NC is None:
        _NC = build()[0]
    inputs = {k: np.asarray(v) for k, v in inputs.items()}
    in_maps = [host_inputs(inputs, b) for b in range(8)]
    res = run_bass_kernel_spmd(_NC, in_maps, core_ids=list(range(8)))
    out = np.stack([np.asarray(r["out"])[:S_LEN] for r in res.results], axis=0)
    return out.astype(np.float32)
```

```python
import numpy as np
import ml_dtypes
import concourse.bass as bass
import concourse.mybir as mybir
from concourse.bass_utils import run_bass_kernel_spmd

F32 = mybir.dt.float32
BF16 = mybir.dt.bfloat16
I32 = mybir.dt.int32
AF = mybir.ActivationFunctionType
ALU = mybir.AluOpType
AX = mybir.AxisListType

D = 2048
S_LEN = 2048
NT = 16
EPS = 1e-6
IN_COLS = 4928
BLK = 128
NBLK = 64
NSLOT = NBLK * BLK
DEBUG = None


class Sync:
    def __init__(self, nc, n_dma_sems=32):
        self.nc = nc
        self.eng = {"pe": nc.tensor, "dve": nc.vector, "act": nc.scalar,
                    "pool": nc.gpsimd, "sp": nc.sync}
        self.sem = {}
        self.cnt = {}
        self._ctx = []
        for e in self.eng:
            cm = nc.semaphore("s_" + e)
            self.sem[e] = cm.__enter__()
            self._ctx.append(cm)
            self.cnt[e] = 0
        self.dma_sems = []
        self.dma_pool = {"sp": [], "pool": [], "act": []}
        self.dma_rr = {"sp": 0, "pool": 0, "act": 0}
        for q, n in (("sp", n_dma_sems // 2), ("pool", n_dma_sems // 2), ("act", 2)):
            for i in range(n):
                cm = nc.semaphore("d%s%d" % (q, i))
                slot = [cm.__enter__(), 0, None]
                self.dma_sems.append(slot)
                self.dma_pool[q].append(slot)
                self._ctx.append(cm)
        self.waited = {}
        self.last_w = {}
        self.readers = {}
        self.prog = {e: [] for e in self.eng}

    def close(self):
        for cm in reversed(self._ctx):
            cm.__exit__(None, None, None)

    def _wait(self, e, tok):
        if tok is None:
            return
        sem, sid, val, src = tok
        if src == e and e == "pe":
            return
        k = (e, sid)
        if self.waited.get(k, 0) >= val:
            return
        self.waited[k] = val
        self.prog[e].append(("w", sem, val))

    def _deps(self, e, reads, writes, skip_same_war=True):
        for r in reads:
            self._wait(e, self.last_w.get(r))
        for w in writes:
            self._wait(e, self.last_w.get(w))
            for tok in self.readers.get(w, ()):
                if skip_same_war and tok[3] == e and e == "pe":
                    continue
                self._wait(e, tok)

    def _commit(self, tok, reads, writes):
        for w in writes:
            self.last_w[w] = tok
            self.readers[w] = []
        for r in reads:
            self.readers.setdefault(r, []).append(tok)

    def op(self, e, fn, reads=(), writes=()):
        self._deps(e, reads, writes)
        self.cnt[e] += 1
        self.prog[e].append(("i", fn, self.sem[e], 1))
        tok = (self.sem[e], e, self.cnt[e], e)
        self._commit(tok, reads, writes)
        return tok

    def dma(self, e, fn, reads=(), writes=()):
        pool = self.dma_pool[e]
        slot = pool[self.dma_rr[e]]
        self.dma_rr[e] = (self.dma_rr[e] + 1) % len(pool)
        self._wait(e, slot[2])
        self._deps(e, reads, writes, skip_same_war=False)
        slot[1] += 16
        self.prog[e].append(("i", fn, slot[0], 16))
        tok = (slot[0], id(slot), slot[1], None)
        slot[2] = tok
        self._commit(tok, reads, writes)
        return tok

    def barrier(self):
        toks = [(self.sem[e], e, self.cnt[e], e) for e in self.eng if self.cnt[e] > 0]
        toks += [s[2] for s in self.dma_sems if s[2] is not None]
        for e in self.eng:
            for t in toks:
                if t[3] == e and e == "pe":
                    continue
                self._wait(e, t)

    def emit(self):
        nc = self.nc
        self.barrier()
        prog = self.prog

        def run(engine, lst):
            for it in lst:
                if it[0] == "w":
                    engine.wait_ge(it[1], it[2])
                else:
                    it[1](engine).then_inc(it[2], it[3])

        with nc.Block() as block:
            @block.sync
            def _(eng):
                run(eng, prog["sp"])

            @block.scalar
            def _(eng):
                run(eng, prog["act"])

            @block.vector
            def _(eng):
                run(eng, prog["dve"])

            @block.gpsimd
            def _(eng):
                run(eng, prog["pool"])

            @block.tensor
            def _(eng):
                run(eng, prog["pe"])


def pipeline(gens, W):
    gens = list(gens)
    active = []
    nxt = 0
    while active or nxt < len(gens):
        while len(active) < W and nxt < len(gens):
            active.append(gens[nxt])
            nxt += 1
        for g in list(active):
            try:
                next(g)
            except StopIteration:
                active.remove(g)


class Arena:
    def __init__(self, ap):
        self.ap = ap
        self.off = 0
        self.cap = ap.shape[1]
        self.peak = 0

    def alloc(self, n, dtype=F32):
        ne = n * (2 if dtype in (F32, I32) else 1)
        ne = (ne + 15) // 16 * 16
        a = self.off
        self.off += ne
        assert self.off <= self.cap, ("arena overflow", self.off, self.cap)
        self.peak = max(self.peak, self.off)
        v = self.ap[:, a:a + n * (2 if dtype in (F32, I32) else 1)]
        if dtype == F32:
            v = v.bitcast(F32)
        elif dtype == I32:
            v = v.bitcast(I32)
        return v


def build(debug=None):
    nc = bass.Bass("TRN2", target_bir_lowering=False)

    def din(name, shape, dt=F32):
        return nc.dram_tensor(name, list(shape), dt, kind="ExternalInput").ap()

    x_d = din("x", [S_LEN, D])
    ccol_d = din("ccol", [128, 16])
    pos_d = din("pos", [1, S_LEN], I32)
    wada_d = din("w_ada", [D, 6 * D])
    bada_d = din("b_ada", [1, 6 * D])
    n1g_d = din("norm1_gc", [128, 16])
    win_d = din("w_in", [D, IN_COLS])
    lbl_d = din("lb_logits", [2, 1024])
    hon_d = din("hgrn_onorm_g", [1, 128])
    qag_d = din("q_a_gc", [128, 4])
    wqu_d = din("w_q_up", [512, 1536])
    kvg_d = din("kv_a_gc", [128, 2])
    wkv_d = din("w_kv_up", [256, 2048])
    qng_d = din("q_norm_gc", [128, 4])
    kng_d = din("k_norm_gc", [128, 4])
    aon_d = din("attn_onorm_g", [1, 128])
    wout_d = din("w_out", [D, D])
    n2g_d = din("norm2_g", [1, D])
    wgr_d = din("w_gr", [D, 36])
    bgr_d = din("b_gr", [1, 36])
    wg_d = din("w_gate", [32 * 128, 16 * 512])
    wu_d = din("w_up", [32 * 128, 16 * 512])
    wd_d = din("w_down", [32 * 128, 4 * 2048])
    cst_d = din("consts", [128, 1024])
    invf_d = din("invf", [64, 1])
    metai_d = din("meta_init", [NSLOT, 32], BF16)
    out_d = nc.dram_tensor("out", [S_LEN + 128, D], F32, kind="ExternalOutput").ap()
    xbuf_d = nc.dram_tensor("xbuf", [NSLOT, D + 32], BF16).ap()
    meta_d = nc.dram_tensor("metabuf", [NSLOT, 16], F32).ap()
    dbg = {}

    def dout(name, shape, dt=F32):
        dbg[name] = nc.dram_tensor("dbg_" + name, list(shape), dt, kind="ExternalOutput").ap()
        return dbg[name]

    S = Sync(nc)
    import contextlib
    es = contextlib.ExitStack()
    arena_t = es.enter_context(nc.sbuf_tensor("arena", [128, 103 * 1024], BF16))
    A = Arena(arena_t[:])
    banks = [es.enter_context(nc.psum_tensor("pb%d" % i, [128, 512], F32)) for i in range(8)]
    PB = [b[:] for b in banks]
    PBH = [b[:].bitcast(BF16) for b in banks]
    PK = ["pb%d" % i for i in range(8)]

    def MM(out, lhsT, rhs, start, stop, r, w):
        return S.op("pe", lambda e: e.matmul(out, lhsT=lhsT, rhs=rhs, start=start, stop=stop,
                                             skip_group_check=True), r, w)

    def TR(out, in_, ident, r, w):
        return S.op("pe", lambda e: e.transpose(out=out, in_=in_, identity=ident), r, w)

    def ACT(out, in_, func, r, w, scale=1.0, bias=0.0, accum=None):
        if accum is None:
            return S.op("act", lambda e: e.activation(out=out, in_=in_, func=func, bias=bias, scale=scale), r, w)
        return S.op("act", lambda e: e.activation(out=out, in_=in_, func=func, bias=bias, scale=scale,
                                                  accum_out=accum), r, w)

    def TS(eng, out, in0, s1, s2, op0, op1, r, w):
        if s2 is None:
            return S.op(eng, lambda e: e.tensor_scalar(out, in0, s1, None, op0), r, w)
        return S.op(eng, lambda e: e.tensor_scalar(out, in0, s1, s2, op0, op1), r, w)

    def TT(eng, out, in0, in1, op, r, w):
        return S.op(eng, lambda e: e.tensor_tensor(out, in0, in1, op), r, w)

    def STT(eng, out, in0, sc, in1, op0, op1, r, w, accum=None):
        if accum is None:
            return S.op(eng, lambda e: e.scalar_tensor_tensor(out, in0, sc, in1, op0, op1), r, w)
        return S.op(eng, lambda e: e.scalar_tensor_tensor(out, in0, sc, in1, op0, op1, accum_out=accum), r, w)

    def CP(eng, out, in_, r, w):
        return S.op(eng, lambda e: e.tensor_copy(out, in_), r, w)

    def RED(eng, out, in_, op, r, w):
        return S.op(eng, lambda e: e.tensor_reduce(out, in_, AX.X, op), r, w)

    def RCP(out, in_, r, w):
        return S.op("dve", lambda e: e.reciprocal(out, in_), r, w)

    def DMA(q, out, in_, r, w):
        return S.dma(q, lambda e: e.dma_start(out=out, in_=in_), r, w)

    _regs = {}

    def breg(e, val):
        if val not in _regs:
            _regs[val] = e.to_reg(val)
        return _regs[val]

    def rstd_col(out, ssq, n, r, w, tmpk):
        ACT(out, ssq, AF.Ln, r, [tmpk], scale=1.0 / n, bias=epsc)
        ACT(out, out, AF.Exp, [tmpk], w, scale=-0.5)

    cst = A.alloc(1024, F32)
    identf = cst[:, 0:128]
    triu = cst[:, 128:256]
    M1 = cst[:, 256:384]
    M2 = cst[:, 384:512]
    tril_strict = cst[:, 512:640]
    iota_p = cst[:, 640:641]
    thr16 = cst[:, 656:672]
    thr64 = cst[:, 672:736]
    eidx = cst[:, 736:768]
    DMA("sp", cst, cst_d, [], ["cst"])
    cb = A.alloc(512, BF16)
    identb = cb[:, 0:128]
    onesb = cb[:, 128:256]
    triub = cb[:, 256:384]
    trilsb = cb[:, 384:512]
    CP("dve", identb, identf, ["cst"], ["cb"])
    CP("dve", triub, triu, ["cst"], ["cb"])
    CP("dve", trilsb, tril_strict, ["cst"], ["cb"])
    S.op("pool", lambda e: e.memset(onesb, 1.0), [], ["cb"])
    small = A.alloc(256, F32)
    epsc = small[:, 0:1]
    onec = small[:, 1:2]
    S.op("pool", lambda e: e.memset(epsc, EPS), [], ["epsc"])
    S.op("pool", lambda e: e.memset(onec, 1.0), [], ["epsc"])
    _sc = [2]

    def col(n=1):
        a = _sc[0]
        _sc[0] += n
        assert _sc[0] <= 256
        return small[:, a:a + n]

    modc = A.alloc(96, F32)
    A1c = A.alloc(16, F32)
    modrow_d = nc.dram_tensor("modrow_d", [1, 6 * D], F32).ap()

    mark = A.off
    ccol = A.alloc(16, F32)
    cact = A.alloc(16, BF16)
    tmp16 = A.alloc(16, F32)
    modrow = A.alloc(6 * D, F32)
    DMA("sp", ccol, ccol_d, [], ["ccol"])
    DMA("sp", modrow[0:1, :], bada_d, [], ["modrow_b"])
    ACT(tmp16, ccol, AF.Exp, ["ccol"], ["tmp16"], scale=-1.0)
    TS("dve", tmp16, tmp16, 1.0, None, ALU.add, None, ["tmp16"], ["tmp16"])
    RCP(tmp16, tmp16, ["tmp16"], ["tmp16"])
    TT("dve", cact, ccol, tmp16, ALU.mult, ["tmp16", "ccol"], ["cact"])
    wa = [A.alloc(16 * 512, BF16).rearrange("p (k n) -> p k n", k=16) for _ in range(2)]
    biasrow = modrow
    for jg in range(8):
        wt = wa[jg % 2]
        wk = "wa%d" % (jg % 2)
        S.dma("pool", (lambda wt=wt, jg=jg: (lambda e: e.dma_start(
            out=wt, in_=wada_d[:, jg * 512:(jg + 1) * 512].rearrange("(k p) n -> p k n", p=128))))(),
            [], [wk])
        pbi = jg % 2
        for k in range(16):
            MM(PB[pbi][0:1, :], cact[:, k:k + 1], wt[:, k, :], k == 0, k == 15, [wk, "cact"], [PK[pbi]])
        TT("dve", modrow[0:1, jg * 512:(jg + 1) * 512], PB[pbi][0:1, :], modrow[0:1, jg * 512:(jg + 1) * 512],
           ALU.add, ["modrow_b"], [PK[pbi], "modrow%d" % jg])
    allrow = ["modrow%d" % j for j in range(8)]
    if debug == "A":
        d = dout("mod", [1, 6 * D])
        DMA("sp", d, modrow[0:1, :], allrow, ["dbg"])
    for j in range(32):
        MM(PB[2][:, j:j + 1], modrow[0:1, j * 128:(j + 1) * 128], onec[0:1, 0:1], True, True, allrow + ["epsc"], [PK[2]])
    CP("dve", modc[:, 0:32], PB[2][:, 0:32], [], [PK[2], "modc"])
    cactp = col(16)
    cactb = cactp.bitcast(BF16)[:, 0:16]
    CP("dve", cactb, cact, ["cact"], ["cactb"])
    n1gc = A.alloc(16, F32)
    DMA("sp", n1gc, n1g_d, [], ["n1gc"])
    STT("dve", A1c, modc[:, 16:32], 1.0, n1gc, ALU.add, ALU.mult, ["modc", "n1gc"], ["A1c"])
    B1c = modc[:, 0:16]
    S.barrier()
    A.off = mark

    if debug == "A":
        d2 = dout("modc", [128, 96])
        DMA("sp", d2, modc, ["modc"], ["dbg"])
        S.emit()
        S.close()
        es.close()
        return nc, dbg

    markMix = A.off
    mixT = A.alloc(16 * S_LEN, BF16).rearrange("p (k t) -> p k t", k=16)
    markHT = A.off
    hT = A.alloc(16 * S_LEN, BF16).rearrange("p (k t) -> p k t", k=16)
    markB = A.off
    xt = [A.alloc(D, F32) for _ in range(2)]
    xn = [A.alloc(D, BF16) for _ in range(2)]
    tmod = [A.alloc(1024, F32) for _ in range(2)]
    ssq1 = col(16)
    rs1 = col(16)
    for i in range(NT):
        sl = i % 2
        DMA("sp", xt[sl], x_d[i * 128:(i + 1) * 128, :], [], ["xt%d" % sl])
        ACT(xn[sl], xt[sl], AF.Square, ["xt%d" % sl], ["xn%d" % sl, "ssq1_%d" % i], accum=ssq1[:, i:i + 1])
        rstd_col(rs1[:, i:i + 1], ssq1[:, i:i + 1], D, ["ssq1_%d" % i, "epsc"], ["rs1_%d" % i], "rs1t_%d" % i)
        ACT(xn[sl], xt[sl], AF.Identity, ["xt%d" % sl, "rs1_%d" % i], ["xn%d" % sl], scale=rs1[:, i:i + 1])
        for hf in range(2):
            for kk in range(8):
                k = hf * 8 + kk
                TR(PBH[hf][:, kk * 128:(kk + 1) * 128], xn[sl][:, k * 128:(k + 1) * 128], identb,
                   ["xn%d" % sl, "cb"], [PK[hf]])
            src = PBH[hf].rearrange("p (k t) -> p k t", k=8)
            tm = tmod[hf].rearrange("p (k t) -> p k t", k=8)
            a1 = A1c[:, hf * 8:(hf + 1) * 8].unsqueeze(2).to_broadcast([128, 8, 128])
            b1 = B1c[:, hf * 8:(hf + 1) * 8].unsqueeze(2).to_broadcast([128, 8, 128])
            TT("dve", tm, src, a1, ALU.mult, ["A1c"], [PK[hf], "tmod%d" % hf])
            TT("pool", hT[:, hf * 8:(hf + 1) * 8, i * 128:(i + 1) * 128], tm, b1, ALU.add,
               ["tmod%d" % hf, "modc"], ["hT%d" % i])
    hTall = ["hT%d" % i for i in range(NT)]
    S.barrier()
    A.off = markB
    if debug == "B":
        d = dout("hT", [128, 16 * S_LEN], BF16)
        DMA("sp", d, hT.rearrange("p k t -> p (k t)"), hTall, ["dbg"])
        S.emit(); S.close(); es.close()
        return nc, dbg

    markC = A.off
    wb = [A.alloc(16 * 512, BF16).rearrange("p (k n) -> p k n", k=16) for _ in range(2)]
    markC1 = A.off
    wsec = [wb[0], wb[1],
            mixT[:, 8:12, :].rearrange("p a t -> p (a t)").rearrange("p (k n) -> p k n", k=16),
            mixT[:, 12:16, :].rearrange("p a t -> p (a t)").rearrange("p (k n) -> p k n", k=16)]
    lbb = A.alloc(1024, F32)
    omlb = A.alloc(1024, F32)
    honb = A.alloc(128, F32)
    DMA("sp", lbb, lbl_d[0:1, :].partition_broadcast(128), [], ["lbb"])
    DMA("sp", omlb, lbl_d[1:2, :].partition_broadcast(128), [], ["omlb"])
    DMA("sp", honb, hon_d.partition_broadcast(128), [], ["honb"])
    TT("dve", omlb, omlb, lbb, ALU.subtract, ["lbb"], ["omlb"])
    ACT(omlb, omlb, AF.Exp, [], ["omlb"])
    TS("dve", omlb, omlb, 1.0, None, ALU.add, None, [], ["omlb"])
    RCP(lbb, omlb, ["omlb"], ["lbb"])
    TS("dve", omlb, lbb, -1.0, 1.0, ALU.mult, ALU.add, ["lbb"], ["omlb"])
    W4 = 512
    en_t, f_t, lf_t, kk_t = A.alloc(W4), A.alloc(W4), A.alloc(W4), A.alloc(W4)
    E1_t, E2_t = A.alloc(W4), A.alloc(W4)
    qin_t, qout_t, kin_t, kout_t, v_t = (A.alloc(W4, BF16) for _ in range(5))
    eng_t, sil_t = A.alloc(W4), A.alloc(W4)
    E3_t, E1n_t = eng_t, f_t
    trq_t, trk_t, tro_t = A.alloc(W4, BF16), A.alloc(W4, BF16), A.alloc(W4, BF16)
    am_t = A.alloc(W4, BF16)
    on_t = en_t
    og_t = A.alloc(W4, BF16)
    Sst = A.alloc(W4)
    Sbf = A.alloc(W4, BF16)
    deccol = col(4)
    ssqo = col(4)
    rso = col(4)
    QSC = 128.0 ** -0.5
    v4 = lambda t: t.rearrange("p (h d) -> p h d", h=4)
    triu4 = triu.unsqueeze(1).to_broadcast([128, 4, 128])
    for hgp in range(2):
        for sec in range(4):
            c0 = sec * 1024 + hgp * 512
            S.dma("pool", (lambda sec=sec, c0=c0: (lambda e: e.dma_start(
                out=wsec[sec], in_=win_d[:, c0:c0 + 512].rearrange("(k p) n -> p k n", p=128))))(), [], ["wsec%d" % sec])
        hs = slice(hgp * 512, (hgp + 1) * 512)
        for i in range(NT):
            tsl = slice(i * 128, (i + 1) * 128)

            def emit_proj(ii):
                for sec in range(4):
                    for k in range(16):
                        MM(PB[sec], hT[:, k, ii * 128:(ii + 1) * 128], wsec[sec][:, k, :], k == 0, k == 15,
                           ["wsec%d" % sec, "hT%d" % ii], [PK[sec]])
            if i == 0:
                emit_proj(0)
            hq, hf_, hi_, hg = PB[0], PB[1], PB[2], PB[3]
            ACT(en_t, hf_, AF.Exp, [], [PK[1], "en"], scale=-1.0)
            ACT(v_t, hi_, AF.Copy, [], [PK[2], "v"])
            ACT(eng_t, hg, AF.Exp, [], [PK[3], "eng"], scale=-1.0)
            ACT(en_t, en_t, AF.Ln, [], ["en"], bias=onec)
            ACT(en_t, en_t, AF.Exp, [], ["en"], scale=-1.0)
            TT("dve", f_t, en_t, omlb[:, hs], ALU.mult, ["en", "omlb"], ["f"])
            TT("dve", f_t, f_t, lbb[:, hs], ALU.add, ["lbb"], ["f"])
            ACT(lf_t, f_t, AF.Ln, ["f"], ["lf"])
            ACT(kk_t, f_t, AF.Identity, ["f"], ["kk"], scale=-1.0, bias=onec)
            ACT(eng_t, eng_t, AF.Ln, [], ["eng"], bias=onec)
            ACT(eng_t, eng_t, AF.Exp, [], ["eng"], scale=-1.0)
            TT("dve", sil_t, hg, eng_t, ALU.mult, ["eng"], [PK[3], "sil"])
            TT("pool", v4(sil_t), v4(sil_t), honb.unsqueeze(1).to_broadcast([128, 4, 128]), ALU.mult, ["honb"], ["sil"])
            MM(PB[4], M1, lf_t, True, True, ["cst", "lf"], [PK[4]])
            MM(PB[5], M2, lf_t, True, True, ["cst", "lf"], [PK[5]])
            MM(PB[6], triu, lf_t, True, True, ["cst", "lf"], [PK[6]])
            for hh in range(4):
                MM(PB[7][:, hh:hh + 1], lf_t[:, hh * 128:(hh + 1) * 128], onec, True, True, ["epsc", "lf"], [PK[7]])
            ACT(E1_t, PB[4], AF.Exp, [], [PK[4], "E1"])
            ACT(E1n_t, PB[4], AF.Exp, [], [PK[4], "f"], scale=-1.0)
            ACT(E2_t, PB[5], AF.Exp, [], [PK[5], "E2"])
            ACT(E3_t, PB[6], AF.Exp, [], [PK[6], "eng"])
            ACT(deccol, PB[7][:, 0:4], AF.Exp, [], [PK[7], "dec"])
            STT("dve", qin_t, hq, QSC, E1_t, ALU.mult, ALU.mult, ["E1"], [PK[0], "qin"])
            STT("dve", qout_t, hq, QSC, E3_t, ALU.mult, ALU.mult, ["eng"], [PK[0], "qout"])
            TT("pool", kin_t, kk_t, E1n_t, ALU.mult, ["kk", "f"], ["kin"])
            TT("pool", kout_t, kk_t, E2_t, ALU.mult, ["kk", "E2"], ["kout"])
            for hh in range(4):
                hsl = slice(hh * 128, (hh + 1) * 128)
                TR(PBH[4][:, hsl], qin_t[:, hsl], identb, ["qin", "cb"], [PK[4]])
                TR(PBH[5][:, hsl], kin_t[:, hsl], identb, ["kin", "cb"], [PK[5]])
                TR(PBH[6][:, hsl], qout_t[:, hsl], identb, ["qout", "cb"], [PK[6]])
            CP("dve", trq_t, PBH[4][:, 0:512], [], [PK[4], "trq"])
            ACT(trk_t, PBH[5][:, 0:512], AF.Copy, [], [PK[5], "trk"])
            CP("dve", tro_t, PBH[6][:, 0:512], [], [PK[6], "tro"])
            for hh in range(4):
                hsl = slice(hh * 128, (hh + 1) * 128)
                MM(PB[4][:, hsl], trk_t[:, hsl], trq_t[:, hsl], True, True, ["trk", "trq"], [PK[4]])
            if i + 1 < NT:
                emit_proj(i + 1)
            TT("dve", v4(am_t), v4(PB[4]), triu4, ALU.mult, ["cst"], [PK[4], "am"])
            for hh in range(4):
                hsl = slice(hh * 128, (hh + 1) * 128)
                if i == 0:
                    MM(PB[5][:, hsl], am_t[:, hsl], v_t[:, hsl], True, True, ["am", "v"], [PK[5]])
                else:
                    MM(PB[5][:, hsl], am_t[:, hsl], v_t[:, hsl], True, False, ["am", "v"], [PK[5]])
                    MM(PB[5][:, hsl], tro_t[:, hsl], Sbf[:, hsl], False, True, ["tro", "Sbf"], [PK[5]])
            for hh in range(4):
                hsl = slice(hh * 128, (hh + 1) * 128)
                MM(PB[6][:, hsl], kout_t[:, hsl], v_t[:, hsl], True, True, ["kout", "v"], [PK[6]])
            if i == 0:
                CP("dve", Sst, PB[6], [], [PK[6], "S"])
            else:
                TT("pool", v4(Sst), v4(Sst), deccol.unsqueeze(2).to_broadcast([128, 4, 128]), ALU.mult, ["dec"], ["S"])
                TT("dve", Sst, Sst, PB[6], ALU.add, [], [PK[6], "S"])
            if i < NT - 1:
                ACT(Sbf, Sst, AF.Copy, ["S"], ["Sbf"])
            ACT(on_t, PB[5], AF.Square, [], [PK[5], "en"])
            RED("dve", ssqo, v4(on_t), ALU.add, ["en"], ["ssqo"])
            ACT(rso, ssqo, AF.Ln, ["ssqo", "epsc"], ["rso"], scale=1.0 / 128, bias=epsc)
            ACT(rso, rso, AF.Exp, [], ["rso"], scale=-0.5)
            TT("dve", v4(on_t), v4(PB[5]), rso.unsqueeze(2).to_broadcast([128, 4, 128]), ALU.mult, ["rso"], [PK[5], "en"])
            TT("dve", og_t, on_t, sil_t, ALU.mult, ["en", "sil"], ["og"])
            for hh in range(4):
                hsl = slice(hh * 128, (hh + 1) * 128)
                TR(PBH[7][:, hsl], og_t[:, hsl], identb, ["og", "cb"], [PK[7]])
            ACT(mixT[:, hgp * 4:(hgp + 1) * 4, tsl], PBH[7][:, 0:512].rearrange("p (h t) -> p h t", h=4), AF.Copy, [],
                [PK[7], "mixT%d_%d" % (hgp, i)])
    S.barrier()
    A.off = markC1
    if debug == "C1":
        d = dout("mixT", [128, 16 * S_LEN], BF16)
        DMA("sp", d, mixT.rearrange("p k t -> p (k t)"), [], ["dbg"])
        S.emit(); S.close(); es.close()
        return nc, dbg

    A.off = markC + 16 * 512
    mla_d = nc.dram_tensor("mla_scratch", [128, 9 * S_LEN], BF16).ap()

    def alloc_mla():
        qaT = A.alloc(4 * S_LEN, BF16).rearrange("p (k t) -> p k t", k=4)
        kvaT = A.alloc(2 * S_LEN, BF16).rearrange("p (k t) -> p k t", k=2)
        return qaT, kvaT, A.alloc(S_LEN, BF16), A.alloc(S_LEN, BF16), A.alloc(S_LEN, BF16)
    mla0 = A.off
    qaT, kvaT, kpeT, kpesT, SQR = alloc_mla()
    mla_all = A.ap[:, mla0:mla0 + 9 * S_LEN]
    qagc = A.alloc(4, F32)
    kvgc = A.alloc(2, F32)
    qng = A.alloc(4, F32)
    kng = A.alloc(4, F32)
    DMA("sp", qagc, qag_d, [], ["qagc"])
    DMA("sp", kvgc, kvg_d, [], ["kvgc"])
    DMA("sp", qng, qng_d, [], ["qng"])
    DMA("sp", kng, kng_d, [], ["kng"])
    TT("dve", qng[:, 2:3], qng[:, 2:3], qng[:, 3:4], ALU.mult, [], ["qng"])
    TT("dve", kng[:, 2:3], kng[:, 2:3], kng[:, 3:4], ALU.mult, [], ["kng"])
    sq_t = [A.alloc(512, BF16) for _ in range(2)]
    rsb_t = [A.alloc(512, F32) for _ in range(2)]
    S.op("pool", lambda e: e.memset(SQR, 0.0), [], ["SQR"])
    wA, wB = wb[0], wb[0]
    S.dma("pool", lambda e: e.dma_start(out=wA, in_=win_d[:, 4096:4608].rearrange("(k p) n -> p k n", p=128)), [], ["wb0"])

    def lowrank(wt, wk, nch, dstT, gcol, gk, nfeat, dk):
        for tg in range(4):
            tsl = slice(tg * 512, (tg + 1) * 512)
            for c in range(nch):
                pbi = c % 2
                for k in range(16):
                    MM(PB[pbi], wt[:, k, c * 128:(c + 1) * 128], hT[:, k, tsl], k == 0, k == 15,
                       [wk] + hTall, [PK[pbi]])
                ACT(sq_t[pbi], PB[pbi], AF.Square, [], [PK[pbi], "sq%d" % pbi])
                TS("dve", dstT[:, c, tsl], PB[pbi], gcol[:, c:c + 1], None, ALU.mult, None, [gk],
                   [PK[pbi], dk + "%d_%d" % (c, tg)])
                MM(PB[2], onesb, sq_t[pbi], c == 0, c == nch - 1, ["cb", "sq%d" % pbi], [PK[2]])
            rb = rsb_t[tg % 2]
            rk = "rsb%d" % (tg % 2)
            ACT(rb, PB[2], AF.Ln, ["epsc"], [PK[2], rk], scale=1.0 / nfeat, bias=epsc)
            ACT(rb, rb, AF.Exp, [], [rk], scale=-0.5)
            for c in range(nch):
                TT("pool" if c % 2 else "dve", dstT[:, c, tsl], dstT[:, c, tsl], rb, ALU.mult, [rk],
                   [dk + "%d_%d" % (c, tg)])
    lowrank(wA, "wb0", 4, qaT, qagc, "qagc", 512, "qaT")
    S.op("pool", lambda e: e.memset(wB[:, :, 256:512], 0.0), [], ["wb0"])
    S.dma("pool", lambda e: e.dma_start(out=wB[:, :, 0:256], in_=win_d[:, 4608:4864].rearrange("(k p) n -> p k n", p=128)), [], ["wb0"])
    S.dma("pool", lambda e: e.dma_start(out=wB[:, :, 256:320], in_=win_d[:, 4864:4928].rearrange("(k p) n -> p k n", p=128)), [], ["wb0"])
    S.dma("pool", lambda e: e.dma_start(out=wB[:, :, 384:416], in_=win_d[:, 4896:4928].rearrange("(k p) n -> p k n", p=128)), [], ["wb0"])
    S.dma("pool", lambda e: e.dma_start(out=wB[:, :, 416:448], in_=win_d[:, 4864:4896].rearrange("(k p) n -> p k n", p=128)), [], ["wb0"])
    lowrank(wB, "wb0", 2, kvaT, kvgc, "kvgc", 256, "kvaT")
    for tg in range(4):
        tsl = slice(tg * 512, (tg + 1) * 512)
        for k in range(16):
            MM(PB[3], wB[:, k, 256:384], hT[:, k, tsl], k == 0, k == 15, ["wb0"] + hTall, [PK[3]])
        for k in range(16):
            MM(PB[4], wB[:, k, 384:512], hT[:, k, tsl], k == 0, k == 15, ["wb0"] + hTall, [PK[4]])
        ACT(SQR[0:64, tsl], PB[3][0:64, :], AF.Square, [], [PK[3], "SQR"])
        TS("dve", kpeT[0:64, tsl], PB[3][0:64, :], kng[0:64, 1:2], None, ALU.mult, None, ["kng"], [PK[3], "kpeT"])
        TS("dve", kpesT[0:64, tsl], PB[4][0:64, :], kng[0:64, 2:3], None, ALU.mult, None, ["kng"], [PK[4], "kpesT"])
    qaT_keys = ["qaT%d_%d" % (c, tg) for c in range(4) for tg in range(4)]
    kvaT_keys = ["kvaT%d_%d" % (c, tg) for c in range(2) for tg in range(4)]
    S.barrier()
    if debug == "C2":
        d = dout("qaT", [128, 4 * S_LEN], BF16)
        DMA("sp", d, qaT.rearrange("p k t -> p (k t)"), [], ["dbg"])
        d = dout("kvaT", [128, 2 * S_LEN], BF16)
        DMA("sp", d, kvaT.rearrange("p k t -> p (k t)"), [], ["dbg"])
        d = dout("kpeT", [64, S_LEN], BF16)
        DMA("sp", d, kpeT[0:64, :], [], ["dbg"])
        d = dout("kpesT", [64, S_LEN], BF16)
        DMA("sp", d, kpesT[0:64, :], [], ["dbg"])
        S.emit(); S.close(); es.close()
        return nc, dbg
    qngp = col(4)
    kngp = col(4)
    CP("dve", qngp, qng, ["qng"], ["qngp"])
    CP("dve", kngp, kng, ["kng"], ["kngp"])
    S.barrier()
    topD = mla0 + 9 * S_LEN
    A.off = markHT
    A.cap = mla0
    if debug == "D00":
        d = dout("qaT", [128, 4 * S_LEN], BF16)
        DMA("sp", d, qaT.rearrange("p k t -> p (k t)"), [], ["dbg"])
        d = dout("kvaT", [128, 2 * S_LEN], BF16)
        DMA("sp", d, kvaT.rearrange("p k t -> p (k t)"), [], ["dbg"])
        d = dout("kpeT", [64, S_LEN], BF16)
        DMA("sp", d, kpeT[0:64, :], [], ["dbg"])
        d = dout("kpesT", [64, S_LEN], BF16)
        DMA("sp", d, kpesT[0:64, :], [], ["dbg"])
        S.emit(); S.close(); es.close()
        return nc, dbg
    PI = float(np.pi)
    cosT = A.alloc(S_LEN, F32)
    sinT = A.alloc(S_LEN, F32)
    RT = A.alloc(S_LEN, BF16)
    aonb = A.alloc(128, F32)
    import os
    SK = os.environ.get("SKIPD", "")
    if "a" not in SK:
        DMA("sp", aonb, aon_d.partition_broadcast(128), [], ["aonb"])
    markD0 = A.off
    posi = A.alloc(S_LEN, I32)
    ang = A.alloc(S_LEN, F32)
    kq = A.alloc(S_LEN, F32)
    kqi = A.alloc(S_LEN, I32)
    msk = A.alloc(S_LEN, F32)
    invf = col(1)
    if "i" not in SK:
        DMA("sp", invf[0:64, :], invf_d, [], ["invf"])
    if "p" not in SK:
        DMA("sp", posi[0:64, :], pos_d.partition_broadcast(64), [], ["posi"])
    def dump_kva(tag):
        if debug == tag:
            S.barrier()
            d = dout("kvaT2", [128, 2 * S_LEN], BF16); DMA("sp", d, kvaT.rearrange("p k t -> p (k t)"), [], ["dbg"])
            S.emit(); S.close(); es.close()
            return True
        return False
    if dump_kva("X1"):
        return nc, dbg
    for (dst, shift, key) in ((sinT, 0.0, "sinT"), (cosT, PI / 2, "cosT")):
        a_, q_, qi_, m_ = ang[0:64, :], kq[0:64, :], kqi[0:64, :], msk[0:64, :]
        CP("dve", a_, posi[0:64, :], ["posi"], ["ang"])
        TS("dve", a_, a_, invf[0:64, :], shift, ALU.mult, ALU.add, ["invf"], ["ang"])
        TS("dve", q_, a_, 1.0 / (2 * PI), None, ALU.mult, None, ["ang"], ["kq"])
        CP("dve", qi_, q_, ["kq"], ["kqi"])
        CP("dve", q_, qi_, ["kqi"], ["kq"])
        if key == "sinT" and dump_kva("X2"):
            return nc, dbg
        STT("dve", a_, q_, -2 * PI, a_, ALU.mult, ALU.add, ["kq"], ["ang"])
        TS("dve", m_, a_, PI, None, ALU.is_gt, None, ["ang"], ["msk"])
        STT("dve", a_, m_, -2 * PI, a_, ALU.mult, ALU.add, ["msk"], ["ang"])
        TS("dve", m_, a_, -PI, None, ALU.is_lt, None, ["ang"], ["msk"])
        STT("dve", a_, m_, 2 * PI, a_, ALU.mult, ALU.add, ["msk"], ["ang"])
        if key == "sinT" and dump_kva("X3"):
            return nc, dbg
        ACT(dst[0:64, :], a_, AF.Sin, ["ang"], [key])
        if key == "sinT" and dump_kva("X4"):
            return nc, dbg
    TT("dve", ang[0:64, :], kpeT[0:64, :], cosT[0:64, :], ALU.mult, ["kpeT", "cosT"], ["ang"])
    TT("dve", kq[0:64, :], kpesT[0:64, :], sinT[0:64, :], ALU.mult, ["kpesT", "sinT"], ["kq"])
    TT("dve", RT[0:64, :], ang[0:64, :], kq[0:64, :], ALU.add, ["kq", "ang"], ["RT"])
    S.barrier()
    A.off = markD0
    if debug == "D0":
        d = dout("cosT", [64, S_LEN]); DMA("sp", d, cosT[0:64, :], [], ["dbg"])
        d = dout("sinT", [64, S_LEN]); DMA("sp", d, sinT[0:64, :], [], ["dbg"])
        d = dout("RT", [64, S_LEN], BF16); DMA("sp", d, RT[0:64, :], [], ["dbg"])
        d = dout("kvaT2", [128, 2 * S_LEN], BF16); DMA("sp", d, kvaT.rearrange("p k t -> p (k t)"), [], ["dbg"])
        print("offsets", markHT, mla0, markD0, A.off)
        S.emit(); S.close(); es.close()
        return nc, dbg

    wq_t = [A.alloc(4 * 384, BF16).rearrange("p (k n) -> p k n", k=4) for _ in range(2)]
    wkv_t = [A.alloc(2 * 256, BF16).rearrange("p (k n) -> p k n", k=2) for _ in range(2)]
    QTn_t = [A.alloc(S_LEN, BF16) for _ in range(2)]
    QTr_t = [A.alloc(S_LEN, BF16) for _ in range(2)]
    KTn_t = [A.alloc(S_LEN, BF16) for _ in range(2)]
    KTr_t = [A.alloc(S_LEN, BF16) for _ in range(2)]
    V_t = [A.alloc(16 * 130, BF16).rearrange("p (t v) -> p t v", t=16) for _ in range(2)]
    for sl in range(2):
        S.op("pool", (lambda sl=sl: (lambda e: e.memset(V_t[sl][:, :, 128:130], 1.0)))(), [], ["V%d" % sl])
        S.op("pool", (lambda sl=sl: (lambda e: e.memset(wq_t[sl], 0.0)))(), [], ["wq%d" % sl])
        S.op("pool", (lambda sl=sl: (lambda e: e.memset(QTr_t[sl], 0.0)))(), [], ["QTr%d" % sl])
        S.op("pool", (lambda sl=sl: (lambda e: e.memset(KTr_t[sl], 0.0)))(), [], ["KTr%d" % sl])
    lowtop = A.off
    A.off = topD
    A.cap = A.ap.shape[1]
    sqn_t = [A.alloc(512, BF16) for _ in range(2)]
    sqr_t = [A.alloc(512, BF16) for _ in range(2)]
    rb_t = [A.alloc(512, F32) for _ in range(2)]
    t1_t = [A.alloc(512, F32) for _ in range(2)]
    t2_t = [A.alloc(512, F32) for _ in range(2)]
    mrowt = A.alloc(256, F32)
    ob_t = [A.alloc(128, BF16) for _ in range(4)]
    A.off = lowtop
    A.cap = mla0
    PT_t = [A.alloc(512, BF16) for _ in range(2)]
    wad = A.alloc(16 * 256, BF16).rearrange("p (k n) -> p k n", k=16)
    ada_next = [0]

    ada_pending = [None]

    def ada_group():
        if ada_pending[0] is not None:
            c0 = ada_pending[0]
            for k in range(16):
                MM(PB[7][0:1, 0:256], cactb[:, k:k + 1], wad[:, k, :], k == 0, k == 15, ["wad", "cactb"], [PK[7]])
            TT("dve", mrowt[0:1, :], PB[7][0:1, 0:256], mrowt[0:1, :], ALU.add, [], [PK[7], "mrowt"])
            DMA("sp", modrow_d[0:1, c0:c0 + 256], mrowt[0:1, :], ["mrowt"], ["modrow_d"])
            ada_pending[0] = None
        g = ada_next[0]
        if g >= 32:
            return
        ada_next[0] += 1
        c0 = 4096 + g * 256
        S.dma("pool", (lambda c0=c0: (lambda e: e.dma_start(
            out=wad, in_=wada_d[:, c0:c0 + 256].rearrange("(k p) n -> p k n", p=128))))(), [], ["wad"])
        DMA("sp", mrowt[0:1, :], bada_d[0:1, c0:c0 + 256], [], ["mrowt"])
        ada_pending[0] = c0
    junk3 = A.alloc(128, F32)
    ocol = col(16)
    SM_SCALE = 192.0 ** -0.5
    nev = 0
    npt = 0
    for h in range(8):
        sl = h % 2
        s_ = str(sl)
        wq, wkv = wq_t[sl], wkv_t[sl]
        QTn, QTr, KTn, KTr, V = QTn_t[sl], QTr_t[sl], KTn_t[sl], KTr_t[sl], V_t[sl]
        b0 = h * 192
        for (a, bnd, c0, n) in ((0, 128, b0, 128), (128, 192, b0 + 128, 64), (256, 288, b0 + 160, 32), (288, 320, b0 + 128, 32)):
            S.dma("pool", (lambda wq=wq, a=a, bnd=bnd, c0=c0, n=n: (lambda e: e.dma_start(
                out=wq[:, :, a:bnd], in_=wqu_d[:, c0:c0 + n].rearrange("(k p) n -> p k n", p=128))))(), [], ["wq" + s_])
        S.dma("pool", (lambda wkv=wkv, h=h: (lambda e: e.dma_start(
            out=wkv, in_=wkv_d[:, h * 256:(h + 1) * 256].rearrange("(k p) n -> p k n", p=128))))(), [], ["wkv" + s_])
        def proj_body(tg, sl=sl, s_=s_, wq=wq, wkv=wkv, QTn=QTn, QTr=QTr, KTn=KTn, KTr=KTr, V=V):
            tsl = slice(tg * 512, (tg + 1) * 512)
            g2 = tg % 2
            gs = str(g2)
            bA, bB, bC, bD = (0, 1, 2, 3) if g2 == 0 else (4, 5, 6, 7)
            for k in range(4):
                MM(PB[bA], wq[:, k, 0:128], qaT[:, k, tsl], k == 0, k == 3, ["wq" + s_] + qaT_keys, [PK[bA]])
            for k in range(4):
                MM(PB[bB], wq[:, k, 128:256], qaT[:, k, tsl], k == 0, k == 3, ["wq" + s_] + qaT_keys, [PK[bB]])
            for k in range(4):
                MM(PB[bC], wq[:, k, 256:384], qaT[:, k, tsl], k == 0, k == 3, ["wq" + s_] + qaT_keys, [PK[bC]])
            yield
            ACT(sqn_t[g2], PB[bA], AF.Square, [], [PK[bA], "sqn" + gs])
            ACT(sqr_t[g2], PB[bB], AF.Square, [], [PK[bB], "sqr" + gs])
            yield
            MM(PB[bD], onesb, sqn_t[g2], True, False, ["cb", "sqn" + gs], [PK[bD]])
            MM(PB[bD], onesb, sqr_t[g2], False, True, ["cb", "sqr" + gs], [PK[bD]])
            yield
            rb = rb_t[g2]
            ACT(rb, PB[bD], AF.Ln, ["epsc"], [PK[bD], "rb" + gs], scale=1.0 / 192, bias=epsc)
            ACT(rb, rb, AF.Exp, [], ["rb" + gs], scale=-0.5)
            yield
            STT("dve", QTn[:, tsl], PB[bA], qngp[:, 0:1], rb, ALU.mult, ALU.mult, ["qngp", "rb" + gs], [PK[bA], "QTn" + s_])
            STT("dve", t1_t[g2][0:64, :], PB[bB][0:64, :], qngp[0:64, 1:2], cosT[0:64, tsl], ALU.mult, ALU.mult,
                ["qngp", "cosT"], [PK[bB], "t1" + gs])
            STT("dve", t2_t[g2][0:64, :], PB[bC][0:64, :], qngp[0:64, 2:3], sinT[0:64, tsl], ALU.mult, ALU.mult,
                ["qngp", "sinT"], [PK[bC], "t2" + gs])
            yield
            TT("pool", t1_t[g2][0:64, :], t1_t[g2][0:64, :], t2_t[g2][0:64, :], ALU.add, ["t2" + gs], ["t1" + gs])
            TT("pool", QTr[0:64, tsl], t1_t[g2][0:64, :], rb[0:64, :], ALU.mult, ["t1" + gs, "rb" + gs], ["QTr" + s_])
            for k in range(2):
                MM(PB[bA], wkv[:, k, 0:128], kvaT[:, k, tsl], k == 0, k == 1, ["wkv" + s_] + kvaT_keys, [PK[bA]])
            for j in range(4):
                t = tg * 4 + j
                for k in range(2):
                    MM(PB[bB][:, j * 128:(j + 1) * 128], kvaT[:, k, t * 128:(t + 1) * 128], wkv[:, k, 128:256],
                       k == 0, k == 1, ["wkv" + s_] + kvaT_keys, [PK[bB]])
            yield
            ACT(sqn_t[g2], PB[bA], AF.Square, [], [PK[bA], "sqn" + gs])
            ACT(V[:, tg * 4:(tg + 1) * 4, 0:128], PB[bB].rearrange("p (t v) -> p t v", t=4), AF.Copy, [],
                [PK[bB], "V" + s_])
            yield
            MM(PB[bD], onesb, sqn_t[g2], True, False, ["cb", "sqn" + gs], [PK[bD]])
            MM(PB[bD], onesb, SQR[:, tsl], False, True, ["cb", "SQR"], [PK[bD]])
            yield
            ACT(rb, PB[bD], AF.Ln, ["epsc"], [PK[bD], "rb" + gs], scale=1.0 / 192, bias=epsc)
            ACT(rb, rb, AF.Exp, [], ["rb" + gs], scale=-0.5)
            yield
            STT("dve", KTn[:, tsl], PB[bA], kngp[:, 0:1], rb, ALU.mult, ALU.mult, ["kngp", "rb" + gs], [PK[bA], "KTn" + s_])
            TT("pool", KTr[0:64, tsl], RT[0:64, tsl], rb[0:64, :], ALU.mult, ["RT", "rb" + gs], ["KTr" + s_])
            yield
        pipeline([proj_body(tg) for tg in range(4)], 2)
        steps = [(G, kt) for G in range(4) for kt in range(4 * G + 4)]

        def geom(G, kt):
            j0 = max(0, kt - 4 * G)
            return j0, (4 - j0) * 128, (4 * G + j0) * 128

        def emit_ST(n):
            G, kt = steps[n]
            j0, ncol, q0 = geom(G, kt)
            sb = n % 2
            MM(PB[sb][:, 0:ncol], KTn[:, kt * 128:(kt + 1) * 128], QTn[:, q0:q0 + ncol], True, False,
               ["KTn" + s_, "QTn" + s_], [PK[sb]])
            MM(PB[sb][:, 0:ncol], KTr[:, kt * 128:(kt + 1) * 128], QTr[:, q0:q0 + ncol], False, True,
               ["KTr" + s_, "QTr" + s_], [PK[sb]])
        emit_ST(0)
        pending = []
        for n in range(len(steps)):
            G, kt = steps[n]
            j0, ncol, q0 = geom(G, kt)
            sb = n % 2
            pt = PT_t[npt % 2]
            pk = "PT%d" % (npt % 2)
            if n % 10 == 5:
                ada_group()
            npt += 1
            if n + 1 < len(steps):
                emit_ST(n + 1)
            ACT(pt[:, 0:ncol], PB[sb][:, 0:ncol], AF.Exp, [], [PK[sb], pk], scale=SM_SCALE)
            if kt >= 4 * G:
                TT("pool", pt[:, 0:128], pt[:, 0:128], triub, ALU.mult, ["cb"], [pk])
            for j in range(j0, 4):
                qt = 4 * G + j
                MM(PB[2 + j][:, 0:129], pt[:, (j - j0) * 128:(j - j0 + 1) * 128], V[:, kt, 0:129],
                   kt == 0, kt == qt, [pk, "V" + s_], [PK[2 + j]])
                if kt == qt:
                    e2 = nev % 4
                    nev += 1

                    def mk(e2=e2, j=j, qt=qt, h=h):
                        es_ = str(e2)
                        O = PB[2 + j]
                        ssq = ocol[:, e2 * 4:e2 * 4 + 1]
                        tt1 = ocol[:, e2 * 4 + 1:e2 * 4 + 2]
                        tt2 = ocol[:, e2 * 4 + 2:e2 * 4 + 3]
                        den = ocol[:, e2 * 4 + 3:e2 * 4 + 4]

                        def s1():
                            ACT(junk3, O[:, 0:128], AF.Square, [], [PK[2 + j], "junk3", "ossq" + es_], accum=ssq)
                            CP("dve", den, O[:, 128:129], [], [PK[2 + j], "oden" + es_])
                            STT("dve", tt1, den, EPS, den, ALU.mult, ALU.mult, ["oden" + es_], ["ott1" + es_])
                            STT("dve", tt2, ssq, 1.0 / 128, tt1, ALU.mult, ALU.add, ["ossq" + es_, "ott1" + es_], ["ott2" + es_])

                        def s2():
                            ACT(tt2, tt2, AF.Ln, [], ["ott2" + es_])
                            ACT(tt2, tt2, AF.Exp, [], ["ott2" + es_], scale=-0.5)

                        def s3():
                            STT("dve", ob_t[e2], O[:, 0:128], tt2, aonb, ALU.mult, ALU.mult, ["ott2" + es_, "aonb"],
                                [PK[2 + j], "ob" + es_])
                            TR(PBH[6][:, 0:128], ob_t[e2], identb, ["ob" + es_, "cb"], [PK[6]])

                        def s4():
                            ACT(mixT[:, 8 + h, qt * 128:(qt + 1) * 128], PBH[6][:, 0:128], AF.Copy, [],
                                [PK[6], "mixT%d_%d" % (8 + h, qt)])
                        return [s1, s2, s3, s4]
                    st = mk()
                    for d_, fn in enumerate(st):
                        pending.append([n + d_, fn])
            last_of_group = (kt == 4 * G + 3)
            keep = []
            for item in pending:
                if item[0] <= n or last_of_group and False:
                    item[1]()
                else:
                    keep.append(item)
            pending[:] = keep
            if last_of_group:
                for item in sorted(pending, key=lambda it: it[0]):
                    item[1]()
                pending[:] = []
    ada_group()
    assert ada_next[0] == 32 and ada_pending[0] is None
    S.barrier()
    A.off = markHT
    A.cap = A.ap.shape[1]
    if debug == "D":
        d = dout("mixT", [128, 16 * S_LEN], BF16)
        DMA("sp", d, mixT.rearrange("p k t -> p (k t)"), [], ["dbg"])
        S.emit(); S.close(); es.close()
        return nc, dbg

    markE = A.off
    wo = A.alloc(16 * D, BF16).rearrange("p (k n) -> p k n", k=16)
    g1b = A.alloc(D, F32)
    xt = [A.alloc(D, F32) for _ in range(2)]
    tmpe = [A.alloc(512, F32) for _ in range(2)]
    for n in range(4):
        S.dma("pool", (lambda n=n: (lambda e: e.dma_start(
            out=wo[:, :, n * 512:(n + 1) * 512],
            in_=wout_d[:, n * 512:(n + 1) * 512].rearrange("(k p) n -> p k n", p=128))))(), [], ["wo%d" % n])
    DMA("sp", g1b, modrow_d[0:1, 2 * D:3 * D].partition_broadcast(128), ["modrow_d"], ["g1b"])
    S.op("pool", lambda e: e.memset(tmpe[0], 0.0), [], ["tmpe0"])
    for n in range(4):
        DMA("sp", out_d[S_LEN:S_LEN + 128, n * 512:(n + 1) * 512], tmpe[0], ["tmpe0"], ["outz"])
    mix_keys = []
    for i in range(NT):
        sl = i % 2
        DMA("sp", xt[sl], x_d[i * 128:(i + 1) * 128, :], [], ["xt%d" % sl])
        for n in range(4):
            pbi = (i * 4 + n) % 4
            for k in range(16):
                MM(PB[pbi], mixT[:, k, i * 128:(i + 1) * 128], wo[:, k, n * 512:(n + 1) * 512], k == 0, k == 15,
                   ["wo%d" % n], [PK[pbi]])
            tp = tmpe[n % 2]
            TT("dve", tp, PB[pbi], g1b[:, n * 512:(n + 1) * 512], ALU.mult, ["g1b"], [PK[pbi], "tmpe%d" % (n % 2)])
            TT("pool", xt[sl][:, n * 512:(n + 1) * 512], xt[sl][:, n * 512:(n + 1) * 512], tp, ALU.add,
               ["tmpe%d" % (n % 2)], ["xt%d" % sl])
        DMA("sp", out_d[i * 128:(i + 1) * 128, :], xt[sl], ["xt%d" % sl], ["out"])
    S.barrier()
    A.off = markMix
    if debug == "E1":
        S.emit(); S.close(); es.close()
        return nc, dbg

    h2_d = nc.dram_tensor("h2_scratch", [S_LEN, D], BF16).ap()
    A2b = A.alloc(D, F32)
    B2b = A.alloc(D, F32)
    g2b = A.alloc(D, F32)
    LG = A.alloc(16 * 36, F32).rearrange("p (t n) -> p t n", t=16)
    IDXW = A.alloc(64, I32)
    zt = A.alloc(D + 32, BF16)
    S.op("pool", lambda e: e.memset(zt, 0.0), [], ["zt"])
    for j in range(NBLK):
        DMA("pool", xbuf_d[j * 128:(j + 1) * 128, :], zt, ["zt"], ["xbz%d" % j])
    xbz_keys = ["xbz%d" % j for j in range(NBLK)]
    markE2 = A.off
    n2gb = A.alloc(D, F32)
    wgr = A.alloc(16 * 36, F32).rearrange("p (k n) -> p k n", k=16)
    bgrb = A.alloc(36, F32)
    DMA("sp", A2b, modrow_d[0:1, 4 * D:5 * D].partition_broadcast(128), ["modrow_d"], ["A2b"])
    DMA("sp", B2b, modrow_d[0:1, 3 * D:4 * D].partition_broadcast(128), ["modrow_d"], ["B2b"])
    DMA("sp", g2b, modrow_d[0:1, 5 * D:6 * D].partition_broadcast(128), ["modrow_d"], ["g2b"])
    DMA("sp", n2gb, n2g_d.partition_broadcast(128), [], ["n2gb"])
    DMA("sp", wgr, wgr_d.rearrange("(k p) n -> p k n", p=128), [], ["wgr"])
    DMA("sp", bgrb, bgr_d.partition_broadcast(128), [], ["bgrb"])
    STT("dve", A2b, A2b, 1.0, n2gb, ALU.add, ALU.mult, ["n2gb"], ["A2b"])
    xt = [A.alloc(D, F32) for _ in range(2)]
    h2f_t = [A.alloc(D, F32) for _ in range(2)]
    h2b = [A.alloc(D, BF16) for _ in range(2)]
    h2T_t = [A.alloc(16 * 128, F32).rearrange("p (k t) -> p k t", k=16) for _ in range(2)]
    ssq2 = col(16)
    rs2 = col(16)

    def e2_body(i):
        sl = i % 2
        s_ = str(sl)
        h2f, h2T = h2f_t[sl], h2T_t[sl]
        b0 = 4 * sl
        DMA("sp", xt[sl], out_d[i * 128:(i + 1) * 128, :], ["out"], ["xt" + s_])
        ACT(h2f, xt[sl], AF.Square, ["xt" + s_], ["h2f" + s_, "ssq2_%d" % i], accum=ssq2[:, i:i + 1])
        yield
        rstd_col(rs2[:, i:i + 1], ssq2[:, i:i + 1], D, ["ssq2_%d" % i, "epsc"], ["rs2_%d" % i], "rs2t_%d" % i)
        yield
        STT("dve", h2f, xt[sl], rs2[:, i:i + 1], A2b, ALU.mult, ALU.mult, ["rs2_%d" % i, "A2b"], ["h2f" + s_])
        yield
        TT("dve", h2f, h2f, B2b, ALU.add, ["B2b"], ["h2f" + s_])
        yield
        ACT(h2b[sl], h2f, AF.Copy, ["h2f" + s_], ["h2b" + s_])
        for q in range(4):
            for kk in range(4):
                k = q * 4 + kk
                TR(PB[b0 + q][:, kk * 128:(kk + 1) * 128], h2f[:, k * 128:(k + 1) * 128], identf, ["h2f" + s_, "cst"],
                   [PK[b0 + q]])
        yield
        DMA("sp", h2_d[i * 128:(i + 1) * 128, :], h2b[sl], ["h2b" + s_], ["h2_d%d" % i])
        for q in range(4):
            if q % 2:
                CP("dve", h2T[:, q * 4:(q + 1) * 4, :], PB[b0 + q].rearrange("p (k t) -> p k t", k=4), [],
                   [PK[b0 + q], "h2T" + s_])
            else:
                ACT(h2T[:, q * 4:(q + 1) * 4, :], PB[b0 + q].rearrange("p (k t) -> p k t", k=4), AF.Copy, [],
                    [PK[b0 + q], "h2T" + s_])
        yield
        for k in range(16):
            MM(PB[b0][:, 0:36], h2T[:, k, :], wgr[:, k, :], k == 0, k == 15, ["h2T" + s_, "wgr"], [PK[b0]])
        yield
        TT("dve", LG[:, i, :], PB[b0][:, 0:36], bgrb, ALU.add, ["bgrb"], [PK[b0], "LG"])
        yield
    pipeline([e2_body(i) for i in range(NT)], 2)
    S.barrier()
    A.off = markE2
    if debug == "E2":
        d = dout("LG", [128, 16 * 36]); DMA("sp", d, LG.rearrange("p t n -> p (t n)"), [], ["dbg"])
        d = dout("h2", [S_LEN, D], BF16); DMA("sp", d, h2_d, [], ["dbg"])
        S.emit(); S.close(); es.close()
        return nc, dbg

    BIG = 1.0e30

    def T3(n, m):
        t = A.alloc(16 * n * m, F32)
        return t.rearrange("p (t n) -> p t n", t=16) if m == 1 else t.rearrange("p (t n m) -> p t n m", t=16, n=n)
    def bc(ap2, shape):
        v = ap2
        for ax in range(2, len(shape)):
            v = v.unsqueeze(ax)
        return v.to_broadcast(shape)
    G = LG[:, :, 0:4]
    EL = LG[:, :, 4:36]
    gmax = A.alloc(16, F32)
    RED("dve", gmax, G, ALU.max, ["LG"], ["gmax"])
    gone = T3(4, 1)
    TT("dve", gone, G, bc(gmax, [128, 16, 4]), ALU.is_equal, ["gmax", "LG"], ["gone"])
    gd = T3(4, 1)
    TT("dve", gd, G, bc(gmax, [128, 16, 4]), ALU.subtract, ["gmax", "LG"], ["gd"])
    ACT(gd, gd, AF.Exp, [], ["gd"])
    pg = A.alloc(16, F32)
    RED("dve", pg, gd, ALU.add, ["gd"], ["pg"])
    RCP(pg, pg, [], ["pg"])
    pen = T3(4, 1)
    TS("dve", pen, gone, -1.0, BIG, ALU.add, ALU.mult, ["gone"], ["pen"])
    EM = T3(32, 1)
    TT("dve", EM.rearrange("p t (g e) -> p t g e", g=4), EL.rearrange("p t (g e) -> p t g e", g=4),
       pen.unsqueeze(3).to_broadcast([128, 16, 4, 8]), ALU.add, ["pen", "LG"], ["EM"])
    v1 = A.alloc(16, F32)
    RED("dve", v1, EM, ALU.max, ["EM"], ["v1"])
    M1 = T3(32, 1)
    TT("dve", M1, EM, bc(v1, [128, 16, 32]), ALU.is_equal, ["EM", "v1"], ["M1"])
    EM2 = T3(32, 1)
    STT("dve", EM2, M1, -BIG, EM, ALU.mult, ALU.add, ["M1", "EM"], ["EM2"])
    v2 = A.alloc(16, F32)
    RED("dve", v2, EM2, ALU.max, ["EM2"], ["v2"])
    M2 = T3(32, 1)
    TT("dve", M2, EM2, bc(v2, [128, 16, 32]), ALU.is_equal, ["EM2", "v2"], ["M2"])
    e21 = A.alloc(16, F32)
    TT("dve", e21, v2, v1, ALU.subtract, ["v1", "v2"], ["e21"])
    ACT(e21, e21, AF.Exp, [], ["e21"])
    w1 = A.alloc(16, F32)
    w2 = A.alloc(16, F32)
    TS("dve", w1, e21, 1.0, None, ALU.add, None, ["e21"], ["w1"])
    RCP(w1, w1, [], ["w1"])
    TT("dve", w1, w1, pg, ALU.mult, ["pg"], ["w1"])
    TT("dve", w2, w1, e21, ALU.mult, ["w1", "e21"], ["w2"])
    Mb = A.alloc(16 * 32, BF16).rearrange("p (t n) -> p t n", t=16)
    TT("dve", Mb, M1, M2, ALU.add, ["M1", "M2"], ["Mb"])
    for i in range(NT):
        MM(PB[0][:, i * 32:(i + 1) * 32], trilsb, Mb[:, i, :], True, i == 0, ["cb", "Mb"], [PK[0]])
        for j in range(i):
            MM(PB[0][:, i * 32:(i + 1) * 32], onesb, Mb[:, j, :], False, j == i - 1, ["cb", "Mb"], [PK[0]])
    POS = T3(32, 1)
    CP("dve", POS.rearrange("p t n -> p (t n)"), PB[0], [], [PK[0], "POS"])
    for j in range(NT):
        MM(PB[1][:, 0:32], onesb, Mb[:, j, :], j == 0, j == NT - 1, ["cb", "Mb"], [PK[1]])
    cnt = A.alloc(32, F32)
    CP("dve", cnt, PB[1][:, 0:32], [], [PK[1], "cnt"])
    cmp1 = A.alloc(32 * 16, F32).rearrange("p (e m) -> p e m", e=32)
    TT("dve", cmp1, cnt.unsqueeze(2).to_broadcast([128, 32, 16]), thr16.unsqueeze(1).to_broadcast([128, 32, 16]),
       ALU.is_gt, ["cnt", "cst"], ["cmp1"])
    padded = A.alloc(32, F32)
    RED("dve", padded, cmp1, ALU.add, ["cmp1"], ["padded"])
    TS("dve", padded, padded, 128.0, None, ALU.mult, None, [], ["padded"])
    cs = [A.alloc(32, F32) for _ in range(2)]
    CP("dve", cs[0], padded, ["padded"], ["cs0"])
    cur = 0
    for sh in (1, 2, 4, 8, 16):
        nx = 1 - cur
        CP("dve", cs[nx][:, 0:sh], cs[cur][:, 0:sh], ["cs%d" % cur], ["cs%d" % nx])
        TT("dve", cs[nx][:, sh:32], cs[cur][:, sh:32], cs[cur][:, 0:32 - sh], ALU.add, ["cs%d" % cur], ["cs%d" % nx])
        cur = nx
    pad_end = cs[cur]
    pek = "cs%d" % cur
    pad_start = A.alloc(32, F32)
    TT("dve", pad_start, pad_end, padded, ALU.subtract, [pek, "padded"], ["pad_start"])
    cmp2 = A.alloc(64 * 32, F32).rearrange("p (j e) -> p j e", j=64)
    TT("dve", cmp2, pad_end.unsqueeze(1).to_broadcast([128, 64, 32]), thr64.unsqueeze(2).to_broadcast([128, 64, 32]),
       ALU.is_le, [pek, "cst"], ["cmp2"])
    blke = A.alloc(64, F32)
    RED("dve", blke, cmp2, ALU.add, ["cmp2"], ["blke"])
    TS("dve", blke, blke, 31.0, None, ALU.min, None, [], ["blke"])
    same = A.alloc(64, F32)
    S.op("pool", lambda e: e.memset(same, 0.0), [], ["same"])
    TT("dve", same[:, 2:64], blke[:, 2:64], blke[:, 0:62], ALU.is_equal, ["blke"], ["same"])
    TS("dve", blke, blke, 128.0, iota_p, ALU.mult, ALU.add, ["cst"], ["blke"])
    STT("dve", blke, same, 8192.0, blke, ALU.mult, ALU.add, ["same"], ["blke"])
    CP("dve", IDXW, blke, ["blke"], ["IDXW"])
    Tt = T3(32, 1)
    TT("dve", Tt, POS, pad_start.unsqueeze(1).to_broadcast([128, 16, 32]), ALU.add, ["POS", "pad_start"], ["Tt"])
    prod = T3(32, 1)
    dstf = A.alloc(32, F32).rearrange("p (t k) -> p t k", t=16)
    TT("dve", prod, M1, Tt, ALU.mult, ["M1", "Tt"], ["prod"])
    RED("dve", dstf[:, :, 0], prod, ALU.add, ["prod"], ["dstf"])
    TT("dve", prod, M2, Tt, ALU.mult, ["M2", "Tt"], ["prod"])
    RED("dve", dstf[:, :, 1], prod, ALU.add, ["prod"], ["dstf"])
    DI = A.alloc(32, I32)
    CP("dve", DI, dstf.rearrange("p t k -> p (t k)"), ["dstf"], ["DI"])
    tokf = A.alloc(16, F32)
    TS("dve", tokf, thr16, iota_p, None, ALU.add, None, ["cst"], ["tokf"])
    with nc.allow_non_contiguous_dma(reason="64B meta tails"):
        pass
    DMA("sp", xbuf_d[:, D:D + 32], metai_d, xbz_keys, ["xbuf"])
    if debug == "E3":
        S.barrier()
        d = dout("LG", [128, 16 * 36]); DMA("sp", d, LG.rearrange("p t n -> p (t n)"), [], ["dbg"])
        d = dout("IDXW", [128, 64], I32); DMA("sp", d, IDXW, [], ["dbg"])
        d = dout("DI", [128, 32], I32); DMA("sp", d, DI, [], ["dbg"])
        d = dout("w1", [128, 16]); DMA("sp", d, w1, [], ["dbg"])
        d = dout("w2", [128, 16]); DMA("sp", d, w2, [], ["dbg"])
        d = dout("cnt", [128, 32]); DMA("sp", d, cnt, [], ["dbg"])
        d = dout("M1", [128, 512]); DMA("sp", d, M1.rearrange("p t n -> p (t n)"), [], ["dbg"])
        d = dout("M2", [128, 512]); DMA("sp", d, M2.rearrange("p t n -> p (t n)"), [], ["dbg"])
        S.emit(); S.close(); es.close()
        return nc, dbg
    hbx = [[A.alloc(D + 32, BF16) for _ in range(2)] for _ in range(2)]
    whi = [A.alloc(16, BF16) for _ in range(2)]
    wlo = [A.alloc(16, BF16) for _ in range(2)]
    whf = A.alloc(16, F32)
    for k in range(2):
        wk_ = (w1, w2)[k]
        CP("dve", whi[k], wk_, ["w1", "w2"], ["whl"])
        CP("dve", whf, whi[k], ["whl"], ["whf"])
        TT("dve", whf, wk_, whf, ALU.subtract, ["w1", "w2"], ["whf"])
        CP("dve", wlo[k], whf, ["whf"], ["whl"])
    for k in range(2):
        for sl in range(2):
            S.op("pool", (lambda k=k, sl=sl: (lambda e: e.memset(hbx[k][sl][:, D:D + 32], 0.0)))(), [], ["hb%d_%d" % (k, sl)])
    for i in range(NT):
        sl = i % 2
        for k in range(2):
            c = i * 2 + k
            hb = hbx[k][sl]
            hk = "hb%d_%d" % (k, sl)
            DMA("sp", hb[:, 0:D], h2_d[i * 128:(i + 1) * 128, :], ["h2_d%d" % i], [hk])
            tailI = hb[:, D:D + 32].bitcast(I32)
            CP("dve", tailI[:, 0:1], tokf[:, i:i + 1], ["tokf"], [hk])
            CP("dve", hb[:, D + 2:D + 3], whi[k][:, i:i + 1], ["whl"], [hk])
            CP("dve", hb[:, D + 3:D + 4], wlo[k][:, i:i + 1], ["whl"], [hk])
            S.dma("pool", (lambda hb=hb, c=c: (lambda e: e.indirect_dma_start(
                out=xbuf_d, out_offset=bass.IndirectOffsetOnAxis(ap=DI[:, c:c + 1], axis=0),
                in_=hb, in_offset=None, bounds_check=breg(e, NSLOT - 1), oob_is_err=False)))(),
                [hk, "DI"], ["xbuf"])
    S.barrier()
    A.off = markE2
    markF = A.off

    wg_t = [A.alloc(16 * 512, BF16) for _ in range(2)]
    wu_t = [A.alloc(16 * 512, BF16) for _ in range(2)]
    wd_t = [A.alloc(4 * D, BF16) for _ in range(2)]
    xb_t = [A.alloc(D + 32, BF16) for _ in range(2)]
    XT_t = [A.alloc(16 * 128, BF16).rearrange("p (k s) -> p k s", k=16) for _ in range(2)]
    en_f = [A.alloc(512, F32) for _ in range(2)]
    tg_f = [A.alloc(512, F32) for _ in range(2)]
    act_b = [A.alloc(512, BF16) for _ in range(2)]
    actT = [A.alloc(4 * 128, BF16).rearrange("p (k s) -> p k s", k=4) for _ in range(2)]
    Y_t = [A.alloc(D, F32) for _ in range(2)]
    wc_t = [col(1) for _ in range(2)]

    def pf(j, which):
        sl = j % 2
        s_ = str(sl)
        lst = []
        if "g" in which:
            lst += [(wg_t[sl], wg_d, "wg"), (wu_t[sl], wu_d, "wu")]
        if "d" in which:
            lst += [(wd_t[sl], wd_d, "wd")]
        for (dst, src, key) in lst:
            S.dma("pool", (lambda dst=dst, src=src, j=j: (lambda e: e.indirect_dma_start(
                out=dst, out_offset=None, in_=src, in_offset=bass.IndirectOffsetOnAxis(ap=IDXW[:, j:j + 1], axis=0),
                bounds_check=breg(e, 32 * 128 - 1), oob_is_err=False)))(), ["IDXW"], [key + s_])
        if "d" in which:
            DMA("sp", xb_t[sl], xbuf_d[j * 128:(j + 1) * 128, :], ["xbuf"], ["xb" + s_])

    def stage_A(j):
        sl = j % 2
        s_ = str(sl)
        bG, bU = 2 * sl, 2 * sl + 1
        xb, XT = xb_t[sl], XT_t[sl]
        xbv = xb[:, 0:D].rearrange("p (f k) -> p k f", k=16)
        for hf, bb in ((0, 4), (1, 5)):
            for kk in range(8):
                TR(PBH[bb][:, kk * 128:(kk + 1) * 128], xbv[:, hf * 8 + kk, :], identb, ["xb" + s_, "cb"], [PK[bb]])
        CP("dve", XT[:, 0:8, :], PBH[4].rearrange("p (k s) -> p k s", k=8), [], [PK[4], "XT" + s_])
        ACT(XT[:, 8:16, :], PBH[5].rearrange("p (k s) -> p k s", k=8), AF.Copy, [], [PK[5], "XT" + s_])
        wg, wu = wg_t[sl], wu_t[sl]
        for k in range(16):
            MM(PB[bG], XT[:, k, :], wg[:, k * 512:(k + 1) * 512], k == 0, k == 15, ["XT" + s_, "wg" + s_], [PK[bG]])
        for k in range(16):
            MM(PB[bU], XT[:, k, :], wu[:, k * 512:(k + 1) * 512], k == 0, k == 15, ["XT" + s_, "wu" + s_], [PK[bU]])

    def stage_B1(j):
        sl = j % 2
        s_ = str(sl)
        bG, bU = 2 * sl, 2 * sl + 1
        ACT(en_f[sl], PB[bG], AF.Exp, [], [PK[bG], "en_f" + s_], scale=-1.0)
        ACT(en_f[sl], en_f[sl], AF.Ln, [], ["en_f" + s_], bias=onec)
        ACT(en_f[sl], en_f[sl], AF.Exp, [], ["en_f" + s_], scale=-1.0)
        TT("dve", tg_f[sl], PB[bG], en_f[sl], ALU.mult, ["en_f" + s_], [PK[bG], "tg_f" + s_])
        TT("dve", act_b[sl], tg_f[sl], PB[bU], ALU.mult, ["tg_f" + s_], [PK[bU], "act_b" + s_])
        abv = act_b[sl].rearrange("p (j k) -> p k j", k=4)
        for kk in range(4):
            TR(PBH[6][:, kk * 128:(kk + 1) * 128], abv[:, kk, :], identb, ["act_b" + s_, "cb"], [PK[6]])
        ACT(actT[sl], PBH[6][:, 0:512].rearrange("p (k s) -> p k s", k=4), AF.Copy, [], [PK[6], "actT" + s_])

    def stage_B2(j):
        sl = j % 2
        s_ = str(sl)
        bG, bU = 2 * sl, 2 * sl + 1
        wd = wd_t[sl]
        xb = xb_t[sl]
        Y = Y_t[sl]
        wcol = wc_t[sl]
        TT("dve", wcol, xb[:, D + 2:D + 3], xb[:, D + 3:D + 4], ALU.add, ["xb" + s_], ["wc" + s_])
        for n in range(4):
            pbi = (bG, bU, 7, bG)[n] if False else (bG if n % 2 == 0 else bU)
            for kk in range(4):
                MM(PB[pbi], actT[sl][:, kk, :], wd[:, kk * D + n * 512: kk * D + (n + 1) * 512], kk == 0, kk == 3,
                   ["actT" + s_, "wd" + s_], [PK[pbi]])
            STT("dve", Y[:, n * 512:(n + 1) * 512], PB[pbi], wcol, g2b[:, n * 512:(n + 1) * 512],
                ALU.mult, ALU.mult, ["wc" + s_, "g2b"], [PK[pbi], "Y" + s_])
        S.dma("pool", (lambda sl=sl, Y=Y: (lambda e: e.indirect_dma_start(
            out=out_d, out_offset=bass.IndirectOffsetOnAxis(ap=xb_t[sl][:, D:D + 32].bitcast(I32)[:, 0:1], axis=0),
            in_=Y, in_offset=None, bounds_check=breg(e, S_LEN + 127), oob_is_err=True, compute_op=ALU.add)))(),
            ["Y" + s_, "xb" + s_], ["out"])
    pf(0, "gd")
    pf(1, "gd")
    stage_A(0)
    pf(2, "g")
    for j in range(NBLK):
        stage_B1(j)
        stage_B2(j)
        if j + 2 < NBLK:
            pf(j + 2, "d")
        if j + 1 < NBLK:
            stage_A(j + 1)
        if j + 3 < NBLK:
            pf(j + 3, "g")
    S.emit()
    S.close()
    es.close()
    return nc, dbg


def host_inputs(inputs, b):
    f = np.float32
    m = {}
    m["x"] = np.ascontiguousarray(inputs["x"][b])
    m["ccol"] = np.ascontiguousarray(inputs["c"][b].reshape(16, 128).T)
    m["pos"] = np.ascontiguousarray(inputs["positions"][b].reshape(1, S_LEN)).astype(np.int32)
    m["w_ada"] = inputs["w_ada"][0]
    m["b_ada"] = inputs["b_ada"][0].reshape(1, -1)
    m["norm1_gc"] = np.ascontiguousarray(inputs["norm1_g"][0].reshape(16, 128).T)
    m["w_in"] = inputs["w_in"][0]
    m["lb_logits"] = inputs["hgrn_lb_logits"]
    m["hgrn_onorm_g"] = inputs["hgrn_onorm_g"][0].reshape(1, 128)
    m["q_a_gc"] = np.ascontiguousarray(inputs["q_a_norm_g"][0].reshape(4, 128).T)
    m["w_q_up"] = inputs["w_q_up"][0]
    m["kv_a_gc"] = np.ascontiguousarray(inputs["kv_a_norm_g"][0].reshape(2, 128).T)
    m["w_kv_up"] = inputs["w_kv_up"][0]

    def qk_cols(g):
        o = np.zeros((128, 4), f)
        o[:, 0] = g[0:128]
        o[0:64, 1] = g[128:192]
        o[0:32, 2] = g[160:192]
        o[32:64, 2] = g[128:160]
        o[0:32, 3] = -1.0
        o[32:64, 3] = 1.0
        return o
    m["q_norm_gc"] = qk_cols(inputs["q_norm_g"][0])
    m["k_norm_gc"] = qk_cols(inputs["k_norm_g"][0])
    m["attn_onorm_g"] = inputs["attn_onorm_g"][0].reshape(1, 128)
    m["w_out"] = inputs["w_out"][0]
    m["norm2_g"] = inputs["norm2_g"][0].reshape(1, D)
    m["w_gr"] = np.ascontiguousarray(np.concatenate([inputs["w_group"][0], inputs["w_router"][0]], axis=1))
    m["b_gr"] = np.concatenate([inputs["b_group"][0], inputs["b_router"][0]]).reshape(1, 36)
    m["w_gate"] = inputs["w_gate"][0].reshape(32 * 128, 16 * 512)
    m["w_up"] = inputs["w_up"][0].reshape(32 * 128, 16 * 512)
    m["w_down"] = inputs["w_down"][0].reshape(32 * 128, 4 * 2048)
    m["consts"] = CONSTS
    m["invf"] = INVF
    m["meta_init"] = META_INIT.view(ml_dtypes.bfloat16)
    return m


def _consts():
    c = np.zeros((128, 1024), np.float32)
    s = np.arange(128)[:, None]
    t = np.arange(128)[None, :]
    c[:, 0:128] = np.eye(128)
    c[:, 128:256] = (s <= t)
    c[:, 256:384] = (s <= t).astype(np.float32) - (s <= 63).astype(np.float32)
    c[:, 384:512] = (s > t)
    c[:, 512:640] = (s < t)
    c[:, 640] = np.arange(128)
    c[:, 656:672] = np.arange(16) * 128
    c[:, 672:736] = np.arange(64) * 128
    c[:, 736:768] = np.arange(32)
    return c


CONSTS = _consts()
INVF = (10000.0 ** (-(np.arange(64) % 32).astype(np.float32) * 2 / 64)).astype(np.float32).reshape(64, 1)
META_INIT = np.zeros((NSLOT, 16), np.int32)
META_INIT[:, 0] = 2048 + (np.arange(NSLOT) % 128)

_NC = None


def kernel(**inputs):
    global _NC
    if _NC is None:
        _NC = build()[0]
    inputs = {k: np.asarray(v) for k, v in inputs.items()}
    in_maps = [host_inputs(inputs, b) for b in range(8)]
    res = run_bass_kernel_spmd(_NC, in_maps, core_ids=list(range(8)))
    out = np.stack([np.asarray(r["out"])[:S_LEN] for r in res.results], axis=0)
    return out.astype(np.float32)
```

```python
import numpy as np
import ml_dtypes
import concourse.bass as bass
import concourse.mybir as mybir
from concourse.bass_utils import run_bass_kernel_spmd

F32 = mybir.dt.float32
BF16 = mybir.dt.bfloat16
I32 = mybir.dt.int32
AF = mybir.ActivationFunctionType
ALU = mybir.AluOpType
AX = mybir.AxisListType

D = 2048
S_LEN = 2048
NT = 16
EPS = 1e-6
IN_COLS = 4928
BLK = 128
NBLK = 63
NSLOT = NBLK * BLK
DEBUG = None


class Sync:
    def __init__(self, nc, n_dma_sems=32):
        self.nc = nc
        self.eng = {"pe": nc.tensor, "dve": nc.vector, "act": nc.scalar,
                    "pool": nc.gpsimd, "sp": nc.sync}
        self.sem = {}
        self.cnt = {}
        self._ctx = []
        for e in self.eng:
            cm = nc.semaphore("s_" + e)
            self.sem[e] = cm.__enter__()
            self._ctx.append(cm)
            self.cnt[e] = 0
        self.dma_sems = []
        self.dma_pool = {"sp": [], "pool": [], "act": []}
        self.dma_rr = {"sp": 0, "pool": 0, "act": 0}
        for q, n in (("sp", n_dma_sems // 2), ("pool", n_dma_sems // 2), ("act", 2)):
            for i in range(n):
                cm = nc.semaphore("d%s%d" % (q, i))
                slot = [cm.__enter__(), 0, None]
                self.dma_sems.append(slot)
                self.dma_pool[q].append(slot)
                self._ctx.append(cm)
        self.waited = {}
        self.last_w = {}
        self.readers = {}
        self.prog = {e: [] for e in self.eng}

    def close(self):
        for cm in reversed(self._ctx):
            cm.__exit__(None, None, None)

    def _wait(self, e, tok):
        if tok is None:
            return
        sem, sid, val, src = tok
        if src == e and e == "pe":
            return
        k = (e, sid)
        if self.waited.get(k, 0) >= val:
            return
        self.waited[k] = val
        self.prog[e].append(("w", sem, val))

    def _deps(self, e, reads, writes, skip_same_war=True):
        for r in reads:
            self._wait(e, self.last_w.get(r))
        for w in writes:
            self._wait(e, self.last_w.get(w))
            for tok in self.readers.get(w, ()):
                if skip_same_war and tok[3] == e and e == "pe":
                    continue
                self._wait(e, tok)

    def _commit(self, tok, reads, writes):
        for w in writes:
            self.last_w[w] = tok
            self.readers[w] = []
        for r in reads:
            self.readers.setdefault(r, []).append(tok)

    def op(self, e, fn, reads=(), writes=()):
        self._deps(e, reads, writes)
        self.cnt[e] += 1
        self.prog[e].append(("i", fn, self.sem[e], 1))
        tok = (self.sem[e], e, self.cnt[e], e)
        self._commit(tok, reads, writes)
        return tok

    def dma(self, e, fn, reads=(), writes=()):
        pool = self.dma_pool[e]
        slot = pool[self.dma_rr[e]]
        self.dma_rr[e] = (self.dma_rr[e] + 1) % len(pool)
        self._wait(e, slot[2])
        self._deps(e, reads, writes, skip_same_war=False)
        slot[1] += 16
        self.prog[e].append(("i", fn, slot[0], 16))
        tok = (slot[0], id(slot), slot[1], None)
        slot[2] = tok
        self._commit(tok, reads, writes)
        return tok

    def barrier(self):
        toks = [(self.sem[e], e, self.cnt[e], e) for e in self.eng if self.cnt[e] > 0]
        toks += [s[2] for s in self.dma_sems if s[2] is not None]
        for e in self.eng:
            for t in toks:
                if t[3] == e and e == "pe":
                    continue
                self._wait(e, t)

    def emit(self):
        nc = self.nc
        self.barrier()
        prog = self.prog

        def run(engine, lst):
            for it in lst:
                if it[0] == "w":
                    engine.wait_ge(it[1], it[2])
                else:
                    it[1](engine).then_inc(it[2], it[3])

        with nc.Block() as block:
            @block.sync
            def _(eng):
                run(eng, prog["sp"])

            @block.scalar
            def _(eng):
                run(eng, prog["act"])

            @block.vector
            def _(eng):
                run(eng, prog["dve"])

            @block.gpsimd
            def _(eng):
                run(eng, prog["pool"])

            @block.tensor
            def _(eng):
                run(eng, prog["pe"])


def pipeline(gens, W):
    gens = list(gens)
    active = []
    nxt = 0
    while active or nxt < len(gens):
        while len(active) < W and nxt < len(gens):
            active.append(gens[nxt])
            nxt += 1
        for g in list(active):
            try:
                next(g)
            except StopIteration:
                active.remove(g)


class Arena:
    def __init__(self, ap):
        self.ap = ap
        self.off = 0
        self.cap = ap.shape[1]
        self.peak = 0

    def alloc(self, n, dtype=F32):
        ne = n * (2 if dtype in (F32, I32) else 1)
        ne = (ne + 15) // 16 * 16
        a = self.off
        self.off += ne
        assert self.off <= self.cap, ("arena overflow", self.off, self.cap)
        self.peak = max(self.peak, self.off)
        v = self.ap[:, a:a + n * (2 if dtype in (F32, I32) else 1)]
        if dtype == F32:
            v = v.bitcast(F32)
        elif dtype == I32:
            v = v.bitcast(I32)
        return v


def build(debug=None):
    nc = bass.Bass("TRN2", target_bir_lowering=False)

    def din(name, shape, dt=F32):
        return nc.dram_tensor(name, list(shape), dt, kind="ExternalInput").ap()

    x_d = din("x", [S_LEN, D])
    ccol_d = din("ccol", [128, 16])
    pos_d = din("pos", [1, S_LEN], I32)
    wada_d = din("w_ada", [D, 6 * D])
    bada_d = din("b_ada", [1, 6 * D])
    n1g_d = din("norm1_gc", [128, 16])
    win_d = din("w_in", [D, IN_COLS])
    lbl_d = din("lb_logits", [2, 1024])
    hon_d = din("hgrn_onorm_g", [1, 128])
    qag_d = din("q_a_gc", [128, 4])
    wqu_d = din("w_q_up", [512, 1536])
    kvg_d = din("kv_a_gc", [128, 2])
    wkv_d = din("w_kv_up", [256, 2048])
    qng_d = din("q_norm_gc", [128, 4])
    kng_d = din("k_norm_gc", [128, 4])
    aon_d = din("attn_onorm_g", [1, 128])
    wout_d = din("w_out", [D, D])
    n2g_d = din("norm2_g", [1, D])
    wgr_d = din("w_gr", [D, 36])
    bgr_d = din("b_gr", [1, 36])
    wg_d = din("w_gate", [32 * 128, 16 * 512])
    wu_d = din("w_up", [32 * 128, 16 * 512])
    wd_d = din("w_down", [32 * 128, 4 * 2048])
    cst_d = din("consts", [128, 1024])
    invf_d = din("invf", [64, 1])
    metai_d = din("meta_init", [NSLOT, 32], BF16)
    out_d = nc.dram_tensor("out", [S_LEN + 128, D], F32, kind="ExternalOutput").ap()
    xbuf_d = nc.dram_tensor("xbuf", [NSLOT, D + 32], BF16).ap()
    meta_d = nc.dram_tensor("metabuf", [NSLOT, 16], F32).ap()
    dbg = {}

    def dout(name, shape, dt=F32):
        dbg[name] = nc.dram_tensor("dbg_" + name, list(shape), dt, kind="ExternalOutput").ap()
        return dbg[name]

    S = Sync(nc)
    import contextlib
    es = contextlib.ExitStack()
    arena_t = es.enter_context(nc.sbuf_tensor("arena", [128, 103 * 1024], BF16))
    A = Arena(arena_t[:])
    banks = [es.enter_context(nc.psum_tensor("pb%d" % i, [128, 512], F32)) for i in range(8)]
    PB = [b[:] for b in banks]
    PBH = [b[:].bitcast(BF16) for b in banks]
    PK = ["pb%d" % i for i in range(8)]

    def MM(out, lhsT, rhs, start, stop, r, w):
        return S.op("pe", lambda e: e.matmul(out, lhsT=lhsT, rhs=rhs, start=start, stop=stop,
                                             skip_group_check=True), r, w)

    def TR(out, in_, ident, r, w):
        return S.op("pe", lambda e: e.transpose(out=out, in_=in_, identity=ident), r, w)

    def ACT(out, in_, func, r, w, scale=1.0, bias=0.0, accum=None):
        if accum is None:
            return S.op("act", lambda e: e.activation(out=out, in_=in_, func=func, bias=bias, scale=scale), r, w)
        return S.op("act", lambda e: e.activation(out=out, in_=in_, func=func, bias=bias, scale=scale,
                                                  accum_out=accum), r, w)

    def TS(eng, out, in0, s1, s2, op0, op1, r, w):
        if s2 is None:
            return S.op(eng, lambda e: e.tensor_scalar(out, in0, s1, None, op0), r, w)
        return S.op(eng, lambda e: e.tensor_scalar(out, in0, s1, s2, op0, op1), r, w)

    def TT(eng, out, in0, in1, op, r, w):
        return S.op(eng, lambda e: e.tensor_tensor(out, in0, in1, op), r, w)

    def STT(eng, out, in0, sc, in1, op0, op1, r, w, accum=None):
        if accum is None:
            return S.op(eng, lambda e: e.scalar_tensor_tensor(out, in0, sc, in1, op0, op1), r, w)
        return S.op(eng, lambda e: e.scalar_tensor_tensor(out, in0, sc, in1, op0, op1, accum_out=accum), r, w)

    def CP(eng, out, in_, r, w):
        return S.op(eng, lambda e: e.tensor_copy(out, in_), r, w)

    def RED(eng, out, in_, op, r, w):
        return S.op(eng, lambda e: e.tensor_reduce(out, in_, AX.X, op), r, w)

    def RCP(out, in_, r, w):
        return S.op("dve", lambda e: e.reciprocal(out, in_), r, w)

    def DMA(q, out, in_, r, w):
        return S.dma(q, lambda e: e.dma_start(out=out, in_=in_), r, w)

    _regs = {}

    def breg(e, val):
        if val not in _regs:
            _regs[val] = e.to_reg(val)
        return _regs[val]

    def rstd_col(out, ssq, n, r, w, tmpk):
        ACT(out, ssq, AF.Ln, r, [tmpk], scale=1.0 / n, bias=epsc)
        ACT(out, out, AF.Exp, [tmpk], w, scale=-0.5)

    cst = A.alloc(1024, F32)
    identf = cst[:, 0:128]
    triu = cst[:, 128:256]
    M1 = cst[:, 256:384]
    M2 = cst[:, 384:512]
    tril_strict = cst[:, 512:640]
    iota_p = cst[:, 640:641]
    thr16 = cst[:, 656:672]
    thr64 = cst[:, 672:736]
    eidx = cst[:, 736:768]
    DMA("sp", cst, cst_d, [], ["cst"])
    cb = A.alloc(512, BF16)
    identb = cb[:, 0:128]
    onesb = cb[:, 128:256]
    triub = cb[:, 256:384]
    trilsb = cb[:, 384:512]
    CP("dve", identb, identf, ["cst"], ["cb"])
    CP("dve", triub, triu, ["cst"], ["cb"])
    CP("dve", trilsb, tril_strict, ["cst"], ["cb"])
    S.op("pool", lambda e: e.memset(onesb, 1.0), [], ["cb"])
    small = A.alloc(256, F32)
    epsc = small[:, 0:1]
    onec = small[:, 1:2]
    S.op("pool", lambda e: e.memset(epsc, EPS), [], ["epsc"])
    S.op("pool", lambda e: e.memset(onec, 1.0), [], ["epsc"])
    _sc = [2]

    def col(n=1):
        a = _sc[0]
        _sc[0] += n
        assert _sc[0] <= 256
        return small[:, a:a + n]

    modc = A.alloc(96, F32)
    A1c = A.alloc(16, F32)
    modrow_d = nc.dram_tensor("modrow_d", [1, 6 * D], F32).ap()

    mark = A.off
    ccol = A.alloc(16, F32)
    cact = A.alloc(16, BF16)
    tmp16 = A.alloc(16, F32)
    modrow = A.alloc(6 * D, F32)
    DMA("sp", ccol, ccol_d, [], ["ccol"])
    DMA("sp", modrow[0:1, :], bada_d, [], ["modrow_b"])
    ACT(tmp16, ccol, AF.Exp, ["ccol"], ["tmp16"], scale=-1.0)
    TS("dve", tmp16, tmp16, 1.0, None, ALU.add, None, ["tmp16"], ["tmp16"])
    RCP(tmp16, tmp16, ["tmp16"], ["tmp16"])
    TT("dve", cact, ccol, tmp16, ALU.mult, ["tmp16", "ccol"], ["cact"])
    wa = [A.alloc(16 * 512, BF16).rearrange("p (k n) -> p k n", k=16) for _ in range(2)]
    biasrow = modrow
    for jg in range(8):
        wt = wa[jg % 2]
        wk = "wa%d" % (jg % 2)
        S.dma("pool", (lambda wt=wt, jg=jg: (lambda e: e.dma_start(
            out=wt, in_=wada_d[:, jg * 512:(jg + 1) * 512].rearrange("(k p) n -> p k n", p=128))))(),
            [], [wk])
        pbi = jg % 2
        for k in range(16):
            MM(PB[pbi][0:1, :], cact[:, k:k + 1], wt[:, k, :], k == 0, k == 15, [wk, "cact"], [PK[pbi]])
        TT("dve", modrow[0:1, jg * 512:(jg + 1) * 512], PB[pbi][0:1, :], modrow[0:1, jg * 512:(jg + 1) * 512],
           ALU.add, ["modrow_b"], [PK[pbi], "modrow%d" % jg])
    allrow = ["modrow%d" % j for j in range(8)]
    if debug == "A":
        d = dout("mod", [1, 6 * D])
        DMA("sp", d, modrow[0:1, :], allrow, ["dbg"])
    for j in range(32):
        MM(PB[2][:, j:j + 1], modrow[0:1, j * 128:(j + 1) * 128], onec[0:1, 0:1], True, True, allrow + ["epsc"], [PK[2]])
    CP("dve", modc[:, 0:32], PB[2][:, 0:32], [], [PK[2], "modc"])
    cactp = col(16)
    cactb = cactp.bitcast(BF16)[:, 0:16]
    CP("dve", cactb, cact, ["cact"], ["cactb"])
    n1gc = A.alloc(16, F32)
    DMA("sp", n1gc, n1g_d, [], ["n1gc"])
    STT("dve", A1c, modc[:, 16:32], 1.0, n1gc, ALU.add, ALU.mult, ["modc", "n1gc"], ["A1c"])
    B1c = modc[:, 0:16]
    S.barrier()
    A.off = mark

    if debug == "A":
        d2 = dout("modc", [128, 96])
        DMA("sp", d2, modc, ["modc"], ["dbg"])
        S.emit()
        S.close()
        es.close()
        return nc, dbg

    markMix = A.off
    mixT = A.alloc(16 * S_LEN, BF16).rearrange("p (k t) -> p k t", k=16)
    markHT = A.off
    hT = A.alloc(16 * S_LEN, BF16).rearrange("p (k t) -> p k t", k=16)
    markB = A.off
    xt = [A.alloc(D, F32) for _ in range(2)]
    xn = [A.alloc(D, BF16) for _ in range(2)]
    tmod = [A.alloc(1024, F32) for _ in range(2)]
    ssq1 = col(16)
    rs1 = col(16)
    for i in range(NT):
        sl = i % 2
        DMA("sp", xt[sl], x_d[i * 128:(i + 1) * 128, :], [], ["xt%d" % sl])
        ACT(xn[sl], xt[sl], AF.Square, ["xt%d" % sl], ["xn%d" % sl, "ssq1_%d" % i], accum=ssq1[:, i:i + 1])
        rstd_col(rs1[:, i:i + 1], ssq1[:, i:i + 1], D, ["ssq1_%d" % i, "epsc"], ["rs1_%d" % i], "rs1t_%d" % i)
        ACT(xn[sl], xt[sl], AF.Identity, ["xt%d" % sl, "rs1_%d" % i], ["xn%d" % sl], scale=rs1[:, i:i + 1])
        for hf in range(2):
            for kk in range(8):
                k = hf * 8 + kk
                TR(PBH[hf][:, kk * 128:(kk + 1) * 128], xn[sl][:, k * 128:(k + 1) * 128], identb,
                   ["xn%d" % sl, "cb"], [PK[hf]])
            src = PBH[hf].rearrange("p (k t) -> p k t", k=8)
            tm = tmod[hf].rearrange("p (k t) -> p k t", k=8)
            a1 = A1c[:, hf * 8:(hf + 1) * 8].unsqueeze(2).to_broadcast([128, 8, 128])
            b1 = B1c[:, hf * 8:(hf + 1) * 8].unsqueeze(2).to_broadcast([128, 8, 128])
            TT("dve", tm, src, a1, ALU.mult, ["A1c"], [PK[hf], "tmod%d" % hf])
            TT("pool", hT[:, hf * 8:(hf + 1) * 8, i * 128:(i + 1) * 128], tm, b1, ALU.add,
               ["tmod%d" % hf, "modc"], ["hT%d" % i])
    hTall = ["hT%d" % i for i in range(NT)]
    S.barrier()
    A.off = markB
    if debug == "B":
        d = dout("hT", [128, 16 * S_LEN], BF16)
        DMA("sp", d, hT.rearrange("p k t -> p (k t)"), hTall, ["dbg"])
        S.emit(); S.close(); es.close()
        return nc, dbg

    markC = A.off
    wb = [A.alloc(16 * 512, BF16).rearrange("p (k n) -> p k n", k=16) for _ in range(2)]
    markC1 = A.off
    wsec = [wb[0], wb[1],
            mixT[:, 8:12, :].rearrange("p a t -> p (a t)").rearrange("p (k n) -> p k n", k=16),
            mixT[:, 12:16, :].rearrange("p a t -> p (a t)").rearrange("p (k n) -> p k n", k=16)]
    lbb = A.alloc(1024, F32)
    omlb = A.alloc(1024, F32)
    honb = A.alloc(128, F32)
    DMA("sp", lbb, lbl_d[0:1, :].partition_broadcast(128), [], ["lbb"])
    DMA("sp", omlb, lbl_d[1:2, :].partition_broadcast(128), [], ["omlb"])
    DMA("sp", honb, hon_d.partition_broadcast(128), [], ["honb"])
    TT("dve", omlb, omlb, lbb, ALU.subtract, ["lbb"], ["omlb"])
    ACT(omlb, omlb, AF.Exp, [], ["omlb"])
    TS("dve", omlb, omlb, 1.0, None, ALU.add, None, [], ["omlb"])
    RCP(lbb, omlb, ["omlb"], ["lbb"])
    TS("dve", omlb, lbb, -1.0, 1.0, ALU.mult, ALU.add, ["lbb"], ["omlb"])
    W4 = 512
    en_t, f_t, lf_t, kk_t = A.alloc(W4), A.alloc(W4), A.alloc(W4), A.alloc(W4)
    E1_t, E2_t = A.alloc(W4), A.alloc(W4)
    qin_t, qout_t, kin_t, kout_t, v_t = (A.alloc(W4, BF16) for _ in range(5))
    eng_t, sil_t = A.alloc(W4), A.alloc(W4)
    E3_t, E1n_t = eng_t, f_t
    trq_t, trk_t, tro_t = A.alloc(W4, BF16), A.alloc(W4, BF16), A.alloc(W4, BF16)
    am_t = A.alloc(W4, BF16)
    on_t = en_t
    og_t = A.alloc(W4, BF16)
    Sst = A.alloc(W4)
    Sbf = A.alloc(W4, BF16)
    deccol = col(4)
    ssqo = col(4)
    rso = col(4)
    QSC = 128.0 ** -0.5
    v4 = lambda t: t.rearrange("p (h d) -> p h d", h=4)
    triu4 = triu.unsqueeze(1).to_broadcast([128, 4, 128])
    for hgp in range(2):
        for sec in range(4):
            c0 = sec * 1024 + hgp * 512
            S.dma("pool", (lambda sec=sec, c0=c0: (lambda e: e.dma_start(
                out=wsec[sec], in_=win_d[:, c0:c0 + 512].rearrange("(k p) n -> p k n", p=128))))(), [], ["wsec%d" % sec])
        hs = slice(hgp * 512, (hgp + 1) * 512)
        for i in range(NT):
            tsl = slice(i * 128, (i + 1) * 128)

            def emit_proj(ii):
                for sec in range(4):
                    for k in range(16):
                        MM(PB[sec], hT[:, k, ii * 128:(ii + 1) * 128], wsec[sec][:, k, :], k == 0, k == 15,
                           ["wsec%d" % sec, "hT%d" % ii], [PK[sec]])
            if i == 0:
                emit_proj(0)
            hq, hf_, hi_, hg = PB[0], PB[1], PB[2], PB[3]
            ACT(en_t, hf_, AF.Exp, [], [PK[1], "en"], scale=-1.0)
            ACT(v_t, hi_, AF.Copy, [], [PK[2], "v"])
            ACT(eng_t, hg, AF.Exp, [], [PK[3], "eng"], scale=-1.0)
            ACT(en_t, en_t, AF.Ln, [], ["en"], bias=onec)
            ACT(en_t, en_t, AF.Exp, [], ["en"], scale=-1.0)
            TT("dve", f_t, en_t, omlb[:, hs], ALU.mult, ["en", "omlb"], ["f"])
            TT("dve", f_t, f_t, lbb[:, hs], ALU.add, ["lbb"], ["f"])
            ACT(lf_t, f_t, AF.Ln, ["f"], ["lf"])
            ACT(kk_t, f_t, AF.Identity, ["f"], ["kk"], scale=-1.0, bias=onec)
            ACT(eng_t, eng_t, AF.Ln, [], ["eng"], bias=onec)
            ACT(eng_t, eng_t, AF.Exp, [], ["eng"], scale=-1.0)
            TT("dve", sil_t, hg, eng_t, ALU.mult, ["eng"], [PK[3], "sil"])
            TT("pool", v4(sil_t), v4(sil_t), honb.unsqueeze(1).to_broadcast([128, 4, 128]), ALU.mult, ["honb"], ["sil"])
            MM(PB[4], M1, lf_t, True, True, ["cst", "lf"], [PK[4]])
            MM(PB[5], M2, lf_t, True, True, ["cst", "lf"], [PK[5]])
            MM(PB[6], triu, lf_t, True, True, ["cst", "lf"], [PK[6]])
            for hh in range(4):
                MM(PB[7][:, hh:hh + 1], lf_t[:, hh * 128:(hh + 1) * 128], onec, True, True, ["epsc", "lf"], [PK[7]])
            ACT(E1_t, PB[4], AF.Exp, [], [PK[4], "E1"])
            ACT(E1n_t, PB[4], AF.Exp, [], [PK[4], "f"], scale=-1.0)
            ACT(E2_t, PB[5], AF.Exp, [], [PK[5], "E2"])
            ACT(E3_t, PB[6], AF.Exp, [], [PK[6], "eng"])
            ACT(deccol, PB[7][:, 0:4], AF.Exp, [], [PK[7], "dec"])
            STT("dve", qin_t, hq, QSC, E1_t, ALU.mult, ALU.mult, ["E1"], [PK[0], "qin"])
            STT("dve", qout_t, hq, QSC, E3_t, ALU.mult, ALU.mult, ["eng"], [PK[0], "qout"])
            TT("pool", kin_t, kk_t, E1n_t, ALU.mult, ["kk", "f"], ["kin"])
            TT("pool", kout_t, kk_t, E2_t, ALU.mult, ["kk", "E2"], ["kout"])
            for hh in range(4):
                hsl = slice(hh * 128, (hh + 1) * 128)
                TR(PBH[4][:, hsl], qin_t[:, hsl], identb, ["qin", "cb"], [PK[4]])
                TR(PBH[5][:, hsl], kin_t[:, hsl], identb, ["kin", "cb"], [PK[5]])
                TR(PBH[6][:, hsl], qout_t[:, hsl], identb, ["qout", "cb"], [PK[6]])
            CP("dve", trq_t, PBH[4][:, 0:512], [], [PK[4], "trq"])
            ACT(trk_t, PBH[5][:, 0:512], AF.Copy, [], [PK[5], "trk"])
            CP("dve", tro_t, PBH[6][:, 0:512], [], [PK[6], "tro"])
            for hh in range(4):
                hsl = slice(hh * 128, (hh + 1) * 128)
                MM(PB[4][:, hsl], trk_t[:, hsl], trq_t[:, hsl], True, True, ["trk", "trq"], [PK[4]])
            if i + 1 < NT:
                emit_proj(i + 1)
            TT("dve", v4(am_t), v4(PB[4]), triu4, ALU.mult, ["cst"], [PK[4], "am"])
            for hh in range(4):
                hsl = slice(hh * 128, (hh + 1) * 128)
                if i == 0:
                    MM(PB[5][:, hsl], am_t[:, hsl], v_t[:, hsl], True, True, ["am", "v"], [PK[5]])
                else:
                    MM(PB[5][:, hsl], am_t[:, hsl], v_t[:, hsl], True, False, ["am", "v"], [PK[5]])
                    MM(PB[5][:, hsl], tro_t[:, hsl], Sbf[:, hsl], False, True, ["tro", "Sbf"], [PK[5]])
            for hh in range(4):
                hsl = slice(hh * 128, (hh + 1) * 128)
                MM(PB[6][:, hsl], kout_t[:, hsl], v_t[:, hsl], True, True, ["kout", "v"], [PK[6]])
            if i == 0:
                CP("dve", Sst, PB[6], [], [PK[6], "S"])
            else:
                TT("pool", v4(Sst), v4(Sst), deccol.unsqueeze(2).to_broadcast([128, 4, 128]), ALU.mult, ["dec"], ["S"])
                TT("dve", Sst, Sst, PB[6], ALU.add, [], [PK[6], "S"])
            if i < NT - 1:
                ACT(Sbf, Sst, AF.Copy, ["S"], ["Sbf"])
            ACT(on_t, PB[5], AF.Square, [], [PK[5], "en"])
            RED("dve", ssqo, v4(on_t), ALU.add, ["en"], ["ssqo"])
            ACT(rso, ssqo, AF.Ln, ["ssqo", "epsc"], ["rso"], scale=1.0 / 128, bias=epsc)
            ACT(rso, rso, AF.Exp, [], ["rso"], scale=-0.5)
            TT("dve", v4(on_t), v4(PB[5]), rso.unsqueeze(2).to_broadcast([128, 4, 128]), ALU.mult, ["rso"], [PK[5], "en"])
            TT("dve", og_t, on_t, sil_t, ALU.mult, ["en", "sil"], ["og"])
            for hh in range(4):
                hsl = slice(hh * 128, (hh + 1) * 128)
                TR(PBH[7][:, hsl], og_t[:, hsl], identb, ["og", "cb"], [PK[7]])
            ACT(mixT[:, hgp * 4:(hgp + 1) * 4, tsl], PBH[7][:, 0:512].rearrange("p (h t) -> p h t", h=4), AF.Copy, [],
                [PK[7], "mixT%d_%d" % (hgp, i)])
    S.barrier()
    A.off = markC1
    if debug == "C1":
        d = dout("mixT", [128, 16 * S_LEN], BF16)
        DMA("sp", d, mixT.rearrange("p k t -> p (k t)"), [], ["dbg"])
        S.emit(); S.close(); es.close()
        return nc, dbg

    A.off = markC + 16 * 512
    mla_d = nc.dram_tensor("mla_scratch", [128, 9 * S_LEN], BF16).ap()

    def alloc_mla():
        qaT = A.alloc(4 * S_LEN, BF16).rearrange("p (k t) -> p k t", k=4)
        kvaT = A.alloc(2 * S_LEN, BF16).rearrange("p (k t) -> p k t", k=2)
        return qaT, kvaT, A.alloc(S_LEN, BF16), A.alloc(S_LEN, BF16), A.alloc(S_LEN, BF16)
    mla0 = A.off
    qaT, kvaT, kpeT, kpesT, SQR = alloc_mla()
    mla_all = A.ap[:, mla0:mla0 + 9 * S_LEN]
    qagc = A.alloc(4, F32)
    kvgc = A.alloc(2, F32)
    qng = A.alloc(4, F32)
    kng = A.alloc(4, F32)
    DMA("sp", qagc, qag_d, [], ["qagc"])
    DMA("sp", kvgc, kvg_d, [], ["kvgc"])
    DMA("sp", qng, qng_d, [], ["qng"])
    DMA("sp", kng, kng_d, [], ["kng"])
    TT("dve", qng[:, 2:3], qng[:, 2:3], qng[:, 3:4], ALU.mult, [], ["qng"])
    TT("dve", kng[:, 2:3], kng[:, 2:3], kng[:, 3:4], ALU.mult, [], ["kng"])
    sq_t = [A.alloc(512, BF16) for _ in range(2)]
    rsb_t = [A.alloc(512, F32) for _ in range(2)]
    S.op("pool", lambda e: e.memset(SQR, 0.0), [], ["SQR"])
    wA, wB = wb[0], wb[0]
    S.dma("pool", lambda e: e.dma_start(out=wA, in_=win_d[:, 4096:4608].rearrange("(k p) n -> p k n", p=128)), [], ["wb0"])

    def lowrank(wt, wk, nch, dstT, gcol, gk, nfeat, dk):
        for tg in range(4):
            tsl = slice(tg * 512, (tg + 1) * 512)
            for c in range(nch):
                pbi = c % 2
                for k in range(16):
                    MM(PB[pbi], wt[:, k, c * 128:(c + 1) * 128], hT[:, k, tsl], k == 0, k == 15,
                       [wk] + hTall, [PK[pbi]])
                ACT(sq_t[pbi], PB[pbi], AF.Square, [], [PK[pbi], "sq%d" % pbi])
                TS("dve", dstT[:, c, tsl], PB[pbi], gcol[:, c:c + 1], None, ALU.mult, None, [gk],
                   [PK[pbi], dk + "%d_%d" % (c, tg)])
                MM(PB[2], onesb, sq_t[pbi], c == 0, c == nch - 1, ["cb", "sq%d" % pbi], [PK[2]])
            rb = rsb_t[tg % 2]
            rk = "rsb%d" % (tg % 2)
            ACT(rb, PB[2], AF.Ln, ["epsc"], [PK[2], rk], scale=1.0 / nfeat, bias=epsc)
            ACT(rb, rb, AF.Exp, [], [rk], scale=-0.5)
            for c in range(nch):
                TT("pool" if c % 2 else "dve", dstT[:, c, tsl], dstT[:, c, tsl], rb, ALU.mult, [rk],
                   [dk + "%d_%d" % (c, tg)])
    lowrank(wA, "wb0", 4, qaT, qagc, "qagc", 512, "qaT")
    S.op("pool", lambda e: e.memset(wB[:, :, 256:512], 0.0), [], ["wb0"])
    S.dma("pool", lambda e: e.dma_start(out=wB[:, :, 0:256], in_=win_d[:, 4608:4864].rearrange("(k p) n -> p k n", p=128)), [], ["wb0"])
    S.dma("pool", lambda e: e.dma_start(out=wB[:, :, 256:320], in_=win_d[:, 4864:4928].rearrange("(k p) n -> p k n", p=128)), [], ["wb0"])
    S.dma("pool", lambda e: e.dma_start(out=wB[:, :, 384:416], in_=win_d[:, 4896:4928].rearrange("(k p) n -> p k n", p=128)), [], ["wb0"])
    S.dma("pool", lambda e: e.dma_start(out=wB[:, :, 416:448], in_=win_d[:, 4864:4896].rearrange("(k p) n -> p k n", p=128)), [], ["wb0"])
    lowrank(wB, "wb0", 2, kvaT, kvgc, "kvgc", 256, "kvaT")
    for tg in range(4):
        tsl = slice(tg * 512, (tg + 1) * 512)
        for k in range(16):
            MM(PB[3], wB[:, k, 256:384], hT[:, k, tsl], k == 0, k == 15, ["wb0"] + hTall, [PK[3]])
        for k in range(16):
            MM(PB[4], wB[:, k, 384:512], hT[:, k, tsl], k == 0, k == 15, ["wb0"] + hTall, [PK[4]])
        ACT(SQR[0:64, tsl], PB[3][0:64, :], AF.Square, [], [PK[3], "SQR"])
        TS("dve", kpeT[0:64, tsl], PB[3][0:64, :], kng[0:64, 1:2], None, ALU.mult, None, ["kng"], [PK[3], "kpeT"])
        TS("dve", kpesT[0:64, tsl], PB[4][0:64, :], kng[0:64, 2:3], None, ALU.mult, None, ["kng"], [PK[4], "kpesT"])
    qaT_keys = ["qaT%d_%d" % (c, tg) for c in range(4) for tg in range(4)]
    kvaT_keys = ["kvaT%d_%d" % (c, tg) for c in range(2) for tg in range(4)]
    S.barrier()
    if debug == "C2":
        d = dout("qaT", [128, 4 * S_LEN], BF16)
        DMA("sp", d, qaT.rearrange("p k t -> p (k t)"), [], ["dbg"])
        d = dout("kvaT", [128, 2 * S_LEN], BF16)
        DMA("sp", d, kvaT.rearrange("p k t -> p (k t)"), [], ["dbg"])
        d = dout("kpeT", [64, S_LEN], BF16)
        DMA("sp", d, kpeT[0:64, :], [], ["dbg"])
        d = dout("kpesT", [64, S_LEN], BF16)
        DMA("sp", d, kpesT[0:64, :], [], ["dbg"])
        S.emit(); S.close(); es.close()
        return nc, dbg
    qngp = col(4)
    kngp = col(4)
    CP("dve", qngp, qng, ["qng"], ["qngp"])
    CP("dve", kngp, kng, ["kng"], ["kngp"])
    S.barrier()
    topD = mla0 + 9 * S_LEN
    A.off = markHT
    A.cap = mla0
    if debug == "D00":
        d = dout("qaT", [128, 4 * S_LEN], BF16)
        DMA("sp", d, qaT.rearrange("p k t -> p (k t)"), [], ["dbg"])
        d = dout("kvaT", [128, 2 * S_LEN], BF16)
        DMA("sp", d, kvaT.rearrange("p k t -> p (k t)"), [], ["dbg"])
        d = dout("kpeT", [64, S_LEN], BF16)
        DMA("sp", d, kpeT[0:64, :], [], ["dbg"])
        d = dout("kpesT", [64, S_LEN], BF16)
        DMA("sp", d, kpesT[0:64, :], [], ["dbg"])
        S.emit(); S.close(); es.close()
        return nc, dbg
    PI = float(np.pi)
    cosT = A.alloc(S_LEN, F32)
    sinT = A.alloc(S_LEN, F32)
    RT = A.alloc(S_LEN, BF16)
    aonb = A.alloc(128, F32)
    import os
    SK = os.environ.get("SKIPD", "")
    if "a" not in SK:
        DMA("sp", aonb, aon_d.partition_broadcast(128), [], ["aonb"])
    markD0 = A.off
    posi = A.alloc(S_LEN, I32)
    ang = A.alloc(S_LEN, F32)
    kq = A.alloc(S_LEN, F32)
    kqi = A.alloc(S_LEN, I32)
    msk = A.alloc(S_LEN, F32)
    invf = col(1)
    if "i" not in SK:
        DMA("sp", invf[0:64, :], invf_d, [], ["invf"])
    if "p" not in SK:
        DMA("sp", posi[0:64, :], pos_d.partition_broadcast(64), [], ["posi"])
    def dump_kva(tag):
        if debug == tag:
            S.barrier()
            d = dout("kvaT2", [128, 2 * S_LEN], BF16); DMA("sp", d, kvaT.rearrange("p k t -> p (k t)"), [], ["dbg"])
            S.emit(); S.close(); es.close()
            return True
        return False
    if dump_kva("X1"):
        return nc, dbg
    for (dst, shift, key) in ((sinT, 0.0, "sinT"), (cosT, PI / 2, "cosT")):
        a_, q_, qi_, m_ = ang[0:64, :], kq[0:64, :], kqi[0:64, :], msk[0:64, :]
        CP("dve", a_, posi[0:64, :], ["posi"], ["ang"])
        TS("dve", a_, a_, invf[0:64, :], shift, ALU.mult, ALU.add, ["invf"], ["ang"])
        TS("dve", q_, a_, 1.0 / (2 * PI), None, ALU.mult, None, ["ang"], ["kq"])
        CP("dve", qi_, q_, ["kq"], ["kqi"])
        CP("dve", q_, qi_, ["kqi"], ["kq"])
        if key == "sinT" and dump_kva("X2"):
            return nc, dbg
        STT("dve", a_, q_, -2 * PI, a_, ALU.mult, ALU.add, ["kq"], ["ang"])
        TS("dve", m_, a_, PI, None, ALU.is_gt, None, ["ang"], ["msk"])
        STT("dve", a_, m_, -2 * PI, a_, ALU.mult, ALU.add, ["msk"], ["ang"])
        TS("dve", m_, a_, -PI, None, ALU.is_lt, None, ["ang"], ["msk"])
        STT("dve", a_, m_, 2 * PI, a_, ALU.mult, ALU.add, ["msk"], ["ang"])
        if key == "sinT" and dump_kva("X3"):
            return nc, dbg
        ACT(dst[0:64, :], a_, AF.Sin, ["ang"], [key])
        if key == "sinT" and dump_kva("X4"):
            return nc, dbg
    TT("dve", ang[0:64, :], kpeT[0:64, :], cosT[0:64, :], ALU.mult, ["kpeT", "cosT"], ["ang"])
    TT("dve", kq[0:64, :], kpesT[0:64, :], sinT[0:64, :], ALU.mult, ["kpesT", "sinT"], ["kq"])
    TT("dve", RT[0:64, :], ang[0:64, :], kq[0:64, :], ALU.add, ["kq", "ang"], ["RT"])
    S.barrier()
    A.off = markD0
    if debug == "D0":
        d = dout("cosT", [64, S_LEN]); DMA("sp", d, cosT[0:64, :], [], ["dbg"])
        d = dout("sinT", [64, S_LEN]); DMA("sp", d, sinT[0:64, :], [], ["dbg"])
        d = dout("RT", [64, S_LEN], BF16); DMA("sp", d, RT[0:64, :], [], ["dbg"])
        d = dout("kvaT2", [128, 2 * S_LEN], BF16); DMA("sp", d, kvaT.rearrange("p k t -> p (k t)"), [], ["dbg"])
        print("offsets", markHT, mla0, markD0, A.off)
        S.emit(); S.close(); es.close()
        return nc, dbg

    wq_t = [A.alloc(4 * 384, BF16).rearrange("p (k n) -> p k n", k=4) for _ in range(2)]
    wkv_t = [A.alloc(2 * 256, BF16).rearrange("p (k n) -> p k n", k=2) for _ in range(2)]
    QTn_t = [A.alloc(S_LEN, BF16) for _ in range(2)]
    QTr_t = [A.alloc(S_LEN, BF16) for _ in range(2)]
    KTn_t = [A.alloc(S_LEN, BF16) for _ in range(2)]
    KTr_t = [A.alloc(S_LEN, BF16) for _ in range(2)]
    V_t = [A.alloc(16 * 130, BF16).rearrange("p (t v) -> p t v", t=16) for _ in range(2)]
    for sl in range(2):
        S.op("pool", (lambda sl=sl: (lambda e: e.memset(V_t[sl][:, :, 128:130], 1.0)))(), [], ["V%d" % sl])
        S.op("pool", (lambda sl=sl: (lambda e: e.memset(wq_t[sl], 0.0)))(), [], ["wq%d" % sl])
        S.op("pool", (lambda sl=sl: (lambda e: e.memset(QTr_t[sl], 0.0)))(), [], ["QTr%d" % sl])
        S.op("pool", (lambda sl=sl: (lambda e: e.memset(KTr_t[sl], 0.0)))(), [], ["KTr%d" % sl])
    lowtop = A.off
    A.off = topD
    A.cap = A.ap.shape[1]
    sqn_t = [A.alloc(512, BF16) for _ in range(2)]
    sqr_t = [A.alloc(512, BF16) for _ in range(2)]
    rb_t = [A.alloc(512, F32) for _ in range(2)]
    t1_t = [A.alloc(512, F32) for _ in range(2)]
    t2_t = [A.alloc(512, F32) for _ in range(2)]
    mrowt = A.alloc(256, F32)
    ob_t = [A.alloc(128, BF16) for _ in range(4)]
    A.off = lowtop
    A.cap = mla0
    PT_t = [A.alloc(512, BF16) for _ in range(2)]
    wad = A.alloc(16 * 256, BF16).rearrange("p (k n) -> p k n", k=16)
    ada_next = [0]

    ada_pending = [None]

    def ada_group():
        if ada_pending[0] is not None:
            c0 = ada_pending[0]
            for k in range(16):
                MM(PB[7][0:1, 0:256], cactb[:, k:k + 1], wad[:, k, :], k == 0, k == 15, ["wad", "cactb"], [PK[7]])
            TT("dve", mrowt[0:1, :], PB[7][0:1, 0:256], mrowt[0:1, :], ALU.add, [], [PK[7], "mrowt"])
            DMA("sp", modrow_d[0:1, c0:c0 + 256], mrowt[0:1, :], ["mrowt"], ["modrow_d"])
            ada_pending[0] = None
        g = ada_next[0]
        if g >= 32:
            return
        ada_next[0] += 1
        c0 = 4096 + g * 256
        S.dma("pool", (lambda c0=c0: (lambda e: e.dma_start(
            out=wad, in_=wada_d[:, c0:c0 + 256].rearrange("(k p) n -> p k n", p=128))))(), [], ["wad"])
        DMA("sp", mrowt[0:1, :], bada_d[0:1, c0:c0 + 256], [], ["mrowt"])
        ada_pending[0] = c0
    junk3 = A.alloc(128, F32)
    ocol = col(16)
    SM_SCALE = 192.0 ** -0.5
    nev = 0
    npt = 0
    for h in range(8):
        sl = h % 2
        s_ = str(sl)
        wq, wkv = wq_t[sl], wkv_t[sl]
        QTn, QTr, KTn, KTr, V = QTn_t[sl], QTr_t[sl], KTn_t[sl], KTr_t[sl], V_t[sl]
        b0 = h * 192
        for (a, bnd, c0, n) in ((0, 128, b0, 128), (128, 192, b0 + 128, 64), (256, 288, b0 + 160, 32), (288, 320, b0 + 128, 32)):
            S.dma("pool", (lambda wq=wq, a=a, bnd=bnd, c0=c0, n=n: (lambda e: e.dma_start(
                out=wq[:, :, a:bnd], in_=wqu_d[:, c0:c0 + n].rearrange("(k p) n -> p k n", p=128))))(), [], ["wq" + s_])
        S.dma("pool", (lambda wkv=wkv, h=h: (lambda e: e.dma_start(
            out=wkv, in_=wkv_d[:, h * 256:(h + 1) * 256].rearrange("(k p) n -> p k n", p=128))))(), [], ["wkv" + s_])
        def proj_body(tg, sl=sl, s_=s_, wq=wq, wkv=wkv, QTn=QTn, QTr=QTr, KTn=KTn, KTr=KTr, V=V):
            tsl = slice(tg * 512, (tg + 1) * 512)
            g2 = tg % 2
            gs = str(g2)
            bA, bB, bC, bD = (0, 1, 2, 3) if g2 == 0 else (4, 5, 6, 7)
            for k in range(4):
                MM(PB[bA], wq[:, k, 0:128], qaT[:, k, tsl], k == 0, k == 3, ["wq" + s_] + qaT_keys, [PK[bA]])
            for k in range(4):
                MM(PB[bB], wq[:, k, 128:256], qaT[:, k, tsl], k == 0, k == 3, ["wq" + s_] + qaT_keys, [PK[bB]])
            for k in range(4):
                MM(PB[bC], wq[:, k, 256:384], qaT[:, k, tsl], k == 0, k == 3, ["wq" + s_] + qaT_keys, [PK[bC]])
            yield
            ACT(sqn_t[g2], PB[bA], AF.Square, [], [PK[bA], "sqn" + gs])
            ACT(sqr_t[g2], PB[bB], AF.Square, [], [PK[bB], "sqr" + gs])
            yield
            MM(PB[bD], onesb, sqn_t[g2], True, False, ["cb", "sqn" + gs], [PK[bD]])
            MM(PB[bD], onesb, sqr_t[g2], False, True, ["cb", "sqr" + gs], [PK[bD]])
            yield
            rb = rb_t[g2]
            ACT(rb, PB[bD], AF.Ln, ["epsc"], [PK[bD], "rb" + gs], scale=1.0 / 192, bias=epsc)
            ACT(rb, rb, AF.Exp, [], ["rb" + gs], scale=-0.5)
            yield
            STT("dve", QTn[:, tsl], PB[bA], qngp[:, 0:1], rb, ALU.mult, ALU.mult, ["qngp", "rb" + gs], [PK[bA], "QTn" + s_])
            STT("dve", t1_t[g2][0:64, :], PB[bB][0:64, :], qngp[0:64, 1:2], cosT[0:64, tsl], ALU.mult, ALU.mult,
                ["qngp", "cosT"], [PK[bB], "t1" + gs])
            STT("dve", t2_t[g2][0:64, :], PB[bC][0:64, :], qngp[0:64, 2:3], sinT[0:64, tsl], ALU.mult, ALU.mult,
                ["qngp", "sinT"], [PK[bC], "t2" + gs])
            yield
            TT("pool", t1_t[g2][0:64, :], t1_t[g2][0:64, :], t2_t[g2][0:64, :], ALU.add, ["t2" + gs], ["t1" + gs])
            TT("pool", QTr[0:64, tsl], t1_t[g2][0:64, :], rb[0:64, :], ALU.mult, ["t1" + gs, "rb" + gs], ["QTr" + s_])
            for k in range(2):
                MM(PB[bA], wkv[:, k, 0:128], kvaT[:, k, tsl], k == 0, k == 1, ["wkv" + s_] + kvaT_keys, [PK[bA]])
            for j in range(4):
                t = tg * 4 + j
                for k in range(2):
                    MM(PB[bB][:, j * 128:(j + 1) * 128], kvaT[:, k, t * 128:(t + 1) * 128], wkv[:, k, 128:256],
                       k == 0, k == 1, ["wkv" + s_] + kvaT_keys, [PK[bB]])
            yield
            ACT(sqn_t[g2], PB[bA], AF.Square, [], [PK[bA], "sqn" + gs])
            ACT(V[:, tg * 4:(tg + 1) * 4, 0:128], PB[bB].rearrange("p (t v) -> p t v", t=4), AF.Copy, [],
                [PK[bB], "V" + s_])
            yield
            MM(PB[bD], onesb, sqn_t[g2], True, False, ["cb", "sqn" + gs], [PK[bD]])
            MM(PB[bD], onesb, SQR[:, tsl], False, True, ["cb", "SQR"], [PK[bD]])
            yield
            ACT(rb, PB[bD], AF.Ln, ["epsc"], [PK[bD], "rb" + gs], scale=1.0 / 192, bias=epsc)
            ACT(rb, rb, AF.Exp, [], ["rb" + gs], scale=-0.5)
            yield
            STT("dve", KTn[:, tsl], PB[bA], kngp[:, 0:1], rb, ALU.mult, ALU.mult, ["kngp", "rb" + gs], [PK[bA], "KTn" + s_])
            TT("pool", KTr[0:64, tsl], RT[0:64, tsl], rb[0:64, :], ALU.mult, ["RT", "rb" + gs], ["KTr" + s_])
            yield
        pipeline([proj_body(tg) for tg in range(4)], 2)
        steps = [(G, kt) for G in range(4) for kt in range(4 * G + 4)]

        def geom(G, kt):
            j0 = max(0, kt - 4 * G)
            return j0, (4 - j0) * 128, (4 * G + j0) * 128

        def emit_ST(n):
            G, kt = steps[n]
            j0, ncol, q0 = geom(G, kt)
            sb = n % 2
            MM(PB[sb][:, 0:ncol], KTn[:, kt * 128:(kt + 1) * 128], QTn[:, q0:q0 + ncol], True, False,
               ["KTn" + s_, "QTn" + s_], [PK[sb]])
            MM(PB[sb][:, 0:ncol], KTr[:, kt * 128:(kt + 1) * 128], QTr[:, q0:q0 + ncol], False, True,
               ["KTr" + s_, "QTr" + s_], [PK[sb]])
        emit_ST(0)
        pending = []
        for n in range(len(steps)):
            G, kt = steps[n]
            j0, ncol, q0 = geom(G, kt)
            sb = n % 2
            pt = PT_t[npt % 2]
            pk = "PT%d" % (npt % 2)
            if n % 10 == 5:
                ada_group()
            npt += 1
            if n + 1 < len(steps):
                emit_ST(n + 1)
            ACT(pt[:, 0:ncol], PB[sb][:, 0:ncol], AF.Exp, [], [PK[sb], pk], scale=SM_SCALE)
            if kt >= 4 * G:
                TT("pool", pt[:, 0:128], pt[:, 0:128], triub, ALU.mult, ["cb"], [pk])
            for j in range(j0, 4):
                qt = 4 * G + j
                MM(PB[2 + j][:, 0:129], pt[:, (j - j0) * 128:(j - j0 + 1) * 128], V[:, kt, 0:129],
                   kt == 0, kt == qt, [pk, "V" + s_], [PK[2 + j]])
                if kt == qt:
                    e2 = nev % 4
                    nev += 1

                    def mk(e2=e2, j=j, qt=qt, h=h):
                        es_ = str(e2)
                        O = PB[2 + j]
                        ssq = ocol[:, e2 * 4:e2 * 4 + 1]
                        tt1 = ocol[:, e2 * 4 + 1:e2 * 4 + 2]
                        tt2 = ocol[:, e2 * 4 + 2:e2 * 4 + 3]
                        den = ocol[:, e2 * 4 + 3:e2 * 4 + 4]

                        def s1():
                            ACT(junk3, O[:, 0:128], AF.Square, [], [PK[2 + j], "junk3", "ossq" + es_], accum=ssq)
                            CP("dve", den, O[:, 128:129], [], [PK[2 + j], "oden" + es_])
                            STT("dve", tt1, den, EPS, den, ALU.mult, ALU.mult, ["oden" + es_], ["ott1" + es_])
                            STT("dve", tt2, ssq, 1.0 / 128, tt1, ALU.mult, ALU.add, ["ossq" + es_, "ott1" + es_], ["ott2" + es_])

                        def s2():
                            ACT(tt2, tt2, AF.Ln, [], ["ott2" + es_])
                            ACT(tt2, tt2, AF.Exp, [], ["ott2" + es_], scale=-0.5)

                        def s3():
                            STT("dve", ob_t[e2], O[:, 0:128], tt2, aonb, ALU.mult, ALU.mult, ["ott2" + es_, "aonb"],
                                [PK[2 + j], "ob" + es_])
                            TR(PBH[6][:, 0:128], ob_t[e2], identb, ["ob" + es_, "cb"], [PK[6]])

                        def s4():
                            ACT(mixT[:, 8 + h, qt * 128:(qt + 1) * 128], PBH[6][:, 0:128], AF.Copy, [],
                                [PK[6], "mixT%d_%d" % (8 + h, qt)])
                        return [s1, s2, s3, s4]
                    st = mk()
                    for d_, fn in enumerate(st):
                        pending.append([n + d_, fn])
            last_of_group = (kt == 4 * G + 3)
            keep = []
            for item in pending:
                if item[0] <= n or last_of_group and False:
                    item[1]()
                else:
                    keep.append(item)
            pending[:] = keep
            if last_of_group:
                for item in sorted(pending, key=lambda it: it[0]):
                    item[1]()
                pending[:] = []
    ada_group()
    assert ada_next[0] == 32 and ada_pending[0] is None
    S.barrier()
    A.off = markHT
    A.cap = A.ap.shape[1]
    if debug == "D":
        d = dout("mixT", [128, 16 * S_LEN], BF16)
        DMA("sp", d, mixT.rearrange("p k t -> p (k t)"), [], ["dbg"])
        S.emit(); S.close(); es.close()
        return nc, dbg

    markE = A.off
    wo = A.alloc(16 * D, BF16).rearrange("p (k n) -> p k n", k=16)
    g1b = A.alloc(D, F32)
    xt = [A.alloc(D, F32) for _ in range(2)]
    tmpe = [A.alloc(512, F32) for _ in range(2)]
    for n in range(4):
        S.dma("pool", (lambda n=n: (lambda e: e.dma_start(
            out=wo[:, :, n * 512:(n + 1) * 512],
            in_=wout_d[:, n * 512:(n + 1) * 512].rearrange("(k p) n -> p k n", p=128))))(), [], ["wo%d" % n])
    DMA("sp", g1b, modrow_d[0:1, 2 * D:3 * D].partition_broadcast(128), ["modrow_d"], ["g1b"])
    mix_keys = []
    for i in range(NT):
        sl = i % 2
        DMA("sp", xt[sl], x_d[i * 128:(i + 1) * 128, :], [], ["xt%d" % sl])
        for n in range(4):
            pbi = (i * 4 + n) % 4
            for k in range(16):
                MM(PB[pbi], mixT[:, k, i * 128:(i + 1) * 128], wo[:, k, n * 512:(n + 1) * 512], k == 0, k == 15,
                   ["wo%d" % n], [PK[pbi]])
            tp = tmpe[n % 2]
            TT("dve", tp, PB[pbi], g1b[:, n * 512:(n + 1) * 512], ALU.mult, ["g1b"], [PK[pbi], "tmpe%d" % (n % 2)])
            TT("pool", xt[sl][:, n * 512:(n + 1) * 512], xt[sl][:, n * 512:(n + 1) * 512], tp, ALU.add,
               ["tmpe%d" % (n % 2)], ["xt%d" % sl])
        DMA("sp", out_d[i * 128:(i + 1) * 128, :], xt[sl], ["xt%d" % sl], ["out"])
    S.barrier()
    A.off = markMix
    if debug == "E1":
        S.emit(); S.close(); es.close()
        return nc, dbg

    h2_d = nc.dram_tensor("h2_scratch", [S_LEN, D], BF16).ap()
    A2b = A.alloc(D, F32)
    B2b = A.alloc(D, F32)
    g2b = A.alloc(D, F32)
    LG = A.alloc(16 * 36, F32).rearrange("p (t n) -> p t n", t=16)
    IDXW = A.alloc(64, I32)
    zt = A.alloc(D + 32, BF16)
    S.op("pool", lambda e: e.memset(zt, 0.0), [], ["zt"])
    for j in range(NBLK):
        DMA("pool", xbuf_d[j * 128:(j + 1) * 128, :], zt, ["zt"], ["xbz%d" % j])
    xbz_keys = ["xbz%d" % j for j in range(NBLK)]
    markE2 = A.off
    n2gb = A.alloc(D, F32)
    wgr = A.alloc(16 * 36, F32).rearrange("p (k n) -> p k n", k=16)
    bgrb = A.alloc(36, F32)
    DMA("sp", A2b, modrow_d[0:1, 4 * D:5 * D].partition_broadcast(128), ["modrow_d"], ["A2b"])
    DMA("sp", B2b, modrow_d[0:1, 3 * D:4 * D].partition_broadcast(128), ["modrow_d"], ["B2b"])
    DMA("sp", g2b, modrow_d[0:1, 5 * D:6 * D].partition_broadcast(128), ["modrow_d"], ["g2b"])
    DMA("sp", n2gb, n2g_d.partition_broadcast(128), [], ["n2gb"])
    DMA("sp", wgr, wgr_d.rearrange("(k p) n -> p k n", p=128), [], ["wgr"])
    DMA("sp", bgrb, bgr_d.partition_broadcast(128), [], ["bgrb"])
    STT("dve", A2b, A2b, 1.0, n2gb, ALU.add, ALU.mult, ["n2gb"], ["A2b"])
    xt = [A.alloc(D, F32) for _ in range(2)]
    h2f_t = [A.alloc(D, F32) for _ in range(2)]
    h2b = [A.alloc(D, BF16) for _ in range(2)]
    h2T_t = [A.alloc(16 * 128, F32).rearrange("p (k t) -> p k t", k=16) for _ in range(2)]
    ssq2 = col(16)
    rs2 = col(16)

    def e2_body(i):
        sl = i % 2
        s_ = str(sl)
        h2f, h2T = h2f_t[sl], h2T_t[sl]
        b0 = 4 * sl
        DMA("sp", xt[sl], out_d[i * 128:(i + 1) * 128, :], ["out"], ["xt" + s_])
        ACT(h2f, xt[sl], AF.Square, ["xt" + s_], ["h2f" + s_, "ssq2_%d" % i], accum=ssq2[:, i:i + 1])
        yield
        rstd_col(rs2[:, i:i + 1], ssq2[:, i:i + 1], D, ["ssq2_%d" % i, "epsc"], ["rs2_%d" % i], "rs2t_%d" % i)
        yield
        STT("dve", h2f, xt[sl], rs2[:, i:i + 1], A2b, ALU.mult, ALU.mult, ["rs2_%d" % i, "A2b"], ["h2f" + s_])
        yield
        TT("dve", h2f, h2f, B2b, ALU.add, ["B2b"], ["h2f" + s_])
        yield
        ACT(h2b[sl], h2f, AF.Copy, ["h2f" + s_], ["h2b" + s_])
        for q in range(4):
            for kk in range(4):
                k = q * 4 + kk
                TR(PB[b0 + q][:, kk * 128:(kk + 1) * 128], h2f[:, k * 128:(k + 1) * 128], identf, ["h2f" + s_, "cst"],
                   [PK[b0 + q]])
        yield
        DMA("sp", h2_d[i * 128:(i + 1) * 128, :], h2b[sl], ["h2b" + s_], ["h2_d%d" % i])
        for q in range(4):
            if q % 2:
                CP("dve", h2T[:, q * 4:(q + 1) * 4, :], PB[b0 + q].rearrange("p (k t) -> p k t", k=4), [],
                   [PK[b0 + q], "h2T" + s_])
            else:
                ACT(h2T[:, q * 4:(q + 1) * 4, :], PB[b0 + q].rearrange("p (k t) -> p k t", k=4), AF.Copy, [],
                    [PK[b0 + q], "h2T" + s_])
        yield
        for k in range(16):
            MM(PB[b0][:, 0:36], h2T[:, k, :], wgr[:, k, :], k == 0, k == 15, ["h2T" + s_, "wgr"], [PK[b0]])
        yield
        TT("dve", LG[:, i, :], PB[b0][:, 0:36], bgrb, ALU.add, ["bgrb"], [PK[b0], "LG"])
        yield
    pipeline([e2_body(i) for i in range(NT)], 2)
    S.barrier()
    A.off = markE2
    if debug == "E2":
        d = dout("LG", [128, 16 * 36]); DMA("sp", d, LG.rearrange("p t n -> p (t n)"), [], ["dbg"])
        d = dout("h2", [S_LEN, D], BF16); DMA("sp", d, h2_d, [], ["dbg"])
        S.emit(); S.close(); es.close()
        return nc, dbg

    BIG = 1.0e30

    def T3(n, m):
        t = A.alloc(16 * n * m, F32)
        return t.rearrange("p (t n) -> p t n", t=16) if m == 1 else t.rearrange("p (t n m) -> p t n m", t=16, n=n)
    def bc(ap2, shape):
        v = ap2
        for ax in range(2, len(shape)):
            v = v.unsqueeze(ax)
        return v.to_broadcast(shape)
    G = LG[:, :, 0:4]
    EL = LG[:, :, 4:36]
    gmax = A.alloc(16, F32)
    RED("dve", gmax, G, ALU.max, ["LG"], ["gmax"])
    gone = T3(4, 1)
    TT("dve", gone, G, bc(gmax, [128, 16, 4]), ALU.is_equal, ["gmax", "LG"], ["gone"])
    gd = T3(4, 1)
    TT("dve", gd, G, bc(gmax, [128, 16, 4]), ALU.subtract, ["gmax", "LG"], ["gd"])
    ACT(gd, gd, AF.Exp, [], ["gd"])
    pg = A.alloc(16, F32)
    RED("dve", pg, gd, ALU.add, ["gd"], ["pg"])
    RCP(pg, pg, [], ["pg"])
    pen = T3(4, 1)
    TS("dve", pen, gone, -1.0, BIG, ALU.add, ALU.mult, ["gone"], ["pen"])
    EM = T3(32, 1)
    TT("dve", EM.rearrange("p t (g e) -> p t g e", g=4), EL.rearrange("p t (g e) -> p t g e", g=4),
       pen.unsqueeze(3).to_broadcast([128, 16, 4, 8]), ALU.add, ["pen", "LG"], ["EM"])
    v1 = A.alloc(16, F32)
    RED("dve", v1, EM, ALU.max, ["EM"], ["v1"])
    M1 = T3(32, 1)
    TT("dve", M1, EM, bc(v1, [128, 16, 32]), ALU.is_equal, ["EM", "v1"], ["M1"])
    EM2 = T3(32, 1)
    STT("dve", EM2, M1, -BIG, EM, ALU.mult, ALU.add, ["M1", "EM"], ["EM2"])
    v2 = A.alloc(16, F32)
    RED("dve", v2, EM2, ALU.max, ["EM2"], ["v2"])
    M2 = T3(32, 1)
    TT("dve", M2, EM2, bc(v2, [128, 16, 32]), ALU.is_equal, ["EM2", "v2"], ["M2"])
    e21 = A.alloc(16, F32)
    TT("dve", e21, v2, v1, ALU.subtract, ["v1", "v2"], ["e21"])
    ACT(e21, e21, AF.Exp, [], ["e21"])
    w1 = A.alloc(16, F32)
    w2 = A.alloc(16, F32)
    TS("dve", w1, e21, 1.0, None, ALU.add, None, ["e21"], ["w1"])
    RCP(w1, w1, [], ["w1"])
    TT("dve", w1, w1, pg, ALU.mult, ["pg"], ["w1"])
    TT("dve", w2, w1, e21, ALU.mult, ["w1", "e21"], ["w2"])
    Mb = A.alloc(16 * 32, BF16).rearrange("p (t n) -> p t n", t=16)
    TT("dve", Mb, M1, M2, ALU.add, ["M1", "M2"], ["Mb"])
    for i in range(NT):
        MM(PB[0][:, i * 32:(i + 1) * 32], trilsb, Mb[:, i, :], True, i == 0, ["cb", "Mb"], [PK[0]])
        for j in range(i):
            MM(PB[0][:, i * 32:(i + 1) * 32], onesb, Mb[:, j, :], False, j == i - 1, ["cb", "Mb"], [PK[0]])
    POS = T3(32, 1)
    CP("dve", POS.rearrange("p t n -> p (t n)"), PB[0], [], [PK[0], "POS"])
    for j in range(NT):
        MM(PB[1][:, 0:32], onesb, Mb[:, j, :], j == 0, j == NT - 1, ["cb", "Mb"], [PK[1]])
    cnt = A.alloc(32, F32)
    CP("dve", cnt, PB[1][:, 0:32], [], [PK[1], "cnt"])
    cmp1 = A.alloc(32 * 16, F32).rearrange("p (e m) -> p e m", e=32)
    TT("dve", cmp1, cnt.unsqueeze(2).to_broadcast([128, 32, 16]), thr16.unsqueeze(1).to_broadcast([128, 32, 16]),
       ALU.is_gt, ["cnt", "cst"], ["cmp1"])
    padded = A.alloc(32, F32)
    RED("dve", padded, cmp1, ALU.add, ["cmp1"], ["padded"])
    TS("dve", padded, padded, 128.0, None, ALU.mult, None, [], ["padded"])
    cs = [A.alloc(32, F32) for _ in range(2)]
    CP("dve", cs[0], padded, ["padded"], ["cs0"])
    cur = 0
    for sh in (1, 2, 4, 8, 16):
        nx = 1 - cur
        CP("dve", cs[nx][:, 0:sh], cs[cur][:, 0:sh], ["cs%d" % cur], ["cs%d" % nx])
        TT("dve", cs[nx][:, sh:32], cs[cur][:, sh:32], cs[cur][:, 0:32 - sh], ALU.add, ["cs%d" % cur], ["cs%d" % nx])
        cur = nx
    pad_end = cs[cur]
    pek = "cs%d" % cur
    pad_start = A.alloc(32, F32)
    TT("dve", pad_start, pad_end, padded, ALU.subtract, [pek, "padded"], ["pad_start"])
    cmp2 = A.alloc(64 * 32, F32).rearrange("p (j e) -> p j e", j=64)
    TT("dve", cmp2, pad_end.unsqueeze(1).to_broadcast([128, 64, 32]), thr64.unsqueeze(2).to_broadcast([128, 64, 32]),
       ALU.is_le, [pek, "cst"], ["cmp2"])
    blke = A.alloc(64, F32)
    RED("dve", blke, cmp2, ALU.add, ["cmp2"], ["blke"])
    TS("dve", blke, blke, 31.0, None, ALU.min, None, [], ["blke"])
    same = A.alloc(64, F32)
    S.op("pool", lambda e: e.memset(same, 0.0), [], ["same"])
    TT("dve", same[:, 2:64], blke[:, 2:64], blke[:, 0:62], ALU.is_equal, ["blke"], ["same"])
    TS("dve", blke, blke, 128.0, iota_p, ALU.mult, ALU.add, ["cst"], ["blke"])
    STT("dve", blke, same, 8192.0, blke, ALU.mult, ALU.add, ["same"], ["blke"])
    CP("dve", IDXW, blke, ["blke"], ["IDXW"])
    Tt = T3(32, 1)
    TT("dve", Tt, POS, pad_start.unsqueeze(1).to_broadcast([128, 16, 32]), ALU.add, ["POS", "pad_start"], ["Tt"])
    prod = T3(32, 1)
    dstf = A.alloc(32, F32).rearrange("p (t k) -> p t k", t=16)
    TT("dve", prod, M1, Tt, ALU.mult, ["M1", "Tt"], ["prod"])
    RED("dve", dstf[:, :, 0], prod, ALU.add, ["prod"], ["dstf"])
    TT("dve", prod, M2, Tt, ALU.mult, ["M2", "Tt"], ["prod"])
    RED("dve", dstf[:, :, 1], prod, ALU.add, ["prod"], ["dstf"])
    DI = A.alloc(32, I32)
    CP("dve", DI, dstf.rearrange("p t k -> p (t k)"), ["dstf"], ["DI"])
    tokf = A.alloc(16, F32)
    TS("dve", tokf, thr16, iota_p, None, ALU.add, None, ["cst"], ["tokf"])
    with nc.allow_non_contiguous_dma(reason="64B meta tails"):
        pass
    DMA("sp", xbuf_d[:, D:D + 32], metai_d, xbz_keys, ["xbuf"])
    if debug == "E3":
        S.barrier()
        d = dout("LG", [128, 16 * 36]); DMA("sp", d, LG.rearrange("p t n -> p (t n)"), [], ["dbg"])
        d = dout("IDXW", [128, 64], I32); DMA("sp", d, IDXW, [], ["dbg"])
        d = dout("DI", [128, 32], I32); DMA("sp", d, DI, [], ["dbg"])
        d = dout("w1", [128, 16]); DMA("sp", d, w1, [], ["dbg"])
        d = dout("w2", [128, 16]); DMA("sp", d, w2, [], ["dbg"])
        d = dout("cnt", [128, 32]); DMA("sp", d, cnt, [], ["dbg"])
        d = dout("M1", [128, 512]); DMA("sp", d, M1.rearrange("p t n -> p (t n)"), [], ["dbg"])
        d = dout("M2", [128, 512]); DMA("sp", d, M2.rearrange("p t n -> p (t n)"), [], ["dbg"])
        S.emit(); S.close(); es.close()
        return nc, dbg
    hbx = [[A.alloc(D + 32, BF16) for _ in range(2)] for _ in range(2)]
    whi = [A.alloc(16, BF16) for _ in range(2)]
    wlo = [A.alloc(16, BF16) for _ in range(2)]
    whf = A.alloc(16, F32)
    for k in range(2):
        wk_ = (w1, w2)[k]
        CP("dve", whi[k], wk_, ["w1", "w2"], ["whl"])
        CP("dve", whf, whi[k], ["whl"], ["whf"])
        TT("dve", whf, wk_, whf, ALU.subtract, ["w1", "w2"], ["whf"])
        CP("dve", wlo[k], whf, ["whf"], ["whl"])
    for k in range(2):
        for sl in range(2):
            S.op("pool", (lambda k=k, sl=sl: (lambda e: e.memset(hbx[k][sl][:, D:D + 32], 0.0)))(), [], ["hb%d_%d" % (k, sl)])
    for i in range(NT):
        sl = i % 2
        for k in range(2):
            c = i * 2 + k
            hb = hbx[k][sl]
            hk = "hb%d_%d" % (k, sl)
            DMA("sp", hb[:, 0:D], h2_d[i * 128:(i + 1) * 128, :], ["h2_d%d" % i], [hk])
            tailI = hb[:, D:D + 32].bitcast(I32)
            CP("dve", tailI[:, 0:1], tokf[:, i:i + 1], ["tokf"], [hk])
            CP("dve", hb[:, D + 2:D + 3], whi[k][:, i:i + 1], ["whl"], [hk])
            CP("dve", hb[:, D + 3:D + 4], wlo[k][:, i:i + 1], ["whl"], [hk])
            S.dma("pool", (lambda hb=hb, c=c: (lambda e: e.indirect_dma_start(
                out=xbuf_d, out_offset=bass.IndirectOffsetOnAxis(ap=DI[:, c:c + 1], axis=0),
                in_=hb, in_offset=None, bounds_check=breg(e, NSLOT - 1), oob_is_err=False)))(),
                [hk, "DI"], ["xbuf"])
    S.barrier()
    A.off = markE2
    markF = A.off

    wg_t = [A.alloc(16 * 512, BF16) for _ in range(2)]
    wu_t = [A.alloc(16 * 512, BF16) for _ in range(2)]
    wd_t = [A.alloc(4 * D, BF16) for _ in range(2)]
    xb_t = [A.alloc(D + 32, BF16) for _ in range(2)]
    XT_t = [A.alloc(16 * 128, BF16).rearrange("p (k s) -> p k s", k=16) for _ in range(2)]
    en_f = [A.alloc(512, F32) for _ in range(2)]
    tg_f = [A.alloc(512, F32) for _ in range(2)]
    act_b = [A.alloc(512, BF16) for _ in range(2)]
    actT = [A.alloc(4 * 128, BF16).rearrange("p (k s) -> p k s", k=4) for _ in range(2)]
    Y_t = [A.alloc(D, F32) for _ in range(2)]
    wc_t = [col(1) for _ in range(2)]

    def pf(j, which):
        sl = j % 2
        s_ = str(sl)
        lst = []
        if "g" in which:
            lst += [(wg_t[sl], wg_d, "wg"), (wu_t[sl], wu_d, "wu")]
        if "d" in which:
            lst += [(wd_t[sl], wd_d, "wd")]
        for (dst, src, key) in lst:
            S.dma("pool", (lambda dst=dst, src=src, j=j: (lambda e: e.indirect_dma_start(
                out=dst, out_offset=None, in_=src, in_offset=bass.IndirectOffsetOnAxis(ap=IDXW[:, j:j + 1], axis=0),
                bounds_check=breg(e, 32 * 128 - 1), oob_is_err=False)))(), ["IDXW"], [key + s_])
        if "d" in which:
            DMA("sp", xb_t[sl], xbuf_d[j * 128:(j + 1) * 128, :], ["xbuf"], ["xb" + s_])

    def stage_A(j):
        sl = j % 2
        s_ = str(sl)
        bG, bU = 2 * sl, 2 * sl + 1
        xb, XT = xb_t[sl], XT_t[sl]
        xbv = xb[:, 0:D].rearrange("p (f k) -> p k f", k=16)
        for hf, bb in ((0, 4), (1, 5)):
            for kk in range(8):
                TR(PBH[bb][:, kk * 128:(kk + 1) * 128], xbv[:, hf * 8 + kk, :], identb, ["xb" + s_, "cb"], [PK[bb]])
        CP("dve", XT[:, 0:8, :], PBH[4].rearrange("p (k s) -> p k s", k=8), [], [PK[4], "XT" + s_])
        ACT(XT[:, 8:16, :], PBH[5].rearrange("p (k s) -> p k s", k=8), AF.Copy, [], [PK[5], "XT" + s_])
        wg, wu = wg_t[sl], wu_t[sl]
        for k in range(16):
            MM(PB[bG], XT[:, k, :], wg[:, k * 512:(k + 1) * 512], k == 0, k == 15, ["XT" + s_, "wg" + s_], [PK[bG]])
        for k in range(16):
            MM(PB[bU], XT[:, k, :], wu[:, k * 512:(k + 1) * 512], k == 0, k == 15, ["XT" + s_, "wu" + s_], [PK[bU]])

    def stage_B1(j):
        sl = j % 2
        s_ = str(sl)
        bG, bU = 2 * sl, 2 * sl + 1
        ACT(en_f[sl], PB[bG], AF.Exp, [], [PK[bG], "en_f" + s_], scale=-1.0)
        ACT(en_f[sl], en_f[sl], AF.Ln, [], ["en_f" + s_], bias=onec)
        ACT(en_f[sl], en_f[sl], AF.Exp, [], ["en_f" + s_], scale=-1.0)
        TT("dve", tg_f[sl], PB[bG], en_f[sl], ALU.mult, ["en_f" + s_], [PK[bG], "tg_f" + s_])
        TT("dve", act_b[sl], tg_f[sl], PB[bU], ALU.mult, ["tg_f" + s_], [PK[bU], "act_b" + s_])
        abv = act_b[sl].rearrange("p (j k) -> p k j", k=4)
        for kk in range(4):
            TR(PBH[6][:, kk * 128:(kk + 1) * 128], abv[:, kk, :], identb, ["act_b" + s_, "cb"], [PK[6]])
        ACT(actT[sl], PBH[6][:, 0:512].rearrange("p (k s) -> p k s", k=4), AF.Copy, [], [PK[6], "actT" + s_])

    def stage_B2(j):
        sl = j % 2
        s_ = str(sl)
        bG, bU = 2 * sl, 2 * sl + 1
        wd = wd_t[sl]
        xb = xb_t[sl]
        Y = Y_t[sl]
        wcol = wc_t[sl]
        TT("dve", wcol, xb[:, D + 2:D + 3], xb[:, D + 3:D + 4], ALU.add, ["xb" + s_], ["wc" + s_])
        for n in range(4):
            pbi = (bG, bU, 7, bG)[n] if False else (bG if n % 2 == 0 else bU)
            for kk in range(4):
                MM(PB[pbi], actT[sl][:, kk, :], wd[:, kk * D + n * 512: kk * D + (n + 1) * 512], kk == 0, kk == 3,
                   ["actT" + s_, "wd" + s_], [PK[pbi]])
            STT("dve", Y[:, n * 512:(n + 1) * 512], PB[pbi], wcol, g2b[:, n * 512:(n + 1) * 512],
                ALU.mult, ALU.mult, ["wc" + s_, "g2b"], [PK[pbi], "Y" + s_])
        S.dma("pool", (lambda sl=sl, Y=Y: (lambda e: e.indirect_dma_start(
            out=out_d, out_offset=bass.IndirectOffsetOnAxis(ap=xb_t[sl][:, D:D + 32].bitcast(I32)[:, 0:1], axis=0),
            in_=Y, in_offset=None, bounds_check=breg(e, S_LEN + 127), oob_is_err=True, compute_op=ALU.add)))(),
            ["Y" + s_, "xb" + s_], ["out"])
    pf(0, "gd")
    pf(1, "gd")
    stage_A(0)
    pf(2, "g")
    for j in range(NBLK):
        stage_B1(j)
        stage_B2(j)
        if j + 2 < NBLK:
            pf(j + 2, "d")
        if j + 1 < NBLK:
            stage_A(j + 1)
        if j + 3 < NBLK:
            pf(j + 3, "g")
    S.emit()
    S.close()
    es.close()
    return nc, dbg


def host_inputs(inputs, b):
    f = np.float32
    m = {}
    m["x"] = np.ascontiguousarray(inputs["x"][b])
    m["ccol"] = np.ascontiguousarray(inputs["c"][b].reshape(16, 128).T)
    m["pos"] = np.ascontiguousarray(inputs["positions"][b].reshape(1, S_LEN)).astype(np.int32)
    m["w_ada"] = inputs["w_ada"][0]
    m["b_ada"] = inputs["b_ada"][0].reshape(1, -1)
    m["norm1_gc"] = np.ascontiguousarray(inputs["norm1_g"][0].reshape(16, 128).T)
    m["w_in"] = inputs["w_in"][0]
    m["lb_logits"] = inputs["hgrn_lb_logits"]
    m["hgrn_onorm_g"] = inputs["hgrn_onorm_g"][0].reshape(1, 128)
    m["q_a_gc"] = np.ascontiguousarray(inputs["q_a_norm_g"][0].reshape(4, 128).T)
    m["w_q_up"] = inputs["w_q_up"][0]
    m["kv_a_gc"] = np.ascontiguousarray(inputs["kv_a_norm_g"][0].reshape(2, 128).T)
    m["w_kv_up"] = inputs["w_kv_up"][0]

    def qk_cols(g):
        o = np.zeros((128, 4), f)
        o[:, 0] = g[0:128]
        o[0:64, 1] = g[128:192]
        o[0:32, 2] = g[160:192]
        o[32:64, 2] = g[128:160]
        o[0:32, 3] = -1.0
        o[32:64, 3] = 1.0
        return o
    m["q_norm_gc"] = qk_cols(inputs["q_norm_g"][0])
    m["k_norm_gc"] = qk_cols(inputs["k_norm_g"][0])
    m["attn_onorm_g"] = inputs["attn_onorm_g"][0].reshape(1, 128)
    m["w_out"] = inputs["w_out"][0]
    m["norm2_g"] = inputs["norm2_g"][0].reshape(1, D)
    m["w_gr"] = np.ascontiguousarray(np.concatenate([inputs["w_group"][0], inputs["w_router"][0]], axis=1))
    m["b_gr"] = np.concatenate([inputs["b_group"][0], inputs["b_router"][0]]).reshape(1, 36)
    m["w_gate"] = inputs["w_gate"][0].reshape(32 * 128, 16 * 512)
    m["w_up"] = inputs["w_up"][0].reshape(32 * 128, 16 * 512)
    m["w_down"] = inputs["w_down"][0].reshape(32 * 128, 4 * 2048)
    m["consts"] = CONSTS
    m["invf"] = INVF
    m["meta_init"] = META_INIT.view(ml_dtypes.bfloat16)
    return m


def _consts():
    c = np.zeros((128, 1024), np.float32)
    s = np.arange(128)[:, None]
    t = np.arange(128)[None, :]
    c[:, 0:128] = np.eye(128)
    c[:, 128:256] = (s <= t)
    c[:, 256:384] = (s <= t).astype(np.float32) - (s <= 63).astype(np.float32)
    c[:, 384:512] = (s > t)
    c[:, 512:640] = (s < t)
    c[:, 640] = np.arange(128)
    c[:, 656:672] = np.arange(16) * 128
    c[:, 672:736] = np.arange(64) * 128
    c[:, 736:768] = np.arange(32)
    return c


CONSTS = _consts()
INVF = (10000.0 ** (-(np.arange(64) % 32).astype(np.float32) * 2 / 64)).astype(np.float32).reshape(64, 1)
META_INIT = np.zeros((NSLOT, 16), np.int32)
META_INIT[:, 0] = 2048 + (np.arange(NSLOT) % 128)

_NC = None


def kernel(**inputs):
    global _NC
    if _NC is None:
        _NC = build()[0]
    inputs = {k: np.asarray(v) for k, v in inputs.items()}
    in_maps = [host_inputs(inputs, b) for b in range(8)]
    res = run_bass_kernel_spmd(_NC, in_maps, core_ids=list(range(8)))
    out = np.stack([np.asarray(r["out"])[:S_LEN] for r in res.results], axis=0)
    return out.astype(np.float32)
```
